# Optimizing a Trainium2 kernel written in Bass

```python
import jax
import jax.numpy as jnp
from jax import lax
import numpy as np

D_MODEL = 1024
BATCH = 2
SEQ = 16384
DEPTH = 2

ROPE_THETA = 10000.0
NORM_EPS = 1e-6
NEG_INF = -1e30

RET_HEADS = 4
RET_DK = D_MODEL // (2 * RET_HEADS)
RET_DV = D_MODEL // (2 * RET_HEADS)
RET_CHUNK = 128
S5_WIDTH = D_MODEL // 2
S5_GROUP_CH = 16
S5_GROUPS = S5_WIDTH // S5_GROUP_CH
S5_STATE = 64
S5_STEP_MIN = 1e-3
S5_STEP_MAX = 1e-1
DIL_PATTERNS = ((128, 1), (512, 4), (2048, 16))
DIL_HEADS = 4
DIL_HEAD_DIM = D_MODEL // 16
MLSTM_HEADS = 4
MLSTM_DH = D_MODEL // 8
MLSTM_CHUNK = 128
N_EXPERTS = 16
EXPERT_FF = 2 * D_MODEL
CAPACITY_FACTOR = 2

EVEN_SPLITS = (RET_HEADS * RET_DK, RET_HEADS * RET_DK, RET_HEADS * RET_DV, RET_HEADS * RET_DV, S5_WIDTH)
EVEN_IN = sum(EVEN_SPLITS)
EVEN_OUT = RET_HEADS * RET_DV + S5_WIDTH
DIL_WIDTH = len(DIL_PATTERNS) * DIL_HEADS * DIL_HEAD_DIM
MLSTM_WIDTH = MLSTM_HEADS * MLSTM_DH
ODD_SPLITS = (DIL_WIDTH, DIL_WIDTH, DIL_WIDTH, MLSTM_WIDTH, MLSTM_WIDTH, MLSTM_WIDTH, MLSTM_WIDTH, 4 * MLSTM_HEADS)
ODD_IN = sum(ODD_SPLITS)
ODD_OUT = DIL_HEADS * DIL_HEAD_DIM + MLSTM_WIDTH
N_EVEN = (DEPTH + 1) // 2
N_ODD = DEPTH // 2

kernel_name = 'hybrid_retention_s5_dilated_mlstm_ecmoe'


def _split(z, sizes):
    offsets = np.cumsum(sizes)[:-1].tolist()
    return jnp.split(z, offsets, axis=-1)


def rmsnorm(x, g):
    xf = x.astype(jnp.float32)
    y = xf * lax.rsqrt(jnp.mean(xf * xf, axis=-1, keepdims=True) + NORM_EPS)
    return (y * g.astype(jnp.float32)).astype(x.dtype)


def head_norm(h):
    hf = h.astype(jnp.float32)
    mu = jnp.mean(hf, axis=-1, keepdims=True)
    var = jnp.mean(jnp.square(hf - mu), axis=-1, keepdims=True)
    return (hf - mu) * lax.rsqrt(var + NORM_EPS)


def rotary(x, positions):
    e = x.shape[-1]
    half = e // 2
    inv_freq = ROPE_THETA ** (-jnp.arange(half, dtype=jnp.float32) / half)
    ang = positions.astype(jnp.float32)[:, None] * inv_freq[None, :]
    cos = jnp.cos(ang)[:, None, :]
    sin = jnp.sin(ang)[:, None, :]
    x1 = x[..., :half].astype(jnp.float32)
    x2 = x[..., half:].astype(jnp.float32)
    return jnp.concatenate([x1 * cos - x2 * sin, x1 * sin + x2 * cos], axis=-1).astype(x.dtype)


def retention_direction(q, k, v, log_gamma, strict):
    b, h, l, dk = q.shape
    dv = v.shape[-1]
    c = RET_CHUNK
    n = l // c
    qc = q.reshape(b, h, n, c, dk)
    kc = k.reshape(b, h, n, c, dk)
    vc = v.reshape(b, h, n, c, dv)
    idx = jnp.arange(c)
    diff = idx[:, None] - idx[None, :]
    mask = (diff > 0) if strict else (diff >= 0)
    decay = jnp.where(mask, jnp.exp(jnp.where(mask, diff, 0).astype(jnp.float32)[None] * log_gamma[:, None, None]), 0.0)
    scores = jnp.einsum('bhnid,bhnjd->bhnij', qc, kc) * decay[None, :, None]
    o_intra = jnp.einsum('bhnij,bhnje->bhnie', scores, vc)
    k_w = jnp.exp((c - 1 - idx).astype(jnp.float32)[None, :] * log_gamma[:, None])
    chunk_kv = jnp.einsum('bhnjd,hj,bhnje->bhnde', kc, k_w, vc)
    chunk_decay = jnp.exp(c * log_gamma)[None, :, None, None]

    def step(state, kv_n):
        return chunk_decay * state + kv_n, state

    _, state_before = lax.scan(step, jnp.zeros((b, h, dk, dv), jnp.float32), jnp.moveaxis(chunk_kv, 2, 0))
    state_before = jnp.moveaxis(state_before, 0, 2)
    q_w = jnp.exp((idx + 1).astype(jnp.float32)[None, :] * log_gamma[:, None])
    o_cross = jnp.einsum('bhnid,hi,bhnde->bhnie', qc, q_w, state_before)
    return (o_intra + o_cross).reshape(b, h, l, dv)


def retention_mixer(q, k, v, g, positions):
    b, l, _ = q.shape
    qh = rotary(q.reshape(b, l, RET_HEADS, RET_DK), positions)
    kh = rotary(k.reshape(b, l, RET_HEADS, RET_DK), positions) * (RET_DK ** -0.5)
    vh = v.reshape(b, l, RET_HEADS, RET_DV)
    qh, kh, vh = (t.transpose(0, 2, 1, 3) for t in (qh, kh, vh))
    log_gamma = jnp.log1p(-(2.0 ** (-5.0 - jnp.arange(RET_HEADS, dtype=jnp.float32))))
    fwd = retention_direction(qh, kh, vh, log_gamma, False)
    bwd = jnp.flip(retention_direction(jnp.flip(qh, 2), jnp.flip(kh, 2), jnp.flip(vh, 2), log_gamma, True), 2)
    o = head_norm(fwd + bwd).transpose(0, 2, 1, 3).reshape(b, l, RET_HEADS * RET_DV)
    return (jax.nn.silu(g.astype(jnp.float32)) * o).astype(g.dtype)


def _linear_combine(left, right):
    a1, b1 = left
    a2, b2 = right
    return a1 * a2, a2 * b1 + b2


def s5_mixer(u, a_re, a_im, b_re, b_im, c_re, c_im, log_step, d_skip, glu_w, glu_b):
    b, l, w = u.shape
    uf = u.astype(jnp.float32)
    ug = uf.reshape(b, l, S5_GROUPS, S5_GROUP_CH).astype(jnp.complex64)
    y = d_skip.astype(jnp.float32) * uf
    for direction in range(2):
        lam = lax.complex(a_re[direction].astype(jnp.float32), a_im[direction].astype(jnp.float32))
        step = jnp.exp(log_step[direction].astype(jnp.float32))[:, None]
        a_bar = jnp.exp(lam * step)
        bmat = lax.complex(b_re[direction].astype(jnp.float32), b_im[direction].astype(jnp.float32))
        b_bar = ((a_bar - 1.0) / lam)[..., None] * bmat
        bu = jnp.einsum('gpc,blgc->blgp', b_bar, ug)
        a_seq = jnp.broadcast_to(a_bar, bu.shape)
        _, states = lax.associative_scan(_linear_combine, (a_seq, bu), axis=1, reverse=(direction == 1))
        cmat = lax.complex(c_re[direction].astype(jnp.float32), c_im[direction].astype(jnp.float32))
        y = y + jnp.real(jnp.einsum('gcp,blgp->blgc', cmat, states)).reshape(b, l, w)
    y = jax.nn.gelu(y)
    y = y * jax.nn.sigmoid(y @ glu_w.astype(jnp.float32) + glu_b.astype(jnp.float32))
    return y.astype(u.dtype)


def dilated_window_attention(q, k, v, dilation, half):
    b, l, h, e = q.shape
    blk = half
    unit = dilation * blk
    lp = -(-l // unit) * unit
    j = lp // dilation
    nb = j // blk

    def to_classes(t):
        t = jnp.pad(t, ((0, 0), (0, lp - l), (0, 0), (0, 0)))
        return t.reshape(b, j, dilation, h, e).transpose(0, 2, 3, 1, 4)

    def windows(t):
        t = jnp.pad(t, ((0, 0), (0, 0), (0, 0), (blk, blk), (0, 0))).reshape(b, dilation, h, nb + 2, blk, e)
        return jnp.concatenate([t[:, :, :, :-2], t[:, :, :, 1:-1], t[:, :, :, 2:]], axis=-2)

    qc = to_classes(q).reshape(b, dilation, h, nb, blk, e)
    kw = windows(to_classes(k))
    vw = windows(to_classes(v))
    valid = (jnp.arange(lp) < l).reshape(j, dilation).T
    valid = jnp.pad(valid, ((0, 0), (blk, blk))).reshape(dilation, nb + 2, blk)
    kvalid = jnp.concatenate([valid[:, :-2], valid[:, 1:-1], valid[:, 2:]], axis=-1)
    rel = jnp.arange(3 * blk)[None, :] - blk - jnp.arange(blk)[:, None]
    mask = (jnp.abs(rel) <= half)[None, None] & kvalid[:, :, None, :]
    s = jnp.einsum('brhnie,brhnte->brhnit', qc, kw).astype(jnp.float32) * (e ** -0.5)
    s = jnp.where(mask[None, :, None], s, NEG_INF)
    m = jnp.max(s, axis=-1, keepdims=True)
    p = jnp.exp(s - m)
    den = jnp.sum(p, axis=-1)
    o = jnp.einsum('brhnit,brhnte->brhnie', p, vw) / den[..., None]
    lse = m[..., 0] + jnp.log(den)
    o = o.reshape(b, dilation, h, j, e).transpose(0, 3, 1, 2, 4).reshape(b, lp, h, e)[:, :l]
    lse = lse.reshape(b, dilation, h, j).transpose(0, 3, 1, 2).reshape(b, lp, h)[:, :l]
    return o, lse


def dilated_mixer(q, k, v, positions):
    b, l, _ = q.shape
    n_heads = len(DIL_PATTERNS) * DIL_HEADS
    qh = rotary(q.reshape(b, l, n_heads, DIL_HEAD_DIM), positions)
    kh = rotary(k.reshape(b, l, n_heads, DIL_HEAD_DIM), positions)
    vh = v.reshape(b, l, n_heads, DIL_HEAD_DIM)
    outs, lses = [], []
    for gi, (window, dilation) in enumerate(DIL_PATTERNS):
        sl = slice(gi * DIL_HEADS, (gi + 1) * DIL_HEADS)
        o, lse = dilated_window_attention(qh[:, :, sl], kh[:, :, sl], vh[:, :, sl], dilation, window // (2 * dilation))
        outs.append(o)
        lses.append(lse)
    wts = jax.nn.softmax(jnp.stack(lses), axis=0)
    o = jnp.sum(wts[..., None] * jnp.stack(outs), axis=0)
    return o.reshape(b, l, DIL_HEADS * DIL_HEAD_DIM).astype(q.dtype)


def mlstm_direction(q, k, v, ig, lf):
    b, h, l, dk = q.shape
    dv = v.shape[-1]
    lc = MLSTM_CHUNK
    n = l // lc

    def chunks(t):
        return jnp.moveaxis(t.reshape((b, h, n, lc) + t.shape[3:]), 2, 0)

    causal = jnp.tril(jnp.ones((lc, lc), dtype=bool))

    def step(carry, inp):
        cmem, nvec, m = carry
        qn, kn, vn, ign, lfn = inp
        bcum = jnp.cumsum(lfn, axis=-1)
        log_d = jnp.where(causal, bcum[..., :, None] - bcum[..., None, :] + ign[..., None, :], -jnp.inf)
        log_inter = bcum + m[..., None]
        m_i = jnp.maximum(log_inter, jnp.max(log_d, axis=-1))
        s = jnp.einsum('bhid,bhjd->bhij', qn, kn) * jnp.exp(log_d - m_i[..., None])
        w_inter = jnp.exp(log_inter - m_i)
        num = jnp.einsum('bhij,bhje->bhie', s, vn) + w_inter[..., None] * jnp.einsum('bhid,bhde->bhie', qn, cmem)
        den_raw = jnp.sum(s, axis=-1) + w_inter * jnp.einsum('bhid,bhd->bhi', qn, nvec)
        hout = num / jnp.maximum(jnp.abs(den_raw), jnp.exp(-m_i))[..., None]
        b_last = bcum[..., -1]
        log_w = b_last[..., None] - bcum + ign
        m_new = jnp.maximum(b_last + m, jnp.max(log_w, axis=-1))
        w_old = jnp.exp(b_last + m - m_new)
        w_j = jnp.exp(log_w - m_new[..., None])
        c_new = w_old[..., None, None] * cmem + jnp.einsum('bhj,bhjd,bhje->bhde', w_j, kn, vn)
        n_new = w_old[..., None] * nvec + jnp.einsum('bhj,bhjd->bhd', w_j, kn)
        return (c_new, n_new, m_new), hout

    init = (jnp.zeros((b, h, dk, dv), jnp.float32), jnp.zeros((b, h, dk), jnp.float32), jnp.zeros((b, h), jnp.float32))
    _, hs = lax.scan(step, init, (chunks(q), chunks(k), chunks(v), chunks(ig), chunks(lf)))
    return jnp.moveaxis(hs, 0, 2).reshape(b, h, l, dv)


def mlstm_mixer(q, k, v, o, gates, i_bias, f_bias):
    b, l, _ = q.shape

    def to_heads(t):
        return t.reshape(b, l, MLSTM_HEADS, MLSTM_DH).transpose(0, 2, 1, 3)

    qh, kh, vh = to_heads(q), to_heads(k) * (MLSTM_DH ** -0.5), to_heads(v)
    g = gates.astype(jnp.float32).reshape(b, l, 4, MLSTM_HEADS).transpose(2, 0, 3, 1)
    ig = g[0:2] + i_bias.astype(jnp.float32)[:, None, :, None]
    lf = jax.nn.log_sigmoid(g[2:4] + f_bias.astype(jnp.float32)[:, None, :, None])
    h_f = mlstm_direction(qh, kh, vh, ig[0], lf[0])
    h_b = jnp.flip(mlstm_direction(jnp.flip(qh, 2), jnp.flip(kh, 2), jnp.flip(vh, 2), jnp.flip(ig[1], 2), jnp.flip(lf[1], 2)), 2)
    hn = head_norm(h_f + h_b).transpose(0, 2, 1, 3).reshape(b, l, MLSTM_WIDTH)
    return (jax.nn.sigmoid(o.astype(jnp.float32)) * hn).astype(q.dtype)


def even_mixer(h, w_in, w_out, a_re, a_im, b_re, b_im, c_re, c_im, log_step, d_skip, glu_w, glu_b, positions):
    q, k, v, g, u = _split(h @ w_in, EVEN_SPLITS)
    o_ret = retention_mixer(q, k, v, g, positions)
    o_s5 = s5_mixer(u, a_re, a_im, b_re, b_im, c_re, c_im, log_step, d_skip, glu_w, glu_b)
    return jnp.concatenate([o_ret, o_s5], axis=-1) @ w_out


def odd_mixer(h, w_in, w_out, i_bias, f_bias, positions):
    cq, ck, cv, mq, mk, mv, mo, mg = _split(h @ w_in, ODD_SPLITS)
    o_dil = dilated_mixer(cq, ck, cv, positions)
    o_ml = mlstm_mixer(mq, mk, mv, mo, mg, i_bias, f_bias)
    return jnp.concatenate([o_dil, o_ml], axis=-1) @ w_out


def expert_choice_moe(h, w_router, w1, w3, w2):
    b, t, d = h.shape
    cap = CAPACITY_FACTOR * t // N_EXPERTS
    aff = jax.nn.softmax(jnp.einsum('btd,de->bte', h, w_router).astype(jnp.float32), axis=-1)
    gate, tok = lax.top_k(jnp.swapaxes(aff, 1, 2), cap)
    flat = tok + (jnp.arange(b) * t)[:, None, None]
    xe = h.reshape(b * t, d)[flat]
    a = jnp.einsum('becd,edf->becf', xe, w1)
    g = jnp.einsum('becd,edf->becf', xe, w3)
    ye = jnp.einsum('becf,efd->becd', jax.nn.silu(a) * g, w2) * gate[..., None].astype(h.dtype)
    out = jnp.zeros((b * t, d), ye.dtype).at[flat.reshape(-1)].add(ye.reshape(-1, d))
    return out.reshape(b, t, d).astype(h.dtype)


def setup_inputs(seed: int = 0) -> dict:
    key = jax.random.key(seed)
    ks = jax.random.split(key, 24)

    def nrm(k, shape, scale):
        return jax.random.normal(k, shape, jnp.float32) * scale

    g_, p_, c_ = S5_GROUPS, S5_STATE, S5_GROUP_CH
    x = nrm(ks[0], (BATCH, SEQ, D_MODEL), 1.0)
    norm_mix_g = 1.0 + nrm(ks[1], (DEPTH, D_MODEL), 0.01)
    norm_ffn_g = 1.0 + nrm(ks[2], (DEPTH, D_MODEL), 0.01)
    final_g = 1.0 + nrm(ks[3], (D_MODEL,), 0.01)
    ev_w_in = nrm(ks[4], (N_EVEN, D_MODEL, EVEN_IN), D_MODEL ** -0.5)
    ev_w_out = nrm(ks[5], (N_EVEN, EVEN_OUT, D_MODEL), EVEN_OUT ** -0.5)
    s5_a_re = -0.5 + nrm(ks[6], (N_EVEN, 2, g_, p_), 0.01)
    s5_a_im = jnp.pi * jnp.arange(p_, dtype=jnp.float32) + nrm(ks[7], (N_EVEN, 2, g_, p_), 0.01)
    s5_b_re = nrm(ks[8], (N_EVEN, 2, g_, p_, c_), (2 * c_) ** -0.5)
    s5_b_im = nrm(ks[9], (N_EVEN, 2, g_, p_, c_), (2 * c_) ** -0.5)
    s5_c_re = nrm(ks[10], (N_EVEN, 2, g_, c_, p_), (2 * p_) ** -0.5)
    s5_c_im = nrm(ks[11], (N_EVEN, 2, g_, c_, p_), (2 * p_) ** -0.5)
    s5_log_step = np.log(S5_STEP_MIN) + jax.random.uniform(ks[12], (N_EVEN, 2, g_), jnp.float32) * (np.log(S5_STEP_MAX) - np.log(S5_STEP_MIN))
    s5_d = nrm(ks[13], (N_EVEN, S5_WIDTH), 1.0)
    s5_glu_w = nrm(ks[14], (N_EVEN, S5_WIDTH, S5_WIDTH), S5_WIDTH ** -0.5)
    s5_glu_b = nrm(ks[15], (N_EVEN, S5_WIDTH), 0.01)
    od_w_in = nrm(ks[16], (N_ODD, D_MODEL, ODD_IN), D_MODEL ** -0.5)
    od_w_out = nrm(ks[17], (N_ODD, ODD_OUT, D_MODEL), ODD_OUT ** -0.5)
    ml_i_bias = nrm(ks[18], (N_ODD, 2, MLSTM_HEADS), 0.1)
    ml_f_bias = jnp.linspace(3.0, 6.0, MLSTM_HEADS, dtype=jnp.float32) + nrm(ks[19], (N_ODD, 2, MLSTM_HEADS), 0.01)
    moe_router = nrm(ks[20], (DEPTH, D_MODEL, N_EXPERTS), D_MODEL ** -0.5)
    moe_w1 = nrm(ks[21], (DEPTH, N_EXPERTS, D_MODEL, EXPERT_FF), D_MODEL ** -0.5)
    moe_w3 = nrm(ks[22], (DEPTH, N_EXPERTS, D_MODEL, EXPERT_FF), D_MODEL ** -0.5)
    moe_w2 = nrm(ks[23], (DEPTH, N_EXPERTS, EXPERT_FF, D_MODEL), EXPERT_FF ** -0.5)
    return {'x': x, 'norm_mix_g': norm_mix_g, 'norm_ffn_g': norm_ffn_g, 'final_g': final_g,
            'ev_w_in': ev_w_in, 'ev_w_out': ev_w_out, 's5_a_re': s5_a_re, 's5_a_im': s5_a_im,
            's5_b_re': s5_b_re, 's5_b_im': s5_b_im, 's5_c_re': s5_c_re, 's5_c_im': s5_c_im,
            's5_log_step': s5_log_step, 's5_d': s5_d, 's5_glu_w': s5_glu_w, 's5_glu_b': s5_glu_b,
            'od_w_in': od_w_in, 'od_w_out': od_w_out, 'ml_i_bias': ml_i_bias, 'ml_f_bias': ml_f_bias,
            'moe_router': moe_router, 'moe_w1': moe_w1, 'moe_w3': moe_w3, 'moe_w2': moe_w2}


def reference(x, norm_mix_g, norm_ffn_g, final_g, ev_w_in, ev_w_out, s5_a_re, s5_a_im, s5_b_re, s5_b_im,
              s5_c_re, s5_c_im, s5_log_step, s5_d, s5_glu_w, s5_glu_b, od_w_in, od_w_out, ml_i_bias, ml_f_bias,
              moe_router, moe_w1, moe_w3, moe_w2):
    positions = jnp.arange(x.shape[1])
    for layer in range(DEPTH):
        h = rmsnorm(x, norm_mix_g[layer])
        if layer % 2 == 0:
            e = layer // 2
            mix = even_mixer(h, ev_w_in[e], ev_w_out[e], s5_a_re[e], s5_a_im[e], s5_b_re[e], s5_b_im[e],
                             s5_c_re[e], s5_c_im[e], s5_log_step[e], s5_d[e], s5_glu_w[e], s5_glu_b[e], positions)
        else:
            o = layer // 2
            mix = odd_mixer(h, od_w_in[o], od_w_out[o], ml_i_bias[o], ml_f_bias[o], positions)
        x = x + mix
        x = x + expert_choice_moe(rmsnorm(x, norm_ffn_g[layer]), moe_router[layer], moe_w1[layer], moe_w3[layer], moe_w2[layer])
    return rmsnorm(x, final_g)
```

```python
import contextlib
import numpy as np
import concourse.bass as bass
import concourse.mybir as mybir
from concourse.bass_utils import run_bass_kernel_spmd

F32 = mybir.dt.float32
BF16 = mybir.dt.bfloat16
I32 = mybir.dt.int32
AF = mybir.ActivationFunctionType
ALU = mybir.AluOpType
AX = mybir.AxisListType
D = 1024
EPS = 1e-6
ENGS = ("pe", "act", "dve", "pool", "sp")
ENGOBJ = {"pe": "tensor", "act": "scalar", "dve": "vector", "pool": "gpsimd", "sp": "sync"}


class Buf:
    __slots__ = ("last_w", "readers")

    def __init__(self):
        self.last_w = None
        self.readers = []


class Op:
    __slots__ = ("eng", "fn", "idx", "deps", "dma", "signal", "cnt", "sem", "semval", "stage")


class Prog:
    ND = {"sp": 10, "act": 2, "pool": 2}

    def __init__(self, nc, st):
        self.nc = nc
        self.esem = {e: st.enter_context(nc.semaphore("s_" + e)) for e in ENGS}
        self.dsem = {(q, k): st.enter_context(nc.semaphore("d_%s%d" % (q, k))) for q in self.ND for k in range(self.ND[q])}
        self.base = {e: 0 for e in ENGS}
        self.dk = {q: 0 for q in self.ND}
        self.dval = {k: 0 for k in self.dsem}
        self.waited = {e: {} for e in ENGS}
        self.stage = 0
        self.ops = {e: [] for e in ENGS}
        self.ninst = 0

    def op(self, eng, fn, reads=(), writes=(), dma=False):
        o = Op()
        o.eng, o.fn, o.idx, o.dma, o.signal, o.cnt, o.stage = eng, fn, len(self.ops[eng]), dma, False, 0, self.stage
        o.sem, o.semval = None, 0
        deps = {}
        for b in reads:
            if b.last_w is not None and b.last_w.stage == self.stage:
                deps[id(b.last_w)] = b.last_w
        for b in writes:
            if b.last_w is not None and b.last_w.stage == self.stage:
                deps[id(b.last_w)] = b.last_w
            for r in b.readers:
                if r.stage == self.stage:
                    deps[id(r)] = r
        o.deps = list(deps.values())
        for b in reads:
            b.readers.append(o)
        for b in writes:
            b.last_w = o
            b.readers = []
        if dma:
            k = self.dk[eng]
            self.dk[eng] += 1
            nd = self.ND[eng]
            o.sem = (eng, k % nd)
            o.semval = 16 * (k // nd + 1)
            self.dval[o.sem] = o.semval
        self.ops[eng].append(o)
        self.ninst += 1
        return o

    def pe(self, fn, reads=(), writes=()):
        return self.op("pe", fn, reads, writes)

    def act(self, fn, reads=(), writes=()):
        return self.op("act", fn, reads, writes)

    def dve(self, fn, reads=(), writes=()):
        return self.op("dve", fn, reads, writes)

    def pool(self, fn, reads=(), writes=()):
        return self.op("pool", fn, reads, writes)

    def dma(self, fn, reads=(), writes=(), q="sp"):
        return self.op(q, fn, reads, writes, dma=True)

    def end_stage(self):
        nc = self.nc
        for e in ENGS:
            comp = [o for o in self.ops[e] if not o.dma]
            if comp:
                comp[-1].signal = True
            for o in self.ops[e]:
                for d in o.deps:
                    if d.dma:
                        continue
                    if d.eng == o.eng:
                        if e == "pe":
                            continue
                        if o.idx - d.idx > 2 and not o.dma:
                            continue
                    d.signal = True
        final = {}
        for e in ENGS:
            c = self.base[e]
            for o in self.ops[e]:
                if o.dma:
                    continue
                if o.signal:
                    c += 1
                o.cnt = c
            final[e] = c
        with nc.Block() as block:
            def run_engine(e, eng):
                waited = self.waited[e]

                def need(sem, key, val):
                    if waited.get(key, 0) < val:
                        eng.wait_ge(sem, val)
                        waited[key] = val

                for o in self.ops[e]:
                    for d in o.deps:
                        if d.dma:
                            need(self.dsem[d.sem], d.sem, d.semval)
                        else:
                            if d.eng == e:
                                if e == "pe":
                                    continue
                                if o.idx - d.idx > 2 and not o.dma:
                                    continue
                            need(self.esem[d.eng], d.eng, d.cnt)
                    if o.dma:
                        if o.semval > 16:
                            need(self.dsem[o.sem], o.sem, o.semval - 16)
                        inst = o.fn(eng)
                        inst.then_inc(self.dsem[o.sem], 16)
                    else:
                        inst = o.fn(eng)
                        if o.signal:
                            inst.then_inc(self.esem[e], 1)
                for e2 in ENGS:
                    if e2 != e and final[e2] > 0:
                        need(self.esem[e2], e2, final[e2])
                for k, v in self.dval.items():
                    if v > 0:
                        need(self.dsem[k], k, v)

            for e in ENGS:
                def _f(eng, e=e):
                    run_engine(e, eng)
                getattr(block, ENGOBJ[e])(_f)
        self.base = final
        self.stage += 1
        self.ops = {e: [] for e in ENGS}


class Ctx:
    def __init__(self):
        self.nc = bass.Bass("TRN2", target_bir_lowering=False)
        self.gst = contextlib.ExitStack()
        self.P = Prog(self.nc, self.gst)
        self.st = None
        self.n = 0
        self.dbg = {}

    def din(self, name, shape, dt=F32):
        return self.nc.dram_tensor(name, list(shape), dt, kind="ExternalInput").ap()

    def dout(self, name, shape, dt=F32):
        return self.nc.dram_tensor(name, list(shape), dt, kind="ExternalOutput").ap()

    def dscr(self, name, shape, dt=F32, debug=False):
        if debug:
            return self.dout(name, shape, dt)
        return self.nc.dram_tensor(name, list(shape), dt, kind="Internal").ap()

    def begin(self):
        self.st = contextlib.ExitStack()

    def end(self):
        self.P.end_stage()
        self.st.close()
        self.st = None

    def sb(self, shape, dt=F32):
        self.n += 1
        t = self.st.enter_context(self.nc.sbuf_tensor("t%d" % self.n, list(shape), dt))
        return t, Buf()

    def ps(self, shape, dt=F32):
        self.n += 1
        t = self.st.enter_context(self.nc.psum_tensor("p%d" % self.n, list(shape), dt))
        return t, Buf()

    def finish(self):
        self.gst.close()
        return self.nc


def stage_proj(C, xT, w, wsw, g8, cos, sin, pT, L, NCT, NRT):
    P = C.P
    C.begin()
    TB = min(512, L)
    NB = L // TB
    wb, _ = C.sb([128, 8, NCT * 128], BF16)
    wswb, _ = C.sb([128, 8, NRT * 128], BF16)
    b_w = [Buf() for _ in range(8)]
    b_ws = [Buf() for _ in range(8)]
    gt, b_g = C.sb([128, 8])
    ones, b_ones = C.sb([128, 128], BF16)
    P.dma(lambda e: e.dma_start(out=gt[:], in_=g8), writes=[b_g])
    P.dve(lambda e: e.memset(ones[:], 1.0), writes=[b_ones])
    epsc, b_epsc = C.sb([128, 1])
    P.dve(lambda e: e.memset(epsc[:], EPS), writes=[b_epsc])
    CH = 640
    for kc in range(8):
        for c0 in range(0, NCT * 128, CH):
            P.dma(lambda e, kc=kc, c0=c0: e.dma_start(out=wb[:, kc, c0:c0 + CH], in_=w[kc * 128:(kc + 1) * 128, c0:c0 + CH]),
                  writes=[b_w[kc]], q="pool")
        for c0 in range(0, NRT * 128, 512):
            P.dma(lambda e, kc=kc, c0=c0: e.dma_start(out=wswb[:, kc, c0:c0 + 512], in_=wsw[kc * 128:(kc + 1) * 128, c0:c0 + 512]),
                  writes=[b_ws[kc]], q="pool")
    nxb = 2 if NCT <= 24 else 1
    xts = [C.sb([128, 8, TB]) for _ in range(nxb)]
    xbs = [C.sb([128, 8, TB], BF16) for _ in range(nxb)]
    sq, b_sq = C.sb([128, 8, TB], BF16)
    css = [C.sb([128, TB]) for _ in range(2)]
    sns = [C.sb([128, TB]) for _ in range(2)]
    rss = [C.sb([128, TB]) for _ in range(2)]
    pss, b_pss = C.ps([128, TB])
    ps_a = [C.ps([128, TB]) for _ in range(2)]
    ps_b = [C.ps([128, TB]) for _ in range(2)]
    t1s = [C.sb([128, TB]) for _ in range(2)]
    t2s = [C.sb([128, TB]) for _ in range(2)]
    os_ = [C.sb([128, TB]) for _ in range(4)]
    b_out = Buf()
    xv = xT.rearrange("(kc p) n -> p kc n", p=128)
    no = 0
    na = 0
    for tb in range(NB):
        sl = slice(tb * TB, (tb + 1) * TB)
        xt, b_x = xts[tb % nxb]
        xb, b_xb = xbs[tb % nxb]
        cs, b_cs = css[tb % 2]
        sn, b_sn = sns[tb % 2]
        rs, b_rs = rss[tb % 2]
        P.dma(lambda e, xt=xt, sl=sl: e.dma_start(out=xt[:], in_=xv[:, :, sl]), writes=[b_x])
        if NRT:
            P.dma(lambda e, cs=cs, sl=sl: e.dma_start(out=cs[:], in_=cos[:, sl]), writes=[b_cs])
            P.dma(lambda e, sn=sn, sl=sl: e.dma_start(out=sn[:], in_=sin[:, sl]), writes=[b_sn])
        P.pool(lambda e, xt=xt: e.tensor_tensor(out=sq[:], in0=xt[:], in1=xt[:], op=ALU.mult), reads=[b_x], writes=[b_sq])
        for kc in range(8):
            eng = P.dve if kc % 2 == 0 else P.pool
            eng(lambda e, kc=kc, xt=xt, xb=xb: e.tensor_scalar(out=xb[:, kc, :], in0=xt[:, kc, :], scalar1=gt[:, kc:kc + 1], scalar2=None, op0=ALU.mult),
                reads=[b_x, b_g], writes=[b_xb])
        for kc in range(8):
            P.pe(lambda e, kc=kc: e.matmul(pss[:], ones[:], sq[:, kc, :], start=(kc == 0), stop=(kc == 7)),
                 reads=[b_sq, b_ones], writes=[b_pss])
        P.act(lambda e, rs=rs: e.activation(out=rs[:], in_=pss[:], func=AF.Ln, scale=1.0 / D, bias=epsc[:, 0:1]), reads=[b_pss, b_epsc], writes=[b_rs])
        P.act(lambda e, rs=rs: e.activation(out=rs[:], in_=rs[:], func=AF.Exp, scale=-0.5), reads=[b_rs], writes=[b_rs])
        if NRT:
            P.pool(lambda e, cs=cs, rs=rs: e.tensor_tensor(out=cs[:], in0=cs[:], in1=rs[:], op=ALU.mult), reads=[b_cs, b_rs], writes=[b_cs])
            P.pool(lambda e, sn=sn, rs=rs: e.tensor_tensor(out=sn[:], in0=sn[:], in1=rs[:], op=ALU.mult), reads=[b_sn, b_rs], writes=[b_sn])
        for ct in range(NCT):
            pa, b_pa = ps_a[na % 2]
            pb, b_pb = ps_b[na % 2]
            na += 1
            o, b_o = os_[no % 4]
            no += 1
            for kc in range(8):
                P.pe(lambda e, kc=kc, ct=ct, pa=pa, xb=xb: e.matmul(pa[:], wb[:, kc, ct * 128:(ct + 1) * 128], xb[:, kc, :], start=(kc == 0), stop=(kc == 7)),
                     reads=[b_w[kc], b_xb], writes=[b_pa])
            if ct < NRT:
                for kc in range(8):
                    P.pe(lambda e, kc=kc, ct=ct, pb=pb, xb=xb: e.matmul(pb[:], wswb[:, kc, ct * 128:(ct + 1) * 128], xb[:, kc, :], start=(kc == 0), stop=(kc == 7)),
                         reads=[b_ws[kc], b_xb], writes=[b_pb])
                t1, b_t1 = t1s[ct % 2]
                t2, b_t2 = t2s[ct % 2]
                P.dve(lambda e, t1=t1, pa=pa, cs=cs: e.tensor_tensor(out=t1[:], in0=pa[:], in1=cs[:], op=ALU.mult), reads=[b_pa, b_cs], writes=[b_t1])
                P.dve(lambda e, t2=t2, pb=pb, sn=sn: e.tensor_tensor(out=t2[:], in0=pb[:], in1=sn[:], op=ALU.mult), reads=[b_pb, b_sn], writes=[b_t2])
                P.pool(lambda e, o=o, t1=t1, t2=t2: e.tensor_tensor(out=o[:], in0=t1[:], in1=t2[:], op=ALU.add), reads=[b_t1, b_t2], writes=[b_o])
            else:
                P.dve(lambda e, o=o, pa=pa, rs=rs: e.tensor_tensor(out=o[:], in0=pa[:], in1=rs[:], op=ALU.mult), reads=[b_pa, b_rs], writes=[b_o])
            P.dma(lambda e, o=o, ct=ct, sl=sl: e.dma_start(out=pT[ct * 128:(ct + 1) * 128, sl], in_=o[:]), reads=[b_o], writes=[Buf()])
    C.end()


def stage_gates(C, pT, mlb, ident, tabd, L, mg_row):
    P = C.P
    C.begin()
    NCH = L // 128
    s = 128.0 ** -0.5
    idt, b_id = C.sb([128, 128])
    mb, b_mb = C.sb([128, 16])
    nfb, b_nfb = C.sb([128, 8])
    lns, b_lns = C.sb([128, 1])
    onesq, b_onesq = C.sb([128, 128])
    P.dma(lambda e: e.dma_start(out=idt[:], in_=ident), writes=[b_id])
    P.dma(lambda e: e.dma_start(out=mb[:], in_=mlb), writes=[b_mb])
    P.dve(lambda e: e.tensor_scalar(out=nfb[:], in0=mb[:, 8:16], scalar1=-1.0, scalar2=None, op0=ALU.mult), reads=[b_mb], writes=[b_nfb])
    P.dve(lambda e: e.memset(lns[:], float(np.log(s))), writes=[b_lns])
    P.dve(lambda e: e.memset(onesq[:], 1.0), writes=[b_onesq])
    b_out = Buf()
    gps = [C.ps([128, 128]) for _ in range(3)]
    for h in range(4):
        for dr in range(2):
            gi, b_gi = C.sb([128, 128])
            gf, b_gf = C.sb([128, 128])
            ri = mg_row + dr * 4 + h
            rf = mg_row + 8 + dr * 4 + h
            P.dma(lambda e, gi=gi, ri=ri: e.dma_start(out=gi[0:NCH, :], in_=pT[ri:ri + 1, :].rearrange("o (c t) -> (o c) t", t=128)), writes=[b_gi])
            P.dma(lambda e, gf=gf, rf=rf: e.dma_start(out=gf[0:NCH, :], in_=pT[rf:rf + 1, :].rearrange("o (c t) -> (o c) t", t=128)), writes=[b_gf])
            sp, b_sp = C.sb([128, 128])
            csp, b_csp = C.sb([128, 128])
            col = dr * 4 + h
            P.act(lambda e, sp=sp, gf=gf, col=col: e.activation(out=sp[0:NCH, :], in_=gf[0:NCH, :], func=AF.Exp, scale=-1.0, bias=nfb[0:NCH, col:col + 1]),
                  reads=[b_gf, b_nfb], writes=[b_sp])
            P.act(lambda e, sp=sp: e.activation(out=sp[0:NCH, :], in_=sp[0:NCH, :], func=AF.Ln, scale=1.0, bias=1.0), reads=[b_sp], writes=[b_sp])
            P.dve(lambda e, csp=csp, sp=sp: e.tensor_tensor_scan(out=csp[0:NCH, :], data0=onesq[0:NCH, :], data1=sp[0:NCH, :], initial=0.0, op0=ALU.mult, op1=ALU.add),
                  reads=[b_sp, b_onesq], writes=[b_csp])
            ex, b_ex = C.sb([128, 128])
            if dr == 0:
                P.dve(lambda e, ex=ex, csp=csp: e.tensor_copy(out=ex[0:NCH, :], in_=csp[0:NCH, :]), reads=[b_csp], writes=[b_ex])
            else:
                P.dve(lambda e, ex=ex, sp=sp, csp=csp: e.tensor_tensor(out=ex[0:NCH, :], in0=sp[0:NCH, :], in1=csp[0:NCH, :], op=ALU.subtract), reads=[b_sp, b_csp], writes=[b_ex])
                P.dve(lambda e, ex=ex, csp=csp: e.tensor_scalar(out=ex[0:NCH, :], in0=ex[0:NCH, :], scalar1=csp[0:NCH, 127:128], scalar2=None, op0=ALU.add), reads=[b_ex, b_csp], writes=[b_ex])
            av, b_av = C.sb([128, 128])
            cv, b_cv = C.sb([128, 128])
            ebc, b_ebc = C.sb([128, 1])
            P.act(lambda e, av=av, ex=ex: e.activation(out=av[0:NCH, :], in_=ex[0:NCH, :], func=AF.Exp, scale=-1.0), reads=[b_ex], writes=[b_av])
            P.dve(lambda e, cv=cv, gi=gi, ex=ex: e.tensor_tensor(out=cv[0:NCH, :], in0=gi[0:NCH, :], in1=ex[0:NCH, :], op=ALU.add), reads=[b_gi, b_ex], writes=[b_cv])
            P.dve(lambda e, cv=cv, col=col: e.tensor_scalar(out=cv[0:NCH, :], in0=cv[0:NCH, :], scalar1=mb[0:NCH, col:col + 1], scalar2=lns[0:NCH, 0:1], op0=ALU.add, op1=ALU.add),
                  reads=[b_cv, b_mb, b_lns], writes=[b_cv])
            P.act(lambda e, cv=cv: e.activation(out=cv[0:NCH, :], in_=cv[0:NCH, :], func=AF.Exp), reads=[b_cv], writes=[b_cv])
            P.act(lambda e, ebc=ebc, csp=csp: e.activation(out=ebc[0:NCH, :], in_=csp[0:NCH, 127:128], func=AF.Exp, scale=-1.0), reads=[b_csp], writes=[b_ebc])
            tb_, b_tb = C.sb([128, 3, NCH])
            for k, (src, b_src) in enumerate(((av, b_av), (cv, b_cv))):
                pt, b_pt = gps[k]
                P.pe(lambda e, pt=pt, src=src: e.transpose(pt[:, 0:NCH], src[0:NCH, :], idt[0:NCH, 0:NCH]), reads=[b_src, b_id], writes=[b_pt])
                P.act(lambda e, pt=pt, k=k, tb_=tb_: e.activation(out=tb_[:, k, :], in_=pt[:, 0:NCH], func=AF.Copy), reads=[b_pt], writes=[b_tb])
            tm, b_tm = C.sb([128, 128])
            P.dve(lambda e, tm=tm, ebc=ebc: e.tensor_scalar(out=tm[0:NCH, :], in0=onesq[0:NCH, :], scalar1=ebc[0:NCH, 0:1], scalar2=None, op0=ALU.mult),
                  reads=[b_onesq, b_ebc], writes=[b_tm])
            pt, b_pt = gps[2]
            P.pe(lambda e, pt=pt, tm=tm: e.matmul(pt[:, 0:NCH], tm[0:NCH, :], idt[0:NCH, 0:NCH], start=True, stop=True), reads=[b_tm, b_id], writes=[b_pt])
            P.act(lambda e, pt=pt, tb_=tb_: e.activation(out=tb_[:, 2, :], in_=pt[:, 0:NCH], func=AF.Copy), reads=[b_pt], writes=[b_tb])
            j0 = (h * 2 + dr) * 3
            P.dma(lambda e, tb_=tb_, j0=j0: e.dma_start(out=tabd[:, j0:j0 + 3, :], in_=tb_[:]), reads=[b_tb], writes=[Buf()])
    C.end()


def stage_linattn(C, pT, tab, ident, masks, o1, mixT, L, q_row, k_row, v_row, g_row, out_row, mlstm):
    P = C.P
    C.begin()
    NCH = L // 128
    NV = 129 if mlstm else 128
    idt, b_id = C.sb([128, 128])
    mk, b_mk = C.sb([128, 3, 128])
    tb_, b_tb = C.sb([128, 24, NCH])
    P.dma(lambda e: e.dma_start(out=idt[:], in_=ident), writes=[b_id])
    P.dma(lambda e: e.dma_start(out=mk[:], in_=masks.rearrange("m p n -> p m n")), writes=[b_mk])
    P.dma(lambda e: e.dma_start(out=tb_[:], in_=tab), writes=[b_tb])
    epsc, b_epsc = C.sb([128, 1])
    P.dve(lambda e: e.memset(epsc[:], EPS), writes=[b_epsc])
    NBUF = 2
    qTs = [C.sb([128, 128]) for _ in range(NBUF)]
    kTs = [C.sb([128, 128]) for _ in range(NBUF)]
    vTs = [C.sb([128, 128]) for _ in range(NBUF)]
    gTs = [C.sb([128, 128]) for _ in range(NBUF)]
    hfs = [C.sb([128, 128]) for _ in range(NBUF)]
    ktoks = [C.sb([128, 128]) for _ in range(NBUF)]
    vpps = [C.sb([128, NV]) for _ in range(NBUF)]
    sms = [C.sb([128, 128]) for _ in range(NBUF)]
    os_ = [C.sb([128, NV]) for _ in range(NBUF)]
    hs = [C.sb([128, 128]) for _ in range(NBUF)]
    gas = [C.sb([128, 128]) for _ in range(NBUF)]
    outs = [C.sb([128, 128]) for _ in range(NBUF)]
    p_kt = [C.ps([128, 128]) for _ in range(1)]
    p_vt = [C.ps([128, 128]) for _ in range(1)]
    p_s = [C.ps([128, 128]) for _ in range(2)]
    p_o = [C.ps([128, NV]) for _ in range(2)]
    p_kv = [C.ps([128, NV]) for _ in range(1)]
    p_tr = [C.ps([128, 128]) for _ in range(1)]
    cst, b_cst = C.sb([128, NV])
    tmpc, b_tmpc = C.sb([128, NV])
    st6, b_st6 = C.sb([128, 6])
    mv, b_mv = C.sb([128, 2])
    rsd, b_rsd = C.sb([128, 1])
    dn, b_dn = C.sb([128, 1])
    b_o1 = [[Buf() for _ in range(NCH)] for _ in range(4)]
    it = 0
    for h in range(4):
        for dr in range(2):
            j0 = (h * 2 + dr) * 3
            mi = 0 if dr == 0 else (2 if mlstm else 1)
            P.dve(lambda e: e.memset(cst[:], 0.0), writes=[b_cst])
            order = range(NCH) if dr == 0 else range(NCH - 1, -1, -1)
            for n in order:
                i = it % NBUF
                it += 1
                cs_ = slice(n * 128, (n + 1) * 128)
                qT, b_q = qTs[i]
                kT, b_k = kTs[i]
                vT, b_v = vTs[i]
                P.dma(lambda e, qT=qT, cs_=cs_, h=h: e.dma_start(out=qT[:], in_=pT[q_row + h * 128:q_row + (h + 1) * 128, cs_]), writes=[b_q])
                P.dma(lambda e, kT=kT, cs_=cs_, h=h: e.dma_start(out=kT[:], in_=pT[k_row + h * 128:k_row + (h + 1) * 128, cs_]), writes=[b_k])
                P.dma(lambda e, vT=vT, cs_=cs_, h=h: e.dma_start(out=vT[:], in_=pT[v_row + h * 128:v_row + (h + 1) * 128, cs_]), writes=[b_v])
                pk, b_pk = p_kt[0]
                pv, b_pv = p_vt[0]
                ktok, b_kt = ktoks[i]
                vpp, b_vp = vpps[i]
                P.pe(lambda e, pk=pk, kT=kT: e.transpose(pk[:], kT[:], idt[:]), reads=[b_k, b_id], writes=[b_pk])
                P.pe(lambda e, pv=pv, vT=vT: e.transpose(pv[:], vT[:], idt[:]), reads=[b_v, b_id], writes=[b_pv])
                P.act(lambda e, ktok=ktok, pk=pk: e.activation(out=ktok[:], in_=pk[:], func=AF.Copy), reads=[b_pk], writes=[b_kt])
                P.dve(lambda e, vpp=vpp, pv=pv, n=n, j0=j0: e.tensor_scalar(out=vpp[:, 0:128], in0=pv[:], scalar1=tb_[:, j0 + 1, n:n + 1], scalar2=None, op0=ALU.mult),
                      reads=[b_pv, b_tb], writes=[b_vp])
                if mlstm:
                    P.act(lambda e, vpp=vpp, n=n, j0=j0: e.activation(out=vpp[:, 128:129], in_=tb_[:, j0 + 1, n:n + 1], func=AF.Copy), reads=[b_tb], writes=[b_vp])
                ps_, b_ps = p_s[it % 2]
                sm, b_sm = sms[i]
                P.pe(lambda e, ps_=ps_, kT=kT, qT=qT: e.matmul(ps_[:], kT[:], qT[:], start=True, stop=True), reads=[b_k, b_q], writes=[b_ps])
                P.dve(lambda e, sm=sm, ps_=ps_, mi=mi: e.tensor_tensor(out=sm[:], in0=ps_[:], in1=mk[:, mi, :], op=ALU.mult), reads=[b_ps, b_mk], writes=[b_sm])
                po, b_po = p_o[it % 2]
                P.pe(lambda e, po=po, sm=sm, vpp=vpp: e.matmul(po[:], sm[:], vpp[:], start=True, stop=False), reads=[b_sm, b_vp], writes=[b_po])
                P.pe(lambda e, po=po, qT=qT: e.matmul(po[:], qT[:], cst[:], start=False, stop=True), reads=[b_q, b_cst], writes=[b_po])
                o_, b_o = os_[i]
                P.act(lambda e, o_=o_, po=po, n=n, j0=j0: e.activation(out=o_[:], in_=po[:], func=AF.Copy, scale=tb_[:, j0, n:n + 1]), reads=[b_po, b_tb], writes=[b_o])
                pkv, b_pkv = p_kv[0]
                P.pe(lambda e, pkv=pkv, ktok=ktok, vpp=vpp: e.matmul(pkv[:], ktok[:], vpp[:], start=True, stop=True), reads=[b_kt, b_vp], writes=[b_pkv])
                P.dve(lambda e, pkv=pkv: e.tensor_tensor(out=tmpc[:], in0=pkv[:], in1=cst[:], op=ALU.add), reads=[b_pkv, b_cst], writes=[b_tmpc])
                P.dve(lambda e, n=n, j0=j0: e.tensor_scalar(out=cst[:], in0=tmpc[:], scalar1=tb_[:, j0 + 2, n:n + 1], scalar2=None, op0=ALU.mult),
                      reads=[b_tmpc, b_tb], writes=[b_cst])
                hh, b_h = hs[i]
                if mlstm:
                    P.dve(lambda e, o_=o_: e.tensor_scalar(out=dn[:], in0=o_[:, 128:129], scalar1=-1.0, scalar2=1.0, op0=ALU.mult, op1=ALU.max), reads=[b_o], writes=[b_dn])
                    P.dve(lambda e, o_=o_: e.tensor_tensor(out=dn[:], in0=dn[:], in1=o_[:, 128:129], op=ALU.max), reads=[b_dn, b_o], writes=[b_dn])
                    P.dve(lambda e: e.reciprocal(out=dn[:], in_=dn[:]), reads=[b_dn], writes=[b_dn])
                    P.dve(lambda e, hh=hh, o_=o_: e.tensor_scalar(out=hh[:], in0=o_[:, 0:128], scalar1=dn[:, 0:1], scalar2=None, op0=ALU.mult), reads=[b_o, b_dn], writes=[b_h])
                    src, b_src = hh, b_h
                else:
                    src, b_src = o_, b_o
                if dr == 0:
                    P.dma(lambda e, src=src, cs_=cs_, h=h: e.dma_start(out=o1[h, cs_, :], in_=src[:, 0:128]), reads=[b_src], writes=[b_o1[h][n]])
                else:
                    hf, b_hf = hfs[i]
                    gT, b_g = gTs[i]
                    P.dma(lambda e, hf=hf, cs_=cs_, h=h: e.dma_start(out=hf[:], in_=o1[h, cs_, :]), reads=[b_o1[h][n]], writes=[b_hf])
                    P.dma(lambda e, gT=gT, cs_=cs_, h=h: e.dma_start(out=gT[:], in_=pT[g_row + h * 128:g_row + (h + 1) * 128, cs_]), writes=[b_g])
                    P.pool(lambda e, hf=hf, src=src: e.tensor_tensor(out=hf[:], in0=hf[:], in1=src[:, 0:128], op=ALU.add), reads=[b_hf, b_src], writes=[b_hf])
                    P.dve(lambda e, hf=hf: e.bn_stats(out=st6[:], in_=hf[:]), reads=[b_hf], writes=[b_st6])
                    P.dve(lambda e: e.bn_aggr(out=mv[:], in_=st6[:]), reads=[b_st6], writes=[b_mv])
                    P.act(lambda e: e.activation(out=rsd[:], in_=mv[:, 1:2], func=AF.Ln, scale=1.0, bias=epsc[:, 0:1]), reads=[b_mv, b_epsc], writes=[b_rsd])
                    P.act(lambda e: e.activation(out=rsd[:], in_=rsd[:], func=AF.Exp, scale=-0.5), reads=[b_rsd], writes=[b_rsd])
                    P.dve(lambda e, hf=hf: e.tensor_scalar(out=hf[:], in0=hf[:], scalar1=mv[:, 0:1], scalar2=rsd[:, 0:1], op0=ALU.subtract, op1=ALU.mult),
                          reads=[b_hf, b_mv, b_rsd], writes=[b_hf])
                    ptr, b_ptr = p_tr[0]
                    P.pe(lambda e, ptr=ptr, hf=hf: e.transpose(ptr[:], hf[:], idt[:]), reads=[b_hf, b_id], writes=[b_ptr])
                    ga, b_ga = gas[i]
                    P.act(lambda e, ga=ga, gT=gT: e.activation(out=ga[:], in_=gT[:], func=AF.Exp, scale=-1.0), reads=[b_g], writes=[b_ga])
                    P.pool(lambda e, ga=ga: e.tensor_scalar(out=ga[:], in0=ga[:], scalar1=1.0, scalar2=None, op0=ALU.add), reads=[b_ga], writes=[b_ga])
                    P.dve(lambda e, ga=ga: e.reciprocal(out=ga[:], in_=ga[:]), reads=[b_ga], writes=[b_ga])
                    if not mlstm:
                        P.pool(lambda e, ga=ga, gT=gT: e.tensor_tensor(out=ga[:], in0=ga[:], in1=gT[:], op=ALU.mult), reads=[b_ga, b_g], writes=[b_ga])
                    ot, b_ot = outs[i]
                    P.dve(lambda e, ot=ot, ptr=ptr, ga=ga: e.tensor_tensor(out=ot[:], in0=ptr[:], in1=ga[:], op=ALU.mult), reads=[b_ptr, b_ga], writes=[b_ot])
                    P.dma(lambda e, ot=ot, cs_=cs_, h=h: e.dma_start(out=mixT[out_row + h * 128:out_row + (h + 1) * 128, cs_], in_=ot[:]), reads=[b_ot], writes=[Buf()])
    C.end()


PI = float(np.pi)


def _wrap(P, C, x, b_x, shape, add=0.0, scr=None, key="a"):
    if scr is not None and ("u" + key) in scr:
        (u, b_u), (ki, b_ki), (kf, b_kf), (y, b_y), (m, b_m) = [scr[n + key] for n in "uikym"]
    else:
        u, b_u = C.sb(shape)
        ki, b_ki = C.sb(shape, I32)
        kf, b_kf = C.sb(shape)
        y, b_y = C.sb(shape)
        m, b_m = C.sb(shape)
        if scr is not None:
            for n, v in zip("uikym", ((u, b_u), (ki, b_ki), (kf, b_kf), (y, b_y), (m, b_m))):
                scr[n + key] = v
    P.dve(lambda e: e.tensor_scalar(out=u[:], in0=x[:], scalar1=add, scalar2=1.0 / (2 * PI), op0=ALU.add, op1=ALU.mult), reads=[b_x], writes=[b_u])
    P.dve(lambda e: e.tensor_copy(out=ki[:], in_=u[:]), reads=[b_u], writes=[b_ki])
    P.dve(lambda e: e.tensor_copy(out=kf[:], in_=ki[:]), reads=[b_ki], writes=[b_kf])
    P.dve(lambda e: e.tensor_scalar(out=u[:], in0=x[:], scalar1=add, scalar2=None, op0=ALU.add), reads=[b_x, b_kf], writes=[b_u])
    P.dve(lambda e: e.scalar_tensor_tensor(out=y[:], in0=kf[:], scalar=-2 * PI, in1=u[:], op0=ALU.mult, op1=ALU.add), reads=[b_kf, b_u], writes=[b_y])
    P.dve(lambda e: e.tensor_scalar(out=m[:], in0=y[:], scalar1=PI, scalar2=-2 * PI, op0=ALU.is_gt, op1=ALU.mult), reads=[b_y], writes=[b_m])
    P.dve(lambda e: e.tensor_tensor(out=y[:], in0=y[:], in1=m[:], op=ALU.add), reads=[b_y, b_m], writes=[b_y])
    P.dve(lambda e: e.tensor_scalar(out=m[:], in0=y[:], scalar1=-PI, scalar2=2 * PI, op0=ALU.is_lt, op1=ALU.mult), reads=[b_y], writes=[b_m])
    P.dve(lambda e: e.tensor_tensor(out=y[:], in0=y[:], in1=m[:], op=ALU.add), reads=[b_y, b_m], writes=[b_y])
    P.dve(lambda e: e.tensor_scalar(out=y[:], in0=y[:], scalar1=-PI, scalar2=PI, op0=ALU.max, op1=ALU.min), reads=[b_y], writes=[b_y])
    return y, b_y


def stage_s5(C, pT, prm, consts, yf, mixT, L, u_row, out_row):
    P = C.P
    T = min(512, L)
    NBK = L // T
    for dr in range(2):
        for ct in range(4):
            C.begin()
            idt, b_id = C.sb([128, 128])
            psw, b_psw = C.sb([128, 128])
            tau, b_tau = C.sb([128, T])
            ks, b_ks = C.sb([128, 3])
            lst, b_lst = C.sb([128, 64])
            dsk, b_dsk = C.sb([128, 4])
            onesT, b_onesT = C.sb([128, T])
            P.dma(lambda e: e.dma_start(out=idt[:], in_=consts["ident"]), writes=[b_id])
            P.dma(lambda e: e.dma_start(out=psw[:], in_=consts["psw"]), writes=[b_psw])
            P.dma(lambda e: e.dma_start(out=tau[:], in_=consts["tau"]), writes=[b_tau])
            P.dma(lambda e: e.dma_start(out=ks[:], in_=consts["ksel"]), writes=[b_ks])
            P.dma(lambda e: e.dma_start(out=lst[:], in_=prm["lstep"]), writes=[b_lst])
            P.dma(lambda e: e.dma_start(out=dsk[:], in_=prm["dsk"]), writes=[b_dsk])
            P.dve(lambda e: e.memset(onesT[:], 1.0), writes=[b_onesT])
            G = []
            scr = {}
            ang, b_ang = C.sb([128, T])
            are2, b_are2 = C.sb([128, 64])
            aim2, b_aim2 = C.sb([128, 64])
            P.dma(lambda e: e.dma_start(out=are2[:], in_=prm['are2']), writes=[b_are2])
            P.dma(lambda e: e.dma_start(out=aim2[:], in_=prm['aim2']), writes=[b_aim2])
            ptr = [C.ps([128, 128]) for _ in range(2)]
            for gp in range(8):
                g = ct * 8 + gp
                are, b_are = C.sb([128, 1])
                aim, b_aim = C.sb([128, 1])
                P.dve(lambda e, are=are, cg=dr * 32 + g: e.tensor_copy(out=are[:], in_=are2[:, cg:cg + 1]), reads=[b_are2], writes=[b_are])
                P.dve(lambda e, aim=aim, cg=dr * 32 + g: e.tensor_copy(out=aim[:], in_=aim2[:, cg:cg + 1]), reads=[b_aim2], writes=[b_aim])
                dl, b_dl = C.sb([128, 1])
                r, b_r = C.sb([128, 1])
                th, b_th = C.sb([128, 1])
                col = dr * 32 + g
                P.act(lambda e, dl=dl, col=col: e.activation(out=dl[:], in_=lst[:, col:col + 1], func=AF.Exp), reads=[b_lst], writes=[b_dl])
                P.act(lambda e, r=r, are=are, dl=dl: e.activation(out=r[:], in_=are[:], func=AF.Exp, scale=dl[:, 0:1]), reads=[b_are, b_dl], writes=[b_r])
                P.dve(lambda e, th=th, aim=aim, dl=dl: e.tensor_tensor(out=th[:], in0=aim[:], in1=dl[:], op=ALU.mult), reads=[b_aim, b_dl], writes=[b_th])
                thr0, b_thr0 = _wrap(P, C, th, b_th, [128, 1], scr=scr, key='c')
                thr, b_thr = C.sb([128, 1])
                P.dve(lambda e, thr=thr, thr0=thr0: e.tensor_copy(out=thr[:], in_=thr0[:]), reads=[b_thr0], writes=[b_thr])
                thc, b_thc = _wrap(P, C, thr, b_thr, [128, 1], add=PI / 2, scr=scr, key='d')
                s0, b_s0 = C.sb([128, 1])
                c0, b_c0 = C.sb([128, 1])
                P.act(lambda e, s0=s0, thr=thr: e.activation(out=s0[:], in_=thr[:], func=AF.Sin), reads=[b_thr], writes=[b_s0])
                P.act(lambda e, c0=c0, thc=thc: e.activation(out=c0[:], in_=thc[:], func=AF.Sin), reads=[b_thc], writes=[b_c0])
                nre, b_nre = C.sb([128, 1])
                nim, b_nim = C.sb([128, 1])
                den, b_den = C.sb([128, 1])
                t0, b_t0 = C.sb([128, 1])
                kre, b_kre = C.sb([128, 1])
                kim, b_kim = C.sb([128, 1])
                P.dve(lambda e, nre=nre, r=r, c0=c0: e.tensor_tensor(out=nre[:], in0=r[:], in1=c0[:], op=ALU.mult), reads=[b_r, b_c0], writes=[b_nre])
                P.dve(lambda e, nre=nre: e.tensor_scalar(out=nre[:], in0=nre[:], scalar1=-1.0, scalar2=None, op0=ALU.add), reads=[b_nre], writes=[b_nre])
                P.dve(lambda e, nim=nim, r=r, s0=s0: e.tensor_tensor(out=nim[:], in0=r[:], in1=s0[:], op=ALU.mult), reads=[b_r, b_s0], writes=[b_nim])
                P.dve(lambda e, den=den, are=are: e.tensor_tensor(out=den[:], in0=are[:], in1=are[:], op=ALU.mult), reads=[b_are], writes=[b_den])
                P.dve(lambda e, den=den, aim=aim: e.scalar_tensor_tensor(out=den[:], in0=aim[:], scalar=aim[:, 0:1], in1=den[:], op0=ALU.mult, op1=ALU.add), reads=[b_aim, b_den], writes=[b_den])
                P.dve(lambda e, den=den: e.reciprocal(out=den[:], in_=den[:]), reads=[b_den], writes=[b_den])
                P.dve(lambda e, t0=t0, nre=nre, are=are: e.tensor_tensor(out=t0[:], in0=nre[:], in1=are[:], op=ALU.mult), reads=[b_nre, b_are], writes=[b_t0])
                P.dve(lambda e, kre=kre, nim=nim, aim=aim, t0=t0: e.scalar_tensor_tensor(out=kre[:], in0=nim[:], scalar=aim[:, 0:1], in1=t0[:], op0=ALU.mult, op1=ALU.add), reads=[b_nim, b_aim, b_t0], writes=[b_kre])
                P.dve(lambda e, kre=kre, den=den: e.tensor_tensor(out=kre[:], in0=kre[:], in1=den[:], op=ALU.mult), reads=[b_kre, b_den], writes=[b_kre])
                P.dve(lambda e, t0=t0, nre=nre, aim=aim: e.tensor_tensor(out=t0[:], in0=nre[:], in1=aim[:], op=ALU.mult), reads=[b_nre, b_aim], writes=[b_t0])
                P.dve(lambda e, kim=kim, nim=nim, are=are, t0=t0: e.scalar_tensor_tensor(out=kim[:], in0=nim[:], scalar=are[:, 0:1], in1=t0[:], op0=ALU.mult, op1=ALU.subtract), reads=[b_nim, b_are, b_t0], writes=[b_kim])
                P.dve(lambda e, kim=kim, den=den: e.tensor_tensor(out=kim[:], in0=kim[:], in1=den[:], op=ALU.mult), reads=[b_kim, b_den], writes=[b_kim])
                cA, b_cA = C.sb([128, 1])
                cB, b_cB = C.sb([128, 1])
                cC, b_cC = C.sb([128, 1])
                P.dve(lambda e, cA=cA, kre=kre: e.tensor_tensor(out=cA[:], in0=kre[:], in1=ks[:, 0:1], op=ALU.mult), reads=[b_kre, b_ks], writes=[b_cA])
                P.dve(lambda e, cA=cA, kim=kim: e.scalar_tensor_tensor(out=cA[:], in0=kim[:], scalar=ks[:, 1:2], in1=cA[:], op0=ALU.mult, op1=ALU.add), reads=[b_kim, b_ks, b_cA], writes=[b_cA])
                P.dve(lambda e, cB=cB, kre=kre: e.tensor_tensor(out=cB[:], in0=kre[:], in1=ks[:, 1:2], op=ALU.mult), reads=[b_kre, b_ks], writes=[b_cB])
                P.dve(lambda e, cB=cB, kim=kim: e.scalar_tensor_tensor(out=cB[:], in0=kim[:], scalar=ks[:, 0:1], in1=cB[:], op0=ALU.mult, op1=ALU.subtract), reads=[b_kim, b_ks, b_cB], writes=[b_cB])
                P.dve(lambda e, cC=cC, cB=cB: e.tensor_copy(out=cC[:], in_=cB[:]), reads=[b_cB], writes=[b_cC])
                P.dve(lambda e, cB=cB, cC=cC: e.tensor_scalar(out=cB[:], in0=cC[:], scalar1=-1.0, scalar2=None, op0=ALU.mult), reads=[b_cC], writes=[b_cB])
                P.dve(lambda e, thr=thr: e.tensor_scalar(out=ang[:], in0=tau[:], scalar1=thr[:, 0:1], scalar2=None, op0=ALU.mult), reads=[b_tau, b_thr], writes=[b_ang])
                aw, b_aw = _wrap(P, C, ang, b_ang, [128, T], scr=scr, key='A')
                ac, b_ac = _wrap(P, C, aw, b_aw, [128, T], add=PI / 2, scr=scr, key='B')
                St, b_St = C.sb([128, T])
                Ct, b_Ct = C.sb([128, T])
                Rb, b_Rb = C.sb([128, T])
                P.act(lambda e, St=St, aw=aw: e.activation(out=St[:], in_=aw[:], func=AF.Sin), reads=[b_aw], writes=[b_St])
                P.act(lambda e, Ct=Ct, ac=ac: e.activation(out=Ct[:], in_=ac[:], func=AF.Sin), reads=[b_ac], writes=[b_Ct])
                P.dve(lambda e, Rb=Rb, r=r: e.tensor_scalar(out=Rb[:], in0=onesT[:], scalar1=r[:, 0:1], scalar2=None, op0=ALU.mult), reads=[b_onesT, b_r], writes=[b_Rb])
                bre, b_bre = C.sb([128, 16])
                bim, b_bim = C.sb([128, 16])
                for half in range(2):
                    P.dma(lambda e, half=half, bre=bre, g=g: e.dma_start(out=bre[half * 64:(half + 1) * 64, :], in_=prm["b_re"][dr, g, :, :]), writes=[b_bre])
                    P.dma(lambda e, half=half, bim=bim, g=g: e.dma_start(out=bim[half * 64:(half + 1) * 64, :], in_=prm["b_im"][dr, g, :, :]), writes=[b_bim])
                BT = []
                for (c1, b_c1, c2, b_c2) in ((cA, b_cA, cB, b_cB), (cC, b_cC, cA, b_cA)):
                    bp, b_bp = C.sb([128, 128])
                    tt, b_tt = C.sb([128, 16])
                    P.pool(lambda e, bp=bp: e.memset(bp[:], 0.0), writes=[b_bp])
                    P.dve(lambda e, tt=tt, c1=c1, bre=bre: e.tensor_scalar(out=tt[:], in0=bre[:], scalar1=c1[:, 0:1], scalar2=None, op0=ALU.mult), reads=[b_bre, b_c1], writes=[b_tt])
                    P.dve(lambda e, bp=bp, tt=tt, c2=c2, gp=gp, bim=bim: e.scalar_tensor_tensor(out=bp[:, gp * 16:(gp + 1) * 16], in0=bim[:], scalar=c2[:, 0:1], in1=tt[:], op0=ALU.mult, op1=ALU.add),
                          reads=[b_bim, b_c2, b_tt, b_bp], writes=[b_bp])
                    pt, b_pt = ptr[0]
                    bT, b_bT = C.sb([128, 128])
                    P.pe(lambda e, pt=pt, bp=bp: e.transpose(pt[:], bp[:], idt[:]), reads=[b_bp, b_id], writes=[b_pt])
                    P.act(lambda e, bT=bT, pt=pt: e.activation(out=bT[:], in_=pt[:], func=AF.Copy), reads=[b_pt], writes=[b_bT])
                    BT.append((bT, b_bT))
                cc1, b_cc1 = C.sb([16, 128])
                cc2, b_cc2 = C.sb([16, 128])
                P.dma(lambda e, cc1=cc1, g=g: e.dma_start(out=cc1[:, 0:64], in_=prm["c_re"][dr, g, :, :]), writes=[b_cc1])
                P.dma(lambda e, cc1=cc1, g=g: e.dma_start(out=cc1[:, 64:128], in_=prm["c_im"][dr, g, :, :]), writes=[b_cc1])
                P.dma(lambda e, cc2=cc2, g=g: e.dma_start(out=cc2[:, 0:64], in_=prm["c_im"][dr, g, :, :]), writes=[b_cc2])
                P.dma(lambda e, cc2=cc2, g=g: e.dma_start(out=cc2[:, 64:128], in_=prm["c_re"][dr, g, :, :]), writes=[b_cc2])
                cm1, b_cm1 = C.sb([128, 128])
                cm2, b_cm2 = C.sb([128, 128])
                P.pool(lambda e, cm1=cm1: e.memset(cm1[:], 0.0), writes=[b_cm1])
                P.pool(lambda e, cm2=cm2: e.memset(cm2[:], 0.0), writes=[b_cm2])
                pt, b_pt = ptr[1]
                P.pe(lambda e, pt=pt, cc1=cc1: e.transpose(pt[:, 0:16], cc1[:], idt[0:16, 0:16]), reads=[b_cc1, b_id], writes=[b_pt])
                P.dve(lambda e, cm1=cm1, pt=pt, gp=gp: e.tensor_scalar(out=cm1[:, gp * 16:(gp + 1) * 16], in0=pt[:, 0:16], scalar1=ks[:, 2:3], scalar2=None, op0=ALU.mult),
                      reads=[b_pt, b_ks, b_cm1], writes=[b_cm1])
                P.pe(lambda e, pt=pt, cc2=cc2: e.transpose(pt[:, 0:16], cc2[:], idt[0:16, 0:16]), reads=[b_cc2, b_id], writes=[b_pt])
                P.dve(lambda e, cm2=cm2, pt=pt, gp=gp: e.tensor_scalar(out=cm2[:, gp * 16:(gp + 1) * 16], in0=pt[:, 0:16], scalar1=-1.0, scalar2=None, op0=ALU.mult),
                      reads=[b_pt, b_cm2], writes=[b_cm2])
                q_, b_q = C.sb([128, 1])
                rt, b_rt = C.sb([128, 128])
                P.dve(lambda e, q_=q_, St=St: e.tensor_tensor(out=q_[:], in0=St[:, T - 1:T], in1=ks[:, 2:3], op=ALU.mult), reads=[b_St, b_ks], writes=[b_q])
                P.dve(lambda e, rt=rt, Ct=Ct: e.tensor_scalar(out=rt[:], in0=idt[:], scalar1=Ct[:, T - 1:T], scalar2=None, op0=ALU.mult), reads=[b_id, b_Ct], writes=[b_rt])
                P.dve(lambda e, rt=rt, q_=q_: e.scalar_tensor_tensor(out=rt[:], in0=psw[:], scalar=q_[:, 0:1], in1=rt[:], op0=ALU.mult, op1=ALU.add), reads=[b_psw, b_q, b_rt], writes=[b_rt])
                carry, b_carry = C.sb([128, 1])
                P.dve(lambda e, carry=carry: e.memset(carry[:], 0.0), writes=[b_carry])
                G.append(dict(St=(St, b_St), Ct=(Ct, b_Ct), Rb=(Rb, b_Rb), B1=BT[0], B2=BT[1], C1=(cm1, b_cm1), C2=(cm2, b_cm2), RT=(rt, b_rt), carry=(carry, b_carry)))
            uts = [C.sb([128, T]) for _ in range(2)]
            pbs = [C.ps([128, T]) for _ in range(2)]
            pss_ = [C.ps([128, T]) for _ in range(2)]
            py, b_py = C.ps([128, T])
            pc, b_pc = C.ps([128, 1])
            t1s = [C.sb([128, T]) for _ in range(2)]
            t2s = [C.sb([128, T]) for _ in range(2)]
            bps = [C.sb([128, T]) for _ in range(2)]
            ws_ = [C.sb([128, T]) for _ in range(2)]
            wcs = [C.sb([128, T]) for _ in range(2)]
            wss = [C.sb([128, T]) for _ in range(2)]
            yos = [C.sb([128, T]) for _ in range(2)]
            yfs = [C.sb([128, T]) for _ in range(2)]
            it = 0
            blocks = range(NBK) if dr == 0 else range(NBK - 1, -1, -1)
            rows = slice(u_row + ct * 128, u_row + (ct + 1) * 128)
            for bi, bk in enumerate(blocks):
                cs_ = slice(bk * T, (bk + 1) * T)
                ut, b_ut = uts[bi % 2]
                P.dma(lambda e, ut=ut, cs_=cs_: e.dma_start(out=ut[:], in_=pT[rows, cs_]), writes=[b_ut])
                for gp in range(8):
                    gd = G[gp]
                    i = it % 2
                    it += 1
                    pb, b_pb = pbs[i]
                    pq, b_pq = pss_[i]
                    P.pe(lambda e, pb=pb, gd=gd, ut=ut: e.matmul(pb[:], gd["B1"][0][:], ut[:], start=True, stop=True), reads=[gd["B1"][1], b_ut], writes=[b_pb])
                    P.pe(lambda e, pq=pq, gd=gd, ut=ut: e.matmul(pq[:], gd["B2"][0][:], ut[:], start=True, stop=True), reads=[gd["B2"][1], b_ut], writes=[b_pq])
                    t1, b_t1 = t1s[i]
                    t2, b_t2 = t2s[i]
                    bp, b_bp = bps[i]
                    w_, b_w = ws_[i]
                    wc, b_wc = wcs[i]
                    wsn, b_wsn = wss[i]
                    rv = (lambda a: a[:, ::-1]) if dr == 1 else (lambda a: a[:])
                    P.dve(lambda e, t1=t1, pb=pb, gd=gd, rv=rv: e.tensor_tensor(out=t1[:], in0=rv(pb), in1=gd["Ct"][0][:], op=ALU.mult), reads=[b_pb, gd["Ct"][1]], writes=[b_t1])
                    P.dve(lambda e, t2=t2, pq=pq, gd=gd, rv=rv: e.tensor_tensor(out=t2[:], in0=rv(pq), in1=gd["St"][0][:], op=ALU.mult), reads=[b_pq, gd["St"][1]], writes=[b_t2])
                    P.pool(lambda e, bp=bp, t1=t1, t2=t2: e.tensor_tensor(out=bp[:], in0=t1[:], in1=t2[:], op=ALU.add), reads=[b_t1, b_t2], writes=[b_bp])
                    P.dve(lambda e, w_=w_, bp=bp, gd=gd: e.tensor_tensor_scan(out=w_[:], data0=gd["Rb"][0][:], data1=bp[:], initial=gd["carry"][0][:, 0:1], op0=ALU.mult, op1=ALU.add),
                          reads=[gd["Rb"][1], b_bp, gd["carry"][1]], writes=[b_w])
                    P.pool(lambda e, wc=wc, w_=w_, gd=gd: e.tensor_tensor(out=wc[:], in0=w_[:], in1=gd["Ct"][0][:], op=ALU.mult), reads=[b_w, gd["Ct"][1]], writes=[b_wc])
                    P.dve(lambda e, wsn=wsn, w_=w_, gd=gd: e.tensor_tensor(out=wsn[:], in0=w_[:], in1=gd["St"][0][:], op=ALU.mult), reads=[b_w, gd["St"][1]], writes=[b_wsn])
                    P.pe(lambda e, gd=gd, wc=wc, gp=gp: e.matmul(py[:], gd["C1"][0][:], wc[:], start=(gp == 0), stop=False), reads=[gd["C1"][1], b_wc], writes=[b_py])
                    P.pe(lambda e, gd=gd, wsn=wsn, gp=gp: e.matmul(py[:], gd["C2"][0][:], wsn[:], start=False, stop=(gp == 7)), reads=[gd["C2"][1], b_wsn], writes=[b_py])
                    P.pe(lambda e, gd=gd, w_=w_: e.matmul(pc[:], gd["RT"][0][:], w_[:, T - 1:T], start=True, stop=True), reads=[gd["RT"][1], b_w], writes=[b_pc])
                    P.act(lambda e, gd=gd: e.activation(out=gd["carry"][0][:], in_=pc[:], func=AF.Copy), reads=[b_pc], writes=[gd["carry"][1]])
                yo, b_yo = yos[bi % 2]
                if dr == 0:
                    P.act(lambda e, yo=yo: e.activation(out=yo[:], in_=py[:], func=AF.Copy), reads=[b_py], writes=[b_yo])
                    P.dma(lambda e, yo=yo, cs_=cs_: e.dma_start(out=yf[ct * 128:(ct + 1) * 128, cs_], in_=yo[:]), reads=[b_yo], writes=[Buf()])
                else:
                    yft, b_yft = yfs[bi % 2]
                    P.dma(lambda e, yft=yft, cs_=cs_: e.dma_start(out=yft[:], in_=yf[ct * 128:(ct + 1) * 128, cs_]), writes=[b_yft])
                    P.dve(lambda e, yo=yo, yft=yft: e.tensor_tensor(out=yo[:], in0=py[:, ::-1], in1=yft[:], op=ALU.add), reads=[b_py, b_yft], writes=[b_yo])
                    P.dve(lambda e, yo=yo, ut=ut: e.scalar_tensor_tensor(out=yo[:], in0=ut[:], scalar=dsk[:, ct:ct + 1], in1=yo[:], op0=ALU.mult, op1=ALU.add), reads=[b_ut, b_dsk, b_yo], writes=[b_yo])
                    P.dma(lambda e, yo=yo, cs_=cs_: e.dma_start(out=mixT[out_row + ct * 128:out_row + (ct + 1) * 128, cs_], in_=yo[:]), reads=[b_yo], writes=[Buf()])
            C.end()


def stage_out(C, xT, mixT, w_out, gluw, glub, g8, router, ident, x1T, htok, aff, L, even):
    P = C.P
    C.begin()
    TB = min(512, L)
    NB = L // TB
    NTS = TB // 128
    KC = 8 if even else 6
    woutb, _ = C.sb([128, KC, 1024], BF16)
    b_wo = [Buf() for _ in range(KC)]
    for kc in range(KC):
        for c0 in range(0, 1024, 512):
            P.dma(lambda e, kc=kc, c0=c0: e.dma_start(out=woutb[:, kc, c0:c0 + 512], in_=w_out[kc * 128:(kc + 1) * 128, c0:c0 + 512]), writes=[b_wo[kc]], q="pool")
    if even:
        gluwb, b_gw = C.sb([128, 4, 512], BF16)
        for kc in range(4):
            P.dma(lambda e, kc=kc: e.dma_start(out=gluwb[:, kc, :], in_=gluw[kc * 128:(kc + 1) * 128, :]), writes=[b_gw], q="pool")
        gb, b_gb = C.sb([128, 4])
        P.dma(lambda e: e.dma_start(out=gb[:], in_=glub), writes=[b_gb])
        ngb, b_ngb = C.sb([128, 4])
        P.dve(lambda e: e.tensor_scalar(out=ngb[:], in0=gb[:], scalar1=-1.0, scalar2=None, op0=ALU.mult), reads=[b_gb], writes=[b_ngb])
    gt, b_g = C.sb([128, 8])
    wr, b_wr = C.sb([128, 8, 16])
    idt, b_id = C.sb([128, 128])
    ones, b_ones = C.sb([128, 128], BF16)
    P.dma(lambda e: e.dma_start(out=gt[:], in_=g8), writes=[b_g])
    P.dma(lambda e: e.dma_start(out=wr[:], in_=router.rearrange("(kc p) n -> p kc n", p=128)), writes=[b_wr])
    P.dma(lambda e: e.dma_start(out=idt[:], in_=ident), writes=[b_id])
    P.dve(lambda e: e.memset(ones[:], 1.0), writes=[b_ones])
    epsc, b_epsc = C.sb([128, 1])
    P.dve(lambda e: e.memset(epsc[:], EPS), writes=[b_epsc])
    mix, b_mix = C.sb([128, KC, TB])
    mixb, b_mixb = C.sb([128, KC, TB], BF16)
    xt, b_xt = C.sb([128, 8, TB])
    x1t, b_x1 = C.sb([128, 8, TB])
    ht, b_ht = C.sb([128, 8, TB])
    sq, b_sq = C.sb([128, 8, TB], BF16)
    rs, b_rs = C.sb([128, TB])
    yg, b_yg = C.sb([128, 4, TB])
    ygb, b_ygb = C.sb([128, 4, TB], BF16)
    sg, b_sg = C.sb([128, TB])
    hrows = [C.sb([128, 1024]) for _ in range(2)]
    afts = [C.sb([128, 16]) for _ in range(2)]
    ex, b_ex = C.sb([128, 16])
    mx, b_mx = C.sb([128, 1])
    sm, b_sm = C.sb([128, 1])
    pxs = [C.ps([128, TB]) for _ in range(2)]
    pss, b_pss = C.ps([128, TB])
    pz, b_pz = C.ps([128, TB])
    pl, b_pl = C.ps([128, 16])
    ptrs = [C.ps([128, 128]) for _ in range(2)]
    xv = xT.rearrange("(kc p) n -> p kc n", p=128)
    mv = mixT.rearrange("(kc p) n -> p kc n", p=128)
    x1v = x1T.rearrange("(kc p) n -> p kc n", p=128)
    nt = 0
    for tb in range(NB):
        sl = slice(tb * TB, (tb + 1) * TB)
        P.dma(lambda e, sl=sl: e.dma_start(out=mix[:], in_=mv[:, 0:KC, sl]), writes=[b_mix])
        P.dma(lambda e, sl=sl: e.dma_start(out=xt[:], in_=xv[:, :, sl]), writes=[b_xt])
        if even:
            P.act(lambda e: e.activation(out=mixb[:, 0:4, :], in_=mix[:, 0:4, :], func=AF.Copy), reads=[b_mix], writes=[b_mixb])
            P.act(lambda e: e.activation(out=yg[:], in_=mix[:, 4:8, :], func=AF.Gelu), reads=[b_mix], writes=[b_yg])
            P.dve(lambda e: e.tensor_copy(out=ygb[:], in_=yg[:]), reads=[b_yg], writes=[b_ygb])
            for ct in range(4):
                for kc in range(4):
                    P.pe(lambda e, ct=ct, kc=kc: e.matmul(pz[:], gluwb[:, kc, ct * 128:(ct + 1) * 128], ygb[:, kc, :], start=(kc == 0), stop=(kc == 3)),
                         reads=[b_gw, b_ygb], writes=[b_pz])
                P.act(lambda e, ct=ct: e.activation(out=sg[:], in_=pz[:], func=AF.Exp, scale=-1.0, bias=ngb[:, ct:ct + 1]), reads=[b_pz, b_ngb], writes=[b_sg])
                P.pool(lambda e: e.tensor_scalar(out=sg[:], in0=sg[:], scalar1=1.0, scalar2=None, op0=ALU.add), reads=[b_sg], writes=[b_sg])
                P.dve(lambda e: e.reciprocal(out=sg[:], in_=sg[:]), reads=[b_sg], writes=[b_sg])
                P.dve(lambda e, ct=ct: e.tensor_tensor(out=mixb[:, 4 + ct, :], in0=yg[:, ct, :], in1=sg[:], op=ALU.mult), reads=[b_yg, b_sg], writes=[b_mixb])
        else:
            P.act(lambda e: e.activation(out=mixb[:], in_=mix[:], func=AF.Copy), reads=[b_mix], writes=[b_mixb])
        for dt in range(8):
            px, b_px = pxs[dt % 2]
            for kc in range(KC):
                P.pe(lambda e, px=px, dt=dt, kc=kc: e.matmul(px[:], woutb[:, kc, dt * 128:(dt + 1) * 128], mixb[:, kc, :], start=(kc == 0), stop=(kc == KC - 1)),
                     reads=[b_wo[kc], b_mixb], writes=[b_px])
            P.dve(lambda e, px=px, dt=dt: e.tensor_tensor(out=x1t[:, dt, :], in0=px[:], in1=xt[:, dt, :], op=ALU.add), reads=[b_px, b_xt], writes=[b_x1])
        P.dma(lambda e, sl=sl: e.dma_start(out=x1v[:, :, sl], in_=x1t[:]), reads=[b_x1], writes=[Buf()])
        P.pool(lambda e: e.tensor_tensor(out=sq[:], in0=x1t[:], in1=x1t[:], op=ALU.mult), reads=[b_x1], writes=[b_sq])
        for kc in range(8):
            P.pe(lambda e, kc=kc: e.matmul(pss[:], ones[:], sq[:, kc, :], start=(kc == 0), stop=(kc == 7)), reads=[b_sq, b_ones], writes=[b_pss])
        P.act(lambda e: e.activation(out=rs[:], in_=pss[:], func=AF.Ln, scale=1.0 / D, bias=epsc[:, 0:1]), reads=[b_pss, b_epsc], writes=[b_rs])
        P.act(lambda e: e.activation(out=rs[:], in_=rs[:], func=AF.Exp, scale=-0.5), reads=[b_rs], writes=[b_rs])
        for dt in range(8):
            P.dve(lambda e, dt=dt: e.scalar_tensor_tensor(out=ht[:, dt, :], in0=x1t[:, dt, :], scalar=gt[:, dt:dt + 1], in1=rs[:], op0=ALU.mult, op1=ALU.mult),
                  reads=[b_x1, b_g, b_rs], writes=[b_ht])
        for ts in range(NTS):
            tsl = slice(ts * 128, (ts + 1) * 128)
            r0 = tb * TB + ts * 128
            for kc in range(8):
                P.pe(lambda e, kc=kc, tsl=tsl: e.matmul(pl[:], ht[:, kc, tsl], wr[:, kc, :], start=(kc == 0), stop=(kc == 7)), reads=[b_ht, b_wr], writes=[b_pl])
            aft, b_aft = afts[nt % 2]
            hrow, b_hrow = hrows[nt % 2]
            nt += 1
            P.dve(lambda e: e.reduce_max(out=mx[:], in_=pl[:], axis=AX.X), reads=[b_pl], writes=[b_mx])
            P.dve(lambda e: e.tensor_scalar(out=mx[:], in0=mx[:], scalar1=-1.0, scalar2=None, op0=ALU.mult), reads=[b_mx], writes=[b_mx])
            P.act(lambda e: e.activation(out=ex[:], in_=pl[:], func=AF.Exp, bias=mx[:, 0:1], accum_out=sm[:]), reads=[b_pl, b_mx], writes=[b_ex, b_sm])
            P.dve(lambda e: e.reciprocal(out=sm[:], in_=sm[:]), reads=[b_sm], writes=[b_sm])
            P.dve(lambda e, aft=aft: e.tensor_scalar(out=aft[:], in0=ex[:], scalar1=sm[:, 0:1], scalar2=None, op0=ALU.mult), reads=[b_ex, b_sm], writes=[b_aft])
            P.dma(lambda e, aft=aft, r0=r0: e.dma_start(out=aff[r0:r0 + 128, :], in_=aft[:]), reads=[b_aft], writes=[Buf()])
            for kc in range(8):
                ptr, b_ptr = ptrs[kc % 2]
                P.pe(lambda e, ptr=ptr, kc=kc, tsl=tsl: e.transpose(ptr[:], ht[:, kc, tsl], idt[:]), reads=[b_ht, b_id], writes=[b_ptr])
                if kc % 2 == 0:
                    P.act(lambda e, ptr=ptr, kc=kc, hrow=hrow: e.activation(out=hrow[:, kc * 128:(kc + 1) * 128], in_=ptr[:], func=AF.Copy), reads=[b_ptr], writes=[b_hrow])
                else:
                    P.dve(lambda e, ptr=ptr, kc=kc, hrow=hrow: e.tensor_copy(out=hrow[:, kc * 128:(kc + 1) * 128], in_=ptr[:]), reads=[b_ptr], writes=[b_hrow])
            P.dma(lambda e, hrow=hrow, r0=r0: e.dma_start(out=htok[r0:r0 + 128, :], in_=hrow[:]), reads=[b_hrow], writes=[Buf()])
    C.end()


BIG = 1.0e6


def _breg(e, rc, val):
    if 'r' not in rc:
        rc['r'] = e.to_reg(val)
    return rc['r']


def stage_route(C, aff, ustrict, rmask, posd, gmd, L):
    P = C.P
    C.begin()
    NJ = L // 128
    CAP = L // 8
    NCOL = 16 * NJ
    A, b_A = C.sb([128, NJ, 16])
    Ae, b_Ae = C.sb([128, 16, NJ])
    for jc in range(0, NJ, 16):
        je = min(NJ, jc + 16)
        P.dma(lambda e, jc=jc, je=je: e.dma_start(out=A[:, jc:je, :], in_=aff[jc * 128:je * 128, :].rearrange("(j p) e -> p j e", p=128)), writes=[b_A])
    P.dve(lambda e: e.tensor_copy(out=Ae[:], in_=A[:].rearrange("p j e -> p e j")), reads=[b_A], writes=[b_Ae])
    us, b_us = C.sb([128, 128])
    rm, b_rm = C.sb([128, NCOL])
    onesf, b_of = C.sb([128, 128])
    P.dma(lambda e: e.dma_start(out=us[:], in_=ustrict), writes=[b_us])
    P.dma(lambda e: e.dma_start(out=rm[:], in_=rmask), writes=[b_rm])
    P.dve(lambda e: e.memset(onesf[:], 1.0), writes=[b_of])
    lo, b_lo = C.sb([128, 16])
    hi, b_hi = C.sb([128, 16])
    mid, b_mid = C.sb([128, 16])
    cnt, b_cnt = C.sb([128, 16])
    ge, b_ge = C.sb([128, 16])
    d1, b_d1 = C.sb([128, 16])
    cmps = [C.sb([128, NJ]) for _ in range(2)]
    ptot, b_ptot = C.ps([128, 16])
    P.dve(lambda e: e.memset(lo[:], 0.0), writes=[b_lo])
    P.dve(lambda e: e.memset(hi[:], 2.0), writes=[b_hi])
    for it in range(34):
        P.dve(lambda e: e.tensor_tensor(out=mid[:], in0=lo[:], in1=hi[:], op=ALU.add), reads=[b_lo, b_hi], writes=[b_mid])
        P.dve(lambda e: e.tensor_scalar(out=mid[:], in0=mid[:], scalar1=0.5, scalar2=None, op0=ALU.mult), reads=[b_mid], writes=[b_mid])
        for ex in range(16):
            cm, b_cm = cmps[ex % 2]
            P.dve(lambda e, ex=ex, cm=cm: e.tensor_scalar(out=cm[:], in0=Ae[:, ex, :], scalar1=mid[:, ex:ex + 1], scalar2=None, op0=ALU.is_ge, op1=ALU.add, accum_out=cnt[:, ex:ex + 1]),
                  reads=[b_Ae, b_mid], writes=[b_cm, b_cnt])
        P.pe(lambda e: e.matmul(ptot[:], onesf[:], cnt[:], start=True, stop=True), reads=[b_of, b_cnt], writes=[b_ptot])
        P.dve(lambda e: e.tensor_scalar(out=ge[:], in0=ptot[:], scalar1=float(CAP) - 0.5, scalar2=None, op0=ALU.is_ge), reads=[b_ptot], writes=[b_ge])
        P.dve(lambda e: e.tensor_tensor(out=d1[:], in0=mid[:], in1=lo[:], op=ALU.subtract), reads=[b_mid, b_lo], writes=[b_d1])
        P.dve(lambda e: e.tensor_tensor(out=d1[:], in0=d1[:], in1=ge[:], op=ALU.mult), reads=[b_d1, b_ge], writes=[b_d1])
        P.dve(lambda e: e.tensor_tensor(out=lo[:], in0=lo[:], in1=d1[:], op=ALU.add), reads=[b_lo, b_d1], writes=[b_lo])
        P.dve(lambda e: e.tensor_tensor(out=d1[:], in0=hi[:], in1=mid[:], op=ALU.subtract), reads=[b_hi, b_mid], writes=[b_d1])
        P.dve(lambda e: e.tensor_tensor(out=d1[:], in0=d1[:], in1=ge[:], op=ALU.mult), reads=[b_d1, b_ge], writes=[b_d1])
        P.dve(lambda e: e.tensor_tensor(out=hi[:], in0=mid[:], in1=d1[:], op=ALU.add), reads=[b_mid, b_d1], writes=[b_hi])
    Me, b_Me = C.sb([128, 16, NJ])
    gm, b_gm = C.sb([128, 16, NJ])
    for ex in range(16):
        P.dve(lambda e, ex=ex: e.tensor_scalar(out=Me[:, ex, :], in0=Ae[:, ex, :], scalar1=lo[:, ex:ex + 1], scalar2=None, op0=ALU.is_ge), reads=[b_Ae, b_lo], writes=[b_Me])
    P.dve(lambda e: e.tensor_tensor(out=gm[:], in0=Ae[:], in1=Me[:], op=ALU.mult), reads=[b_Ae, b_Me], writes=[b_gm])
    Mf = Me[:].rearrange("p e j -> p (e j)")
    pre, b_pre = C.sb([128, NCOL])
    cn, b_cn = C.sb([128, NCOL])
    off, b_off = C.sb([128, NCOL])
    pp, b_pp = C.ps([128, min(512, NCOL)])
    pc, b_pc = C.ps([128, min(512, NCOL)])
    CW = min(512, NCOL)
    for c0 in range(0, NCOL, CW):
        P.pe(lambda e, c0=c0: e.matmul(pp[:], us[:], Mf[:, c0:c0 + CW], start=True, stop=True), reads=[b_us, b_Me], writes=[b_pp])
        P.act(lambda e, c0=c0: e.activation(out=pre[:, c0:c0 + CW], in_=pp[:], func=AF.Copy), reads=[b_pp], writes=[b_pre])
        P.pe(lambda e, c0=c0: e.matmul(pc[:], onesf[:], Mf[:, c0:c0 + CW], start=True, stop=True), reads=[b_of, b_Me], writes=[b_pc])
        P.act(lambda e, c0=c0: e.activation(out=cn[:, c0:c0 + CW], in_=pc[:], func=AF.Copy), reads=[b_pc], writes=[b_cn])
    P.dve(lambda e: e.tensor_tensor_scan(out=off[:], data0=rm[:], data1=cn[:], initial=0.0, op0=ALU.mult, op1=ALU.add), reads=[b_rm, b_cn], writes=[b_off])
    P.dve(lambda e: e.tensor_tensor(out=off[:], in0=off[:], in1=cn[:], op=ALU.subtract), reads=[b_off, b_cn], writes=[b_off])
    P.dve(lambda e: e.tensor_tensor(out=pre[:], in0=pre[:], in1=off[:], op=ALU.add), reads=[b_pre, b_off], writes=[b_pre])
    P.dve(lambda e: e.tensor_scalar(out=pre[:], in0=pre[:], scalar1=-BIG, scalar2=None, op0=ALU.add), reads=[b_pre], writes=[b_pre])
    P.dve(lambda e: e.tensor_tensor(out=pre[:], in0=pre[:], in1=Mf, op=ALU.mult), reads=[b_pre, b_Me], writes=[b_pre])
    P.dve(lambda e: e.tensor_scalar(out=pre[:], in0=pre[:], scalar1=BIG, scalar2=None, op0=ALU.add), reads=[b_pre], writes=[b_pre])
    pi, b_pi = C.sb([128, NCOL], I32)
    P.dve(lambda e: e.tensor_copy(out=pi[:], in_=pre[:]), reads=[b_pre], writes=[b_pi])
    P.dma(lambda e: e.dma_start(out=posd, in_=pi[:]), reads=[b_pi], writes=[Buf()])
    P.dma(lambda e: e.dma_start(out=gmd, in_=gm[:].rearrange("p e j -> p (e j)")), reads=[b_gm], writes=[Buf()])
    C.end()


def stage_dispatch(C, htok, posd, xe, L):
    P = C.P
    C.begin()
    NJ = L // 128
    CAP = L // 8
    pi, b_pi = C.sb([128, 16, NJ], I32)
    P.dma(lambda e: e.dma_start(out=pi[:].rearrange("p e j -> p (e j)"), in_=posd), writes=[b_pi])
    rc = {}
    hts = [C.sb([128, 1024]) for _ in range(2)]
    ixs = [C.sb([128, 1], I32) for _ in range(4)]
    ni = 0
    for j in range(NJ):
        ht, b_ht = hts[j % 2]
        P.dma(lambda e, ht=ht, j=j: e.dma_start(out=ht[:], in_=htok[j * 128:(j + 1) * 128, :]), writes=[b_ht])
        for ex in range(16):
            ix, b_ix = ixs[ni % 4]
            ni += 1
            P.dve(lambda e, ix=ix, ex=ex, j=j: e.tensor_copy(out=ix[:], in_=pi[:, ex, j:j + 1]), reads=[b_pi], writes=[b_ix])
            P.dma(lambda e, ht=ht, ix=ix, ex=ex: e.indirect_dma_start(out=xe[ex][:, :], out_offset=bass.IndirectOffsetOnAxis(ap=ix[:, :], axis=0),
                                                                      in_=ht[:, :], in_offset=None, bounds_check=_breg(e, rc, CAP - 1), oob_is_err=False),
                  reads=[b_ht, b_ix], writes=[Buf()], q="pool")
    C.end()


def stage_ffn(C, xe, w1, w3, w2, ye, ident, L, FF):
    P = C.P
    C.begin()
    CAP = L // 8
    SB = min(512, CAP)
    NSB = CAP // SB
    NST = SB // 128
    NF = FF // 128
    idt, b_id = C.sb([128, 128])
    P.dma(lambda e: e.dma_start(out=idt[:], in_=ident), writes=[b_id])
    w1b, _ = C.sb([128, 8, FF], BF16)
    w3b, _ = C.sb([128, 8, FF], BF16)
    w2b, _ = C.sb([128, NF, 1024], BF16)
    b_w1 = [Buf() for _ in range(8)]
    b_w3 = [Buf() for _ in range(8)]
    b_w2 = [Buf() for _ in range(NF)]
    xrs = [C.sb([128, 1024]) for _ in range(2)]
    xeT, b_xeT = C.sb([128, 8, SB], BF16)
    hid, b_hid = C.sb([128, NF, SB], BF16)
    sas = [C.sb([128, SB]) for _ in range(2)]
    yrows = [C.sb([128, 1024]) for _ in range(2)]
    ptrs = [C.ps([128, 128]) for _ in range(2)]
    pas = [C.ps([128, SB]) for _ in range(2)]
    pbs = [C.ps([128, SB]) for _ in range(2)]
    pys = [C.ps([128, 512]) for _ in range(2)]
    CH = min(512, FF)
    nx = 0
    ny = 0
    for ex in range(16):
        for kc in range(8):
            for c0 in range(0, FF, CH):
                P.dma(lambda e, ex=ex, kc=kc, c0=c0: e.dma_start(out=w1b[:, kc, c0:c0 + CH], in_=w1[ex, kc * 128:(kc + 1) * 128, c0:c0 + CH]), writes=[b_w1[kc]], q="pool")
                P.dma(lambda e, ex=ex, kc=kc, c0=c0: e.dma_start(out=w3b[:, kc, c0:c0 + CH], in_=w3[ex, kc * 128:(kc + 1) * 128, c0:c0 + CH]), writes=[b_w3[kc]], q="pool")
        for fc in range(NF):
            for c0 in range(0, 1024, 512):
                P.dma(lambda e, ex=ex, fc=fc, c0=c0: e.dma_start(out=w2b[:, fc, c0:c0 + 512], in_=w2[ex, fc * 128:(fc + 1) * 128, c0:c0 + 512]), writes=[b_w2[fc]], q="pool")
        for sb_ in range(NSB):
            for st in range(NST):
                r0 = sb_ * SB + st * 128
                xr, b_xr = xrs[nx % 2]
                nx += 1
                P.dma(lambda e, xr=xr, ex=ex, r0=r0: e.dma_start(out=xr[:], in_=xe[ex][r0:r0 + 128, :]), writes=[b_xr])
                for kc in range(8):
                    ptr, b_ptr = ptrs[kc % 2]
                    P.pe(lambda e, ptr=ptr, xr=xr, kc=kc: e.transpose(ptr[:], xr[:, kc * 128:(kc + 1) * 128], idt[:]), reads=[b_xr, b_id], writes=[b_ptr])
                    if kc % 2 == 0:
                        P.act(lambda e, ptr=ptr, kc=kc, st=st: e.activation(out=xeT[:, kc, st * 128:(st + 1) * 128], in_=ptr[:], func=AF.Copy), reads=[b_ptr], writes=[b_xeT])
                    else:
                        P.dve(lambda e, ptr=ptr, kc=kc, st=st: e.tensor_copy(out=xeT[:, kc, st * 128:(st + 1) * 128], in_=ptr[:]), reads=[b_ptr], writes=[b_xeT])
            for ft in range(NF):
                pa, b_pa = pas[ft % 2]
                pb, b_pb = pbs[ft % 2]
                sa, b_sa = sas[ft % 2]
                for kc in range(8):
                    P.pe(lambda e, pa=pa, kc=kc, ft=ft: e.matmul(pa[:], w1b[:, kc, ft * 128:(ft + 1) * 128], xeT[:, kc, :], start=(kc == 0), stop=(kc == 7)), reads=[b_w1[kc], b_xeT], writes=[b_pa])
                for kc in range(8):
                    P.pe(lambda e, pb=pb, kc=kc, ft=ft: e.matmul(pb[:], w3b[:, kc, ft * 128:(ft + 1) * 128], xeT[:, kc, :], start=(kc == 0), stop=(kc == 7)), reads=[b_w3[kc], b_xeT], writes=[b_pb])
                P.act(lambda e, sa=sa, pa=pa: e.activation(out=sa[:], in_=pa[:], func=AF.Exp, scale=-1.0), reads=[b_pa], writes=[b_sa])
                P.pool(lambda e, sa=sa: e.tensor_scalar(out=sa[:], in0=sa[:], scalar1=1.0, scalar2=None, op0=ALU.add), reads=[b_sa], writes=[b_sa])
                P.dve(lambda e, sa=sa: e.reciprocal(out=sa[:], in_=sa[:]), reads=[b_sa], writes=[b_sa])
                P.dve(lambda e, sa=sa, pa=pa: e.tensor_tensor(out=sa[:], in0=pa[:], in1=sa[:], op=ALU.mult), reads=[b_pa, b_sa], writes=[b_sa])
                P.dve(lambda e, sa=sa, pb=pb, ft=ft: e.tensor_tensor(out=hid[:, ft, :], in0=pb[:], in1=sa[:], op=ALU.mult), reads=[b_pb, b_sa], writes=[b_hid])
            for st in range(NST):
                r0 = sb_ * SB + st * 128
                yrow, b_yrow = yrows[ny % 2]
                ny += 1
                for dh in range(2):
                    py, b_py = pys[dh]
                    for fc in range(NF):
                        P.pe(lambda e, py=py, fc=fc, st=st, dh=dh: e.matmul(py[:], hid[:, fc, st * 128:(st + 1) * 128], w2b[:, fc, dh * 512:(dh + 1) * 512], start=(fc == 0), stop=(fc == NF - 1)),
                             reads=[b_hid, b_w2[fc]], writes=[b_py])
                    if dh == 0:
                        P.act(lambda e, py=py, yrow=yrow: e.activation(out=yrow[:, 0:512], in_=py[:], func=AF.Copy), reads=[b_py], writes=[b_yrow])
                    else:
                        P.dve(lambda e, py=py, yrow=yrow: e.tensor_copy(out=yrow[:, 512:1024], in_=py[:]), reads=[b_py], writes=[b_yrow])
                P.dma(lambda e, yrow=yrow, ex=ex, r0=r0: e.dma_start(out=ye[ex][r0:r0 + 128, :], in_=yrow[:]), reads=[b_yrow], writes=[Buf()])
    C.end()


def stage_combine(C, ye, posd, gmd, x1T, ident, x2T, outT, gfin, L):
    P = C.P
    C.begin()
    NJ = L // 128
    CAP = L // 8
    idt, b_id = C.sb([128, 128])
    P.dma(lambda e: e.dma_start(out=idt[:], in_=ident), writes=[b_id])
    pi, b_pi = C.sb([128, 16, NJ], I32)
    gm, b_gm = C.sb([128, 16, NJ])
    P.dma(lambda e: e.dma_start(out=pi[:].rearrange("p e j -> p (e j)"), in_=posd), writes=[b_pi])
    P.dma(lambda e: e.dma_start(out=gm[:].rearrange("p e j -> p (e j)"), in_=gmd), writes=[b_gm])
    Gs = [C.sb([128, 1024]) for _ in range(2)]
    for G_, b_G in Gs:
        P.dve(lambda e, G_=G_: e.memset(G_[:], 0.0), writes=[b_G])
    accs = [C.sb([128, 1024]) for _ in range(2)]
    x1s = [C.sb([128, 8, 128]) for _ in range(2)]
    x2s = [C.sb([128, 8, 128]) for _ in range(2)]
    ptrs = [C.ps([128, 128]) for _ in range(2)]
    if outT is not None:
        gt, b_g = C.sb([128, 8])
        ones, b_ones = C.sb([128, 128], BF16)
        sq, b_sq = C.sb([128, 8, 128], BF16)
        rs, b_rs = C.sb([128, 128])
        ots = [C.sb([128, 8, 128]) for _ in range(2)]
        pss, b_pss = C.ps([128, 128])
        P.dma(lambda e: e.dma_start(out=gt[:], in_=gfin), writes=[b_g])
        P.dve(lambda e: e.memset(ones[:], 1.0), writes=[b_ones])
        epsc, b_epsc = C.sb([128, 1])
        P.dve(lambda e: e.memset(epsc[:], EPS), writes=[b_epsc])
        ov = outT.rearrange("(kc p) n -> p kc n", p=128)
    x1v = x1T.rearrange("(kc p) n -> p kc n", p=128)
    x2v = x2T.rearrange("(kc p) n -> p kc n", p=128)
    ng = 0
    rc = {}
    ixs = [C.sb([128, 1], I32) for _ in range(4)]
    for j in range(NJ):
        acc, b_acc = accs[j % 2]
        x1t, b_x1 = x1s[j % 2]
        x2t, b_x2 = x2s[j % 2]
        cs_ = slice(j * 128, (j + 1) * 128)
        P.dma(lambda e, x1t=x1t, cs_=cs_: e.dma_start(out=x1t[:], in_=x1v[:, :, cs_]), writes=[b_x1])
        for ex in range(16):
            G_, b_G = Gs[ng % 2]
            ix, b_ix = ixs[ng % 4]
            ng += 1
            P.act(lambda e, ix=ix, ex=ex, j=j: e.activation(out=ix[:], in_=pi[:, ex, j:j + 1], func=AF.Copy), reads=[b_pi], writes=[b_ix])
            P.dma(lambda e, G_=G_, ex=ex, ix=ix: e.indirect_dma_start(out=G_[:, :], out_offset=None, in_=ye[ex][:, :],
                                                                      in_offset=bass.IndirectOffsetOnAxis(ap=ix[:, :], axis=0), bounds_check=_breg(e, rc, CAP - 1), oob_is_err=False),
                  reads=[b_ix], writes=[b_G], q="pool")
            if ex == 0:
                P.dve(lambda e, acc=acc, G_=G_, ex=ex, j=j: e.tensor_scalar(out=acc[:], in0=G_[:], scalar1=gm[:, ex, j:j + 1], scalar2=None, op0=ALU.mult), reads=[b_G, b_gm], writes=[b_acc])
            else:
                P.dve(lambda e, acc=acc, G_=G_, ex=ex, j=j: e.scalar_tensor_tensor(out=acc[:], in0=G_[:], scalar=gm[:, ex, j:j + 1], in1=acc[:], op0=ALU.mult, op1=ALU.add),
                      reads=[b_G, b_gm, b_acc], writes=[b_acc])
        for kc in range(8):
            ptr, b_ptr = ptrs[kc % 2]
            P.pe(lambda e, ptr=ptr, acc=acc, kc=kc: e.transpose(ptr[:], acc[:, kc * 128:(kc + 1) * 128], idt[:]), reads=[b_acc, b_id], writes=[b_ptr])
            P.dve(lambda e, ptr=ptr, x2t=x2t, x1t=x1t, kc=kc: e.tensor_tensor(out=x2t[:, kc, :], in0=ptr[:], in1=x1t[:, kc, :], op=ALU.add), reads=[b_ptr, b_x1], writes=[b_x2])
        P.dma(lambda e, x2t=x2t, cs_=cs_: e.dma_start(out=x2v[:, :, cs_], in_=x2t[:]), reads=[b_x2], writes=[Buf()])
        if outT is not None:
            ot, b_ot = ots[j % 2]
            P.pool(lambda e, x2t=x2t: e.tensor_tensor(out=sq[:], in0=x2t[:], in1=x2t[:], op=ALU.mult), reads=[b_x2], writes=[b_sq])
            for kc in range(8):
                P.pe(lambda e, kc=kc: e.matmul(pss[:], ones[:], sq[:, kc, :], start=(kc == 0), stop=(kc == 7)), reads=[b_sq, b_ones], writes=[b_pss])
            P.act(lambda e: e.activation(out=rs[:], in_=pss[:], func=AF.Ln, scale=1.0 / D, bias=epsc[:, 0:1]), reads=[b_pss, b_epsc], writes=[b_rs])
            P.act(lambda e: e.activation(out=rs[:], in_=rs[:], func=AF.Exp, scale=-0.5), reads=[b_rs], writes=[b_rs])
            for kc in range(8):
                P.dve(lambda e, ot=ot, x2t=x2t, kc=kc: e.scalar_tensor_tensor(out=ot[:, kc, :], in0=x2t[:, kc, :], scalar=gt[:, kc:kc + 1], in1=rs[:], op0=ALU.mult, op1=ALU.mult),
                      reads=[b_x2, b_g, b_rs], writes=[b_ot])
            P.dma(lambda e, ot=ot, cs_=cs_: e.dma_start(out=ov[:, :, cs_], in_=ot[:]), reads=[b_ot], writes=[Buf()])
    C.end()


DILS = (1, 4, 16)
DSPAN = (1, 2, 8)


def dil_masks():
    kk = np.arange(128)[:, None]
    qq = np.arange(128)[None, :]
    ms = []
    for g, d in enumerate(DILS):
        for dl in range(-DSPAN[g], DSPAN[g] + 1):
            rel = 128 * dl + kk - qq
            ms.append(((rel % d == 0) & (np.abs(rel) <= 64 * d)).astype(np.float32))
    return np.stack(ms)


def stage_vprep(C, pT, ident, vtok, L, v_row):
    P = C.P
    C.begin()
    TB = min(512, L)
    NT = TB // 128
    idt, b_id = C.sb([128, 128])
    P.dma(lambda e: e.dma_start(out=idt[:], in_=ident), writes=[b_id])
    vts = [C.sb([64, TB]) for _ in range(2)]
    vos = [C.sb([128, NT, 65]) for _ in range(2)]
    for vo, b_vo in vos:
        P.dve(lambda e, vo=vo: e.memset(vo[:], 1.0), writes=[b_vo])
    ptrs = [C.ps([128, 64]) for _ in range(2)]
    it = 0
    for hd in range(12):
        for tb in range(L // TB):
            vt, b_vt = vts[it % 2]
            vo, b_vo = vos[it % 2]
            it += 1
            P.dma(lambda e, vt=vt, hd=hd, tb=tb: e.dma_start(out=vt[:], in_=pT[v_row + hd * 64:v_row + (hd + 1) * 64, tb * TB:(tb + 1) * TB]), writes=[b_vt])
            for t in range(NT):
                ptr, b_ptr = ptrs[t % 2]
                P.pe(lambda e, ptr=ptr, vt=vt, t=t: e.transpose(ptr[:], vt[:, t * 128:(t + 1) * 128], idt[0:64, 0:64]), reads=[b_vt, b_id], writes=[b_ptr])
                if t % 2 == 0:
                    P.act(lambda e, ptr=ptr, vo=vo, t=t: e.activation(out=vo[:, t, 0:64], in_=ptr[:], func=AF.Copy), reads=[b_ptr], writes=[b_vo])
                else:
                    P.dve(lambda e, ptr=ptr, vo=vo, t=t: e.tensor_copy(out=vo[:, t, 0:64], in_=ptr[:]), reads=[b_ptr], writes=[b_vo])
            P.dma(lambda e, vo=vo, hd=hd, tb=tb: e.dma_start(out=vtok[hd][tb * TB:(tb + 1) * TB, :].rearrange("(t p) c -> p t c", p=128), in_=vo[:]), reads=[b_vo], writes=[Buf()])
    C.end()


def stage_dil(C, pT, ident, masks, vtok, mixT, L, q_row, k_row, out_row):
    P = C.P
    NCH = L // 128
    for i in range(4):
        C.begin()
        idt, b_id = C.sb([128, 128])
        mk, b_mk = C.sb([128, 25, 128])
        P.dma(lambda e: e.dma_start(out=idt[:], in_=ident), writes=[b_id])
        P.dma(lambda e: e.dma_start(out=mk[:], in_=masks.rearrange("m p n -> p m n")), writes=[b_mk])
        qs = [[C.sb([64, 128]) for _ in range(2)] for _ in range(3)]
        ks = [[C.sb([64, (2 * DSPAN[g] + 1) * 128]) for _ in range(2)] for g in range(3)]
        vs = [[C.sb([128, 2 * DSPAN[g] + 1, 65]) for _ in range(2)] for g in range(3)]
        pss = [C.ps([128, 512]) for _ in range(2)]
        pacc = [C.ps([128, 65]) for _ in range(2)]
        ptr, b_ptr = C.ps([64, 128])
        es = [C.sb([128, 512]) for _ in range(2)]
        pms = [C.sb([128, 512]) for _ in range(2)]
        rd, b_rd = C.sb([128, 1])
        ots = [C.sb([128, 64]) for _ in range(2)]
        oTs = [C.sb([64, 128]) for _ in range(2)]
        moff = (0, 3, 8)
        ngrp = 0
        for n in range(NCH):
            pa, b_pa = pacc[n % 2]
            first = True
            plan = []
            for g in range(3):
                hd = 4 * g + i
                D_ = DSPAN[g]
                m0 = max(0, n - D_)
                m1 = min(NCH - 1, n + D_)
                q_, b_q = qs[g][n % 2]
                k_, b_k = ks[g][n % 2]
                v_, b_v = vs[g][n % 2]
                nm = m1 - m0 + 1
                P.dma(lambda e, q_=q_, hd=hd, n=n: e.dma_start(out=q_[:], in_=pT[q_row + hd * 64:q_row + (hd + 1) * 64, n * 128:(n + 1) * 128]), writes=[b_q])
                P.dma(lambda e, k_=k_, hd=hd, m0=m0, nm=nm: e.dma_start(out=k_[:, 0:nm * 128], in_=pT[k_row + hd * 64:k_row + (hd + 1) * 64, m0 * 128:(m0 + nm) * 128]), writes=[b_k])
                P.dma(lambda e, v_=v_, hd=hd, m0=m0, nm=nm: e.dma_start(out=v_[:, 0:nm, :], in_=vtok[hd][m0 * 128:(m0 + nm) * 128, :].rearrange("(t p) c -> p t c", p=128)), writes=[b_v])
                tiles = [(g, m, m - m0, moff[g] + (m - n) + D_) for m in range(m0, m1 + 1)]
                for c0 in range(0, len(tiles), 4):
                    plan.append((g, tiles[c0:c0 + 4], (q_, b_q), (k_, b_k), (v_, b_v)))
            total = sum(len(p[1]) for p in plan)
            done = 0
            for (g, tl, (q_, b_q), (k_, b_k), (v_, b_v)) in plan:
                ps_, b_ps = pss[ngrp % 2]
                e_, b_e = es[ngrp % 2]
                pm, b_pm = pms[ngrp % 2]
                nt = len(tl)
                for a, (_, m, ml, mi) in enumerate(tl):
                    P.pe(lambda e, ps_=ps_, k_=k_, q_=q_, a=a, ml=ml: e.matmul(ps_[:, a * 128:(a + 1) * 128], k_[:, ml * 128:(ml + 1) * 128], q_[:], start=True, stop=True),
                         reads=[b_k, b_q], writes=[b_ps])
                P.act(lambda e, e_=e_, ps_=ps_, nt=nt: e.activation(out=e_[:, 0:nt * 128], in_=ps_[:, 0:nt * 128], func=AF.Exp, scale=0.125), reads=[b_ps], writes=[b_e])
                mi0 = tl[0][3]
                eng = P.dve if ngrp % 2 == 0 else P.pool
                eng(lambda e, pm=pm, e_=e_, nt=nt, mi0=mi0: e.tensor_tensor(out=pm[:, 0:nt * 128], in0=e_[:, 0:nt * 128], in1=mk[:, mi0:mi0 + nt, :].rearrange("p m n -> p (m n)"), op=ALU.mult),
                    reads=[b_e, b_mk], writes=[b_pm])
                for a, (_, m, ml, mi) in enumerate(tl):
                    done += 1
                    P.pe(lambda e, pa=pa, pm=pm, v_=v_, a=a, ml=ml, st=(done == 1), sp=(done == total): e.matmul(pa[:], pm[:, a * 128:(a + 1) * 128], v_[:, ml, :], start=st, stop=sp),
                         reads=[b_pm, b_v], writes=[b_pa])
                ngrp += 1
            ot, b_ot = ots[n % 2]
            oT, b_oT = oTs[n % 2]
            P.dve(lambda e, pa=pa: e.reciprocal(out=rd[:], in_=pa[:, 64:65]), reads=[b_pa], writes=[b_rd])
            P.dve(lambda e, ot=ot, pa=pa: e.tensor_scalar(out=ot[:], in0=pa[:, 0:64], scalar1=rd[:, 0:1], scalar2=None, op0=ALU.mult), reads=[b_pa, b_rd], writes=[b_ot])
            P.pe(lambda e, ot=ot: e.transpose(ptr[:], ot[:], idt[:]), reads=[b_ot, b_id], writes=[b_ptr])
            P.act(lambda e, oT=oT: e.activation(out=oT[:], in_=ptr[:], func=AF.Copy), reads=[b_ptr], writes=[b_oT])
            P.dma(lambda e, oT=oT, n=n: e.dma_start(out=mixT[out_row + i * 64:out_row + (i + 1) * 64, n * 128:(n + 1) * 128], in_=oT[:]), reads=[b_oT], writes=[Buf()])
        C.end()


def rope_tables(L, half, period):
    inv = (10000.0 ** (-np.arange(half, dtype=np.float32) / half)).astype(np.float32)
    ang = (np.arange(L, dtype=np.float32)[None, :] * inv[:, None]).astype(np.float32)
    cos = np.cos(ang).astype(np.float32)
    sin = np.sin(ang).astype(np.float32)
    rows = np.arange(128) % period
    c = cos[rows % half]
    s = np.where((rows < half)[:, None], -sin[rows % half], sin[rows % half])
    return np.ascontiguousarray(c, np.float32), np.ascontiguousarray(s, np.float32)


def swap_cols(w, ncols, period):
    half = period // 2
    idx = np.arange(ncols)
    src = (idx // period) * period + (idx % period + half) % period
    return np.ascontiguousarray(w[:, src])


def ret_tables(L):
    NCH = L // 128
    s = 128.0 ** -0.5
    tab = np.zeros((128, 24, NCH), np.float64)
    t = np.arange(128, dtype=np.float64)
    for h in range(4):
        lg = np.log1p(-(2.0 ** (-5.0 - h)))
        for dr in range(2):
            e = (t + 1) if dr == 0 else (128 - t)
            j0 = (h * 2 + dr) * 3
            tab[:, j0, :] = np.exp(lg * e)[:, None]
            tab[:, j0 + 1, :] = (np.exp(-lg * e) * s)[:, None]
            tab[:, j0 + 2, :] = np.exp(lg * 128)
    return tab.astype(np.float32)


def la_masks():
    j = np.arange(128)[:, None]
    i = np.arange(128)[None, :]
    return np.stack([(j <= i), (j > i), (j >= i)]).astype(np.float32)


def g8(v):
    return np.ascontiguousarray(v.reshape(8, 128).T)


def s5_host(d, T):
    a_re, a_im = d['s5_a_re'][0], d['s5_a_im'][0]
    are2 = np.concatenate([a_re.reshape(64, 64).T] * 2, 0)
    aim2 = np.concatenate([a_im.reshape(64, 64).T] * 2, 0)
    lstep = np.tile(d['s5_log_step'][0].reshape(1, 64), (128, 1))
    dsk = d['s5_d'][0].reshape(4, 128).T
    prm = {"are2": are2, "aim2": aim2, "lstep": lstep, "dsk": dsk, "b_re": d['s5_b_re'][0], "b_im": d['s5_b_im'][0],
           "c_re": d['s5_c_re'][0], "c_im": d['s5_c_im'][0]}
    psw = np.zeros((128, 128), np.float32)
    for k in range(128):
        psw[k, (k + 64) % 128] = 1
    ksel = np.zeros((128, 3), np.float32)
    ksel[:64, 0] = 1; ksel[64:, 1] = 1; ksel[:64, 2] = 1; ksel[64:, 2] = -1
    tau = np.tile(np.arange(1, T + 1, dtype=np.float32)[None, :], (128, 1))
    consts = {"psw": psw, "ksel": ksel, "tau": tau}
    return {k: np.ascontiguousarray(v, np.float32) for k, v in prm.items()}, consts


def route_consts(L):
    NJ = L // 128
    q = np.arange(128)[:, None]; p = np.arange(128)[None, :]
    us = (q < p).astype(np.float32)
    rm = np.ones((16, NJ), np.float32); rm[:, 0] = 0
    rm = np.tile(rm.reshape(1, -1), (128, 1))
    return us, np.ascontiguousarray(rm)


class RowSplit:
    def __init__(self, C, name, rows, L, chunk=1024):
        self.chunk = chunk
        self.parts = [C.dscr("%s_%d" % (name, k), [min(chunk, rows - k * chunk), L]) for k in range(-(-rows // chunk))]

    def __getitem__(self, key):
        rs, cs = key
        k = rs.start // self.chunk
        assert (rs.stop - 1) // self.chunk == k
        return self.parts[k][rs.start - k * self.chunk:rs.stop - k * self.chunk, cs]


def build_program(L, FF):
    NCH = L // 128
    NJ = L // 128
    CAP = L // 8
    T = min(512, L)
    C = Ctx()
    i = {}
    def din(name, shape, dt=F32):
        i[name] = C.din(name, shape, dt)
        return i[name]
    xT = din("xT", [D, L])
    w_in = [din("w_in0", [D, 2560]), din("w_in1", [D, 4480])]
    wsw = [din("wsw0", [D, 1024]), din("wsw1", [D, 1536])]
    w_out = [din("w_out0", [1024, 1024]), din("w_out1", [768, 1024])]
    gmix = [din("gmix0", [128, 8]), din("gmix1", [128, 8])]
    gffn = [din("gffn0", [128, 8]), din("gffn1", [128, 8])]
    gfin = din("gfin", [128, 8])
    cos = [din("cos0", [128, L]), din("cos1", [128, L])]
    sin = [din("sin0", [128, L]), din("sin1", [128, L])]
    ident = din("ident", [128, 128])
    lam = din("lamask", [3, 128, 128])
    rtab = din("rtab", [128, 24, NCH])
    prm_shapes = {"are2": [128, 64], "aim2": [128, 64], "lstep": [128, 64], "dsk": [128, 4], "b_re": [2, 32, 64, 16], "b_im": [2, 32, 64, 16],
                  "c_re": [2, 32, 16, 64], "c_im": [2, 32, 16, 64]}
    prm = {}
    for k, shp in prm_shapes.items():
        a = din("s5_" + k, shp)
        prm[k] = a[:, :] if len(shp) == 2 else a
    consts = {"ident": ident[:, :], "psw": din("c_psw", [128, 128])[:, :], "ksel": din("c_ksel", [128, 3])[:, :], "tau": din("c_tau", [128, T])[:, :]}
    gluw = din("gluw", [512, 512])
    glub = din("glub", [128, 4])
    router = [din("router0", [1024, 16]), din("router1", [1024, 16])]
    ustrict = din("ustrict", [128, 128])
    rmask = din("rmask", [128, 16 * NJ])
    mlb = din("mlb", [128, 16])
    dmasks = din("dmasks", [25, 128, 128])
    w1 = [din("w1_0", [16, 1024, FF]), din("w1_1", [16, 1024, FF])]
    w3 = [din("w3_0", [16, 1024, FF]), din("w3_1", [16, 1024, FF])]
    w2 = [din("w2_0", [16, FF, 1024]), din("w2_1", [16, FF, 1024])]
    outT = C.dout("outT", [D, L])
    pT = RowSplit(C, "pT", 4480, L)
    mixT = C.dscr("mixT", [1024, L])
    o1 = C.dscr("o1", [4, L, 128])
    yf = C.dscr("yf", [512, L])
    x1T = C.dscr("x1T", [1024, L])
    x2T = C.dscr("x2T", [1024, L])
    x3T = C.dscr("x3T", [1024, L])
    htok = C.dscr("htok", [L, 1024])
    aff = C.dscr("aff", [L, 16])
    posd = C.dscr("posd", [128, 16 * NJ], I32)
    gmd = C.dscr("gmd", [128, 16 * NJ])
    xe = [C.dscr("xe%d" % e, [CAP, 1024]) for e in range(16)]
    ye = [C.dscr("ye%d" % e, [CAP, 1024]) for e in range(16)]
    tabd = C.dscr("tabd", [128, 24, NCH])
    vtok = [C.dscr("vtok%d" % h, [L, 65]) for h in range(12)]

    def moe(layer, xin1T, xoutT, final):
        stage_route(C, aff, ustrict[:, :], rmask[:, :], posd[:, :], gmd[:, :], L)
        stage_dispatch(C, htok, posd[:, :], xe, L)
        stage_ffn(C, xe, w1[layer], w3[layer], w2[layer], ye, ident[:, :], L, FF)
        stage_combine(C, ye, posd[:, :], gmd[:, :], xin1T, ident[:, :], xoutT, outT if final else None, gfin[:, :], L)

    stage_proj(C, xT, w_in[0], wsw[0], gmix[0][:, :], cos[0], sin[0], pT, L, 20, 8)
    stage_linattn(C, pT, rtab[:, :, :], ident[:, :], lam, o1, mixT, L, 0, 512, 1024, 1536, 0, False)
    stage_s5(C, pT, prm, consts, yf, mixT, L, 2048, 512)
    stage_out(C, xT, mixT, w_out[0], gluw, glub[:, :], gffn[0][:, :], router[0], ident[:, :], x1T, htok, aff, L, True)
    moe(0, x1T, x2T, False)
    stage_proj(C, x2T, w_in[1], wsw[1], gmix[1][:, :], cos[1], sin[1], pT, L, 35, 12)
    stage_gates(C, pT, mlb[:, :], ident[:, :], tabd, L, 4352)
    stage_linattn(C, pT, tabd[:, :, :], ident[:, :], lam, o1, mixT, L, 2304, 2816, 3328, 3840, 256, True)
    stage_vprep(C, pT, ident[:, :], vtok, L, 1536)
    stage_dil(C, pT, ident[:, :], dmasks, vtok, mixT, L, 0, 768, 0)
    stage_out(C, x2T, mixT, w_out[1], None, None, gffn[1][:, :], router[1], ident[:, :], x1T, htok, aff, L, False)
    moe(1, x1T, x3T, True)
    ninst = C.P.ninst
    return C.finish(), ninst


def host_inputs(inp, b, L, FF):
    f = lambda a: np.ascontiguousarray(a, np.float32)
    T = min(512, L)
    im = {"xT": f(inp["x"][b].T)}
    W0 = inp["ev_w_in"][0]
    W1 = np.zeros((D, 4480), np.float32)
    W1[:, :4368] = inp["od_w_in"][0]
    im["w_in0"] = f(W0); im["w_in1"] = W1
    im["wsw0"] = swap_cols(W0[:, :1024], 1024, 128); im["wsw1"] = swap_cols(W1[:, :1536], 1536, 64)
    im["w_out0"] = f(inp["ev_w_out"][0]); im["w_out1"] = f(inp["od_w_out"][0])
    for l in range(2):
        im["gmix%d" % l] = g8(inp["norm_mix_g"][l]); im["gffn%d" % l] = g8(inp["norm_ffn_g"][l])
        im["router%d" % l] = f(inp["moe_router"][l])
        im["w1_%d" % l] = f(inp["moe_w1"][l]); im["w3_%d" % l] = f(inp["moe_w3"][l]); im["w2_%d" % l] = f(inp["moe_w2"][l])
    im["gfin"] = g8(inp["final_g"])
    im["cos0"], im["sin0"] = rope_tables(L, 64, 128)
    im["cos1"], im["sin1"] = rope_tables(L, 32, 64)
    im["ident"] = np.eye(128, dtype=np.float32)
    im["lamask"] = la_masks()
    im["rtab"] = ret_tables(L)
    prm, consts = s5_host({k: inp[k] for k in ("s5_a_re", "s5_a_im", "s5_b_re", "s5_b_im", "s5_c_re", "s5_c_im", "s5_log_step", "s5_d")}, T)
    for k, v in prm.items():
        im["s5_" + k] = v
    for k, v in consts.items():
        im["c_" + k] = f(v)
    im["gluw"] = f(inp["s5_glu_w"][0]); im["glub"] = f(inp["s5_glu_b"][0].reshape(4, 128).T)
    im["ustrict"], im["rmask"] = route_consts(L)
    im["mlb"] = f(np.tile(np.concatenate([inp["ml_i_bias"][0].reshape(-1), inp["ml_f_bias"][0].reshape(-1)])[None, :], (128, 1)))
    im["dmasks"] = dil_masks()
    return im


def run_model(inp, batches):
    L = inp["x"].shape[1]
    FF = inp["moe_w1"].shape[-1]
    nc, ninst = build_program(L, FF)
    ims = [host_inputs(inp, b, L, FF) for b in batches]
    res = run_bass_kernel_spmd(nc, ims, core_ids=list(range(len(batches))))
    return [np.ascontiguousarray(r["outT"].T) for r in res.results]


def kernel(**inputs):
    inp = {k: np.asarray(v) for k, v in inputs.items()}
    B = inp["x"].shape[0]
    outs = run_model(inp, list(range(B)))
    return np.stack(outs, 0).astype(np.float32)
```

```python
import contextlib
import numpy as np
import concourse.bass as bass
import concourse.mybir as mybir
from concourse.bass_utils import run_bass_kernel_spmd

F32 = mybir.dt.float32
BF16 = mybir.dt.bfloat16
I32 = mybir.dt.int32
AF = mybir.ActivationFunctionType
ALU = mybir.AluOpType
AX = mybir.AxisListType
D = 1024
EPS = 1e-6
ENGS = ("pe", "act", "dve", "pool", "sp")
ENGOBJ = {"pe": "tensor", "act": "scalar", "dve": "vector", "pool": "gpsimd", "sp": "sync"}


class Buf:
    __slots__ = ("last_w", "readers")

    def __init__(self):
        self.last_w = None
        self.readers = []


class Op:
    __slots__ = ("eng", "fn", "idx", "deps", "dma", "signal", "cnt", "sem", "semval", "stage")


class Prog:
    ND = {"sp": 10, "act": 2, "pool": 2}

    def __init__(self, nc, st):
        self.nc = nc
        self.esem = {e: st.enter_context(nc.semaphore("s_" + e)) for e in ENGS}
        self.dsem = {(q, k): st.enter_context(nc.semaphore("d_%s%d" % (q, k))) for q in self.ND for k in range(self.ND[q])}
        self.base = {e: 0 for e in ENGS}
        self.dk = {q: 0 for q in self.ND}
        self.dval = {k: 0 for k in self.dsem}
        self.waited = {e: {} for e in ENGS}
        self.stage = 0
        self.ops = {e: [] for e in ENGS}
        self.ninst = 0

    def op(self, eng, fn, reads=(), writes=(), dma=False):
        o = Op()
        o.eng, o.fn, o.idx, o.dma, o.signal, o.cnt, o.stage = eng, fn, len(self.ops[eng]), dma, False, 0, self.stage
        o.sem, o.semval = None, 0
        deps = {}
        for b in reads:
            if b.last_w is not None and b.last_w.stage == self.stage:
                deps[id(b.last_w)] = b.last_w
        for b in writes:
            if b.last_w is not None and b.last_w.stage == self.stage:
                deps[id(b.last_w)] = b.last_w
            for r in b.readers:
                if r.stage == self.stage:
                    deps[id(r)] = r
        o.deps = list(deps.values())
        for b in reads:
            b.readers.append(o)
        for b in writes:
            b.last_w = o
            b.readers = []
        if dma:
            k = self.dk[eng]
            self.dk[eng] += 1
            nd = self.ND[eng]
            o.sem = (eng, k % nd)
            o.semval = 16 * (k // nd + 1)
            self.dval[o.sem] = o.semval
        self.ops[eng].append(o)
        self.ninst += 1
        return o

    def pe(self, fn, reads=(), writes=()):
        return self.op("pe", fn, reads, writes)

    def act(self, fn, reads=(), writes=()):
        return self.op("act", fn, reads, writes)

    def dve(self, fn, reads=(), writes=()):
        return self.op("dve", fn, reads, writes)

    def pool(self, fn, reads=(), writes=()):
        return self.op("pool", fn, reads, writes)

    def dma(self, fn, reads=(), writes=(), q="sp"):
        return self.op(q, fn, reads, writes, dma=True)

    def end_stage(self):
        nc = self.nc
        for e in ENGS:
            comp = [o for o in self.ops[e] if not o.dma]
            if comp:
                comp[-1].signal = True
            for o in self.ops[e]:
                for d in o.deps:
                    if d.dma:
                        continue
                    if d.eng == o.eng:
                        if e == "pe":
                            continue
                        if o.idx - d.idx > 2 and not o.dma:
                            continue
                    d.signal = True
        final = {}
        for e in ENGS:
            c = self.base[e]
            for o in self.ops[e]:
                if o.dma:
                    continue
                if o.signal:
                    c += 1
                o.cnt = c
            final[e] = c
        with nc.Block() as block:
            def run_engine(e, eng):
                waited = self.waited[e]

                def need(sem, key, val):
                    if waited.get(key, 0) < val:
                        eng.wait_ge(sem, val)
                        waited[key] = val

                for o in self.ops[e]:
                    for d in o.deps:
                        if d.dma:
                            need(self.dsem[d.sem], d.sem, d.semval)
                        else:
                            if d.eng == e:
                                if e == "pe":
                                    continue
                                if o.idx - d.idx > 2 and not o.dma:
                                    continue
                            need(self.esem[d.eng], d.eng, d.cnt)
                    if o.dma:
                        if o.semval > 16:
                            need(self.dsem[o.sem], o.sem, o.semval - 16)
                        inst = o.fn(eng)
                        inst.then_inc(self.dsem[o.sem], 16)
                    else:
                        inst = o.fn(eng)
                        if o.signal:
                            inst.then_inc(self.esem[e], 1)
                for e2 in ENGS:
                    if e2 != e and final[e2] > 0:
                        need(self.esem[e2], e2, final[e2])
                for k, v in self.dval.items():
                    if v > 0:
                        need(self.dsem[k], k, v)

            for e in ENGS:
                def _f(eng, e=e):
                    run_engine(e, eng)
                getattr(block, ENGOBJ[e])(_f)
        self.base = final
        self.stage += 1
        self.ops = {e: [] for e in ENGS}


class Ctx:
    def __init__(self):
        self.nc = bass.Bass("TRN2", target_bir_lowering=False)
        self.gst = contextlib.ExitStack()
        self.P = Prog(self.nc, self.gst)
        self.st = None
        self.n = 0
        self.dbg = {}

    def din(self, name, shape, dt=F32):
        return self.nc.dram_tensor(name, list(shape), dt, kind="ExternalInput").ap()

    def dout(self, name, shape, dt=F32):
        return self.nc.dram_tensor(name, list(shape), dt, kind="ExternalOutput").ap()

    def dscr(self, name, shape, dt=F32, debug=False):
        if debug:
            return self.dout(name, shape, dt)
        return self.nc.dram_tensor(name, list(shape), dt, kind="Internal").ap()

    def begin(self):
        self.st = contextlib.ExitStack()

    def end(self):
        self.P.end_stage()
        self.st.close()
        self.st = None

    def sb(self, shape, dt=F32):
        self.n += 1
        t = self.st.enter_context(self.nc.sbuf_tensor("t%d" % self.n, list(shape), dt))
        return t, Buf()

    def ps(self, shape, dt=F32):
        self.n += 1
        t = self.st.enter_context(self.nc.psum_tensor("p%d" % self.n, list(shape), dt))
        return t, Buf()

    def finish(self):
        self.gst.close()
        return self.nc


def stage_proj(C, xT, w, wsw, g8, cos, sin, pT, L, NCT, NRT):
    P = C.P
    C.begin()
    TB = min(512, L)
    NB = L // TB
    wb, _ = C.sb([128, 8, NCT * 128], BF16)
    wswb, _ = C.sb([128, 8, NRT * 128], BF16)
    b_w = [Buf() for _ in range(8)]
    b_ws = [Buf() for _ in range(8)]
    gt, b_g = C.sb([128, 8])
    ones, b_ones = C.sb([128, 128], BF16)
    P.dma(lambda e: e.dma_start(out=gt[:], in_=g8), writes=[b_g])
    P.dve(lambda e: e.memset(ones[:], 1.0), writes=[b_ones])
    epsc, b_epsc = C.sb([128, 1])
    P.dve(lambda e: e.memset(epsc[:], EPS), writes=[b_epsc])
    CH = 640
    for kc in range(8):
        for c0 in range(0, NCT * 128, CH):
            P.dma(lambda e, kc=kc, c0=c0: e.dma_start(out=wb[:, kc, c0:c0 + CH], in_=w[kc * 128:(kc + 1) * 128, c0:c0 + CH]),
                  writes=[b_w[kc]], q="pool")
        for c0 in range(0, NRT * 128, 512):
            P.dma(lambda e, kc=kc, c0=c0: e.dma_start(out=wswb[:, kc, c0:c0 + 512], in_=wsw[kc * 128:(kc + 1) * 128, c0:c0 + 512]),
                  writes=[b_ws[kc]], q="pool")
    nxb = 2 if NCT <= 24 else 1
    xts = [C.sb([128, 8, TB]) for _ in range(nxb)]
    xbs = [C.sb([128, 8, TB], BF16) for _ in range(nxb)]
    sq, b_sq = C.sb([128, 8, TB], BF16)
    css = [C.sb([128, TB]) for _ in range(2)]
    sns = [C.sb([128, TB]) for _ in range(2)]
    rss = [C.sb([128, TB]) for _ in range(2)]
    pss, b_pss = C.ps([128, TB])
    ps_a = [C.ps([128, TB]) for _ in range(2)]
    ps_b = [C.ps([128, TB]) for _ in range(2)]
    t1s = [C.sb([128, TB]) for _ in range(2)]
    t2s = [C.sb([128, TB]) for _ in range(2)]
    os_ = [C.sb([128, TB]) for _ in range(4)]
    b_out = Buf()
    xv = xT.rearrange("(kc p) n -> p kc n", p=128)
    no = 0
    na = 0
    for tb in range(NB):
        sl = slice(tb * TB, (tb + 1) * TB)
        xt, b_x = xts[tb % nxb]
        xb, b_xb = xbs[tb % nxb]
        cs, b_cs = css[tb % 2]
        sn, b_sn = sns[tb % 2]
        rs, b_rs = rss[tb % 2]
        P.dma(lambda e, xt=xt, sl=sl: e.dma_start(out=xt[:], in_=xv[:, :, sl]), writes=[b_x])
        if NRT:
            P.dma(lambda e, cs=cs, sl=sl: e.dma_start(out=cs[:], in_=cos[:, sl]), writes=[b_cs])
            P.dma(lambda e, sn=sn, sl=sl: e.dma_start(out=sn[:], in_=sin[:, sl]), writes=[b_sn])
        P.pool(lambda e, xt=xt: e.tensor_tensor(out=sq[:], in0=xt[:], in1=xt[:], op=ALU.mult), reads=[b_x], writes=[b_sq])
        for kc in range(8):
            eng = P.dve if kc % 2 == 0 else P.pool
            eng(lambda e, kc=kc, xt=xt, xb=xb: e.tensor_scalar(out=xb[:, kc, :], in0=xt[:, kc, :], scalar1=gt[:, kc:kc + 1], scalar2=None, op0=ALU.mult),
                reads=[b_x, b_g], writes=[b_xb])
        for kc in range(8):
            P.pe(lambda e, kc=kc: e.matmul(pss[:], ones[:], sq[:, kc, :], start=(kc == 0), stop=(kc == 7)),
                 reads=[b_sq, b_ones], writes=[b_pss])
        P.act(lambda e, rs=rs: e.activation(out=rs[:], in_=pss[:], func=AF.Ln, scale=1.0 / D, bias=epsc[:, 0:1]), reads=[b_pss, b_epsc], writes=[b_rs])
        P.act(lambda e, rs=rs: e.activation(out=rs[:], in_=rs[:], func=AF.Exp, scale=-0.5), reads=[b_rs], writes=[b_rs])
        if NRT:
            P.pool(lambda e, cs=cs, rs=rs: e.tensor_tensor(out=cs[:], in0=cs[:], in1=rs[:], op=ALU.mult), reads=[b_cs, b_rs], writes=[b_cs])
            P.pool(lambda e, sn=sn, rs=rs: e.tensor_tensor(out=sn[:], in0=sn[:], in1=rs[:], op=ALU.mult), reads=[b_sn, b_rs], writes=[b_sn])
        for ct in range(NCT):
            pa, b_pa = ps_a[na % 2]
            pb, b_pb = ps_b[na % 2]
            na += 1
            o, b_o = os_[no % 4]
            no += 1
            for kc in range(8):
                P.pe(lambda e, kc=kc, ct=ct, pa=pa, xb=xb: e.matmul(pa[:], wb[:, kc, ct * 128:(ct + 1) * 128], xb[:, kc, :], start=(kc == 0), stop=(kc == 7)),
                     reads=[b_w[kc], b_xb], writes=[b_pa])
            if ct < NRT:
                for kc in range(8):
                    P.pe(lambda e, kc=kc, ct=ct, pb=pb, xb=xb: e.matmul(pb[:], wswb[:, kc, ct * 128:(ct + 1) * 128], xb[:, kc, :], start=(kc == 0), stop=(kc == 7)),
                         reads=[b_ws[kc], b_xb], writes=[b_pb])
                t1, b_t1 = t1s[ct % 2]
                t2, b_t2 = t2s[ct % 2]
                P.dve(lambda e, t1=t1, pa=pa, cs=cs: e.tensor_tensor(out=t1[:], in0=pa[:], in1=cs[:], op=ALU.mult), reads=[b_pa, b_cs], writes=[b_t1])
                P.dve(lambda e, t2=t2, pb=pb, sn=sn: e.tensor_tensor(out=t2[:], in0=pb[:], in1=sn[:], op=ALU.mult), reads=[b_pb, b_sn], writes=[b_t2])
                P.pool(lambda e, o=o, t1=t1, t2=t2: e.tensor_tensor(out=o[:], in0=t1[:], in1=t2[:], op=ALU.add), reads=[b_t1, b_t2], writes=[b_o])
            else:
                P.dve(lambda e, o=o, pa=pa, rs=rs: e.tensor_tensor(out=o[:], in0=pa[:], in1=rs[:], op=ALU.mult), reads=[b_pa, b_rs], writes=[b_o])
            P.dma(lambda e, o=o, ct=ct, sl=sl: e.dma_start(out=pT[ct * 128:(ct + 1) * 128, sl], in_=o[:]), reads=[b_o], writes=[Buf()])
    C.end()


def stage_gates(C, pT, mlb, ident, tabd, L, mg_row):
    P = C.P
    C.begin()
    NCH = L // 128
    s = 128.0 ** -0.5
    idt, b_id = C.sb([128, 128])
    mb, b_mb = C.sb([128, 16])
    nfb, b_nfb = C.sb([128, 8])
    lns, b_lns = C.sb([128, 1])
    onesq, b_onesq = C.sb([128, 128])
    P.dma(lambda e: e.dma_start(out=idt[:], in_=ident), writes=[b_id])
    P.dma(lambda e: e.dma_start(out=mb[:], in_=mlb), writes=[b_mb])
    P.dve(lambda e: e.tensor_scalar(out=nfb[:], in0=mb[:, 8:16], scalar1=-1.0, scalar2=None, op0=ALU.mult), reads=[b_mb], writes=[b_nfb])
    P.dve(lambda e: e.memset(lns[:], float(np.log(s))), writes=[b_lns])
    P.dve(lambda e: e.memset(onesq[:], 1.0), writes=[b_onesq])
    b_out = Buf()
    gps = [C.ps([128, 128]) for _ in range(3)]
    for h in range(4):
        for dr in range(2):
            gi, b_gi = C.sb([128, 128])
            gf, b_gf = C.sb([128, 128])
            ri = mg_row + dr * 4 + h
            rf = mg_row + 8 + dr * 4 + h
            P.dma(lambda e, gi=gi, ri=ri: e.dma_start(out=gi[0:NCH, :], in_=pT[ri:ri + 1, :].rearrange("o (c t) -> (o c) t", t=128)), writes=[b_gi])
            P.dma(lambda e, gf=gf, rf=rf: e.dma_start(out=gf[0:NCH, :], in_=pT[rf:rf + 1, :].rearrange("o (c t) -> (o c) t", t=128)), writes=[b_gf])
            sp, b_sp = C.sb([128, 128])
            csp, b_csp = C.sb([128, 128])
            col = dr * 4 + h
            P.act(lambda e, sp=sp, gf=gf, col=col: e.activation(out=sp[0:NCH, :], in_=gf[0:NCH, :], func=AF.Exp, scale=-1.0, bias=nfb[0:NCH, col:col + 1]),
                  reads=[b_gf, b_nfb], writes=[b_sp])
            P.act(lambda e, sp=sp: e.activation(out=sp[0:NCH, :], in_=sp[0:NCH, :], func=AF.Ln, scale=1.0, bias=1.0), reads=[b_sp], writes=[b_sp])
            P.dve(lambda e, csp=csp, sp=sp: e.tensor_tensor_scan(out=csp[0:NCH, :], data0=onesq[0:NCH, :], data1=sp[0:NCH, :], initial=0.0, op0=ALU.mult, op1=ALU.add),
                  reads=[b_sp, b_onesq], writes=[b_csp])
            ex, b_ex = C.sb([128, 128])
            if dr == 0:
                P.dve(lambda e, ex=ex, csp=csp: e.tensor_copy(out=ex[0:NCH, :], in_=csp[0:NCH, :]), reads=[b_csp], writes=[b_ex])
            else:
                P.dve(lambda e, ex=ex, sp=sp, csp=csp: e.tensor_tensor(out=ex[0:NCH, :], in0=sp[0:NCH, :], in1=csp[0:NCH, :], op=ALU.subtract), reads=[b_sp, b_csp], writes=[b_ex])
                P.dve(lambda e, ex=ex, csp=csp: e.tensor_scalar(out=ex[0:NCH, :], in0=ex[0:NCH, :], scalar1=csp[0:NCH, 127:128], scalar2=None, op0=ALU.add), reads=[b_ex, b_csp], writes=[b_ex])
            av, b_av = C.sb([128, 128])
            cv, b_cv = C.sb([128, 128])
            ebc, b_ebc = C.sb([128, 1])
            P.act(lambda e, av=av, ex=ex: e.activation(out=av[0:NCH, :], in_=ex[0:NCH, :], func=AF.Exp, scale=-1.0), reads=[b_ex], writes=[b_av])
            P.dve(lambda e, cv=cv, gi=gi, ex=ex: e.tensor_tensor(out=cv[0:NCH, :], in0=gi[0:NCH, :], in1=ex[0:NCH, :], op=ALU.add), reads=[b_gi, b_ex], writes=[b_cv])
            P.dve(lambda e, cv=cv, col=col: e.tensor_scalar(out=cv[0:NCH, :], in0=cv[0:NCH, :], scalar1=mb[0:NCH, col:col + 1], scalar2=lns[0:NCH, 0:1], op0=ALU.add, op1=ALU.add),
                  reads=[b_cv, b_mb, b_lns], writes=[b_cv])
            P.act(lambda e, cv=cv: e.activation(out=cv[0:NCH, :], in_=cv[0:NCH, :], func=AF.Exp), reads=[b_cv], writes=[b_cv])
            P.act(lambda e, ebc=ebc, csp=csp: e.activation(out=ebc[0:NCH, :], in_=csp[0:NCH, 127:128], func=AF.Exp, scale=-1.0), reads=[b_csp], writes=[b_ebc])
            tb_, b_tb = C.sb([128, 3, NCH])
            for k, (src, b_src) in enumerate(((av, b_av), (cv, b_cv))):
                pt, b_pt = gps[k]
                P.pe(lambda e, pt=pt, src=src: e.transpose(pt[:, 0:NCH], src[0:NCH, :], idt[0:NCH, 0:NCH]), reads=[b_src, b_id], writes=[b_pt])
                P.act(lambda e, pt=pt, k=k, tb_=tb_: e.activation(out=tb_[:, k, :], in_=pt[:, 0:NCH], func=AF.Copy), reads=[b_pt], writes=[b_tb])
            tm, b_tm = C.sb([128, 128])
            P.dve(lambda e, tm=tm, ebc=ebc: e.tensor_scalar(out=tm[0:NCH, :], in0=onesq[0:NCH, :], scalar1=ebc[0:NCH, 0:1], scalar2=None, op0=ALU.mult),
                  reads=[b_onesq, b_ebc], writes=[b_tm])
            pt, b_pt = gps[2]
            P.pe(lambda e, pt=pt, tm=tm: e.matmul(pt[:, 0:NCH], tm[0:NCH, :], idt[0:NCH, 0:NCH], start=True, stop=True), reads=[b_tm, b_id], writes=[b_pt])
            P.act(lambda e, pt=pt, tb_=tb_: e.activation(out=tb_[:, 2, :], in_=pt[:, 0:NCH], func=AF.Copy), reads=[b_pt], writes=[b_tb])
            j0 = (h * 2 + dr) * 3
            P.dma(lambda e, tb_=tb_, j0=j0: e.dma_start(out=tabd[:, j0:j0 + 3, :], in_=tb_[:]), reads=[b_tb], writes=[Buf()])
    C.end()


def stage_linattn(C, pT, tab, ident, masks, o1, mixT, L, q_row, k_row, v_row, g_row, out_row, mlstm):
    P = C.P
    C.begin()
    NCH = L // 128
    NV = 129 if mlstm else 128
    idt, b_id = C.sb([128, 128])
    mk, b_mk = C.sb([128, 3, 128])
    tb_, b_tb = C.sb([128, 24, NCH])
    P.dma(lambda e: e.dma_start(out=idt[:], in_=ident), writes=[b_id])
    P.dma(lambda e: e.dma_start(out=mk[:], in_=masks.rearrange("m p n -> p m n")), writes=[b_mk])
    P.dma(lambda e: e.dma_start(out=tb_[:], in_=tab), writes=[b_tb])
    epsc, b_epsc = C.sb([128, 1])
    P.dve(lambda e: e.memset(epsc[:], EPS), writes=[b_epsc])
    NBUF = 4
    qTs = [C.sb([128, 128]) for _ in range(NBUF)]
    kTs = [C.sb([128, 128]) for _ in range(NBUF)]
    vTs = [C.sb([128, 128]) for _ in range(NBUF)]
    gTs = [C.sb([128, 128]) for _ in range(NBUF)]
    hfs = [C.sb([128, 128]) for _ in range(NBUF)]
    ktoks = [C.sb([128, 128]) for _ in range(NBUF)]
    vpps = [C.sb([128, NV]) for _ in range(NBUF)]
    sms = [C.sb([128, 128]) for _ in range(NBUF)]
    os_ = [C.sb([128, NV]) for _ in range(NBUF)]
    hs = [C.sb([128, 128]) for _ in range(NBUF)]
    gas = [C.sb([128, 128]) for _ in range(NBUF)]
    outs = [C.sb([128, 128]) for _ in range(NBUF)]
    p_kt = [C.ps([128, 128]) for _ in range(1)]
    p_vt = [C.ps([128, 128]) for _ in range(1)]
    p_s = [C.ps([128, 128]) for _ in range(2)]
    p_o = [C.ps([128, NV]) for _ in range(2)]
    p_kv = [C.ps([128, NV]) for _ in range(1)]
    p_tr = [C.ps([128, 128]) for _ in range(1)]
    cst, b_cst = C.sb([128, NV])
    tmpc, b_tmpc = C.sb([128, NV])
    st6, b_st6 = C.sb([128, 6])
    mv, b_mv = C.sb([128, 2])
    rsd, b_rsd = C.sb([128, 1])
    dn, b_dn = C.sb([128, 1])
    b_o1 = [[Buf() for _ in range(NCH)] for _ in range(4)]
    it = 0
    csts = [(cst, b_cst)] + [C.sb([128, NV]) for _ in range(3)]
    for dr in range(2):
        mi = 0 if dr == 0 else (2 if mlstm else 1)
        for h in range(4):
            P.dve(lambda e, c_=csts[h][0]: e.memset(c_[:], 0.0), writes=[csts[h][1]])
        order = range(NCH) if dr == 0 else range(NCH - 1, -1, -1)
        for n in order:
            for h in range(4):
                j0 = (h * 2 + dr) * 3
                cst, b_cst = csts[h]
                i = it % NBUF
                it += 1
                cs_ = slice(n * 128, (n + 1) * 128)
                qT, b_q = qTs[i]
                kT, b_k = kTs[i]
                vT, b_v = vTs[i]
                P.dma(lambda e, qT=qT, cs_=cs_, h=h: e.dma_start(out=qT[:], in_=pT[q_row + h * 128:q_row + (h + 1) * 128, cs_]), writes=[b_q])
                P.dma(lambda e, kT=kT, cs_=cs_, h=h: e.dma_start(out=kT[:], in_=pT[k_row + h * 128:k_row + (h + 1) * 128, cs_]), writes=[b_k])
                P.dma(lambda e, vT=vT, cs_=cs_, h=h: e.dma_start(out=vT[:], in_=pT[v_row + h * 128:v_row + (h + 1) * 128, cs_]), writes=[b_v])
                pk, b_pk = p_kt[0]
                pv, b_pv = p_vt[0]
                ktok, b_kt = ktoks[i]
                vpp, b_vp = vpps[i]
                P.pe(lambda e, pk=pk, kT=kT: e.transpose(pk[:], kT[:], idt[:]), reads=[b_k, b_id], writes=[b_pk])
                P.pe(lambda e, pv=pv, vT=vT: e.transpose(pv[:], vT[:], idt[:]), reads=[b_v, b_id], writes=[b_pv])
                P.act(lambda e, ktok=ktok, pk=pk: e.activation(out=ktok[:], in_=pk[:], func=AF.Copy), reads=[b_pk], writes=[b_kt])
                P.dve(lambda e, vpp=vpp, pv=pv, n=n, j0=j0: e.tensor_scalar(out=vpp[:, 0:128], in0=pv[:], scalar1=tb_[:, j0 + 1, n:n + 1], scalar2=None, op0=ALU.mult),
                      reads=[b_pv, b_tb], writes=[b_vp])
                if mlstm:
                    P.act(lambda e, vpp=vpp, n=n, j0=j0: e.activation(out=vpp[:, 128:129], in_=tb_[:, j0 + 1, n:n + 1], func=AF.Copy), reads=[b_tb], writes=[b_vp])
                ps_, b_ps = p_s[it % 2]
                sm, b_sm = sms[i]
                P.pe(lambda e, ps_=ps_, kT=kT, qT=qT: e.matmul(ps_[:], kT[:], qT[:], start=True, stop=True), reads=[b_k, b_q], writes=[b_ps])
                P.dve(lambda e, sm=sm, ps_=ps_, mi=mi: e.tensor_tensor(out=sm[:], in0=ps_[:], in1=mk[:, mi, :], op=ALU.mult), reads=[b_ps, b_mk], writes=[b_sm])
                po, b_po = p_o[it % 2]
                P.pe(lambda e, po=po, sm=sm, vpp=vpp: e.matmul(po[:], sm[:], vpp[:], start=True, stop=False), reads=[b_sm, b_vp], writes=[b_po])
                P.pe(lambda e, po=po, qT=qT, cst=cst: e.matmul(po[:], qT[:], cst[:], start=False, stop=True), reads=[b_q, b_cst], writes=[b_po])
                o_, b_o = os_[i]
                P.act(lambda e, o_=o_, po=po, n=n, j0=j0: e.activation(out=o_[:], in_=po[:], func=AF.Copy, scale=tb_[:, j0, n:n + 1]), reads=[b_po, b_tb], writes=[b_o])
                pkv, b_pkv = p_kv[0]
                P.pe(lambda e, pkv=pkv, ktok=ktok, vpp=vpp: e.matmul(pkv[:], ktok[:], vpp[:], start=True, stop=True), reads=[b_kt, b_vp], writes=[b_pkv])
                P.dve(lambda e, pkv=pkv, cst=cst: e.tensor_tensor(out=tmpc[:], in0=pkv[:], in1=cst[:], op=ALU.add), reads=[b_pkv, b_cst], writes=[b_tmpc])
                P.dve(lambda e, n=n, j0=j0, cst=cst: e.tensor_scalar(out=cst[:], in0=tmpc[:], scalar1=tb_[:, j0 + 2, n:n + 1], scalar2=None, op0=ALU.mult),
                      reads=[b_tmpc, b_tb], writes=[b_cst])
                hh, b_h = hs[i]
                if mlstm:
                    P.dve(lambda e, o_=o_: e.tensor_scalar(out=dn[:], in0=o_[:, 128:129], scalar1=-1.0, scalar2=1.0, op0=ALU.mult, op1=ALU.max), reads=[b_o], writes=[b_dn])
                    P.dve(lambda e, o_=o_: e.tensor_tensor(out=dn[:], in0=dn[:], in1=o_[:, 128:129], op=ALU.max), reads=[b_dn, b_o], writes=[b_dn])
                    P.dve(lambda e: e.reciprocal(out=dn[:], in_=dn[:]), reads=[b_dn], writes=[b_dn])
                    P.dve(lambda e, hh=hh, o_=o_: e.tensor_scalar(out=hh[:], in0=o_[:, 0:128], scalar1=dn[:, 0:1], scalar2=None, op0=ALU.mult), reads=[b_o, b_dn], writes=[b_h])
                    src, b_src = hh, b_h
                else:
                    src, b_src = o_, b_o
                if dr == 0:
                    P.dma(lambda e, src=src, cs_=cs_, h=h: e.dma_start(out=o1[h, cs_, :], in_=src[:, 0:128]), reads=[b_src], writes=[b_o1[h][n]])
                else:
                    hf, b_hf = hfs[i]
                    gT, b_g = gTs[i]
                    P.dma(lambda e, hf=hf, cs_=cs_, h=h: e.dma_start(out=hf[:], in_=o1[h, cs_, :]), reads=[b_o1[h][n]], writes=[b_hf])
                    P.dma(lambda e, gT=gT, cs_=cs_, h=h: e.dma_start(out=gT[:], in_=pT[g_row + h * 128:g_row + (h + 1) * 128, cs_]), writes=[b_g])
                    P.pool(lambda e, hf=hf, src=src: e.tensor_tensor(out=hf[:], in0=hf[:], in1=src[:, 0:128], op=ALU.add), reads=[b_hf, b_src], writes=[b_hf])
                    P.dve(lambda e, hf=hf: e.bn_stats(out=st6[:], in_=hf[:]), reads=[b_hf], writes=[b_st6])
                    P.dve(lambda e: e.bn_aggr(out=mv[:], in_=st6[:]), reads=[b_st6], writes=[b_mv])
                    P.act(lambda e: e.activation(out=rsd[:], in_=mv[:, 1:2], func=AF.Ln, scale=1.0, bias=epsc[:, 0:1]), reads=[b_mv, b_epsc], writes=[b_rsd])
                    P.act(lambda e: e.activation(out=rsd[:], in_=rsd[:], func=AF.Exp, scale=-0.5), reads=[b_rsd], writes=[b_rsd])
                    P.dve(lambda e, hf=hf: e.tensor_scalar(out=hf[:], in0=hf[:], scalar1=mv[:, 0:1], scalar2=rsd[:, 0:1], op0=ALU.subtract, op1=ALU.mult),
                          reads=[b_hf, b_mv, b_rsd], writes=[b_hf])
                    ptr, b_ptr = p_tr[0]
                    P.pe(lambda e, ptr=ptr, hf=hf: e.transpose(ptr[:], hf[:], idt[:]), reads=[b_hf, b_id], writes=[b_ptr])
                    ga, b_ga = gas[i]
                    P.act(lambda e, ga=ga, gT=gT: e.activation(out=ga[:], in_=gT[:], func=AF.Exp, scale=-1.0), reads=[b_g], writes=[b_ga])
                    P.pool(lambda e, ga=ga: e.tensor_scalar(out=ga[:], in0=ga[:], scalar1=1.0, scalar2=None, op0=ALU.add), reads=[b_ga], writes=[b_ga])
                    P.dve(lambda e, ga=ga: e.reciprocal(out=ga[:], in_=ga[:]), reads=[b_ga], writes=[b_ga])
                    if not mlstm:
                        P.pool(lambda e, ga=ga, gT=gT: e.tensor_tensor(out=ga[:], in0=ga[:], in1=gT[:], op=ALU.mult), reads=[b_ga, b_g], writes=[b_ga])
                    ot, b_ot = outs[i]
                    P.dve(lambda e, ot=ot, ptr=ptr, ga=ga: e.tensor_tensor(out=ot[:], in0=ptr[:], in1=ga[:], op=ALU.mult), reads=[b_ptr, b_ga], writes=[b_ot])
                    P.dma(lambda e, ot=ot, cs_=cs_, h=h: e.dma_start(out=mixT[out_row + h * 128:out_row + (h + 1) * 128, cs_], in_=ot[:]), reads=[b_ot], writes=[Buf()])
    C.end()


PI = float(np.pi)


def _wrap(P, C, x, b_x, shape, add=0.0, scr=None, key="a"):
    if scr is not None and ("u" + key) in scr:
        (u, b_u), (ki, b_ki), (kf, b_kf), (y, b_y), (m, b_m) = [scr[n + key] for n in "uikym"]
    else:
        u, b_u = C.sb(shape)
        ki, b_ki = C.sb(shape, I32)
        kf, b_kf = C.sb(shape)
        y, b_y = C.sb(shape)
        m, b_m = C.sb(shape)
        if scr is not None:
            for n, v in zip("uikym", ((u, b_u), (ki, b_ki), (kf, b_kf), (y, b_y), (m, b_m))):
                scr[n + key] = v
    P.dve(lambda e: e.tensor_scalar(out=u[:], in0=x[:], scalar1=add, scalar2=1.0 / (2 * PI), op0=ALU.add, op1=ALU.mult), reads=[b_x], writes=[b_u])
    P.dve(lambda e: e.tensor_copy(out=ki[:], in_=u[:]), reads=[b_u], writes=[b_ki])
    P.dve(lambda e: e.tensor_copy(out=kf[:], in_=ki[:]), reads=[b_ki], writes=[b_kf])
    P.dve(lambda e: e.tensor_scalar(out=u[:], in0=x[:], scalar1=add, scalar2=None, op0=ALU.add), reads=[b_x, b_kf], writes=[b_u])
    P.dve(lambda e: e.scalar_tensor_tensor(out=y[:], in0=kf[:], scalar=-2 * PI, in1=u[:], op0=ALU.mult, op1=ALU.add), reads=[b_kf, b_u], writes=[b_y])
    P.dve(lambda e: e.tensor_scalar(out=m[:], in0=y[:], scalar1=PI, scalar2=-2 * PI, op0=ALU.is_gt, op1=ALU.mult), reads=[b_y], writes=[b_m])
    P.dve(lambda e: e.tensor_tensor(out=y[:], in0=y[:], in1=m[:], op=ALU.add), reads=[b_y, b_m], writes=[b_y])
    P.dve(lambda e: e.tensor_scalar(out=m[:], in0=y[:], scalar1=-PI, scalar2=2 * PI, op0=ALU.is_lt, op1=ALU.mult), reads=[b_y], writes=[b_m])
    P.dve(lambda e: e.tensor_tensor(out=y[:], in0=y[:], in1=m[:], op=ALU.add), reads=[b_y, b_m], writes=[b_y])
    P.dve(lambda e: e.tensor_scalar(out=y[:], in0=y[:], scalar1=-PI, scalar2=PI, op0=ALU.max, op1=ALU.min), reads=[b_y], writes=[b_y])
    return y, b_y


def stage_s5(C, pT, prm, consts, yf, mixT, L, u_row, out_row):
    P = C.P
    T = min(512, L)
    NBK = L // T
    for dr in range(2):
        for ct in range(4):
            C.begin()
            idt, b_id = C.sb([128, 128])
            psw, b_psw = C.sb([128, 128])
            tau, b_tau = C.sb([128, T])
            ks, b_ks = C.sb([128, 3])
            lst, b_lst = C.sb([128, 64])
            dsk, b_dsk = C.sb([128, 4])
            onesT, b_onesT = C.sb([128, T])
            P.dma(lambda e: e.dma_start(out=idt[:], in_=consts["ident"]), writes=[b_id])
            P.dma(lambda e: e.dma_start(out=psw[:], in_=consts["psw"]), writes=[b_psw])
            P.dma(lambda e: e.dma_start(out=tau[:], in_=consts["tau"]), writes=[b_tau])
            P.dma(lambda e: e.dma_start(out=ks[:], in_=consts["ksel"]), writes=[b_ks])
            P.dma(lambda e: e.dma_start(out=lst[:], in_=prm["lstep"]), writes=[b_lst])
            P.dma(lambda e: e.dma_start(out=dsk[:], in_=prm["dsk"]), writes=[b_dsk])
            P.dve(lambda e: e.memset(onesT[:], 1.0), writes=[b_onesT])
            G = []
            scr = {}
            ang, b_ang = C.sb([128, T])
            are2, b_are2 = C.sb([128, 64])
            aim2, b_aim2 = C.sb([128, 64])
            P.dma(lambda e: e.dma_start(out=are2[:], in_=prm['are2']), writes=[b_are2])
            P.dma(lambda e: e.dma_start(out=aim2[:], in_=prm['aim2']), writes=[b_aim2])
            ptr = [C.ps([128, 128]) for _ in range(2)]
            for gp in range(8):
                g = ct * 8 + gp
                are, b_are = C.sb([128, 1])
                aim, b_aim = C.sb([128, 1])
                P.dve(lambda e, are=are, cg=dr * 32 + g: e.tensor_copy(out=are[:], in_=are2[:, cg:cg + 1]), reads=[b_are2], writes=[b_are])
                P.dve(lambda e, aim=aim, cg=dr * 32 + g: e.tensor_copy(out=aim[:], in_=aim2[:, cg:cg + 1]), reads=[b_aim2], writes=[b_aim])
                dl, b_dl = C.sb([128, 1])
                r, b_r = C.sb([128, 1])
                th, b_th = C.sb([128, 1])
                col = dr * 32 + g
                P.act(lambda e, dl=dl, col=col: e.activation(out=dl[:], in_=lst[:, col:col + 1], func=AF.Exp), reads=[b_lst], writes=[b_dl])
                P.act(lambda e, r=r, are=are, dl=dl: e.activation(out=r[:], in_=are[:], func=AF.Exp, scale=dl[:, 0:1]), reads=[b_are, b_dl], writes=[b_r])
                P.dve(lambda e, th=th, aim=aim, dl=dl: e.tensor_tensor(out=th[:], in0=aim[:], in1=dl[:], op=ALU.mult), reads=[b_aim, b_dl], writes=[b_th])
                thr0, b_thr0 = _wrap(P, C, th, b_th, [128, 1], scr=scr, key='c')
                thr, b_thr = C.sb([128, 1])
                P.dve(lambda e, thr=thr, thr0=thr0: e.tensor_copy(out=thr[:], in_=thr0[:]), reads=[b_thr0], writes=[b_thr])
                thc, b_thc = _wrap(P, C, thr, b_thr, [128, 1], add=PI / 2, scr=scr, key='d')
                s0, b_s0 = C.sb([128, 1])
                c0, b_c0 = C.sb([128, 1])
                P.act(lambda e, s0=s0, thr=thr: e.activation(out=s0[:], in_=thr[:], func=AF.Sin), reads=[b_thr], writes=[b_s0])
                P.act(lambda e, c0=c0, thc=thc: e.activation(out=c0[:], in_=thc[:], func=AF.Sin), reads=[b_thc], writes=[b_c0])
                nre, b_nre = C.sb([128, 1])
                nim, b_nim = C.sb([128, 1])
                den, b_den = C.sb([128, 1])
                t0, b_t0 = C.sb([128, 1])
                kre, b_kre = C.sb([128, 1])
                kim, b_kim = C.sb([128, 1])
                P.dve(lambda e, nre=nre, r=r, c0=c0: e.tensor_tensor(out=nre[:], in0=r[:], in1=c0[:], op=ALU.mult), reads=[b_r, b_c0], writes=[b_nre])
                P.dve(lambda e, nre=nre: e.tensor_scalar(out=nre[:], in0=nre[:], scalar1=-1.0, scalar2=None, op0=ALU.add), reads=[b_nre], writes=[b_nre])
                P.dve(lambda e, nim=nim, r=r, s0=s0: e.tensor_tensor(out=nim[:], in0=r[:], in1=s0[:], op=ALU.mult), reads=[b_r, b_s0], writes=[b_nim])
                P.dve(lambda e, den=den, are=are: e.tensor_tensor(out=den[:], in0=are[:], in1=are[:], op=ALU.mult), reads=[b_are], writes=[b_den])
                P.dve(lambda e, den=den, aim=aim: e.scalar_tensor_tensor(out=den[:], in0=aim[:], scalar=aim[:, 0:1], in1=den[:], op0=ALU.mult, op1=ALU.add), reads=[b_aim, b_den], writes=[b_den])
                P.dve(lambda e, den=den: e.reciprocal(out=den[:], in_=den[:]), reads=[b_den], writes=[b_den])
                P.dve(lambda e, t0=t0, nre=nre, are=are: e.tensor_tensor(out=t0[:], in0=nre[:], in1=are[:], op=ALU.mult), reads=[b_nre, b_are], writes=[b_t0])
                P.dve(lambda e, kre=kre, nim=nim, aim=aim, t0=t0: e.scalar_tensor_tensor(out=kre[:], in0=nim[:], scalar=aim[:, 0:1], in1=t0[:], op0=ALU.mult, op1=ALU.add), reads=[b_nim, b_aim, b_t0], writes=[b_kre])
                P.dve(lambda e, kre=kre, den=den: e.tensor_tensor(out=kre[:], in0=kre[:], in1=den[:], op=ALU.mult), reads=[b_kre, b_den], writes=[b_kre])
                P.dve(lambda e, t0=t0, nre=nre, aim=aim: e.tensor_tensor(out=t0[:], in0=nre[:], in1=aim[:], op=ALU.mult), reads=[b_nre, b_aim], writes=[b_t0])
                P.dve(lambda e, kim=kim, nim=nim, are=are, t0=t0: e.scalar_tensor_tensor(out=kim[:], in0=nim[:], scalar=are[:, 0:1], in1=t0[:], op0=ALU.mult, op1=ALU.subtract), reads=[b_nim, b_are, b_t0], writes=[b_kim])
                P.dve(lambda e, kim=kim, den=den: e.tensor_tensor(out=kim[:], in0=kim[:], in1=den[:], op=ALU.mult), reads=[b_kim, b_den], writes=[b_kim])
                cA, b_cA = C.sb([128, 1])
                cB, b_cB = C.sb([128, 1])
                cC, b_cC = C.sb([128, 1])
                P.dve(lambda e, cA=cA, kre=kre: e.tensor_tensor(out=cA[:], in0=kre[:], in1=ks[:, 0:1], op=ALU.mult), reads=[b_kre, b_ks], writes=[b_cA])
                P.dve(lambda e, cA=cA, kim=kim: e.scalar_tensor_tensor(out=cA[:], in0=kim[:], scalar=ks[:, 1:2], in1=cA[:], op0=ALU.mult, op1=ALU.add), reads=[b_kim, b_ks, b_cA], writes=[b_cA])
                P.dve(lambda e, cB=cB, kre=kre: e.tensor_tensor(out=cB[:], in0=kre[:], in1=ks[:, 1:2], op=ALU.mult), reads=[b_kre, b_ks], writes=[b_cB])
                P.dve(lambda e, cB=cB, kim=kim: e.scalar_tensor_tensor(out=cB[:], in0=kim[:], scalar=ks[:, 0:1], in1=cB[:], op0=ALU.mult, op1=ALU.subtract), reads=[b_kim, b_ks, b_cB], writes=[b_cB])
                P.dve(lambda e, cC=cC, cB=cB: e.tensor_copy(out=cC[:], in_=cB[:]), reads=[b_cB], writes=[b_cC])
                P.dve(lambda e, cB=cB, cC=cC: e.tensor_scalar(out=cB[:], in0=cC[:], scalar1=-1.0, scalar2=None, op0=ALU.mult), reads=[b_cC], writes=[b_cB])
                P.dve(lambda e, thr=thr: e.tensor_scalar(out=ang[:], in0=tau[:], scalar1=thr[:, 0:1], scalar2=None, op0=ALU.mult), reads=[b_tau, b_thr], writes=[b_ang])
                aw, b_aw = _wrap(P, C, ang, b_ang, [128, T], scr=scr, key='A')
                ac, b_ac = _wrap(P, C, aw, b_aw, [128, T], add=PI / 2, scr=scr, key='B')
                St, b_St = C.sb([128, T])
                Ct, b_Ct = C.sb([128, T])
                Rb, b_Rb = C.sb([128, T])
                P.act(lambda e, St=St, aw=aw: e.activation(out=St[:], in_=aw[:], func=AF.Sin), reads=[b_aw], writes=[b_St])
                P.act(lambda e, Ct=Ct, ac=ac: e.activation(out=Ct[:], in_=ac[:], func=AF.Sin), reads=[b_ac], writes=[b_Ct])
                P.dve(lambda e, Rb=Rb, r=r: e.tensor_scalar(out=Rb[:], in0=onesT[:], scalar1=r[:, 0:1], scalar2=None, op0=ALU.mult), reads=[b_onesT, b_r], writes=[b_Rb])
                bre, b_bre = C.sb([128, 16])
                bim, b_bim = C.sb([128, 16])
                for half in range(2):
                    P.dma(lambda e, half=half, bre=bre, g=g: e.dma_start(out=bre[half * 64:(half + 1) * 64, :], in_=prm["b_re"][dr, g, :, :]), writes=[b_bre])
                    P.dma(lambda e, half=half, bim=bim, g=g: e.dma_start(out=bim[half * 64:(half + 1) * 64, :], in_=prm["b_im"][dr, g, :, :]), writes=[b_bim])
                BT = []
                for (c1, b_c1, c2, b_c2) in ((cA, b_cA, cB, b_cB), (cC, b_cC, cA, b_cA)):
                    bp, b_bp = C.sb([128, 128])
                    tt, b_tt = C.sb([128, 16])
                    P.pool(lambda e, bp=bp: e.memset(bp[:], 0.0), writes=[b_bp])
                    P.dve(lambda e, tt=tt, c1=c1, bre=bre: e.tensor_scalar(out=tt[:], in0=bre[:], scalar1=c1[:, 0:1], scalar2=None, op0=ALU.mult), reads=[b_bre, b_c1], writes=[b_tt])
                    P.dve(lambda e, bp=bp, tt=tt, c2=c2, gp=gp, bim=bim: e.scalar_tensor_tensor(out=bp[:, gp * 16:(gp + 1) * 16], in0=bim[:], scalar=c2[:, 0:1], in1=tt[:], op0=ALU.mult, op1=ALU.add),
                          reads=[b_bim, b_c2, b_tt, b_bp], writes=[b_bp])
                    pt, b_pt = ptr[0]
                    bT, b_bT = C.sb([128, 128], BF16)
                    P.pe(lambda e, pt=pt, bp=bp: e.transpose(pt[:], bp[:], idt[:]), reads=[b_bp, b_id], writes=[b_pt])
                    P.act(lambda e, bT=bT, pt=pt: e.activation(out=bT[:], in_=pt[:], func=AF.Copy), reads=[b_pt], writes=[b_bT])
                    BT.append((bT, b_bT))
                cc1, b_cc1 = C.sb([16, 128])
                cc2, b_cc2 = C.sb([16, 128])
                P.dma(lambda e, cc1=cc1, g=g: e.dma_start(out=cc1[:, 0:64], in_=prm["c_re"][dr, g, :, :]), writes=[b_cc1])
                P.dma(lambda e, cc1=cc1, g=g: e.dma_start(out=cc1[:, 64:128], in_=prm["c_im"][dr, g, :, :]), writes=[b_cc1])
                P.dma(lambda e, cc2=cc2, g=g: e.dma_start(out=cc2[:, 0:64], in_=prm["c_im"][dr, g, :, :]), writes=[b_cc2])
                P.dma(lambda e, cc2=cc2, g=g: e.dma_start(out=cc2[:, 64:128], in_=prm["c_re"][dr, g, :, :]), writes=[b_cc2])
                cm1, b_cm1 = C.sb([128, 128], BF16)
                cm2, b_cm2 = C.sb([128, 128], BF16)
                P.pool(lambda e, cm1=cm1: e.memset(cm1[:], 0.0), writes=[b_cm1])
                P.pool(lambda e, cm2=cm2: e.memset(cm2[:], 0.0), writes=[b_cm2])
                pt, b_pt = ptr[1]
                P.pe(lambda e, pt=pt, cc1=cc1: e.transpose(pt[:, 0:16], cc1[:], idt[0:16, 0:16]), reads=[b_cc1, b_id], writes=[b_pt])
                P.dve(lambda e, cm1=cm1, pt=pt, gp=gp: e.tensor_scalar(out=cm1[:, gp * 16:(gp + 1) * 16], in0=pt[:, 0:16], scalar1=ks[:, 2:3], scalar2=None, op0=ALU.mult),
                      reads=[b_pt, b_ks, b_cm1], writes=[b_cm1])
                P.pe(lambda e, pt=pt, cc2=cc2: e.transpose(pt[:, 0:16], cc2[:], idt[0:16, 0:16]), reads=[b_cc2, b_id], writes=[b_pt])
                P.dve(lambda e, cm2=cm2, pt=pt, gp=gp: e.tensor_scalar(out=cm2[:, gp * 16:(gp + 1) * 16], in0=pt[:, 0:16], scalar1=-1.0, scalar2=None, op0=ALU.mult),
                      reads=[b_pt, b_cm2], writes=[b_cm2])
                q_, b_q = C.sb([128, 1])
                rt, b_rt = C.sb([128, 128])
                P.dve(lambda e, q_=q_, St=St: e.tensor_tensor(out=q_[:], in0=St[:, T - 1:T], in1=ks[:, 2:3], op=ALU.mult), reads=[b_St, b_ks], writes=[b_q])
                P.dve(lambda e, rt=rt, Ct=Ct: e.tensor_scalar(out=rt[:], in0=idt[:], scalar1=Ct[:, T - 1:T], scalar2=None, op0=ALU.mult), reads=[b_id, b_Ct], writes=[b_rt])
                P.dve(lambda e, rt=rt, q_=q_: e.scalar_tensor_tensor(out=rt[:], in0=psw[:], scalar=q_[:, 0:1], in1=rt[:], op0=ALU.mult, op1=ALU.add), reads=[b_psw, b_q, b_rt], writes=[b_rt])
                carry, b_carry = C.sb([128, 1])
                P.dve(lambda e, carry=carry: e.memset(carry[:], 0.0), writes=[b_carry])
                G.append(dict(St=(St, b_St), Ct=(Ct, b_Ct), Rb=(Rb, b_Rb), B1=BT[0], B2=BT[1], C1=(cm1, b_cm1), C2=(cm2, b_cm2), RT=(rt, b_rt), carry=(carry, b_carry)))
            uts = [C.sb([128, T]) for _ in range(2)]
            utbs = [C.sb([128, T], BF16) for _ in range(2)]
            pbs = [C.ps([128, T]) for _ in range(2)]
            pss_ = [C.ps([128, T]) for _ in range(2)]
            py, b_py = C.ps([128, T])
            pc, b_pc = C.ps([128, 1])
            NB3 = 3
            t1s = [C.sb([128, T]) for _ in range(NB3)]
            t2s = [C.sb([128, T]) for _ in range(NB3)]
            bps = [C.sb([128, T]) for _ in range(NB3)]
            ws_ = [C.sb([128, T]) for _ in range(NB3)]
            wcs = [C.sb([128, T], BF16) for _ in range(NB3)]
            wss = [C.sb([128, T], BF16) for _ in range(NB3)]
            yos = [C.sb([128, T]) for _ in range(2)]
            yfs = [C.sb([128, T]) for _ in range(2)]
            it = 0
            blocks = range(NBK) if dr == 0 else range(NBK - 1, -1, -1)
            rows = slice(u_row + ct * 128, u_row + (ct + 1) * 128)
            for bi, bk in enumerate(blocks):
                cs_ = slice(bk * T, (bk + 1) * T)
                ut, b_ut = uts[bi % 2]
                P.dma(lambda e, ut=ut, cs_=cs_: e.dma_start(out=ut[:], in_=pT[rows, cs_]), writes=[b_ut])
                utb, b_utb = utbs[bi % 2]
                P.act(lambda e, ut=ut, utb=utb: e.activation(out=utb[:], in_=ut[:], func=AF.Copy), reads=[b_ut], writes=[b_utb])
                for gp in range(8):
                    gd = G[gp]
                    i = it % 2
                    it += 1
                    pb, b_pb = pbs[i]
                    pq, b_pq = pss_[i]
                    P.pe(lambda e, pb=pb, gd=gd, utb=utb: e.matmul(pb[:], gd["B1"][0][:], utb[:], start=True, stop=True), reads=[gd["B1"][1], b_utb], writes=[b_pb])
                    P.pe(lambda e, pq=pq, gd=gd, utb=utb: e.matmul(pq[:], gd["B2"][0][:], utb[:], start=True, stop=True), reads=[gd["B2"][1], b_utb], writes=[b_pq])
                    i3 = (it - 1) % NB3
                    t1, b_t1 = t1s[i3]
                    t2, b_t2 = t2s[i3]
                    bp, b_bp = bps[i3]
                    w_, b_w = ws_[i3]
                    wc, b_wc = wcs[i3]
                    wsn, b_wsn = wss[i3]
                    rv = (lambda a: a[:, ::-1]) if dr == 1 else (lambda a: a[:])
                    P.dve(lambda e, t1=t1, pb=pb, gd=gd, rv=rv: e.tensor_tensor(out=t1[:], in0=rv(pb), in1=gd["Ct"][0][:], op=ALU.mult), reads=[b_pb, gd["Ct"][1]], writes=[b_t1])
                    P.dve(lambda e, t2=t2, pq=pq, gd=gd, rv=rv: e.tensor_tensor(out=t2[:], in0=rv(pq), in1=gd["St"][0][:], op=ALU.mult), reads=[b_pq, gd["St"][1]], writes=[b_t2])
                    P.pool(lambda e, bp=bp, t1=t1, t2=t2: e.tensor_tensor(out=bp[:], in0=t1[:], in1=t2[:], op=ALU.add), reads=[b_t1, b_t2], writes=[b_bp])
                    P.dve(lambda e, w_=w_, bp=bp, gd=gd: e.tensor_tensor_scan(out=w_[:], data0=gd["Rb"][0][:], data1=bp[:], initial=gd["carry"][0][:, 0:1], op0=ALU.mult, op1=ALU.add),
                          reads=[gd["Rb"][1], b_bp, gd["carry"][1]], writes=[b_w])
                    P.pool(lambda e, wc=wc, w_=w_, gd=gd: e.tensor_tensor(out=wc[:], in0=w_[:], in1=gd["Ct"][0][:], op=ALU.mult), reads=[b_w, gd["Ct"][1]], writes=[b_wc])
                    P.dve(lambda e, wsn=wsn, w_=w_, gd=gd: e.tensor_tensor(out=wsn[:], in0=w_[:], in1=gd["St"][0][:], op=ALU.mult), reads=[b_w, gd["St"][1]], writes=[b_wsn])
                    P.pe(lambda e, gd=gd, wc=wc, gp=gp: e.matmul(py[:], gd["C1"][0][:], wc[:], start=(gp == 0), stop=False), reads=[gd["C1"][1], b_wc], writes=[b_py])
                    P.pe(lambda e, gd=gd, wsn=wsn, gp=gp: e.matmul(py[:], gd["C2"][0][:], wsn[:], start=False, stop=(gp == 7)), reads=[gd["C2"][1], b_wsn], writes=[b_py])
                    P.pe(lambda e, gd=gd, w_=w_: e.matmul(pc[:], gd["RT"][0][:], w_[:, T - 1:T], start=True, stop=True), reads=[gd["RT"][1], b_w], writes=[b_pc])
                    P.act(lambda e, gd=gd: e.activation(out=gd["carry"][0][:], in_=pc[:], func=AF.Copy), reads=[b_pc], writes=[gd["carry"][1]])
                yo, b_yo = yos[bi % 2]
                if dr == 0:
                    P.act(lambda e, yo=yo: e.activation(out=yo[:], in_=py[:], func=AF.Copy), reads=[b_py], writes=[b_yo])
                    P.dma(lambda e, yo=yo, cs_=cs_: e.dma_start(out=yf[ct * 128:(ct + 1) * 128, cs_], in_=yo[:]), reads=[b_yo], writes=[Buf()])
                else:
                    yft, b_yft = yfs[bi % 2]
                    P.dma(lambda e, yft=yft, cs_=cs_: e.dma_start(out=yft[:], in_=yf[ct * 128:(ct + 1) * 128, cs_]), writes=[b_yft])
                    P.dve(lambda e, yo=yo, yft=yft: e.tensor_tensor(out=yo[:], in0=py[:, ::-1], in1=yft[:], op=ALU.add), reads=[b_py, b_yft], writes=[b_yo])
                    P.dve(lambda e, yo=yo, ut=ut: e.scalar_tensor_tensor(out=yo[:], in0=ut[:], scalar=dsk[:, ct:ct + 1], in1=yo[:], op0=ALU.mult, op1=ALU.add), reads=[b_ut, b_dsk, b_yo], writes=[b_yo])
                    P.dma(lambda e, yo=yo, cs_=cs_: e.dma_start(out=mixT[out_row + ct * 128:out_row + (ct + 1) * 128, cs_], in_=yo[:]), reads=[b_yo], writes=[Buf()])
            C.end()


def stage_out(C, xT, mixT, w_out, gluw, glub, g8, router, ident, x1T, htok, aff, L, even):
    P = C.P
    C.begin()
    TB = min(512, L)
    NB = L // TB
    NTS = TB // 128
    KC = 8 if even else 6
    woutb, _ = C.sb([128, KC, 1024], BF16)
    b_wo = [Buf() for _ in range(KC)]
    for kc in range(KC):
        for c0 in range(0, 1024, 512):
            P.dma(lambda e, kc=kc, c0=c0: e.dma_start(out=woutb[:, kc, c0:c0 + 512], in_=w_out[kc * 128:(kc + 1) * 128, c0:c0 + 512]), writes=[b_wo[kc]], q="pool")
    if even:
        gluwb, b_gw = C.sb([128, 4, 512], BF16)
        for kc in range(4):
            P.dma(lambda e, kc=kc: e.dma_start(out=gluwb[:, kc, :], in_=gluw[kc * 128:(kc + 1) * 128, :]), writes=[b_gw], q="pool")
        gb, b_gb = C.sb([128, 4])
        P.dma(lambda e: e.dma_start(out=gb[:], in_=glub), writes=[b_gb])
        ngb, b_ngb = C.sb([128, 4])
        P.dve(lambda e: e.tensor_scalar(out=ngb[:], in0=gb[:], scalar1=-1.0, scalar2=None, op0=ALU.mult), reads=[b_gb], writes=[b_ngb])
    gt, b_g = C.sb([128, 8])
    wr, b_wr = C.sb([128, 8, 16])
    idt, b_id = C.sb([128, 128])
    ones, b_ones = C.sb([128, 128], BF16)
    P.dma(lambda e: e.dma_start(out=gt[:], in_=g8), writes=[b_g])
    P.dma(lambda e: e.dma_start(out=wr[:], in_=router.rearrange("(kc p) n -> p kc n", p=128)), writes=[b_wr])
    P.dma(lambda e: e.dma_start(out=idt[:], in_=ident), writes=[b_id])
    P.dve(lambda e: e.memset(ones[:], 1.0), writes=[b_ones])
    epsc, b_epsc = C.sb([128, 1])
    P.dve(lambda e: e.memset(epsc[:], EPS), writes=[b_epsc])
    mix, b_mix = C.sb([128, KC, TB])
    mixb, b_mixb = C.sb([128, KC, TB], BF16)
    xt, b_xt = C.sb([128, 8, TB])
    x1t, b_x1 = C.sb([128, 8, TB])
    ht, b_ht = C.sb([128, 8, TB])
    sq, b_sq = C.sb([128, 8, TB], BF16)
    rs, b_rs = C.sb([128, TB])
    yg, b_yg = C.sb([128, 4, TB])
    ygb, b_ygb = C.sb([128, 4, TB], BF16)
    sg, b_sg = C.sb([128, TB])
    hrows = [C.sb([128, 1024]) for _ in range(2)]
    afts = [C.sb([128, 16]) for _ in range(2)]
    ex, b_ex = C.sb([128, 16])
    mx, b_mx = C.sb([128, 1])
    sm, b_sm = C.sb([128, 1])
    pxs = [C.ps([128, TB]) for _ in range(2)]
    pss, b_pss = C.ps([128, TB])
    pz, b_pz = C.ps([128, TB])
    pl, b_pl = C.ps([128, 16])
    ptrs = [C.ps([128, 128]) for _ in range(2)]
    xv = xT.rearrange("(kc p) n -> p kc n", p=128)
    mv = mixT.rearrange("(kc p) n -> p kc n", p=128)
    x1v = x1T.rearrange("(kc p) n -> p kc n", p=128)
    nt = 0
    for tb in range(NB):
        sl = slice(tb * TB, (tb + 1) * TB)
        P.dma(lambda e, sl=sl: e.dma_start(out=mix[:], in_=mv[:, 0:KC, sl]), writes=[b_mix])
        P.dma(lambda e, sl=sl: e.dma_start(out=xt[:], in_=xv[:, :, sl]), writes=[b_xt])
        if even:
            P.act(lambda e: e.activation(out=mixb[:, 0:4, :], in_=mix[:, 0:4, :], func=AF.Copy), reads=[b_mix], writes=[b_mixb])
            P.act(lambda e: e.activation(out=yg[:], in_=mix[:, 4:8, :], func=AF.Gelu), reads=[b_mix], writes=[b_yg])
            P.dve(lambda e: e.tensor_copy(out=ygb[:], in_=yg[:]), reads=[b_yg], writes=[b_ygb])
            for ct in range(4):
                for kc in range(4):
                    P.pe(lambda e, ct=ct, kc=kc: e.matmul(pz[:], gluwb[:, kc, ct * 128:(ct + 1) * 128], ygb[:, kc, :], start=(kc == 0), stop=(kc == 3)),
                         reads=[b_gw, b_ygb], writes=[b_pz])
                P.act(lambda e, ct=ct: e.activation(out=sg[:], in_=pz[:], func=AF.Exp, scale=-1.0, bias=ngb[:, ct:ct + 1]), reads=[b_pz, b_ngb], writes=[b_sg])
                P.pool(lambda e: e.tensor_scalar(out=sg[:], in0=sg[:], scalar1=1.0, scalar2=None, op0=ALU.add), reads=[b_sg], writes=[b_sg])
                P.dve(lambda e: e.reciprocal(out=sg[:], in_=sg[:]), reads=[b_sg], writes=[b_sg])
                P.dve(lambda e, ct=ct: e.tensor_tensor(out=mixb[:, 4 + ct, :], in0=yg[:, ct, :], in1=sg[:], op=ALU.mult), reads=[b_yg, b_sg], writes=[b_mixb])
        else:
            P.act(lambda e: e.activation(out=mixb[:], in_=mix[:], func=AF.Copy), reads=[b_mix], writes=[b_mixb])
        for dt in range(8):
            px, b_px = pxs[dt % 2]
            for kc in range(KC):
                P.pe(lambda e, px=px, dt=dt, kc=kc: e.matmul(px[:], woutb[:, kc, dt * 128:(dt + 1) * 128], mixb[:, kc, :], start=(kc == 0), stop=(kc == KC - 1)),
                     reads=[b_wo[kc], b_mixb], writes=[b_px])
            P.dve(lambda e, px=px, dt=dt: e.tensor_tensor(out=x1t[:, dt, :], in0=px[:], in1=xt[:, dt, :], op=ALU.add), reads=[b_px, b_xt], writes=[b_x1])
        P.dma(lambda e, sl=sl: e.dma_start(out=x1v[:, :, sl], in_=x1t[:]), reads=[b_x1], writes=[Buf()])
        P.pool(lambda e: e.tensor_tensor(out=sq[:], in0=x1t[:], in1=x1t[:], op=ALU.mult), reads=[b_x1], writes=[b_sq])
        for kc in range(8):
            P.pe(lambda e, kc=kc: e.matmul(pss[:], ones[:], sq[:, kc, :], start=(kc == 0), stop=(kc == 7)), reads=[b_sq, b_ones], writes=[b_pss])
        P.act(lambda e: e.activation(out=rs[:], in_=pss[:], func=AF.Ln, scale=1.0 / D, bias=epsc[:, 0:1]), reads=[b_pss, b_epsc], writes=[b_rs])
        P.act(lambda e: e.activation(out=rs[:], in_=rs[:], func=AF.Exp, scale=-0.5), reads=[b_rs], writes=[b_rs])
        for dt in range(8):
            P.dve(lambda e, dt=dt: e.scalar_tensor_tensor(out=ht[:, dt, :], in0=x1t[:, dt, :], scalar=gt[:, dt:dt + 1], in1=rs[:], op0=ALU.mult, op1=ALU.mult),
                  reads=[b_x1, b_g, b_rs], writes=[b_ht])
        for ts in range(NTS):
            tsl = slice(ts * 128, (ts + 1) * 128)
            r0 = tb * TB + ts * 128
            for kc in range(8):
                P.pe(lambda e, kc=kc, tsl=tsl: e.matmul(pl[:], ht[:, kc, tsl], wr[:, kc, :], start=(kc == 0), stop=(kc == 7)), reads=[b_ht, b_wr], writes=[b_pl])
            aft, b_aft = afts[nt % 2]
            hrow, b_hrow = hrows[nt % 2]
            nt += 1
            P.dve(lambda e: e.reduce_max(out=mx[:], in_=pl[:], axis=AX.X), reads=[b_pl], writes=[b_mx])
            P.dve(lambda e: e.tensor_scalar(out=mx[:], in0=mx[:], scalar1=-1.0, scalar2=None, op0=ALU.mult), reads=[b_mx], writes=[b_mx])
            P.act(lambda e: e.activation(out=ex[:], in_=pl[:], func=AF.Exp, bias=mx[:, 0:1], accum_out=sm[:]), reads=[b_pl, b_mx], writes=[b_ex, b_sm])
            P.dve(lambda e: e.reciprocal(out=sm[:], in_=sm[:]), reads=[b_sm], writes=[b_sm])
            P.dve(lambda e, aft=aft: e.tensor_scalar(out=aft[:], in0=ex[:], scalar1=sm[:, 0:1], scalar2=None, op0=ALU.mult), reads=[b_ex, b_sm], writes=[b_aft])
            P.dma(lambda e, aft=aft, r0=r0: e.dma_start(out=aff[r0:r0 + 128, :], in_=aft[:]), reads=[b_aft], writes=[Buf()])
            for kc in range(8):
                ptr, b_ptr = ptrs[kc % 2]
                P.pe(lambda e, ptr=ptr, kc=kc, tsl=tsl: e.transpose(ptr[:], ht[:, kc, tsl], idt[:]), reads=[b_ht, b_id], writes=[b_ptr])
                if kc % 2 == 0:
                    P.act(lambda e, ptr=ptr, kc=kc, hrow=hrow: e.activation(out=hrow[:, kc * 128:(kc + 1) * 128], in_=ptr[:], func=AF.Copy), reads=[b_ptr], writes=[b_hrow])
                else:
                    P.dve(lambda e, ptr=ptr, kc=kc, hrow=hrow: e.tensor_copy(out=hrow[:, kc * 128:(kc + 1) * 128], in_=ptr[:]), reads=[b_ptr], writes=[b_hrow])
            P.dma(lambda e, hrow=hrow, r0=r0: e.dma_start(out=htok[r0:r0 + 128, :], in_=hrow[:]), reads=[b_hrow], writes=[Buf()])
    C.end()


BIG = 1.0e6


def _breg(e, rc, val):
    if 'r' not in rc:
        rc['r'] = e.to_reg(val)
    return rc['r']


def stage_route(C, aff, ustrict, rmask, posd, gmd, L):
    P = C.P
    C.begin()
    NJ = L // 128
    CAP = L // 8
    NCOL = 16 * NJ
    A, b_A = C.sb([128, NJ, 16])
    Ae, b_Ae = C.sb([128, 16, NJ])
    for jc in range(0, NJ, 16):
        je = min(NJ, jc + 16)
        P.dma(lambda e, jc=jc, je=je: e.dma_start(out=A[:, jc:je, :], in_=aff[jc * 128:je * 128, :].rearrange("(j p) e -> p j e", p=128)), writes=[b_A])
    P.dve(lambda e: e.tensor_copy(out=Ae[:], in_=A[:].rearrange("p j e -> p e j")), reads=[b_A], writes=[b_Ae])
    us, b_us = C.sb([128, 128])
    rm, b_rm = C.sb([128, NCOL])
    onesf, b_of = C.sb([128, 128])
    P.dma(lambda e: e.dma_start(out=us[:], in_=ustrict), writes=[b_us])
    P.dma(lambda e: e.dma_start(out=rm[:], in_=rmask), writes=[b_rm])
    P.dve(lambda e: e.memset(onesf[:], 1.0), writes=[b_of])
    lo, b_lo = C.sb([128, 16])
    hi, b_hi = C.sb([128, 16])
    mid, b_mid = C.sb([128, 16])
    cnt, b_cnt = C.sb([128, 16])
    ge, b_ge = C.sb([128, 16])
    d1, b_d1 = C.sb([128, 16])
    cmps = [C.sb([128, NJ]) for _ in range(2)]
    ptot, b_ptot = C.ps([128, 16])
    P.dve(lambda e: e.memset(lo[:], 0.0), writes=[b_lo])
    P.dve(lambda e: e.memset(hi[:], 2.0), writes=[b_hi])
    for it in range(34):
        P.dve(lambda e: e.tensor_tensor(out=mid[:], in0=lo[:], in1=hi[:], op=ALU.add), reads=[b_lo, b_hi], writes=[b_mid])
        P.dve(lambda e: e.tensor_scalar(out=mid[:], in0=mid[:], scalar1=0.5, scalar2=None, op0=ALU.mult), reads=[b_mid], writes=[b_mid])
        for ex in range(16):
            cm, b_cm = cmps[ex % 2]
            P.dve(lambda e, ex=ex, cm=cm: e.tensor_scalar(out=cm[:], in0=Ae[:, ex, :], scalar1=mid[:, ex:ex + 1], scalar2=None, op0=ALU.is_ge, op1=ALU.add, accum_out=cnt[:, ex:ex + 1]),
                  reads=[b_Ae, b_mid], writes=[b_cm, b_cnt])
        P.pe(lambda e: e.matmul(ptot[:], onesf[:], cnt[:], start=True, stop=True), reads=[b_of, b_cnt], writes=[b_ptot])
        P.dve(lambda e: e.tensor_scalar(out=ge[:], in0=ptot[:], scalar1=float(CAP) - 0.5, scalar2=None, op0=ALU.is_ge), reads=[b_ptot], writes=[b_ge])
        P.dve(lambda e: e.tensor_tensor(out=d1[:], in0=mid[:], in1=lo[:], op=ALU.subtract), reads=[b_mid, b_lo], writes=[b_d1])
        P.dve(lambda e: e.tensor_tensor(out=d1[:], in0=d1[:], in1=ge[:], op=ALU.mult), reads=[b_d1, b_ge], writes=[b_d1])
        P.dve(lambda e: e.tensor_tensor(out=lo[:], in0=lo[:], in1=d1[:], op=ALU.add), reads=[b_lo, b_d1], writes=[b_lo])
        P.dve(lambda e: e.tensor_tensor(out=d1[:], in0=hi[:], in1=mid[:], op=ALU.subtract), reads=[b_hi, b_mid], writes=[b_d1])
        P.dve(lambda e: e.tensor_tensor(out=d1[:], in0=d1[:], in1=ge[:], op=ALU.mult), reads=[b_d1, b_ge], writes=[b_d1])
        P.dve(lambda e: e.tensor_tensor(out=hi[:], in0=mid[:], in1=d1[:], op=ALU.add), reads=[b_mid, b_d1], writes=[b_hi])
    Me, b_Me = C.sb([128, 16, NJ])
    gm, b_gm = C.sb([128, 16, NJ])
    for ex in range(16):
        P.dve(lambda e, ex=ex: e.tensor_scalar(out=Me[:, ex, :], in0=Ae[:, ex, :], scalar1=lo[:, ex:ex + 1], scalar2=None, op0=ALU.is_ge), reads=[b_Ae, b_lo], writes=[b_Me])
    P.dve(lambda e: e.tensor_tensor(out=gm[:], in0=Ae[:], in1=Me[:], op=ALU.mult), reads=[b_Ae, b_Me], writes=[b_gm])
    Mf = Me[:].rearrange("p e j -> p (e j)")
    pre, b_pre = C.sb([128, NCOL])
    cn, b_cn = C.sb([128, NCOL])
    off, b_off = C.sb([128, NCOL])
    pp, b_pp = C.ps([128, min(512, NCOL)])
    pc, b_pc = C.ps([128, min(512, NCOL)])
    CW = min(512, NCOL)
    for c0 in range(0, NCOL, CW):
        P.pe(lambda e, c0=c0: e.matmul(pp[:], us[:], Mf[:, c0:c0 + CW], start=True, stop=True), reads=[b_us, b_Me], writes=[b_pp])
        P.act(lambda e, c0=c0: e.activation(out=pre[:, c0:c0 + CW], in_=pp[:], func=AF.Copy), reads=[b_pp], writes=[b_pre])
        P.pe(lambda e, c0=c0: e.matmul(pc[:], onesf[:], Mf[:, c0:c0 + CW], start=True, stop=True), reads=[b_of, b_Me], writes=[b_pc])
        P.act(lambda e, c0=c0: e.activation(out=cn[:, c0:c0 + CW], in_=pc[:], func=AF.Copy), reads=[b_pc], writes=[b_cn])
    P.dve(lambda e: e.tensor_tensor_scan(out=off[:], data0=rm[:], data1=cn[:], initial=0.0, op0=ALU.mult, op1=ALU.add), reads=[b_rm, b_cn], writes=[b_off])
    P.dve(lambda e: e.tensor_tensor(out=off[:], in0=off[:], in1=cn[:], op=ALU.subtract), reads=[b_off, b_cn], writes=[b_off])
    P.dve(lambda e: e.tensor_tensor(out=pre[:], in0=pre[:], in1=off[:], op=ALU.add), reads=[b_pre, b_off], writes=[b_pre])
    P.dve(lambda e: e.tensor_scalar(out=pre[:], in0=pre[:], scalar1=-BIG, scalar2=None, op0=ALU.add), reads=[b_pre], writes=[b_pre])
    P.dve(lambda e: e.tensor_tensor(out=pre[:], in0=pre[:], in1=Mf, op=ALU.mult), reads=[b_pre, b_Me], writes=[b_pre])
    P.dve(lambda e: e.tensor_scalar(out=pre[:], in0=pre[:], scalar1=BIG, scalar2=None, op0=ALU.add), reads=[b_pre], writes=[b_pre])
    pi, b_pi = C.sb([128, NCOL], I32)
    P.dve(lambda e: e.tensor_copy(out=pi[:], in_=pre[:]), reads=[b_pre], writes=[b_pi])
    P.dma(lambda e: e.dma_start(out=posd, in_=pi[:]), reads=[b_pi], writes=[Buf()])
    P.dma(lambda e: e.dma_start(out=gmd, in_=gm[:].rearrange("p e j -> p (e j)")), reads=[b_gm], writes=[Buf()])
    C.end()


def stage_dispatch(C, htok, posd, xe, L):
    P = C.P
    C.begin()
    NJ = L // 128
    CAP = L // 8
    pi, b_pi = C.sb([128, 16, NJ], I32)
    P.dma(lambda e: e.dma_start(out=pi[:].rearrange("p e j -> p (e j)"), in_=posd), writes=[b_pi])
    rc = {}
    hts = [C.sb([128, 1024]) for _ in range(2)]
    ixs = [C.sb([128, 1], I32) for _ in range(4)]
    ni = 0
    for j in range(NJ):
        ht, b_ht = hts[j % 2]
        P.dma(lambda e, ht=ht, j=j: e.dma_start(out=ht[:], in_=htok[j * 128:(j + 1) * 128, :]), writes=[b_ht])
        for ex in range(16):
            ix, b_ix = ixs[ni % 4]
            ni += 1
            P.dve(lambda e, ix=ix, ex=ex, j=j: e.tensor_copy(out=ix[:], in_=pi[:, ex, j:j + 1]), reads=[b_pi], writes=[b_ix])
            P.dma(lambda e, ht=ht, ix=ix, ex=ex: e.indirect_dma_start(out=xe[ex][:, :], out_offset=bass.IndirectOffsetOnAxis(ap=ix[:, :], axis=0),
                                                                      in_=ht[:, :], in_offset=None, bounds_check=_breg(e, rc, CAP - 1), oob_is_err=False),
                  reads=[b_ht, b_ix], writes=[Buf()], q="pool")
    C.end()


def stage_ffn(C, xe, w1, w3, w2, ye, ident, L, FF):
    P = C.P
    C.begin()
    CAP = L // 8
    SB = min(512, CAP)
    NSB = CAP // SB
    NST = SB // 128
    NF = FF // 128
    idt, b_id = C.sb([128, 128])
    P.dma(lambda e: e.dma_start(out=idt[:], in_=ident), writes=[b_id])
    w1b, _ = C.sb([128, 8, FF], BF16)
    w3b, _ = C.sb([128, 8, FF], BF16)
    w2b, _ = C.sb([128, NF, 1024], BF16)
    b_w1 = [Buf() for _ in range(8)]
    b_w3 = [Buf() for _ in range(8)]
    b_w2 = [Buf() for _ in range(NF)]
    xrs = [C.sb([128, 1024]) for _ in range(2)]
    xeT, b_xeT = C.sb([128, 8, SB], BF16)
    hid, b_hid = C.sb([128, NF, SB], BF16)
    sas = [C.sb([128, SB]) for _ in range(2)]
    yrows = [C.sb([128, 1024]) for _ in range(2)]
    ptrs = [C.ps([128, 128]) for _ in range(2)]
    pas = [C.ps([128, SB]) for _ in range(2)]
    pbs = [C.ps([128, SB]) for _ in range(2)]
    pys = [C.ps([128, 512]) for _ in range(2)]
    nx = 0
    ny = 0
    NSTG = 3
    stg = [C.sb([128, max(FF, 1024)]) for _ in range(NSTG)]
    ns = 0
    for ex in range(16):
        jobs = []
        for kc in range(8):
            jobs.append((w1[ex, kc * 128:(kc + 1) * 128, :], FF, w1b, kc, b_w1[kc]))
            jobs.append((w3[ex, kc * 128:(kc + 1) * 128, :], FF, w3b, kc, b_w3[kc]))
        for fc in range(NF):
            jobs.append((w2[ex, fc * 128:(fc + 1) * 128, :], 1024, w2b, fc, b_w2[fc]))
        for (src, width, dstt, di, b_dst) in jobs:
            sg_, b_sg = stg[ns % NSTG]
            ns += 1
            P.dma(lambda e, sg_=sg_, src=src, width=width: e.dma_start(out=sg_[:, 0:width], in_=src), writes=[b_sg])
            P.act(lambda e, sg_=sg_, dstt=dstt, di=di, width=width: e.activation(out=dstt[:, di, :], in_=sg_[:, 0:width], func=AF.Copy), reads=[b_sg], writes=[b_dst])
        for sb_ in range(NSB):
            for st in range(NST):
                r0 = sb_ * SB + st * 128
                xr, b_xr = xrs[nx % 2]
                nx += 1
                P.dma(lambda e, xr=xr, ex=ex, r0=r0: e.dma_start(out=xr[:], in_=xe[ex][r0:r0 + 128, :]), writes=[b_xr], q="pool")
                for kc in range(8):
                    ptr, b_ptr = ptrs[kc % 2]
                    P.pe(lambda e, ptr=ptr, xr=xr, kc=kc: e.transpose(ptr[:], xr[:, kc * 128:(kc + 1) * 128], idt[:]), reads=[b_xr, b_id], writes=[b_ptr])
                    if kc % 2 == 0:
                        P.act(lambda e, ptr=ptr, kc=kc, st=st: e.activation(out=xeT[:, kc, st * 128:(st + 1) * 128], in_=ptr[:], func=AF.Copy), reads=[b_ptr], writes=[b_xeT])
                    else:
                        P.dve(lambda e, ptr=ptr, kc=kc, st=st: e.tensor_copy(out=xeT[:, kc, st * 128:(st + 1) * 128], in_=ptr[:]), reads=[b_ptr], writes=[b_xeT])
            for ft in range(NF):
                pa, b_pa = pas[ft % 2]
                pb, b_pb = pbs[ft % 2]
                sa, b_sa = sas[ft % 2]
                for kc in range(8):
                    P.pe(lambda e, pa=pa, kc=kc, ft=ft: e.matmul(pa[:], w1b[:, kc, ft * 128:(ft + 1) * 128], xeT[:, kc, :], start=(kc == 0), stop=(kc == 7)), reads=[b_w1[kc], b_xeT], writes=[b_pa])
                for kc in range(8):
                    P.pe(lambda e, pb=pb, kc=kc, ft=ft: e.matmul(pb[:], w3b[:, kc, ft * 128:(ft + 1) * 128], xeT[:, kc, :], start=(kc == 0), stop=(kc == 7)), reads=[b_w3[kc], b_xeT], writes=[b_pb])
                P.act(lambda e, sa=sa, pa=pa: e.activation(out=sa[:], in_=pa[:], func=AF.Exp, scale=-1.0), reads=[b_pa], writes=[b_sa])
                P.dve(lambda e, sa=sa: e.tensor_scalar(out=sa[:], in0=sa[:], scalar1=1.0, scalar2=None, op0=ALU.add), reads=[b_sa], writes=[b_sa])
                P.dve(lambda e, sa=sa: e.reciprocal(out=sa[:], in_=sa[:]), reads=[b_sa], writes=[b_sa])
                P.dve(lambda e, sa=sa, pa=pa: e.tensor_tensor(out=sa[:], in0=pa[:], in1=sa[:], op=ALU.mult), reads=[b_pa, b_sa], writes=[b_sa])
                P.dve(lambda e, sa=sa, pb=pb, ft=ft: e.tensor_tensor(out=hid[:, ft, :], in0=pb[:], in1=sa[:], op=ALU.mult), reads=[b_pb, b_sa], writes=[b_hid])
            for st in range(NST):
                r0 = sb_ * SB + st * 128
                yrow, b_yrow = yrows[ny % 2]
                ny += 1
                for dh in range(2):
                    py, b_py = pys[dh]
                    for fc in range(NF):
                        P.pe(lambda e, py=py, fc=fc, st=st, dh=dh: e.matmul(py[:], hid[:, fc, st * 128:(st + 1) * 128], w2b[:, fc, dh * 512:(dh + 1) * 512], start=(fc == 0), stop=(fc == NF - 1)),
                             reads=[b_hid, b_w2[fc]], writes=[b_py])
                    if dh == 0:
                        P.act(lambda e, py=py, yrow=yrow: e.activation(out=yrow[:, 0:512], in_=py[:], func=AF.Copy), reads=[b_py], writes=[b_yrow])
                    else:
                        P.dve(lambda e, py=py, yrow=yrow: e.tensor_copy(out=yrow[:, 512:1024], in_=py[:]), reads=[b_py], writes=[b_yrow])
                P.dma(lambda e, yrow=yrow, ex=ex, r0=r0: e.dma_start(out=ye[ex][r0:r0 + 128, :], in_=yrow[:]), reads=[b_yrow], writes=[Buf()], q="pool")
    C.end()


def stage_combine(C, ye, posd, gmd, x1T, ident, x2T, outT, gfin, L):
    P = C.P
    C.begin()
    NJ = L // 128
    CAP = L // 8
    idt, b_id = C.sb([128, 128])
    P.dma(lambda e: e.dma_start(out=idt[:], in_=ident), writes=[b_id])
    pi, b_pi = C.sb([128, 16, NJ], I32)
    gm, b_gm = C.sb([128, 16, NJ])
    P.dma(lambda e: e.dma_start(out=pi[:].rearrange("p e j -> p (e j)"), in_=posd), writes=[b_pi])
    P.dma(lambda e: e.dma_start(out=gm[:].rearrange("p e j -> p (e j)"), in_=gmd), writes=[b_gm])
    Gs = [C.sb([128, 1024]) for _ in range(2)]
    for G_, b_G in Gs:
        P.dve(lambda e, G_=G_: e.memset(G_[:], 0.0), writes=[b_G])
    accs = [C.sb([128, 1024]) for _ in range(2)]
    x1s = [C.sb([128, 8, 128]) for _ in range(2)]
    x2s = [C.sb([128, 8, 128]) for _ in range(2)]
    ptrs = [C.ps([128, 128]) for _ in range(2)]
    if outT is not None:
        gt, b_g = C.sb([128, 8])
        ones, b_ones = C.sb([128, 128], BF16)
        sq, b_sq = C.sb([128, 8, 128], BF16)
        rs, b_rs = C.sb([128, 128])
        ots = [C.sb([128, 8, 128]) for _ in range(2)]
        pss, b_pss = C.ps([128, 128])
        P.dma(lambda e: e.dma_start(out=gt[:], in_=gfin), writes=[b_g])
        P.dve(lambda e: e.memset(ones[:], 1.0), writes=[b_ones])
        epsc, b_epsc = C.sb([128, 1])
        P.dve(lambda e: e.memset(epsc[:], EPS), writes=[b_epsc])
        ov = outT.rearrange("(kc p) n -> p kc n", p=128)
    x1v = x1T.rearrange("(kc p) n -> p kc n", p=128)
    x2v = x2T.rearrange("(kc p) n -> p kc n", p=128)
    ng = 0
    rc = {}
    ixs = [C.sb([128, 1], I32) for _ in range(4)]
    for j in range(NJ):
        acc, b_acc = accs[j % 2]
        x1t, b_x1 = x1s[j % 2]
        x2t, b_x2 = x2s[j % 2]
        cs_ = slice(j * 128, (j + 1) * 128)
        P.dma(lambda e, x1t=x1t, cs_=cs_: e.dma_start(out=x1t[:], in_=x1v[:, :, cs_]), writes=[b_x1])
        for ex in range(16):
            G_, b_G = Gs[ng % 2]
            ix, b_ix = ixs[ng % 4]
            ng += 1
            P.act(lambda e, ix=ix, ex=ex, j=j: e.activation(out=ix[:], in_=pi[:, ex, j:j + 1], func=AF.Copy), reads=[b_pi], writes=[b_ix])
            P.dma(lambda e, G_=G_, ex=ex, ix=ix: e.indirect_dma_start(out=G_[:, :], out_offset=None, in_=ye[ex][:, :],
                                                                      in_offset=bass.IndirectOffsetOnAxis(ap=ix[:, :], axis=0), bounds_check=_breg(e, rc, CAP - 1), oob_is_err=False),
                  reads=[b_ix], writes=[b_G], q="pool")
            if ex == 0:
                P.dve(lambda e, acc=acc, G_=G_, ex=ex, j=j: e.tensor_scalar(out=acc[:], in0=G_[:], scalar1=gm[:, ex, j:j + 1], scalar2=None, op0=ALU.mult), reads=[b_G, b_gm], writes=[b_acc])
            else:
                P.dve(lambda e, acc=acc, G_=G_, ex=ex, j=j: e.scalar_tensor_tensor(out=acc[:], in0=G_[:], scalar=gm[:, ex, j:j + 1], in1=acc[:], op0=ALU.mult, op1=ALU.add),
                      reads=[b_G, b_gm, b_acc], writes=[b_acc])
        for kc in range(8):
            ptr, b_ptr = ptrs[kc % 2]
            P.pe(lambda e, ptr=ptr, acc=acc, kc=kc: e.transpose(ptr[:], acc[:, kc * 128:(kc + 1) * 128], idt[:]), reads=[b_acc, b_id], writes=[b_ptr])
            P.dve(lambda e, ptr=ptr, x2t=x2t, x1t=x1t, kc=kc: e.tensor_tensor(out=x2t[:, kc, :], in0=ptr[:], in1=x1t[:, kc, :], op=ALU.add), reads=[b_ptr, b_x1], writes=[b_x2])
        P.dma(lambda e, x2t=x2t, cs_=cs_: e.dma_start(out=x2v[:, :, cs_], in_=x2t[:]), reads=[b_x2], writes=[Buf()])
        if outT is not None:
            ot, b_ot = ots[j % 2]
            P.pool(lambda e, x2t=x2t: e.tensor_tensor(out=sq[:], in0=x2t[:], in1=x2t[:], op=ALU.mult), reads=[b_x2], writes=[b_sq])
            for kc in range(8):
                P.pe(lambda e, kc=kc: e.matmul(pss[:], ones[:], sq[:, kc, :], start=(kc == 0), stop=(kc == 7)), reads=[b_sq, b_ones], writes=[b_pss])
            P.act(lambda e: e.activation(out=rs[:], in_=pss[:], func=AF.Ln, scale=1.0 / D, bias=epsc[:, 0:1]), reads=[b_pss, b_epsc], writes=[b_rs])
            P.act(lambda e: e.activation(out=rs[:], in_=rs[:], func=AF.Exp, scale=-0.5), reads=[b_rs], writes=[b_rs])
            for kc in range(8):
                P.dve(lambda e, ot=ot, x2t=x2t, kc=kc: e.scalar_tensor_tensor(out=ot[:, kc, :], in0=x2t[:, kc, :], scalar=gt[:, kc:kc + 1], in1=rs[:], op0=ALU.mult, op1=ALU.mult),
                      reads=[b_x2, b_g, b_rs], writes=[b_ot])
            P.dma(lambda e, ot=ot, cs_=cs_: e.dma_start(out=ov[:, :, cs_], in_=ot[:]), reads=[b_ot], writes=[Buf()])
    C.end()


DILS = (1, 4, 16)
DSPAN = (1, 2, 8)


def dil_masks():
    kk = np.arange(128)[:, None]
    qq = np.arange(128)[None, :]
    ms = []
    for g, d in enumerate(DILS):
        for dl in range(-DSPAN[g], DSPAN[g] + 1):
            rel = 128 * dl + kk - qq
            ms.append(((rel % d == 0) & (np.abs(rel) <= 64 * d)).astype(np.float32))
    return np.stack(ms)


def stage_vprep(C, pT, ident, vtok, L, v_row):
    P = C.P
    C.begin()
    TB = min(512, L)
    NT = TB // 128
    idt, b_id = C.sb([128, 128])
    P.dma(lambda e: e.dma_start(out=idt[:], in_=ident), writes=[b_id])
    vts = [C.sb([64, TB]) for _ in range(2)]
    vos = [C.sb([128, NT, 65]) for _ in range(2)]
    for vo, b_vo in vos:
        P.dve(lambda e, vo=vo: e.memset(vo[:], 1.0), writes=[b_vo])
    ptrs = [C.ps([128, 64]) for _ in range(2)]
    it = 0
    for hd in range(12):
        for tb in range(L // TB):
            vt, b_vt = vts[it % 2]
            vo, b_vo = vos[it % 2]
            it += 1
            P.dma(lambda e, vt=vt, hd=hd, tb=tb: e.dma_start(out=vt[:], in_=pT[v_row + hd * 64:v_row + (hd + 1) * 64, tb * TB:(tb + 1) * TB]), writes=[b_vt])
            for t in range(NT):
                ptr, b_ptr = ptrs[t % 2]
                P.pe(lambda e, ptr=ptr, vt=vt, t=t: e.transpose(ptr[:], vt[:, t * 128:(t + 1) * 128], idt[0:64, 0:64]), reads=[b_vt, b_id], writes=[b_ptr])
                if t % 2 == 0:
                    P.act(lambda e, ptr=ptr, vo=vo, t=t: e.activation(out=vo[:, t, 0:64], in_=ptr[:], func=AF.Copy), reads=[b_ptr], writes=[b_vo])
                else:
                    P.dve(lambda e, ptr=ptr, vo=vo, t=t: e.tensor_copy(out=vo[:, t, 0:64], in_=ptr[:]), reads=[b_ptr], writes=[b_vo])
            P.dma(lambda e, vo=vo, hd=hd, tb=tb: e.dma_start(out=vtok[hd][tb * TB:(tb + 1) * TB, :].rearrange("(t p) c -> p t c", p=128), in_=vo[:]), reads=[b_vo], writes=[Buf()])
    C.end()


def stage_dil(C, pT, ident, masks, vtok, mixT, L, q_row, k_row, out_row):
    P = C.P
    NCH = L // 128
    for i in range(4):
        C.begin()
        idt, b_id = C.sb([128, 128])
        mk, b_mk = C.sb([128, 25, 128])
        P.dma(lambda e: e.dma_start(out=idt[:], in_=ident), writes=[b_id])
        P.dma(lambda e: e.dma_start(out=mk[:], in_=masks.rearrange("m p n -> p m n")), writes=[b_mk])
        qs = [[C.sb([64, 128]) for _ in range(2)] for _ in range(3)]
        ks = [[C.sb([64, (2 * DSPAN[g] + 1) * 128]) for _ in range(2)] for g in range(3)]
        vs = [[C.sb([128, 2 * DSPAN[g] + 1, 65]) for _ in range(2)] for g in range(3)]
        pss = [C.ps([128, 512]) for _ in range(2)]
        pacc = [C.ps([128, 65]) for _ in range(2)]
        ptr, b_ptr = C.ps([64, 128])
        es = [C.sb([128, 512]) for _ in range(2)]
        pms = [C.sb([128, 512]) for _ in range(2)]
        rd, b_rd = C.sb([128, 1])
        ots = [C.sb([128, 64]) for _ in range(2)]
        oTs = [C.sb([64, 128]) for _ in range(2)]
        moff = (0, 3, 8)
        ngrp = 0
        for n in range(NCH):
            pa, b_pa = pacc[n % 2]
            first = True
            plan = []
            for g in range(3):
                hd = 4 * g + i
                D_ = DSPAN[g]
                m0 = max(0, n - D_)
                m1 = min(NCH - 1, n + D_)
                q_, b_q = qs[g][n % 2]
                k_, b_k = ks[g][n % 2]
                v_, b_v = vs[g][n % 2]
                nm = m1 - m0 + 1
                P.dma(lambda e, q_=q_, hd=hd, n=n: e.dma_start(out=q_[:], in_=pT[q_row + hd * 64:q_row + (hd + 1) * 64, n * 128:(n + 1) * 128]), writes=[b_q])
                P.dma(lambda e, k_=k_, hd=hd, m0=m0, nm=nm: e.dma_start(out=k_[:, 0:nm * 128], in_=pT[k_row + hd * 64:k_row + (hd + 1) * 64, m0 * 128:(m0 + nm) * 128]), writes=[b_k])
                P.dma(lambda e, v_=v_, hd=hd, m0=m0, nm=nm: e.dma_start(out=v_[:, 0:nm, :], in_=vtok[hd][m0 * 128:(m0 + nm) * 128, :].rearrange("(t p) c -> p t c", p=128)), writes=[b_v])
                tiles = [(g, m, m - m0, moff[g] + (m - n) + D_) for m in range(m0, m1 + 1)]
                for c0 in range(0, len(tiles), 4):
                    plan.append((g, tiles[c0:c0 + 4], (q_, b_q), (k_, b_k), (v_, b_v)))
            total = sum(len(p[1]) for p in plan)
            done = 0
            for (g, tl, (q_, b_q), (k_, b_k), (v_, b_v)) in plan:
                ps_, b_ps = pss[ngrp % 2]
                e_, b_e = es[ngrp % 2]
                pm, b_pm = pms[ngrp % 2]
                nt = len(tl)
                for a, (_, m, ml, mi) in enumerate(tl):
                    P.pe(lambda e, ps_=ps_, k_=k_, q_=q_, a=a, ml=ml: e.matmul(ps_[:, a * 128:(a + 1) * 128], k_[:, ml * 128:(ml + 1) * 128], q_[:], start=True, stop=True),
                         reads=[b_k, b_q], writes=[b_ps])
                P.act(lambda e, e_=e_, ps_=ps_, nt=nt: e.activation(out=e_[:, 0:nt * 128], in_=ps_[:, 0:nt * 128], func=AF.Exp, scale=0.125), reads=[b_ps], writes=[b_e])
                mi0 = tl[0][3]
                eng = P.dve if ngrp % 2 == 0 else P.pool
                eng(lambda e, pm=pm, e_=e_, nt=nt, mi0=mi0: e.tensor_tensor(out=pm[:, 0:nt * 128], in0=e_[:, 0:nt * 128], in1=mk[:, mi0:mi0 + nt, :].rearrange("p m n -> p (m n)"), op=ALU.mult),
                    reads=[b_e, b_mk], writes=[b_pm])
                for a, (_, m, ml, mi) in enumerate(tl):
                    done += 1
                    P.pe(lambda e, pa=pa, pm=pm, v_=v_, a=a, ml=ml, st=(done == 1), sp=(done == total): e.matmul(pa[:], pm[:, a * 128:(a + 1) * 128], v_[:, ml, :], start=st, stop=sp),
                         reads=[b_pm, b_v], writes=[b_pa])
                ngrp += 1
            ot, b_ot = ots[n % 2]
            oT, b_oT = oTs[n % 2]
            P.dve(lambda e, pa=pa: e.reciprocal(out=rd[:], in_=pa[:, 64:65]), reads=[b_pa], writes=[b_rd])
            P.dve(lambda e, ot=ot, pa=pa: e.tensor_scalar(out=ot[:], in0=pa[:, 0:64], scalar1=rd[:, 0:1], scalar2=None, op0=ALU.mult), reads=[b_pa, b_rd], writes=[b_ot])
            P.pe(lambda e, ot=ot: e.transpose(ptr[:], ot[:], idt[:]), reads=[b_ot, b_id], writes=[b_ptr])
            P.act(lambda e, oT=oT: e.activation(out=oT[:], in_=ptr[:], func=AF.Copy), reads=[b_ptr], writes=[b_oT])
            P.dma(lambda e, oT=oT, n=n: e.dma_start(out=mixT[out_row + i * 64:out_row + (i + 1) * 64, n * 128:(n + 1) * 128], in_=oT[:]), reads=[b_oT], writes=[Buf()])
        C.end()


def rope_tables(L, half, period):
    inv = (10000.0 ** (-np.arange(half, dtype=np.float32) / half)).astype(np.float32)
    ang = (np.arange(L, dtype=np.float32)[None, :] * inv[:, None]).astype(np.float32)
    cos = np.cos(ang).astype(np.float32)
    sin = np.sin(ang).astype(np.float32)
    rows = np.arange(128) % period
    c = cos[rows % half]
    s = np.where((rows < half)[:, None], -sin[rows % half], sin[rows % half])
    return np.ascontiguousarray(c, np.float32), np.ascontiguousarray(s, np.float32)


def swap_cols(w, ncols, period):
    half = period // 2
    idx = np.arange(ncols)
    src = (idx // period) * period + (idx % period + half) % period
    return np.ascontiguousarray(w[:, src])


def ret_tables(L):
    NCH = L // 128
    s = 128.0 ** -0.5
    tab = np.zeros((128, 24, NCH), np.float64)
    t = np.arange(128, dtype=np.float64)
    for h in range(4):
        lg = np.log1p(-(2.0 ** (-5.0 - h)))
        for dr in range(2):
            e = (t + 1) if dr == 0 else (128 - t)
            j0 = (h * 2 + dr) * 3
            tab[:, j0, :] = np.exp(lg * e)[:, None]
            tab[:, j0 + 1, :] = (np.exp(-lg * e) * s)[:, None]
            tab[:, j0 + 2, :] = np.exp(lg * 128)
    return tab.astype(np.float32)


def la_masks():
    j = np.arange(128)[:, None]
    i = np.arange(128)[None, :]
    return np.stack([(j <= i), (j > i), (j >= i)]).astype(np.float32)


def g8(v):
    return np.ascontiguousarray(v.reshape(8, 128).T)


def s5_host(d, T):
    a_re, a_im = d['s5_a_re'][0], d['s5_a_im'][0]
    are2 = np.concatenate([a_re.reshape(64, 64).T] * 2, 0)
    aim2 = np.concatenate([a_im.reshape(64, 64).T] * 2, 0)
    lstep = np.tile(d['s5_log_step'][0].reshape(1, 64), (128, 1))
    dsk = d['s5_d'][0].reshape(4, 128).T
    prm = {"are2": are2, "aim2": aim2, "lstep": lstep, "dsk": dsk, "b_re": d['s5_b_re'][0], "b_im": d['s5_b_im'][0],
           "c_re": d['s5_c_re'][0], "c_im": d['s5_c_im'][0]}
    psw = np.zeros((128, 128), np.float32)
    for k in range(128):
        psw[k, (k + 64) % 128] = 1
    ksel = np.zeros((128, 3), np.float32)
    ksel[:64, 0] = 1; ksel[64:, 1] = 1; ksel[:64, 2] = 1; ksel[64:, 2] = -1
    tau = np.tile(np.arange(1, T + 1, dtype=np.float32)[None, :], (128, 1))
    consts = {"psw": psw, "ksel": ksel, "tau": tau}
    return {k: np.ascontiguousarray(v, np.float32) for k, v in prm.items()}, consts


def route_consts(L):
    NJ = L // 128
    q = np.arange(128)[:, None]; p = np.arange(128)[None, :]
    us = (q < p).astype(np.float32)
    rm = np.ones((16, NJ), np.float32); rm[:, 0] = 0
    rm = np.tile(rm.reshape(1, -1), (128, 1))
    return us, np.ascontiguousarray(rm)


class RowSplit:
    def __init__(self, C, name, rows, L, chunk=1024):
        self.chunk = chunk
        self.parts = [C.dscr("%s_%d" % (name, k), [min(chunk, rows - k * chunk), L]) for k in range(-(-rows // chunk))]

    def __getitem__(self, key):
        rs, cs = key
        k = rs.start // self.chunk
        assert (rs.stop - 1) // self.chunk == k
        return self.parts[k][rs.start - k * self.chunk:rs.stop - k * self.chunk, cs]


def build_program(L, FF):
    NCH = L // 128
    NJ = L // 128
    CAP = L // 8
    T = min(512, L)
    C = Ctx()
    i = {}
    def din(name, shape, dt=F32):
        i[name] = C.din(name, shape, dt)
        return i[name]
    xT = din("xT", [D, L])
    w_in = [din("w_in0", [D, 2560]), din("w_in1", [D, 4480])]
    wsw = [din("wsw0", [D, 1024]), din("wsw1", [D, 1536])]
    w_out = [din("w_out0", [1024, 1024]), din("w_out1", [768, 1024])]
    gmix = [din("gmix0", [128, 8]), din("gmix1", [128, 8])]
    gffn = [din("gffn0", [128, 8]), din("gffn1", [128, 8])]
    gfin = din("gfin", [128, 8])
    cos = [din("cos0", [128, L]), din("cos1", [128, L])]
    sin = [din("sin0", [128, L]), din("sin1", [128, L])]
    ident = din("ident", [128, 128])
    lam = din("lamask", [3, 128, 128])
    rtab = din("rtab", [128, 24, NCH])
    prm_shapes = {"are2": [128, 64], "aim2": [128, 64], "lstep": [128, 64], "dsk": [128, 4], "b_re": [2, 32, 64, 16], "b_im": [2, 32, 64, 16],
                  "c_re": [2, 32, 16, 64], "c_im": [2, 32, 16, 64]}
    prm = {}
    for k, shp in prm_shapes.items():
        a = din("s5_" + k, shp)
        prm[k] = a[:, :] if len(shp) == 2 else a
    consts = {"ident": ident[:, :], "psw": din("c_psw", [128, 128])[:, :], "ksel": din("c_ksel", [128, 3])[:, :], "tau": din("c_tau", [128, T])[:, :]}
    gluw = din("gluw", [512, 512])
    glub = din("glub", [128, 4])
    router = [din("router0", [1024, 16]), din("router1", [1024, 16])]
    ustrict = din("ustrict", [128, 128])
    rmask = din("rmask", [128, 16 * NJ])
    mlb = din("mlb", [128, 16])
    dmasks = din("dmasks", [25, 128, 128])
    w1 = [din("w1_0", [16, 1024, FF]), din("w1_1", [16, 1024, FF])]
    w3 = [din("w3_0", [16, 1024, FF]), din("w3_1", [16, 1024, FF])]
    w2 = [din("w2_0", [16, FF, 1024]), din("w2_1", [16, FF, 1024])]
    outT = C.dout("outT", [D, L])
    pT = RowSplit(C, "pT", 4480, L)
    mixT = C.dscr("mixT", [1024, L])
    o1 = C.dscr("o1", [4, L, 128])
    yf = C.dscr("yf", [512, L])
    x1T = C.dscr("x1T", [1024, L])
    x2T = C.dscr("x2T", [1024, L])
    x3T = C.dscr("x3T", [1024, L])
    htok = C.dscr("htok", [L, 1024])
    aff = C.dscr("aff", [L, 16])
    posd = C.dscr("posd", [128, 16 * NJ], I32)
    gmd = C.dscr("gmd", [128, 16 * NJ])
    xe = [C.dscr("xe%d" % e, [CAP, 1024]) for e in range(16)]
    ye = [C.dscr("ye%d" % e, [CAP, 1024]) for e in range(16)]
    tabd = C.dscr("tabd", [128, 24, NCH])
    vtok = [C.dscr("vtok%d" % h, [L, 65]) for h in range(12)]

    def moe(layer, xin1T, xoutT, final):
        stage_route(C, aff, ustrict[:, :], rmask[:, :], posd[:, :], gmd[:, :], L)
        stage_dispatch(C, htok, posd[:, :], xe, L)
        stage_ffn(C, xe, w1[layer], w3[layer], w2[layer], ye, ident[:, :], L, FF)
        stage_combine(C, ye, posd[:, :], gmd[:, :], xin1T, ident[:, :], xoutT, outT if final else None, gfin[:, :], L)

    stage_proj(C, xT, w_in[0], wsw[0], gmix[0][:, :], cos[0], sin[0], pT, L, 20, 8)
    stage_linattn(C, pT, rtab[:, :, :], ident[:, :], lam, o1, mixT, L, 0, 512, 1024, 1536, 0, False)
    stage_s5(C, pT, prm, consts, yf, mixT, L, 2048, 512)
    stage_out(C, xT, mixT, w_out[0], gluw, glub[:, :], gffn[0][:, :], router[0], ident[:, :], x1T, htok, aff, L, True)
    moe(0, x1T, x2T, False)
    stage_proj(C, x2T, w_in[1], wsw[1], gmix[1][:, :], cos[1], sin[1], pT, L, 35, 12)
    stage_gates(C, pT, mlb[:, :], ident[:, :], tabd, L, 4352)
    stage_linattn(C, pT, tabd[:, :, :], ident[:, :], lam, o1, mixT, L, 2304, 2816, 3328, 3840, 256, True)
    stage_vprep(C, pT, ident[:, :], vtok, L, 1536)
    stage_dil(C, pT, ident[:, :], dmasks, vtok, mixT, L, 0, 768, 0)
    stage_out(C, x2T, mixT, w_out[1], None, None, gffn[1][:, :], router[1], ident[:, :], x1T, htok, aff, L, False)
    moe(1, x1T, x3T, True)
    ninst = C.P.ninst
    return C.finish(), ninst


def host_inputs(inp, b, L, FF):
    f = lambda a: np.ascontiguousarray(a, np.float32)
    T = min(512, L)
    im = {"xT": f(inp["x"][b].T)}
    W0 = inp["ev_w_in"][0]
    W1 = np.zeros((D, 4480), np.float32)
    W1[:, :4368] = inp["od_w_in"][0]
    im["w_in0"] = f(W0); im["w_in1"] = W1
    im["wsw0"] = swap_cols(W0[:, :1024], 1024, 128); im["wsw1"] = swap_cols(W1[:, :1536], 1536, 64)
    im["w_out0"] = f(inp["ev_w_out"][0]); im["w_out1"] = f(inp["od_w_out"][0])
    for l in range(2):
        im["gmix%d" % l] = g8(inp["norm_mix_g"][l]); im["gffn%d" % l] = g8(inp["norm_ffn_g"][l])
        im["router%d" % l] = f(inp["moe_router"][l])
        im["w1_%d" % l] = f(inp["moe_w1"][l]); im["w3_%d" % l] = f(inp["moe_w3"][l]); im["w2_%d" % l] = f(inp["moe_w2"][l])
    im["gfin"] = g8(inp["final_g"])
    im["cos0"], im["sin0"] = rope_tables(L, 64, 128)
    im["cos1"], im["sin1"] = rope_tables(L, 32, 64)
    im["ident"] = np.eye(128, dtype=np.float32)
    im["lamask"] = la_masks()
    im["rtab"] = ret_tables(L)
    prm, consts = s5_host({k: inp[k] for k in ("s5_a_re", "s5_a_im", "s5_b_re", "s5_b_im", "s5_c_re", "s5_c_im", "s5_log_step", "s5_d")}, T)
    for k, v in prm.items():
        im["s5_" + k] = v
    for k, v in consts.items():
        im["c_" + k] = f(v)
    im["gluw"] = f(inp["s5_glu_w"][0]); im["glub"] = f(inp["s5_glu_b"][0].reshape(4, 128).T)
    im["ustrict"], im["rmask"] = route_consts(L)
    im["mlb"] = f(np.tile(np.concatenate([inp["ml_i_bias"][0].reshape(-1), inp["ml_f_bias"][0].reshape(-1)])[None, :], (128, 1)))
    im["dmasks"] = dil_masks()
    return im


def run_model(inp, batches):
    L = inp["x"].shape[1]
    FF = inp["moe_w1"].shape[-1]
    nc, ninst = build_program(L, FF)
    ims = [host_inputs(inp, b, L, FF) for b in batches]
    res = run_bass_kernel_spmd(nc, ims, core_ids=list(range(len(batches))))
    return [np.ascontiguousarray(r["outT"].T) for r in res.results]


def kernel(**inputs):
    inp = {k: np.asarray(v) for k, v in inputs.items()}
    B = inp["x"].shape[0]
    outs = run_model(inp, list(range(B)))
    return np.stack(outs, 0).astype(np.float32)
```

```python
import contextlib
import numpy as np
import concourse.bass as bass
import concourse.mybir as mybir
from concourse.bass_utils import run_bass_kernel_spmd

F32 = mybir.dt.float32
BF16 = mybir.dt.bfloat16
I32 = mybir.dt.int32
AF = mybir.ActivationFunctionType
ALU = mybir.AluOpType
AX = mybir.AxisListType
D = 1024
EPS = 1e-6
ENGS = ("pe", "act", "dve", "pool", "sp")
ENGOBJ = {"pe": "tensor", "act": "scalar", "dve": "vector", "pool": "gpsimd", "sp": "sync"}


class Buf:
    __slots__ = ("last_w", "readers")

    def __init__(self):
        self.last_w = None
        self.readers = []


class Op:
    __slots__ = ("eng", "fn", "idx", "deps", "dma", "signal", "cnt", "sem", "semval", "stage")


class Prog:
    ND = {"sp": 10, "act": 2, "pool": 2}

    def __init__(self, nc, st):
        self.nc = nc
        self.esem = {e: st.enter_context(nc.semaphore("s_" + e)) for e in ENGS}
        self.dsem = {(q, k): st.enter_context(nc.semaphore("d_%s%d" % (q, k))) for q in self.ND for k in range(self.ND[q])}
        self.base = {e: 0 for e in ENGS}
        self.dk = {q: 0 for q in self.ND}
        self.dval = {k: 0 for k in self.dsem}
        self.waited = {e: {} for e in ENGS}
        self.stage = 0
        self.ops = {e: [] for e in ENGS}
        self.ninst = 0

    def op(self, eng, fn, reads=(), writes=(), dma=False):
        o = Op()
        o.eng, o.fn, o.idx, o.dma, o.signal, o.cnt, o.stage = eng, fn, len(self.ops[eng]), dma, False, 0, self.stage
        o.sem, o.semval = None, 0
        deps = {}
        for b in reads:
            if b.last_w is not None and b.last_w.stage == self.stage:
                deps[id(b.last_w)] = b.last_w
        for b in writes:
            if b.last_w is not None and b.last_w.stage == self.stage:
                deps[id(b.last_w)] = b.last_w
            for r in b.readers:
                if r.stage == self.stage:
                    deps[id(r)] = r
        o.deps = list(deps.values())
        for b in reads:
            b.readers.append(o)
        for b in writes:
            b.last_w = o
            b.readers = []
        if dma:
            k = self.dk[eng]
            self.dk[eng] += 1
            nd = self.ND[eng]
            o.sem = (eng, k % nd)
            o.semval = 16 * (k // nd + 1)
            self.dval[o.sem] = o.semval
        self.ops[eng].append(o)
        self.ninst += 1
        return o

    def pe(self, fn, reads=(), writes=()):
        return self.op("pe", fn, reads, writes)

    def act(self, fn, reads=(), writes=()):
        return self.op("act", fn, reads, writes)

    def dve(self, fn, reads=(), writes=()):
        return self.op("dve", fn, reads, writes)

    def pool(self, fn, reads=(), writes=()):
        return self.op("pool", fn, reads, writes)

    def dma(self, fn, reads=(), writes=(), q="sp"):
        return self.op(q, fn, reads, writes, dma=True)

    def end_stage(self):
        nc = self.nc
        for e in ENGS:
            comp = [o for o in self.ops[e] if not o.dma]
            if comp:
                comp[-1].signal = True
            for o in self.ops[e]:
                for d in o.deps:
                    if d.dma:
                        continue
                    if d.eng == o.eng:
                        if e == "pe":
                            continue
                        if o.idx - d.idx > 2 and not o.dma:
                            continue
                    d.signal = True
        final = {}
        for e in ENGS:
            c = self.base[e]
            for o in self.ops[e]:
                if o.dma:
                    continue
                if o.signal:
                    c += 1
                o.cnt = c
            final[e] = c
        with nc.Block() as block:
            def run_engine(e, eng):
                waited = self.waited[e]

                def need(sem, key, val):
                    if waited.get(key, 0) < val:
                        eng.wait_ge(sem, val)
                        waited[key] = val

                for o in self.ops[e]:
                    for d in o.deps:
                        if d.dma:
                            need(self.dsem[d.sem], d.sem, d.semval)
                        else:
                            if d.eng == e:
                                if e == "pe":
                                    continue
                                if o.idx - d.idx > 2 and not o.dma:
                                    continue
                            need(self.esem[d.eng], d.eng, d.cnt)
                    if o.dma:
                        if o.semval > 16:
                            need(self.dsem[o.sem], o.sem, o.semval - 16)
                        inst = o.fn(eng)
                        inst.then_inc(self.dsem[o.sem], 16)
                    else:
                        inst = o.fn(eng)
                        if o.signal:
                            inst.then_inc(self.esem[e], 1)
                for e2 in ENGS:
                    if e2 != e and final[e2] > 0:
                        need(self.esem[e2], e2, final[e2])
                for k, v in self.dval.items():
                    if v > 0:
                        need(self.dsem[k], k, v)

            for e in ENGS:
                def _f(eng, e=e):
                    run_engine(e, eng)
                getattr(block, ENGOBJ[e])(_f)
        self.base = final
        self.stage += 1
        self.ops = {e: [] for e in ENGS}


class Ctx:
    def __init__(self):
        self.nc = bass.Bass("TRN2", target_bir_lowering=False)
        self.gst = contextlib.ExitStack()
        self.P = Prog(self.nc, self.gst)
        self.st = None
        self.n = 0
        self.dbg = {}

    def din(self, name, shape, dt=F32):
        return self.nc.dram_tensor(name, list(shape), dt, kind="ExternalInput").ap()

    def dout(self, name, shape, dt=F32):
        return self.nc.dram_tensor(name, list(shape), dt, kind="ExternalOutput").ap()

    def dscr(self, name, shape, dt=F32, debug=False):
        if debug:
            return self.dout(name, shape, dt)
        return self.nc.dram_tensor(name, list(shape), dt, kind="Internal").ap()

    def begin(self):
        self.st = contextlib.ExitStack()

    def end(self):
        self.P.end_stage()
        self.st.close()
        self.st = None

    def sb(self, shape, dt=F32):
        self.n += 1
        t = self.st.enter_context(self.nc.sbuf_tensor("t%d" % self.n, list(shape), dt))
        return t, Buf()

    def ps(self, shape, dt=F32):
        self.n += 1
        t = self.st.enter_context(self.nc.psum_tensor("p%d" % self.n, list(shape), dt))
        return t, Buf()

    def finish(self):
        self.gst.close()
        return self.nc


def stage_proj(C, xT, w, wsw, g8, cos, sin, pT, L, NCT, NRT):
    P = C.P
    C.begin()
    TB = min(512, L)
    NB = L // TB
    wb, _ = C.sb([128, 8, NCT * 128], BF16)
    wswb, _ = C.sb([128, 8, NRT * 128], BF16)
    b_w = [Buf() for _ in range(8)]
    b_ws = [Buf() for _ in range(8)]
    gt, b_g = C.sb([128, 8])
    ones, b_ones = C.sb([128, 128], BF16)
    P.dma(lambda e: e.dma_start(out=gt[:], in_=g8), writes=[b_g])
    P.dve(lambda e: e.memset(ones[:], 1.0), writes=[b_ones])
    epsc, b_epsc = C.sb([128, 1])
    P.dve(lambda e: e.memset(epsc[:], EPS), writes=[b_epsc])
    CH = 640
    for kc in range(8):
        for c0 in range(0, NCT * 128, CH):
            P.dma(lambda e, kc=kc, c0=c0: e.dma_start(out=wb[:, kc, c0:c0 + CH], in_=w[kc * 128:(kc + 1) * 128, c0:c0 + CH]),
                  writes=[b_w[kc]], q="pool")
        for c0 in range(0, NRT * 128, 512):
            P.dma(lambda e, kc=kc, c0=c0: e.dma_start(out=wswb[:, kc, c0:c0 + 512], in_=wsw[kc * 128:(kc + 1) * 128, c0:c0 + 512]),
                  writes=[b_ws[kc]], q="pool")
    nxb = 2 if NCT <= 24 else 1
    xts = [C.sb([128, 8, TB]) for _ in range(nxb)]
    xbs = [C.sb([128, 8, TB], BF16) for _ in range(nxb)]
    sq, b_sq = C.sb([128, 8, TB], BF16)
    css = [C.sb([128, TB]) for _ in range(2)]
    sns = [C.sb([128, TB]) for _ in range(2)]
    rss = [C.sb([128, TB]) for _ in range(2)]
    pss, b_pss = C.ps([128, TB])
    ps_a = [C.ps([128, TB]) for _ in range(2)]
    ps_b = [C.ps([128, TB]) for _ in range(2)]
    t1s = [C.sb([128, TB]) for _ in range(2)]
    t2s = [C.sb([128, TB]) for _ in range(2)]
    os_ = [C.sb([128, TB]) for _ in range(4)]
    b_out = Buf()
    xv = xT.rearrange("(kc p) n -> p kc n", p=128)
    no = 0
    na = 0
    for tb in range(NB):
        sl = slice(tb * TB, (tb + 1) * TB)
        xt, b_x = xts[tb % nxb]
        xb, b_xb = xbs[tb % nxb]
        cs, b_cs = css[tb % 2]
        sn, b_sn = sns[tb % 2]
        rs, b_rs = rss[tb % 2]
        P.dma(lambda e, xt=xt, sl=sl: e.dma_start(out=xt[:], in_=xv[:, :, sl]), writes=[b_x])
        if NRT:
            P.dma(lambda e, cs=cs, sl=sl: e.dma_start(out=cs[:], in_=cos[:, sl]), writes=[b_cs])
            P.dma(lambda e, sn=sn, sl=sl: e.dma_start(out=sn[:], in_=sin[:, sl]), writes=[b_sn])
        P.pool(lambda e, xt=xt: e.tensor_tensor(out=sq[:], in0=xt[:], in1=xt[:], op=ALU.mult), reads=[b_x], writes=[b_sq])
        for kc in range(8):
            eng = P.dve if kc % 2 == 0 else P.pool
            eng(lambda e, kc=kc, xt=xt, xb=xb: e.tensor_scalar(out=xb[:, kc, :], in0=xt[:, kc, :], scalar1=gt[:, kc:kc + 1], scalar2=None, op0=ALU.mult),
                reads=[b_x, b_g], writes=[b_xb])
        for kc in range(8):
            P.pe(lambda e, kc=kc: e.matmul(pss[:], ones[:], sq[:, kc, :], start=(kc == 0), stop=(kc == 7)),
                 reads=[b_sq, b_ones], writes=[b_pss])
        P.act(lambda e, rs=rs: e.activation(out=rs[:], in_=pss[:], func=AF.Ln, scale=1.0 / D, bias=epsc[:, 0:1]), reads=[b_pss, b_epsc], writes=[b_rs])
        P.act(lambda e, rs=rs: e.activation(out=rs[:], in_=rs[:], func=AF.Exp, scale=-0.5), reads=[b_rs], writes=[b_rs])
        if NRT:
            P.pool(lambda e, cs=cs, rs=rs: e.tensor_tensor(out=cs[:], in0=cs[:], in1=rs[:], op=ALU.mult), reads=[b_cs, b_rs], writes=[b_cs])
            P.pool(lambda e, sn=sn, rs=rs: e.tensor_tensor(out=sn[:], in0=sn[:], in1=rs[:], op=ALU.mult), reads=[b_sn, b_rs], writes=[b_sn])
        for ct in range(NCT):
            pa, b_pa = ps_a[na % 2]
            pb, b_pb = ps_b[na % 2]
            na += 1
            o, b_o = os_[no % 4]
            no += 1
            for kc in range(8):
                P.pe(lambda e, kc=kc, ct=ct, pa=pa, xb=xb: e.matmul(pa[:], wb[:, kc, ct * 128:(ct + 1) * 128], xb[:, kc, :], start=(kc == 0), stop=(kc == 7)),
                     reads=[b_w[kc], b_xb], writes=[b_pa])
            if ct < NRT:
                for kc in range(8):
                    P.pe(lambda e, kc=kc, ct=ct, pb=pb, xb=xb: e.matmul(pb[:], wswb[:, kc, ct * 128:(ct + 1) * 128], xb[:, kc, :], start=(kc == 0), stop=(kc == 7)),
                         reads=[b_ws[kc], b_xb], writes=[b_pb])
                t1, b_t1 = t1s[ct % 2]
                t2, b_t2 = t2s[ct % 2]
                P.dve(lambda e, t1=t1, pa=pa, cs=cs: e.tensor_tensor(out=t1[:], in0=pa[:], in1=cs[:], op=ALU.mult), reads=[b_pa, b_cs], writes=[b_t1])
                P.dve(lambda e, t2=t2, pb=pb, sn=sn: e.tensor_tensor(out=t2[:], in0=pb[:], in1=sn[:], op=ALU.mult), reads=[b_pb, b_sn], writes=[b_t2])
                P.pool(lambda e, o=o, t1=t1, t2=t2: e.tensor_tensor(out=o[:], in0=t1[:], in1=t2[:], op=ALU.add), reads=[b_t1, b_t2], writes=[b_o])
            else:
                P.dve(lambda e, o=o, pa=pa, rs=rs: e.tensor_tensor(out=o[:], in0=pa[:], in1=rs[:], op=ALU.mult), reads=[b_pa, b_rs], writes=[b_o])
            P.dma(lambda e, o=o, ct=ct, sl=sl: e.dma_start(out=pT[ct * 128:(ct + 1) * 128, sl], in_=o[:]), reads=[b_o], writes=[Buf()])
    C.end()


def stage_gates(C, pT, mlb, ident, tabd, L, mg_row):
    P = C.P
    C.begin()
    NCH = L // 128
    s = 128.0 ** -0.5
    idt, b_id = C.sb([128, 128])
    mb, b_mb = C.sb([128, 16])
    nfb, b_nfb = C.sb([128, 8])
    lns, b_lns = C.sb([128, 1])
    onesq, b_onesq = C.sb([128, 128])
    P.dma(lambda e: e.dma_start(out=idt[:], in_=ident), writes=[b_id])
    P.dma(lambda e: e.dma_start(out=mb[:], in_=mlb), writes=[b_mb])
    P.dve(lambda e: e.tensor_scalar(out=nfb[:], in0=mb[:, 8:16], scalar1=-1.0, scalar2=None, op0=ALU.mult), reads=[b_mb], writes=[b_nfb])
    P.dve(lambda e: e.memset(lns[:], float(np.log(s))), writes=[b_lns])
    P.dve(lambda e: e.memset(onesq[:], 1.0), writes=[b_onesq])
    b_out = Buf()
    gps = [C.ps([128, 128]) for _ in range(3)]
    for h in range(4):
        for dr in range(2):
            gi, b_gi = C.sb([128, 128])
            gf, b_gf = C.sb([128, 128])
            ri = mg_row + dr * 4 + h
            rf = mg_row + 8 + dr * 4 + h
            P.dma(lambda e, gi=gi, ri=ri: e.dma_start(out=gi[0:NCH, :], in_=pT[ri:ri + 1, :].rearrange("o (c t) -> (o c) t", t=128)), writes=[b_gi])
            P.dma(lambda e, gf=gf, rf=rf: e.dma_start(out=gf[0:NCH, :], in_=pT[rf:rf + 1, :].rearrange("o (c t) -> (o c) t", t=128)), writes=[b_gf])
            sp, b_sp = C.sb([128, 128])
            csp, b_csp = C.sb([128, 128])
            col = dr * 4 + h
            P.act(lambda e, sp=sp, gf=gf, col=col: e.activation(out=sp[0:NCH, :], in_=gf[0:NCH, :], func=AF.Exp, scale=-1.0, bias=nfb[0:NCH, col:col + 1]),
                  reads=[b_gf, b_nfb], writes=[b_sp])
            P.act(lambda e, sp=sp: e.activation(out=sp[0:NCH, :], in_=sp[0:NCH, :], func=AF.Ln, scale=1.0, bias=1.0), reads=[b_sp], writes=[b_sp])
            P.dve(lambda e, csp=csp, sp=sp: e.tensor_tensor_scan(out=csp[0:NCH, :], data0=onesq[0:NCH, :], data1=sp[0:NCH, :], initial=0.0, op0=ALU.mult, op1=ALU.add),
                  reads=[b_sp, b_onesq], writes=[b_csp])
            ex, b_ex = C.sb([128, 128])
            if dr == 0:
                P.dve(lambda e, ex=ex, csp=csp: e.tensor_copy(out=ex[0:NCH, :], in_=csp[0:NCH, :]), reads=[b_csp], writes=[b_ex])
            else:
                P.dve(lambda e, ex=ex, sp=sp, csp=csp: e.tensor_tensor(out=ex[0:NCH, :], in0=sp[0:NCH, :], in1=csp[0:NCH, :], op=ALU.subtract), reads=[b_sp, b_csp], writes=[b_ex])
                P.dve(lambda e, ex=ex, csp=csp: e.tensor_scalar(out=ex[0:NCH, :], in0=ex[0:NCH, :], scalar1=csp[0:NCH, 127:128], scalar2=None, op0=ALU.add), reads=[b_ex, b_csp], writes=[b_ex])
            av, b_av = C.sb([128, 128])
            cv, b_cv = C.sb([128, 128])
            ebc, b_ebc = C.sb([128, 1])
            P.act(lambda e, av=av, ex=ex: e.activation(out=av[0:NCH, :], in_=ex[0:NCH, :], func=AF.Exp, scale=-1.0), reads=[b_ex], writes=[b_av])
            P.dve(lambda e, cv=cv, gi=gi, ex=ex: e.tensor_tensor(out=cv[0:NCH, :], in0=gi[0:NCH, :], in1=ex[0:NCH, :], op=ALU.add), reads=[b_gi, b_ex], writes=[b_cv])
            P.dve(lambda e, cv=cv, col=col: e.tensor_scalar(out=cv[0:NCH, :], in0=cv[0:NCH, :], scalar1=mb[0:NCH, col:col + 1], scalar2=lns[0:NCH, 0:1], op0=ALU.add, op1=ALU.add),
                  reads=[b_cv, b_mb, b_lns], writes=[b_cv])
            P.act(lambda e, cv=cv: e.activation(out=cv[0:NCH, :], in_=cv[0:NCH, :], func=AF.Exp), reads=[b_cv], writes=[b_cv])
            P.act(lambda e, ebc=ebc, csp=csp: e.activation(out=ebc[0:NCH, :], in_=csp[0:NCH, 127:128], func=AF.Exp, scale=-1.0), reads=[b_csp], writes=[b_ebc])
            tb_, b_tb = C.sb([128, 3, NCH])
            for k, (src, b_src) in enumerate(((av, b_av), (cv, b_cv))):
                pt, b_pt = gps[k]
                P.pe(lambda e, pt=pt, src=src: e.transpose(pt[:, 0:NCH], src[0:NCH, :], idt[0:NCH, 0:NCH]), reads=[b_src, b_id], writes=[b_pt])
                P.act(lambda e, pt=pt, k=k, tb_=tb_: e.activation(out=tb_[:, k, :], in_=pt[:, 0:NCH], func=AF.Copy), reads=[b_pt], writes=[b_tb])
            tm, b_tm = C.sb([128, 128])
            P.dve(lambda e, tm=tm, ebc=ebc: e.tensor_scalar(out=tm[0:NCH, :], in0=onesq[0:NCH, :], scalar1=ebc[0:NCH, 0:1], scalar2=None, op0=ALU.mult),
                  reads=[b_onesq, b_ebc], writes=[b_tm])
            pt, b_pt = gps[2]
            P.pe(lambda e, pt=pt, tm=tm: e.matmul(pt[:, 0:NCH], tm[0:NCH, :], idt[0:NCH, 0:NCH], start=True, stop=True), reads=[b_tm, b_id], writes=[b_pt])
            P.act(lambda e, pt=pt, tb_=tb_: e.activation(out=tb_[:, 2, :], in_=pt[:, 0:NCH], func=AF.Copy), reads=[b_pt], writes=[b_tb])
            j0 = (h * 2 + dr) * 3
            P.dma(lambda e, tb_=tb_, j0=j0: e.dma_start(out=tabd[:, j0:j0 + 3, :], in_=tb_[:]), reads=[b_tb], writes=[Buf()])
    C.end()


def stage_linattn(C, pT, tab, ident, masks, o1, mixT, L, q_row, k_row, v_row, g_row, out_row, mlstm):
    P = C.P
    C.begin()
    NCH = L // 128
    NV = 129 if mlstm else 128
    idt, b_id = C.sb([128, 128])
    mk, b_mk = C.sb([128, 3, 128])
    tb_, b_tb = C.sb([128, 24, NCH])
    P.dma(lambda e: e.dma_start(out=idt[:], in_=ident), writes=[b_id])
    P.dma(lambda e: e.dma_start(out=mk[:], in_=masks.rearrange("m p n -> p m n")), writes=[b_mk])
    P.dma(lambda e: e.dma_start(out=tb_[:], in_=tab), writes=[b_tb])
    epsc, b_epsc = C.sb([128, 1])
    P.dve(lambda e: e.memset(epsc[:], EPS), writes=[b_epsc])
    NBUF = 4
    qTs = [C.sb([128, 128]) for _ in range(NBUF)]
    kTs = [C.sb([128, 128]) for _ in range(NBUF)]
    vTs = [C.sb([128, 128]) for _ in range(NBUF)]
    gTs = [C.sb([128, 128]) for _ in range(NBUF)]
    hfs = [C.sb([128, 128]) for _ in range(NBUF)]
    ktoks = [C.sb([128, 128]) for _ in range(NBUF)]
    vpps = [C.sb([128, NV]) for _ in range(NBUF)]
    sms = [C.sb([128, 128]) for _ in range(NBUF)]
    os_ = [C.sb([128, NV]) for _ in range(NBUF)]
    hs = [C.sb([128, 128]) for _ in range(NBUF)]
    gas = [C.sb([128, 128]) for _ in range(NBUF)]
    outs = [C.sb([128, 128]) for _ in range(NBUF)]
    p_kt = [C.ps([128, 128]) for _ in range(1)]
    p_vt = [C.ps([128, 128]) for _ in range(1)]
    p_s = [C.ps([128, 128]) for _ in range(2)]
    p_o = [C.ps([128, NV]) for _ in range(2)]
    p_kv = [C.ps([128, NV]) for _ in range(1)]
    p_tr = [C.ps([128, 128]) for _ in range(1)]
    cst, b_cst = C.sb([128, NV])
    tmpc, b_tmpc = C.sb([128, NV])
    st6, b_st6 = C.sb([128, 6])
    mv, b_mv = C.sb([128, 2])
    rsd, b_rsd = C.sb([128, 1])
    dn, b_dn = C.sb([128, 1])
    b_o1 = [[Buf() for _ in range(NCH)] for _ in range(4)]
    it = 0
    csts = [(cst, b_cst)] + [C.sb([128, NV]) for _ in range(3)]
    for dr in range(2):
        mi = 0 if dr == 0 else (2 if mlstm else 1)
        for h in range(4):
            P.dve(lambda e, c_=csts[h][0]: e.memset(c_[:], 0.0), writes=[csts[h][1]])
        order = range(NCH) if dr == 0 else range(NCH - 1, -1, -1)
        for n in order:
            for h in range(4):
                j0 = (h * 2 + dr) * 3
                cst, b_cst = csts[h]
                i = it % NBUF
                it += 1
                cs_ = slice(n * 128, (n + 1) * 128)
                qT, b_q = qTs[i]
                kT, b_k = kTs[i]
                vT, b_v = vTs[i]
                P.dma(lambda e, qT=qT, cs_=cs_, h=h: e.dma_start(out=qT[:], in_=pT[q_row + h * 128:q_row + (h + 1) * 128, cs_]), writes=[b_q])
                P.dma(lambda e, kT=kT, cs_=cs_, h=h: e.dma_start(out=kT[:], in_=pT[k_row + h * 128:k_row + (h + 1) * 128, cs_]), writes=[b_k])
                P.dma(lambda e, vT=vT, cs_=cs_, h=h: e.dma_start(out=vT[:], in_=pT[v_row + h * 128:v_row + (h + 1) * 128, cs_]), writes=[b_v])
                pk, b_pk = p_kt[0]
                pv, b_pv = p_vt[0]
                ktok, b_kt = ktoks[i]
                vpp, b_vp = vpps[i]
                P.pe(lambda e, pk=pk, kT=kT: e.transpose(pk[:], kT[:], idt[:]), reads=[b_k, b_id], writes=[b_pk])
                P.pe(lambda e, pv=pv, vT=vT: e.transpose(pv[:], vT[:], idt[:]), reads=[b_v, b_id], writes=[b_pv])
                P.act(lambda e, ktok=ktok, pk=pk: e.activation(out=ktok[:], in_=pk[:], func=AF.Copy), reads=[b_pk], writes=[b_kt])
                P.dve(lambda e, vpp=vpp, pv=pv, n=n, j0=j0: e.tensor_scalar(out=vpp[:, 0:128], in0=pv[:], scalar1=tb_[:, j0 + 1, n:n + 1], scalar2=None, op0=ALU.mult),
                      reads=[b_pv, b_tb], writes=[b_vp])
                if mlstm:
                    P.act(lambda e, vpp=vpp, n=n, j0=j0: e.activation(out=vpp[:, 128:129], in_=tb_[:, j0 + 1, n:n + 1], func=AF.Copy), reads=[b_tb], writes=[b_vp])
                ps_, b_ps = p_s[it % 2]
                sm, b_sm = sms[i]
                P.pe(lambda e, ps_=ps_, kT=kT, qT=qT: e.matmul(ps_[:], kT[:], qT[:], start=True, stop=True), reads=[b_k, b_q], writes=[b_ps])
                P.dve(lambda e, sm=sm, ps_=ps_, mi=mi: e.tensor_tensor(out=sm[:], in0=ps_[:], in1=mk[:, mi, :], op=ALU.mult), reads=[b_ps, b_mk], writes=[b_sm])
                po, b_po = p_o[it % 2]
                P.pe(lambda e, po=po, sm=sm, vpp=vpp: e.matmul(po[:], sm[:], vpp[:], start=True, stop=False), reads=[b_sm, b_vp], writes=[b_po])
                P.pe(lambda e, po=po, qT=qT, cst=cst: e.matmul(po[:], qT[:], cst[:], start=False, stop=True), reads=[b_q, b_cst], writes=[b_po])
                o_, b_o = os_[i]
                P.act(lambda e, o_=o_, po=po, n=n, j0=j0: e.activation(out=o_[:], in_=po[:], func=AF.Copy, scale=tb_[:, j0, n:n + 1]), reads=[b_po, b_tb], writes=[b_o])
                pkv, b_pkv = p_kv[0]
                P.pe(lambda e, pkv=pkv, ktok=ktok, vpp=vpp: e.matmul(pkv[:], ktok[:], vpp[:], start=True, stop=True), reads=[b_kt, b_vp], writes=[b_pkv])
                P.dve(lambda e, pkv=pkv, cst=cst: e.tensor_tensor(out=tmpc[:], in0=pkv[:], in1=cst[:], op=ALU.add), reads=[b_pkv, b_cst], writes=[b_tmpc])
                P.dve(lambda e, n=n, j0=j0, cst=cst: e.tensor_scalar(out=cst[:], in0=tmpc[:], scalar1=tb_[:, j0 + 2, n:n + 1], scalar2=None, op0=ALU.mult),
                      reads=[b_tmpc, b_tb], writes=[b_cst])
                hh, b_h = hs[i]
                if mlstm:
                    P.dve(lambda e, o_=o_: e.tensor_scalar(out=dn[:], in0=o_[:, 128:129], scalar1=-1.0, scalar2=1.0, op0=ALU.mult, op1=ALU.max), reads=[b_o], writes=[b_dn])
                    P.dve(lambda e, o_=o_: e.tensor_tensor(out=dn[:], in0=dn[:], in1=o_[:, 128:129], op=ALU.max), reads=[b_dn, b_o], writes=[b_dn])
                    P.dve(lambda e: e.reciprocal(out=dn[:], in_=dn[:]), reads=[b_dn], writes=[b_dn])
                    P.dve(lambda e, hh=hh, o_=o_: e.tensor_scalar(out=hh[:], in0=o_[:, 0:128], scalar1=dn[:, 0:1], scalar2=None, op0=ALU.mult), reads=[b_o, b_dn], writes=[b_h])
                    src, b_src = hh, b_h
                else:
                    src, b_src = o_, b_o
                if dr == 0:
                    P.dma(lambda e, src=src, cs_=cs_, h=h: e.dma_start(out=o1[h, cs_, :], in_=src[:, 0:128]), reads=[b_src], writes=[b_o1[h][n]])
                else:
                    hf, b_hf = hfs[i]
                    gT, b_g = gTs[i]
                    P.dma(lambda e, hf=hf, cs_=cs_, h=h: e.dma_start(out=hf[:], in_=o1[h, cs_, :]), reads=[b_o1[h][n]], writes=[b_hf])
                    P.dma(lambda e, gT=gT, cs_=cs_, h=h: e.dma_start(out=gT[:], in_=pT[g_row + h * 128:g_row + (h + 1) * 128, cs_]), writes=[b_g])
                    P.pool(lambda e, hf=hf, src=src: e.tensor_tensor(out=hf[:], in0=hf[:], in1=src[:, 0:128], op=ALU.add), reads=[b_hf, b_src], writes=[b_hf])
                    P.dve(lambda e, hf=hf: e.bn_stats(out=st6[:], in_=hf[:]), reads=[b_hf], writes=[b_st6])
                    P.dve(lambda e: e.bn_aggr(out=mv[:], in_=st6[:]), reads=[b_st6], writes=[b_mv])
                    P.act(lambda e: e.activation(out=rsd[:], in_=mv[:, 1:2], func=AF.Ln, scale=1.0, bias=epsc[:, 0:1]), reads=[b_mv, b_epsc], writes=[b_rsd])
                    P.act(lambda e: e.activation(out=rsd[:], in_=rsd[:], func=AF.Exp, scale=-0.5), reads=[b_rsd], writes=[b_rsd])
                    P.dve(lambda e, hf=hf: e.tensor_scalar(out=hf[:], in0=hf[:], scalar1=mv[:, 0:1], scalar2=rsd[:, 0:1], op0=ALU.subtract, op1=ALU.mult),
                          reads=[b_hf, b_mv, b_rsd], writes=[b_hf])
                    ptr, b_ptr = p_tr[0]
                    P.pe(lambda e, ptr=ptr, hf=hf: e.transpose(ptr[:], hf[:], idt[:]), reads=[b_hf, b_id], writes=[b_ptr])
                    ga, b_ga = gas[i]
                    P.act(lambda e, ga=ga, gT=gT: e.activation(out=ga[:], in_=gT[:], func=AF.Exp, scale=-1.0), reads=[b_g], writes=[b_ga])
                    P.pool(lambda e, ga=ga: e.tensor_scalar(out=ga[:], in0=ga[:], scalar1=1.0, scalar2=None, op0=ALU.add), reads=[b_ga], writes=[b_ga])
                    P.dve(lambda e, ga=ga: e.reciprocal(out=ga[:], in_=ga[:]), reads=[b_ga], writes=[b_ga])
                    if not mlstm:
                        P.pool(lambda e, ga=ga, gT=gT: e.tensor_tensor(out=ga[:], in0=ga[:], in1=gT[:], op=ALU.mult), reads=[b_ga, b_g], writes=[b_ga])
                    ot, b_ot = outs[i]
                    P.dve(lambda e, ot=ot, ptr=ptr, ga=ga: e.tensor_tensor(out=ot[:], in0=ptr[:], in1=ga[:], op=ALU.mult), reads=[b_ptr, b_ga], writes=[b_ot])
                    P.dma(lambda e, ot=ot, cs_=cs_, h=h: e.dma_start(out=mixT[out_row + h * 128:out_row + (h + 1) * 128, cs_], in_=ot[:]), reads=[b_ot], writes=[Buf()])
    C.end()


PI = float(np.pi)


def _wrap(P, C, x, b_x, shape, add=0.0, scr=None, key="a"):
    if scr is not None and ("u" + key) in scr:
        (u, b_u), (ki, b_ki), (kf, b_kf), (y, b_y), (m, b_m) = [scr[n + key] for n in "uikym"]
    else:
        u, b_u = C.sb(shape)
        ki, b_ki = C.sb(shape, I32)
        kf, b_kf = C.sb(shape)
        y, b_y = C.sb(shape)
        m, b_m = C.sb(shape)
        if scr is not None:
            for n, v in zip("uikym", ((u, b_u), (ki, b_ki), (kf, b_kf), (y, b_y), (m, b_m))):
                scr[n + key] = v
    P.dve(lambda e: e.tensor_scalar(out=u[:], in0=x[:], scalar1=add, scalar2=1.0 / (2 * PI), op0=ALU.add, op1=ALU.mult), reads=[b_x], writes=[b_u])
    P.dve(lambda e: e.tensor_copy(out=ki[:], in_=u[:]), reads=[b_u], writes=[b_ki])
    P.dve(lambda e: e.tensor_copy(out=kf[:], in_=ki[:]), reads=[b_ki], writes=[b_kf])
    P.dve(lambda e: e.tensor_scalar(out=u[:], in0=x[:], scalar1=add, scalar2=None, op0=ALU.add), reads=[b_x, b_kf], writes=[b_u])
    P.dve(lambda e: e.scalar_tensor_tensor(out=y[:], in0=kf[:], scalar=-2 * PI, in1=u[:], op0=ALU.mult, op1=ALU.add), reads=[b_kf, b_u], writes=[b_y])
    P.dve(lambda e: e.tensor_scalar(out=m[:], in0=y[:], scalar1=PI, scalar2=-2 * PI, op0=ALU.is_gt, op1=ALU.mult), reads=[b_y], writes=[b_m])
    P.dve(lambda e: e.tensor_tensor(out=y[:], in0=y[:], in1=m[:], op=ALU.add), reads=[b_y, b_m], writes=[b_y])
    P.dve(lambda e: e.tensor_scalar(out=m[:], in0=y[:], scalar1=-PI, scalar2=2 * PI, op0=ALU.is_lt, op1=ALU.mult), reads=[b_y], writes=[b_m])
    P.dve(lambda e: e.tensor_tensor(out=y[:], in0=y[:], in1=m[:], op=ALU.add), reads=[b_y, b_m], writes=[b_y])
    P.dve(lambda e: e.tensor_scalar(out=y[:], in0=y[:], scalar1=-PI, scalar2=PI, op0=ALU.max, op1=ALU.min), reads=[b_y], writes=[b_y])
    return y, b_y


def stage_s5(C, pT, prm, consts, yf, mixT, L, u_row, out_row):
    P = C.P
    T = min(512, L)
    NBK = L // T
    for dr in range(2):
        for ct in range(4):
            C.begin()
            idt, b_id = C.sb([128, 128])
            psw, b_psw = C.sb([128, 128])
            tau, b_tau = C.sb([128, T])
            ks, b_ks = C.sb([128, 3])
            lst, b_lst = C.sb([128, 64])
            dsk, b_dsk = C.sb([128, 4])
            onesT, b_onesT = C.sb([128, T])
            P.dma(lambda e: e.dma_start(out=idt[:], in_=consts["ident"]), writes=[b_id])
            P.dma(lambda e: e.dma_start(out=psw[:], in_=consts["psw"]), writes=[b_psw])
            P.dma(lambda e: e.dma_start(out=tau[:], in_=consts["tau"]), writes=[b_tau])
            P.dma(lambda e: e.dma_start(out=ks[:], in_=consts["ksel"]), writes=[b_ks])
            P.dma(lambda e: e.dma_start(out=lst[:], in_=prm["lstep"]), writes=[b_lst])
            P.dma(lambda e: e.dma_start(out=dsk[:], in_=prm["dsk"]), writes=[b_dsk])
            P.dve(lambda e: e.memset(onesT[:], 1.0), writes=[b_onesT])
            G = []
            scr = {}
            ang, b_ang = C.sb([128, T])
            are2, b_are2 = C.sb([128, 64])
            aim2, b_aim2 = C.sb([128, 64])
            P.dma(lambda e: e.dma_start(out=are2[:], in_=prm['are2']), writes=[b_are2])
            P.dma(lambda e: e.dma_start(out=aim2[:], in_=prm['aim2']), writes=[b_aim2])
            ptr = [C.ps([128, 128]) for _ in range(2)]
            for gp in range(8):
                g = ct * 8 + gp
                are, b_are = C.sb([128, 1])
                aim, b_aim = C.sb([128, 1])
                P.dve(lambda e, are=are, cg=dr * 32 + g: e.tensor_copy(out=are[:], in_=are2[:, cg:cg + 1]), reads=[b_are2], writes=[b_are])
                P.dve(lambda e, aim=aim, cg=dr * 32 + g: e.tensor_copy(out=aim[:], in_=aim2[:, cg:cg + 1]), reads=[b_aim2], writes=[b_aim])
                dl, b_dl = C.sb([128, 1])
                r, b_r = C.sb([128, 1])
                th, b_th = C.sb([128, 1])
                col = dr * 32 + g
                P.act(lambda e, dl=dl, col=col: e.activation(out=dl[:], in_=lst[:, col:col + 1], func=AF.Exp), reads=[b_lst], writes=[b_dl])
                P.act(lambda e, r=r, are=are, dl=dl: e.activation(out=r[:], in_=are[:], func=AF.Exp, scale=dl[:, 0:1]), reads=[b_are, b_dl], writes=[b_r])
                P.dve(lambda e, th=th, aim=aim, dl=dl: e.tensor_tensor(out=th[:], in0=aim[:], in1=dl[:], op=ALU.mult), reads=[b_aim, b_dl], writes=[b_th])
                thr0, b_thr0 = _wrap(P, C, th, b_th, [128, 1], scr=scr, key='c')
                thr, b_thr = C.sb([128, 1])
                P.dve(lambda e, thr=thr, thr0=thr0: e.tensor_copy(out=thr[:], in_=thr0[:]), reads=[b_thr0], writes=[b_thr])
                thc, b_thc = _wrap(P, C, thr, b_thr, [128, 1], add=PI / 2, scr=scr, key='d')
                s0, b_s0 = C.sb([128, 1])
                c0, b_c0 = C.sb([128, 1])
                P.act(lambda e, s0=s0, thr=thr: e.activation(out=s0[:], in_=thr[:], func=AF.Sin), reads=[b_thr], writes=[b_s0])
                P.act(lambda e, c0=c0, thc=thc: e.activation(out=c0[:], in_=thc[:], func=AF.Sin), reads=[b_thc], writes=[b_c0])
                nre, b_nre = C.sb([128, 1])
                nim, b_nim = C.sb([128, 1])
                den, b_den = C.sb([128, 1])
                t0, b_t0 = C.sb([128, 1])
                kre, b_kre = C.sb([128, 1])
                kim, b_kim = C.sb([128, 1])
                P.dve(lambda e, nre=nre, r=r, c0=c0: e.tensor_tensor(out=nre[:], in0=r[:], in1=c0[:], op=ALU.mult), reads=[b_r, b_c0], writes=[b_nre])
                P.dve(lambda e, nre=nre: e.tensor_scalar(out=nre[:], in0=nre[:], scalar1=-1.0, scalar2=None, op0=ALU.add), reads=[b_nre], writes=[b_nre])
                P.dve(lambda e, nim=nim, r=r, s0=s0: e.tensor_tensor(out=nim[:], in0=r[:], in1=s0[:], op=ALU.mult), reads=[b_r, b_s0], writes=[b_nim])
                P.dve(lambda e, den=den, are=are: e.tensor_tensor(out=den[:], in0=are[:], in1=are[:], op=ALU.mult), reads=[b_are], writes=[b_den])
                P.dve(lambda e, den=den, aim=aim: e.scalar_tensor_tensor(out=den[:], in0=aim[:], scalar=aim[:, 0:1], in1=den[:], op0=ALU.mult, op1=ALU.add), reads=[b_aim, b_den], writes=[b_den])
                P.dve(lambda e, den=den: e.reciprocal(out=den[:], in_=den[:]), reads=[b_den], writes=[b_den])
                P.dve(lambda e, t0=t0, nre=nre, are=are: e.tensor_tensor(out=t0[:], in0=nre[:], in1=are[:], op=ALU.mult), reads=[b_nre, b_are], writes=[b_t0])
                P.dve(lambda e, kre=kre, nim=nim, aim=aim, t0=t0: e.scalar_tensor_tensor(out=kre[:], in0=nim[:], scalar=aim[:, 0:1], in1=t0[:], op0=ALU.mult, op1=ALU.add), reads=[b_nim, b_aim, b_t0], writes=[b_kre])
                P.dve(lambda e, kre=kre, den=den: e.tensor_tensor(out=kre[:], in0=kre[:], in1=den[:], op=ALU.mult), reads=[b_kre, b_den], writes=[b_kre])
                P.dve(lambda e, t0=t0, nre=nre, aim=aim: e.tensor_tensor(out=t0[:], in0=nre[:], in1=aim[:], op=ALU.mult), reads=[b_nre, b_aim], writes=[b_t0])
                P.dve(lambda e, kim=kim, nim=nim, are=are, t0=t0: e.scalar_tensor_tensor(out=kim[:], in0=nim[:], scalar=are[:, 0:1], in1=t0[:], op0=ALU.mult, op1=ALU.subtract), reads=[b_nim, b_are, b_t0], writes=[b_kim])
                P.dve(lambda e, kim=kim, den=den: e.tensor_tensor(out=kim[:], in0=kim[:], in1=den[:], op=ALU.mult), reads=[b_kim, b_den], writes=[b_kim])
                cA, b_cA = C.sb([128, 1])
                cB, b_cB = C.sb([128, 1])
                cC, b_cC = C.sb([128, 1])
                P.dve(lambda e, cA=cA, kre=kre: e.tensor_tensor(out=cA[:], in0=kre[:], in1=ks[:, 0:1], op=ALU.mult), reads=[b_kre, b_ks], writes=[b_cA])
                P.dve(lambda e, cA=cA, kim=kim: e.scalar_tensor_tensor(out=cA[:], in0=kim[:], scalar=ks[:, 1:2], in1=cA[:], op0=ALU.mult, op1=ALU.add), reads=[b_kim, b_ks, b_cA], writes=[b_cA])
                P.dve(lambda e, cB=cB, kre=kre: e.tensor_tensor(out=cB[:], in0=kre[:], in1=ks[:, 1:2], op=ALU.mult), reads=[b_kre, b_ks], writes=[b_cB])
                P.dve(lambda e, cB=cB, kim=kim: e.scalar_tensor_tensor(out=cB[:], in0=kim[:], scalar=ks[:, 0:1], in1=cB[:], op0=ALU.mult, op1=ALU.subtract), reads=[b_kim, b_ks, b_cB], writes=[b_cB])
                P.dve(lambda e, cC=cC, cB=cB: e.tensor_copy(out=cC[:], in_=cB[:]), reads=[b_cB], writes=[b_cC])
                P.dve(lambda e, cB=cB, cC=cC: e.tensor_scalar(out=cB[:], in0=cC[:], scalar1=-1.0, scalar2=None, op0=ALU.mult), reads=[b_cC], writes=[b_cB])
                P.dve(lambda e, thr=thr: e.tensor_scalar(out=ang[:], in0=tau[:], scalar1=thr[:, 0:1], scalar2=None, op0=ALU.mult), reads=[b_tau, b_thr], writes=[b_ang])
                aw, b_aw = _wrap(P, C, ang, b_ang, [128, T], scr=scr, key='A')
                ac, b_ac = _wrap(P, C, aw, b_aw, [128, T], add=PI / 2, scr=scr, key='B')
                St, b_St = C.sb([128, T])
                Ct, b_Ct = C.sb([128, T])
                Rb, b_Rb = C.sb([128, T])
                P.act(lambda e, St=St, aw=aw: e.activation(out=St[:], in_=aw[:], func=AF.Sin), reads=[b_aw], writes=[b_St])
                P.act(lambda e, Ct=Ct, ac=ac: e.activation(out=Ct[:], in_=ac[:], func=AF.Sin), reads=[b_ac], writes=[b_Ct])
                P.dve(lambda e, Rb=Rb, r=r: e.tensor_scalar(out=Rb[:], in0=onesT[:], scalar1=r[:, 0:1], scalar2=None, op0=ALU.mult), reads=[b_onesT, b_r], writes=[b_Rb])
                bre, b_bre = C.sb([128, 16])
                bim, b_bim = C.sb([128, 16])
                for half in range(2):
                    P.dma(lambda e, half=half, bre=bre, g=g: e.dma_start(out=bre[half * 64:(half + 1) * 64, :], in_=prm["b_re"][dr, g, :, :]), writes=[b_bre])
                    P.dma(lambda e, half=half, bim=bim, g=g: e.dma_start(out=bim[half * 64:(half + 1) * 64, :], in_=prm["b_im"][dr, g, :, :]), writes=[b_bim])
                BT = []
                for (c1, b_c1, c2, b_c2) in ((cA, b_cA, cB, b_cB), (cC, b_cC, cA, b_cA)):
                    bp, b_bp = C.sb([128, 128])
                    tt, b_tt = C.sb([128, 16])
                    P.pool(lambda e, bp=bp: e.memset(bp[:], 0.0), writes=[b_bp])
                    P.dve(lambda e, tt=tt, c1=c1, bre=bre: e.tensor_scalar(out=tt[:], in0=bre[:], scalar1=c1[:, 0:1], scalar2=None, op0=ALU.mult), reads=[b_bre, b_c1], writes=[b_tt])
                    P.dve(lambda e, bp=bp, tt=tt, c2=c2, gp=gp, bim=bim: e.scalar_tensor_tensor(out=bp[:, gp * 16:(gp + 1) * 16], in0=bim[:], scalar=c2[:, 0:1], in1=tt[:], op0=ALU.mult, op1=ALU.add),
                          reads=[b_bim, b_c2, b_tt, b_bp], writes=[b_bp])
                    pt, b_pt = ptr[0]
                    bT, b_bT = C.sb([128, 128], BF16)
                    P.pe(lambda e, pt=pt, bp=bp: e.transpose(pt[:], bp[:], idt[:]), reads=[b_bp, b_id], writes=[b_pt])
                    P.act(lambda e, bT=bT, pt=pt: e.activation(out=bT[:], in_=pt[:], func=AF.Copy), reads=[b_pt], writes=[b_bT])
                    BT.append((bT, b_bT))
                cc1, b_cc1 = C.sb([16, 128])
                cc2, b_cc2 = C.sb([16, 128])
                P.dma(lambda e, cc1=cc1, g=g: e.dma_start(out=cc1[:, 0:64], in_=prm["c_re"][dr, g, :, :]), writes=[b_cc1])
                P.dma(lambda e, cc1=cc1, g=g: e.dma_start(out=cc1[:, 64:128], in_=prm["c_im"][dr, g, :, :]), writes=[b_cc1])
                P.dma(lambda e, cc2=cc2, g=g: e.dma_start(out=cc2[:, 0:64], in_=prm["c_im"][dr, g, :, :]), writes=[b_cc2])
                P.dma(lambda e, cc2=cc2, g=g: e.dma_start(out=cc2[:, 64:128], in_=prm["c_re"][dr, g, :, :]), writes=[b_cc2])
                cm1, b_cm1 = C.sb([128, 128], BF16)
                cm2, b_cm2 = C.sb([128, 128], BF16)
                P.pool(lambda e, cm1=cm1: e.memset(cm1[:], 0.0), writes=[b_cm1])
                P.pool(lambda e, cm2=cm2: e.memset(cm2[:], 0.0), writes=[b_cm2])
                pt, b_pt = ptr[1]
                P.pe(lambda e, pt=pt, cc1=cc1: e.transpose(pt[:, 0:16], cc1[:], idt[0:16, 0:16]), reads=[b_cc1, b_id], writes=[b_pt])
                P.dve(lambda e, cm1=cm1, pt=pt, gp=gp: e.tensor_scalar(out=cm1[:, gp * 16:(gp + 1) * 16], in0=pt[:, 0:16], scalar1=ks[:, 2:3], scalar2=None, op0=ALU.mult),
                      reads=[b_pt, b_ks, b_cm1], writes=[b_cm1])
                P.pe(lambda e, pt=pt, cc2=cc2: e.transpose(pt[:, 0:16], cc2[:], idt[0:16, 0:16]), reads=[b_cc2, b_id], writes=[b_pt])
                P.dve(lambda e, cm2=cm2, pt=pt, gp=gp: e.tensor_scalar(out=cm2[:, gp * 16:(gp + 1) * 16], in0=pt[:, 0:16], scalar1=-1.0, scalar2=None, op0=ALU.mult),
                      reads=[b_pt, b_cm2], writes=[b_cm2])
                q_, b_q = C.sb([128, 1])
                rt, b_rt = C.sb([128, 128])
                P.dve(lambda e, q_=q_, St=St: e.tensor_tensor(out=q_[:], in0=St[:, T - 1:T], in1=ks[:, 2:3], op=ALU.mult), reads=[b_St, b_ks], writes=[b_q])
                P.dve(lambda e, rt=rt, Ct=Ct: e.tensor_scalar(out=rt[:], in0=idt[:], scalar1=Ct[:, T - 1:T], scalar2=None, op0=ALU.mult), reads=[b_id, b_Ct], writes=[b_rt])
                P.dve(lambda e, rt=rt, q_=q_: e.scalar_tensor_tensor(out=rt[:], in0=psw[:], scalar=q_[:, 0:1], in1=rt[:], op0=ALU.mult, op1=ALU.add), reads=[b_psw, b_q, b_rt], writes=[b_rt])
                carry, b_carry = C.sb([128, 1])
                P.dve(lambda e, carry=carry: e.memset(carry[:], 0.0), writes=[b_carry])
                G.append(dict(St=(St, b_St), Ct=(Ct, b_Ct), Rb=(Rb, b_Rb), B1=BT[0], B2=BT[1], C1=(cm1, b_cm1), C2=(cm2, b_cm2), RT=(rt, b_rt), carry=(carry, b_carry)))
            uts = [C.sb([128, T]) for _ in range(2)]
            utbs = [C.sb([128, T], BF16) for _ in range(2)]
            pbs = [C.ps([128, T]) for _ in range(2)]
            pss_ = [C.ps([128, T]) for _ in range(2)]
            py, b_py = C.ps([128, T])
            pc, b_pc = C.ps([128, 1])
            NB3 = 3
            t1s = [C.sb([128, T]) for _ in range(NB3)]
            t2s = [C.sb([128, T]) for _ in range(NB3)]
            bps = [C.sb([128, T]) for _ in range(NB3)]
            ws_ = [C.sb([128, T]) for _ in range(NB3)]
            wcs = [C.sb([128, T], BF16) for _ in range(NB3)]
            wss = [C.sb([128, T], BF16) for _ in range(NB3)]
            yos = [C.sb([128, T]) for _ in range(2)]
            yfs = [C.sb([128, T]) for _ in range(2)]
            blocks = list(range(NBK)) if dr == 0 else list(range(NBK - 1, -1, -1))
            rows = slice(u_row + ct * 128, u_row + (ct + 1) * 128)
            rv = (lambda a: a[:, ::-1]) if dr == 1 else (lambda a: a[:])
            items = [(bi, bk, gp) for bi, bk in enumerate(blocks) for gp in range(8)]
            st_ = {}
            SK = 2
            NB4 = 4
            bps = [C.sb([128, T]) for _ in range(NB4)]

            def phaseA(k):
                bi, bk, gp = items[k]
                cs_ = slice(bk * T, (bk + 1) * T)
                if gp == 0:
                    ut, b_ut = uts[bi % 2]
                    utb, b_utb = utbs[bi % 2]
                    P.dma(lambda e, ut=ut, cs_=cs_: e.dma_start(out=ut[:], in_=pT[rows, cs_]), writes=[b_ut])
                    P.act(lambda e, ut=ut, utb=utb: e.activation(out=utb[:], in_=ut[:], func=AF.Copy), reads=[b_ut], writes=[b_utb])
                utb, b_utb = utbs[bi % 2]
                gd = G[gp]
                pb, b_pb = pbs[k % 2]
                pq, b_pq = pss_[k % 2]
                t1, b_t1 = t1s[k % 2]
                t2, b_t2 = t2s[k % 2]
                bp, b_bp = bps[k % NB4]
                P.pe(lambda e, pb=pb, gd=gd, utb=utb: e.matmul(pb[:], gd["B1"][0][:], utb[:], start=True, stop=True), reads=[gd["B1"][1], b_utb], writes=[b_pb])
                P.pe(lambda e, pq=pq, gd=gd, utb=utb: e.matmul(pq[:], gd["B2"][0][:], utb[:], start=True, stop=True), reads=[gd["B2"][1], b_utb], writes=[b_pq])
                P.dve(lambda e, t1=t1, pb=pb, gd=gd: e.tensor_tensor(out=t1[:], in0=rv(pb), in1=gd["Ct"][0][:], op=ALU.mult), reads=[b_pb, gd["Ct"][1]], writes=[b_t1])
                P.dve(lambda e, t2=t2, pq=pq, gd=gd: e.tensor_tensor(out=t2[:], in0=rv(pq), in1=gd["St"][0][:], op=ALU.mult), reads=[b_pq, gd["St"][1]], writes=[b_t2])
                P.pool(lambda e, bp=bp, t1=t1, t2=t2: e.tensor_tensor(out=bp[:], in0=t1[:], in1=t2[:], op=ALU.add), reads=[b_t1, b_t2], writes=[b_bp])

            def phaseB(k):
                bi, bk, gp = items[k]
                cs_ = slice(bk * T, (bk + 1) * T)
                gd = G[gp]
                bp, b_bp = bps[k % NB4]
                w_, b_w = ws_[k % 3]
                wc, b_wc = wcs[k % 3]
                wsn, b_wsn = wss[k % 3]
                P.dve(lambda e, w_=w_, bp=bp, gd=gd: e.tensor_tensor_scan(out=w_[:], data0=gd["Rb"][0][:], data1=bp[:], initial=gd["carry"][0][:, 0:1], op0=ALU.mult, op1=ALU.add),
                      reads=[gd["Rb"][1], b_bp, gd["carry"][1]], writes=[b_w])
                P.pool(lambda e, wc=wc, w_=w_, gd=gd: e.tensor_tensor(out=wc[:], in0=w_[:], in1=gd["Ct"][0][:], op=ALU.mult), reads=[b_w, gd["Ct"][1]], writes=[b_wc])
                P.dve(lambda e, wsn=wsn, w_=w_, gd=gd: e.tensor_tensor(out=wsn[:], in0=w_[:], in1=gd["St"][0][:], op=ALU.mult), reads=[b_w, gd["St"][1]], writes=[b_wsn])
                P.pe(lambda e, gd=gd, wc=wc, gp=gp: e.matmul(py[:], gd["C1"][0][:], wc[:], start=(gp == 0), stop=False), reads=[gd["C1"][1], b_wc], writes=[b_py])
                P.pe(lambda e, gd=gd, wsn=wsn, gp=gp: e.matmul(py[:], gd["C2"][0][:], wsn[:], start=False, stop=(gp == 7)), reads=[gd["C2"][1], b_wsn], writes=[b_py])
                P.pe(lambda e, gd=gd, w_=w_: e.matmul(pc[:], gd["RT"][0][:], w_[:, T - 1:T], start=True, stop=True), reads=[gd["RT"][1], b_w], writes=[b_pc])
                P.act(lambda e, gd=gd: e.activation(out=gd["carry"][0][:], in_=pc[:], func=AF.Copy), reads=[b_pc], writes=[gd["carry"][1]])
                if gp == 7:
                    ut, b_ut = uts[bi % 2]
                    yo, b_yo = yos[bi % 2]
                    if dr == 0:
                        P.act(lambda e, yo=yo: e.activation(out=yo[:], in_=py[:], func=AF.Copy), reads=[b_py], writes=[b_yo])
                        P.dma(lambda e, yo=yo, cs_=cs_: e.dma_start(out=yf[ct * 128:(ct + 1) * 128, cs_], in_=yo[:]), reads=[b_yo], writes=[Buf()])
                    else:
                        yft, b_yft = yfs[bi % 2]
                        P.dma(lambda e, yft=yft, cs_=cs_: e.dma_start(out=yft[:], in_=yf[ct * 128:(ct + 1) * 128, cs_]), writes=[b_yft])
                        P.dve(lambda e, yo=yo, yft=yft: e.tensor_tensor(out=yo[:], in0=py[:, ::-1], in1=yft[:], op=ALU.add), reads=[b_py, b_yft], writes=[b_yo])
                        P.dve(lambda e, yo=yo, ut=ut: e.scalar_tensor_tensor(out=yo[:], in0=ut[:], scalar=dsk[:, ct:ct + 1], in1=yo[:], op0=ALU.mult, op1=ALU.add), reads=[b_ut, b_dsk, b_yo], writes=[b_yo])
                        P.dma(lambda e, yo=yo, cs_=cs_: e.dma_start(out=mixT[out_row + ct * 128:out_row + (ct + 1) * 128, cs_], in_=yo[:]), reads=[b_yo], writes=[Buf()])

            for k in range(len(items) + SK):
                if k < len(items):
                    phaseA(k)
                if k - SK >= 0:
                    phaseB(k - SK)
            C.end()


def stage_out(C, xT, mixT, w_out, gluw, glub, g8, router, ident, x1T, htok, aff, L, even):
    P = C.P
    C.begin()
    TB = min(512, L)
    NB = L // TB
    NTS = TB // 128
    KC = 8 if even else 6
    woutb, _ = C.sb([128, KC, 1024], BF16)
    b_wo = [Buf() for _ in range(KC)]
    for kc in range(KC):
        for c0 in range(0, 1024, 512):
            P.dma(lambda e, kc=kc, c0=c0: e.dma_start(out=woutb[:, kc, c0:c0 + 512], in_=w_out[kc * 128:(kc + 1) * 128, c0:c0 + 512]), writes=[b_wo[kc]], q="pool")
    if even:
        gluwb, b_gw = C.sb([128, 4, 512], BF16)
        for kc in range(4):
            P.dma(lambda e, kc=kc: e.dma_start(out=gluwb[:, kc, :], in_=gluw[kc * 128:(kc + 1) * 128, :]), writes=[b_gw], q="pool")
        gb, b_gb = C.sb([128, 4])
        P.dma(lambda e: e.dma_start(out=gb[:], in_=glub), writes=[b_gb])
        ngb, b_ngb = C.sb([128, 4])
        P.dve(lambda e: e.tensor_scalar(out=ngb[:], in0=gb[:], scalar1=-1.0, scalar2=None, op0=ALU.mult), reads=[b_gb], writes=[b_ngb])
    gt, b_g = C.sb([128, 8])
    wr, b_wr = C.sb([128, 8, 16])
    idt, b_id = C.sb([128, 128])
    ones, b_ones = C.sb([128, 128], BF16)
    P.dma(lambda e: e.dma_start(out=gt[:], in_=g8), writes=[b_g])
    P.dma(lambda e: e.dma_start(out=wr[:], in_=router.rearrange("(kc p) n -> p kc n", p=128)), writes=[b_wr])
    P.dma(lambda e: e.dma_start(out=idt[:], in_=ident), writes=[b_id])
    P.dve(lambda e: e.memset(ones[:], 1.0), writes=[b_ones])
    epsc, b_epsc = C.sb([128, 1])
    P.dve(lambda e: e.memset(epsc[:], EPS), writes=[b_epsc])
    mix, b_mix = C.sb([128, KC, TB])
    mixb, b_mixb = C.sb([128, KC, TB], BF16)
    xt, b_xt = C.sb([128, 8, TB])
    x1t, b_x1 = C.sb([128, 8, TB])
    ht, b_ht = C.sb([128, 8, TB])
    sq, b_sq = C.sb([128, 8, TB], BF16)
    rs, b_rs = C.sb([128, TB])
    yg, b_yg = C.sb([128, 4, TB])
    ygb, b_ygb = C.sb([128, 4, TB], BF16)
    sg, b_sg = C.sb([128, TB])
    hrows = [C.sb([128, 1024]) for _ in range(2)]
    afts = [C.sb([128, 16]) for _ in range(2)]
    ex, b_ex = C.sb([128, 16])
    mx, b_mx = C.sb([128, 1])
    sm, b_sm = C.sb([128, 1])
    pxs = [C.ps([128, TB]) for _ in range(2)]
    pss, b_pss = C.ps([128, TB])
    pz, b_pz = C.ps([128, TB])
    pl, b_pl = C.ps([128, 16])
    ptrs = [C.ps([128, 128]) for _ in range(2)]
    xv = xT.rearrange("(kc p) n -> p kc n", p=128)
    mv = mixT.rearrange("(kc p) n -> p kc n", p=128)
    x1v = x1T.rearrange("(kc p) n -> p kc n", p=128)
    nt = 0
    for tb in range(NB):
        sl = slice(tb * TB, (tb + 1) * TB)
        P.dma(lambda e, sl=sl: e.dma_start(out=mix[:], in_=mv[:, 0:KC, sl]), writes=[b_mix])
        P.dma(lambda e, sl=sl: e.dma_start(out=xt[:], in_=xv[:, :, sl]), writes=[b_xt])
        if even:
            P.act(lambda e: e.activation(out=mixb[:, 0:4, :], in_=mix[:, 0:4, :], func=AF.Copy), reads=[b_mix], writes=[b_mixb])
            P.act(lambda e: e.activation(out=yg[:], in_=mix[:, 4:8, :], func=AF.Gelu), reads=[b_mix], writes=[b_yg])
            P.dve(lambda e: e.tensor_copy(out=ygb[:], in_=yg[:]), reads=[b_yg], writes=[b_ygb])
            for ct in range(4):
                for kc in range(4):
                    P.pe(lambda e, ct=ct, kc=kc: e.matmul(pz[:], gluwb[:, kc, ct * 128:(ct + 1) * 128], ygb[:, kc, :], start=(kc == 0), stop=(kc == 3)),
                         reads=[b_gw, b_ygb], writes=[b_pz])
                P.act(lambda e, ct=ct: e.activation(out=sg[:], in_=pz[:], func=AF.Exp, scale=-1.0, bias=ngb[:, ct:ct + 1]), reads=[b_pz, b_ngb], writes=[b_sg])
                P.pool(lambda e: e.tensor_scalar(out=sg[:], in0=sg[:], scalar1=1.0, scalar2=None, op0=ALU.add), reads=[b_sg], writes=[b_sg])
                P.dve(lambda e: e.reciprocal(out=sg[:], in_=sg[:]), reads=[b_sg], writes=[b_sg])
                P.dve(lambda e, ct=ct: e.tensor_tensor(out=mixb[:, 4 + ct, :], in0=yg[:, ct, :], in1=sg[:], op=ALU.mult), reads=[b_yg, b_sg], writes=[b_mixb])
        else:
            P.act(lambda e: e.activation(out=mixb[:], in_=mix[:], func=AF.Copy), reads=[b_mix], writes=[b_mixb])
        for dt in range(8):
            px, b_px = pxs[dt % 2]
            for kc in range(KC):
                P.pe(lambda e, px=px, dt=dt, kc=kc: e.matmul(px[:], woutb[:, kc, dt * 128:(dt + 1) * 128], mixb[:, kc, :], start=(kc == 0), stop=(kc == KC - 1)),
                     reads=[b_wo[kc], b_mixb], writes=[b_px])
            P.dve(lambda e, px=px, dt=dt: e.tensor_tensor(out=x1t[:, dt, :], in0=px[:], in1=xt[:, dt, :], op=ALU.add), reads=[b_px, b_xt], writes=[b_x1])
        P.dma(lambda e, sl=sl: e.dma_start(out=x1v[:, :, sl], in_=x1t[:]), reads=[b_x1], writes=[Buf()])
        P.pool(lambda e: e.tensor_tensor(out=sq[:], in0=x1t[:], in1=x1t[:], op=ALU.mult), reads=[b_x1], writes=[b_sq])
        for kc in range(8):
            P.pe(lambda e, kc=kc: e.matmul(pss[:], ones[:], sq[:, kc, :], start=(kc == 0), stop=(kc == 7)), reads=[b_sq, b_ones], writes=[b_pss])
        P.act(lambda e: e.activation(out=rs[:], in_=pss[:], func=AF.Ln, scale=1.0 / D, bias=epsc[:, 0:1]), reads=[b_pss, b_epsc], writes=[b_rs])
        P.act(lambda e: e.activation(out=rs[:], in_=rs[:], func=AF.Exp, scale=-0.5), reads=[b_rs], writes=[b_rs])
        for dt in range(8):
            P.dve(lambda e, dt=dt: e.scalar_tensor_tensor(out=ht[:, dt, :], in0=x1t[:, dt, :], scalar=gt[:, dt:dt + 1], in1=rs[:], op0=ALU.mult, op1=ALU.mult),
                  reads=[b_x1, b_g, b_rs], writes=[b_ht])
        for ts in range(NTS):
            tsl = slice(ts * 128, (ts + 1) * 128)
            r0 = tb * TB + ts * 128
            for kc in range(8):
                P.pe(lambda e, kc=kc, tsl=tsl: e.matmul(pl[:], ht[:, kc, tsl], wr[:, kc, :], start=(kc == 0), stop=(kc == 7)), reads=[b_ht, b_wr], writes=[b_pl])
            aft, b_aft = afts[nt % 2]
            hrow, b_hrow = hrows[nt % 2]
            nt += 1
            P.dve(lambda e: e.reduce_max(out=mx[:], in_=pl[:], axis=AX.X), reads=[b_pl], writes=[b_mx])
            P.dve(lambda e: e.tensor_scalar(out=mx[:], in0=mx[:], scalar1=-1.0, scalar2=None, op0=ALU.mult), reads=[b_mx], writes=[b_mx])
            P.act(lambda e: e.activation(out=ex[:], in_=pl[:], func=AF.Exp, bias=mx[:, 0:1], accum_out=sm[:]), reads=[b_pl, b_mx], writes=[b_ex, b_sm])
            P.dve(lambda e: e.reciprocal(out=sm[:], in_=sm[:]), reads=[b_sm], writes=[b_sm])
            P.dve(lambda e, aft=aft: e.tensor_scalar(out=aft[:], in0=ex[:], scalar1=sm[:, 0:1], scalar2=None, op0=ALU.mult), reads=[b_ex, b_sm], writes=[b_aft])
            P.dma(lambda e, aft=aft, r0=r0: e.dma_start(out=aff[r0:r0 + 128, :], in_=aft[:]), reads=[b_aft], writes=[Buf()])
            for kc in range(8):
                ptr, b_ptr = ptrs[kc % 2]
                P.pe(lambda e, ptr=ptr, kc=kc, tsl=tsl: e.transpose(ptr[:], ht[:, kc, tsl], idt[:]), reads=[b_ht, b_id], writes=[b_ptr])
                if kc % 2 == 0:
                    P.act(lambda e, ptr=ptr, kc=kc, hrow=hrow: e.activation(out=hrow[:, kc * 128:(kc + 1) * 128], in_=ptr[:], func=AF.Copy), reads=[b_ptr], writes=[b_hrow])
                else:
                    P.dve(lambda e, ptr=ptr, kc=kc, hrow=hrow: e.tensor_copy(out=hrow[:, kc * 128:(kc + 1) * 128], in_=ptr[:]), reads=[b_ptr], writes=[b_hrow])
            P.dma(lambda e, hrow=hrow, r0=r0: e.dma_start(out=htok[r0:r0 + 128, :], in_=hrow[:]), reads=[b_hrow], writes=[Buf()])
    C.end()


BIG = 1.0e6


def _breg(e, rc, val):
    if 'r' not in rc:
        rc['r'] = e.to_reg(val)
    return rc['r']


def stage_route(C, aff, ustrict, rmask, posd, gmd, L):
    P = C.P
    C.begin()
    NJ = L // 128
    CAP = L // 8
    NCOL = 16 * NJ
    A, b_A = C.sb([128, NJ, 16])
    Ae, b_Ae = C.sb([128, 16, NJ])
    for jc in range(0, NJ, 16):
        je = min(NJ, jc + 16)
        P.dma(lambda e, jc=jc, je=je: e.dma_start(out=A[:, jc:je, :], in_=aff[jc * 128:je * 128, :].rearrange("(j p) e -> p j e", p=128)), writes=[b_A])
    P.dve(lambda e: e.tensor_copy(out=Ae[:], in_=A[:].rearrange("p j e -> p e j")), reads=[b_A], writes=[b_Ae])
    us, b_us = C.sb([128, 128])
    rm, b_rm = C.sb([128, NCOL])
    onesf, b_of = C.sb([128, 128])
    P.dma(lambda e: e.dma_start(out=us[:], in_=ustrict), writes=[b_us])
    P.dma(lambda e: e.dma_start(out=rm[:], in_=rmask), writes=[b_rm])
    P.dve(lambda e: e.memset(onesf[:], 1.0), writes=[b_of])
    lo, b_lo = C.sb([128, 16])
    hi, b_hi = C.sb([128, 16])
    mid, b_mid = C.sb([128, 16])
    cnt, b_cnt = C.sb([128, 16])
    ge, b_ge = C.sb([128, 16])
    d1, b_d1 = C.sb([128, 16])
    cmps = [C.sb([128, NJ]) for _ in range(2)]
    ptot, b_ptot = C.ps([128, 16])
    P.dve(lambda e: e.memset(lo[:], 0.0), writes=[b_lo])
    P.dve(lambda e: e.memset(hi[:], 2.0), writes=[b_hi])
    for it in range(34):
        P.dve(lambda e: e.tensor_tensor(out=mid[:], in0=lo[:], in1=hi[:], op=ALU.add), reads=[b_lo, b_hi], writes=[b_mid])
        P.dve(lambda e: e.tensor_scalar(out=mid[:], in0=mid[:], scalar1=0.5, scalar2=None, op0=ALU.mult), reads=[b_mid], writes=[b_mid])
        for ex in range(16):
            cm, b_cm = cmps[ex % 2]
            P.dve(lambda e, ex=ex, cm=cm: e.tensor_scalar(out=cm[:], in0=Ae[:, ex, :], scalar1=mid[:, ex:ex + 1], scalar2=None, op0=ALU.is_ge, op1=ALU.add, accum_out=cnt[:, ex:ex + 1]),
                  reads=[b_Ae, b_mid], writes=[b_cm, b_cnt])
        P.pe(lambda e: e.matmul(ptot[:], onesf[:], cnt[:], start=True, stop=True), reads=[b_of, b_cnt], writes=[b_ptot])
        P.dve(lambda e: e.tensor_scalar(out=ge[:], in0=ptot[:], scalar1=float(CAP) - 0.5, scalar2=None, op0=ALU.is_ge), reads=[b_ptot], writes=[b_ge])
        P.dve(lambda e: e.tensor_tensor(out=d1[:], in0=mid[:], in1=lo[:], op=ALU.subtract), reads=[b_mid, b_lo], writes=[b_d1])
        P.dve(lambda e: e.tensor_tensor(out=d1[:], in0=d1[:], in1=ge[:], op=ALU.mult), reads=[b_d1, b_ge], writes=[b_d1])
        P.dve(lambda e: e.tensor_tensor(out=lo[:], in0=lo[:], in1=d1[:], op=ALU.add), reads=[b_lo, b_d1], writes=[b_lo])
        P.dve(lambda e: e.tensor_tensor(out=d1[:], in0=hi[:], in1=mid[:], op=ALU.subtract), reads=[b_hi, b_mid], writes=[b_d1])
        P.dve(lambda e: e.tensor_tensor(out=d1[:], in0=d1[:], in1=ge[:], op=ALU.mult), reads=[b_d1, b_ge], writes=[b_d1])
        P.dve(lambda e: e.tensor_tensor(out=hi[:], in0=mid[:], in1=d1[:], op=ALU.add), reads=[b_mid, b_d1], writes=[b_hi])
    Me, b_Me = C.sb([128, 16, NJ])
    gm, b_gm = C.sb([128, 16, NJ])
    for ex in range(16):
        P.dve(lambda e, ex=ex: e.tensor_scalar(out=Me[:, ex, :], in0=Ae[:, ex, :], scalar1=lo[:, ex:ex + 1], scalar2=None, op0=ALU.is_ge), reads=[b_Ae, b_lo], writes=[b_Me])
    P.dve(lambda e: e.tensor_tensor(out=gm[:], in0=Ae[:], in1=Me[:], op=ALU.mult), reads=[b_Ae, b_Me], writes=[b_gm])
    Mf = Me[:].rearrange("p e j -> p (e j)")
    pre, b_pre = C.sb([128, NCOL])
    cn, b_cn = C.sb([128, NCOL])
    off, b_off = C.sb([128, NCOL])
    pp, b_pp = C.ps([128, min(512, NCOL)])
    pc, b_pc = C.ps([128, min(512, NCOL)])
    CW = min(512, NCOL)
    for c0 in range(0, NCOL, CW):
        P.pe(lambda e, c0=c0: e.matmul(pp[:], us[:], Mf[:, c0:c0 + CW], start=True, stop=True), reads=[b_us, b_Me], writes=[b_pp])
        P.act(lambda e, c0=c0: e.activation(out=pre[:, c0:c0 + CW], in_=pp[:], func=AF.Copy), reads=[b_pp], writes=[b_pre])
        P.pe(lambda e, c0=c0: e.matmul(pc[:], onesf[:], Mf[:, c0:c0 + CW], start=True, stop=True), reads=[b_of, b_Me], writes=[b_pc])
        P.act(lambda e, c0=c0: e.activation(out=cn[:, c0:c0 + CW], in_=pc[:], func=AF.Copy), reads=[b_pc], writes=[b_cn])
    P.dve(lambda e: e.tensor_tensor_scan(out=off[:], data0=rm[:], data1=cn[:], initial=0.0, op0=ALU.mult, op1=ALU.add), reads=[b_rm, b_cn], writes=[b_off])
    P.dve(lambda e: e.tensor_tensor(out=off[:], in0=off[:], in1=cn[:], op=ALU.subtract), reads=[b_off, b_cn], writes=[b_off])
    P.dve(lambda e: e.tensor_tensor(out=pre[:], in0=pre[:], in1=off[:], op=ALU.add), reads=[b_pre, b_off], writes=[b_pre])
    P.dve(lambda e: e.tensor_scalar(out=pre[:], in0=pre[:], scalar1=-BIG, scalar2=None, op0=ALU.add), reads=[b_pre], writes=[b_pre])
    P.dve(lambda e: e.tensor_tensor(out=pre[:], in0=pre[:], in1=Mf, op=ALU.mult), reads=[b_pre, b_Me], writes=[b_pre])
    P.dve(lambda e: e.tensor_scalar(out=pre[:], in0=pre[:], scalar1=BIG, scalar2=None, op0=ALU.add), reads=[b_pre], writes=[b_pre])
    pi, b_pi = C.sb([128, NCOL], I32)
    P.dve(lambda e: e.tensor_copy(out=pi[:], in_=pre[:]), reads=[b_pre], writes=[b_pi])
    P.dma(lambda e: e.dma_start(out=posd, in_=pi[:]), reads=[b_pi], writes=[Buf()])
    P.dma(lambda e: e.dma_start(out=gmd, in_=gm[:].rearrange("p e j -> p (e j)")), reads=[b_gm], writes=[Buf()])
    C.end()


def stage_dispatch(C, htok, posd, xe, L):
    P = C.P
    C.begin()
    NJ = L // 128
    CAP = L // 8
    pi, b_pi = C.sb([128, 16, NJ], I32)
    P.dma(lambda e: e.dma_start(out=pi[:].rearrange("p e j -> p (e j)"), in_=posd), writes=[b_pi])
    rc = {}
    hts = [C.sb([128, 1024]) for _ in range(2)]
    ixs = [C.sb([128, 1], I32) for _ in range(4)]
    ni = 0
    for j in range(NJ):
        ht, b_ht = hts[j % 2]
        P.dma(lambda e, ht=ht, j=j: e.dma_start(out=ht[:], in_=htok[j * 128:(j + 1) * 128, :]), writes=[b_ht])
        for ex in range(16):
            ix, b_ix = ixs[ni % 4]
            ni += 1
            P.dve(lambda e, ix=ix, ex=ex, j=j: e.tensor_copy(out=ix[:], in_=pi[:, ex, j:j + 1]), reads=[b_pi], writes=[b_ix])
            P.dma(lambda e, ht=ht, ix=ix, ex=ex: e.indirect_dma_start(out=xe[ex][:, :], out_offset=bass.IndirectOffsetOnAxis(ap=ix[:, :], axis=0),
                                                                      in_=ht[:, :], in_offset=None, bounds_check=_breg(e, rc, CAP - 1), oob_is_err=False),
                  reads=[b_ht, b_ix], writes=[Buf()], q="pool")
    C.end()


def stage_ffn(C, xe, w1, w3, w2, ye, ident, L, FF):
    P = C.P
    C.begin()
    CAP = L // 8
    SB = min(512, CAP)
    NSB = CAP // SB
    NST = SB // 128
    NF = FF // 128
    idt, b_id = C.sb([128, 128])
    P.dma(lambda e: e.dma_start(out=idt[:], in_=ident), writes=[b_id])
    w1b, _ = C.sb([128, 8, FF], BF16)
    w3b, _ = C.sb([128, 8, FF], BF16)
    w2b, _ = C.sb([128, NF, 1024], BF16)
    b_w1 = [Buf() for _ in range(8)]
    b_w3 = [Buf() for _ in range(8)]
    b_w2 = [Buf() for _ in range(NF)]
    xrs = [C.sb([128, 1024]) for _ in range(2)]
    xeT, b_xeT = C.sb([128, 8, SB], BF16)
    hid, b_hid = C.sb([128, NF, SB], BF16)
    sas = [C.sb([128, SB]) for _ in range(2)]
    yrows = [C.sb([128, 1024]) for _ in range(2)]
    ptrs = [C.ps([128, 128]) for _ in range(2)]
    pas = [C.ps([128, SB]) for _ in range(2)]
    pbs = [C.ps([128, SB]) for _ in range(2)]
    pys = [C.ps([128, 512]) for _ in range(2)]
    nx = 0
    ny = 0
    NSTG = 3
    stg = [C.sb([128, max(FF, 1024)]) for _ in range(NSTG)]
    ns = 0
    for ex in range(16):
        jobs = []
        for kc in range(8):
            jobs.append((w1[ex, kc * 128:(kc + 1) * 128, :], FF, w1b, kc, b_w1[kc]))
            jobs.append((w3[ex, kc * 128:(kc + 1) * 128, :], FF, w3b, kc, b_w3[kc]))
        for fc in range(NF):
            jobs.append((w2[ex, fc * 128:(fc + 1) * 128, :], 1024, w2b, fc, b_w2[fc]))
        for (src, width, dstt, di, b_dst) in jobs:
            sg_, b_sg = stg[ns % NSTG]
            ns += 1
            P.dma(lambda e, sg_=sg_, src=src, width=width: e.dma_start(out=sg_[:, 0:width], in_=src), writes=[b_sg])
            P.act(lambda e, sg_=sg_, dstt=dstt, di=di, width=width: e.activation(out=dstt[:, di, :], in_=sg_[:, 0:width], func=AF.Copy), reads=[b_sg], writes=[b_dst])
        for sb_ in range(NSB):
            for st in range(NST):
                r0 = sb_ * SB + st * 128
                xr, b_xr = xrs[nx % 2]
                nx += 1
                P.dma(lambda e, xr=xr, ex=ex, r0=r0: e.dma_start(out=xr[:], in_=xe[ex][r0:r0 + 128, :]), writes=[b_xr], q="pool")
                for kc in range(8):
                    ptr, b_ptr = ptrs[kc % 2]
                    P.pe(lambda e, ptr=ptr, xr=xr, kc=kc: e.transpose(ptr[:], xr[:, kc * 128:(kc + 1) * 128], idt[:]), reads=[b_xr, b_id], writes=[b_ptr])
                    if kc % 2 == 0:
                        P.act(lambda e, ptr=ptr, kc=kc, st=st: e.activation(out=xeT[:, kc, st * 128:(st + 1) * 128], in_=ptr[:], func=AF.Copy), reads=[b_ptr], writes=[b_xeT])
                    else:
                        P.dve(lambda e, ptr=ptr, kc=kc, st=st: e.tensor_copy(out=xeT[:, kc, st * 128:(st + 1) * 128], in_=ptr[:]), reads=[b_ptr], writes=[b_xeT])
            for ft in range(NF):
                pa, b_pa = pas[ft % 2]
                pb, b_pb = pbs[ft % 2]
                sa, b_sa = sas[ft % 2]
                for kc in range(8):
                    P.pe(lambda e, pa=pa, kc=kc, ft=ft: e.matmul(pa[:], w1b[:, kc, ft * 128:(ft + 1) * 128], xeT[:, kc, :], start=(kc == 0), stop=(kc == 7)), reads=[b_w1[kc], b_xeT], writes=[b_pa])
                for kc in range(8):
                    P.pe(lambda e, pb=pb, kc=kc, ft=ft: e.matmul(pb[:], w3b[:, kc, ft * 128:(ft + 1) * 128], xeT[:, kc, :], start=(kc == 0), stop=(kc == 7)), reads=[b_w3[kc], b_xeT], writes=[b_pb])
                P.act(lambda e, sa=sa, pa=pa: e.activation(out=sa[:], in_=pa[:], func=AF.Exp, scale=-1.0), reads=[b_pa], writes=[b_sa])
                P.dve(lambda e, sa=sa: e.tensor_scalar(out=sa[:], in0=sa[:], scalar1=1.0, scalar2=None, op0=ALU.add), reads=[b_sa], writes=[b_sa])
                P.dve(lambda e, sa=sa: e.reciprocal(out=sa[:], in_=sa[:]), reads=[b_sa], writes=[b_sa])
                P.dve(lambda e, sa=sa, pa=pa: e.tensor_tensor(out=sa[:], in0=pa[:], in1=sa[:], op=ALU.mult), reads=[b_pa, b_sa], writes=[b_sa])
                P.dve(lambda e, sa=sa, pb=pb, ft=ft: e.tensor_tensor(out=hid[:, ft, :], in0=pb[:], in1=sa[:], op=ALU.mult), reads=[b_pb, b_sa], writes=[b_hid])
            for st in range(NST):
                r0 = sb_ * SB + st * 128
                yrow, b_yrow = yrows[ny % 2]
                ny += 1
                for dh in range(2):
                    py, b_py = pys[dh]
                    for fc in range(NF):
                        P.pe(lambda e, py=py, fc=fc, st=st, dh=dh: e.matmul(py[:], hid[:, fc, st * 128:(st + 1) * 128], w2b[:, fc, dh * 512:(dh + 1) * 512], start=(fc == 0), stop=(fc == NF - 1)),
                             reads=[b_hid, b_w2[fc]], writes=[b_py])
                    if dh == 0:
                        P.act(lambda e, py=py, yrow=yrow: e.activation(out=yrow[:, 0:512], in_=py[:], func=AF.Copy), reads=[b_py], writes=[b_yrow])
                    else:
                        P.dve(lambda e, py=py, yrow=yrow: e.tensor_copy(out=yrow[:, 512:1024], in_=py[:]), reads=[b_py], writes=[b_yrow])
                P.dma(lambda e, yrow=yrow, ex=ex, r0=r0: e.dma_start(out=ye[ex][r0:r0 + 128, :], in_=yrow[:]), reads=[b_yrow], writes=[Buf()], q="pool")
    C.end()


def stage_combine(C, ye, posd, gmd, x1T, ident, x2T, outT, gfin, L):
    P = C.P
    C.begin()
    NJ = L // 128
    CAP = L // 8
    idt, b_id = C.sb([128, 128])
    P.dma(lambda e: e.dma_start(out=idt[:], in_=ident), writes=[b_id])
    pi, b_pi = C.sb([128, 16, NJ], I32)
    gm, b_gm = C.sb([128, 16, NJ])
    P.dma(lambda e: e.dma_start(out=pi[:].rearrange("p e j -> p (e j)"), in_=posd), writes=[b_pi])
    P.dma(lambda e: e.dma_start(out=gm[:].rearrange("p e j -> p (e j)"), in_=gmd), writes=[b_gm])
    Gs = [C.sb([128, 1024]) for _ in range(2)]
    for G_, b_G in Gs:
        P.dve(lambda e, G_=G_: e.memset(G_[:], 0.0), writes=[b_G])
    accs = [C.sb([128, 1024]) for _ in range(2)]
    x1s = [C.sb([128, 8, 128]) for _ in range(2)]
    x2s = [C.sb([128, 8, 128]) for _ in range(2)]
    ptrs = [C.ps([128, 128]) for _ in range(2)]
    if outT is not None:
        gt, b_g = C.sb([128, 8])
        ones, b_ones = C.sb([128, 128], BF16)
        sq, b_sq = C.sb([128, 8, 128], BF16)
        rs, b_rs = C.sb([128, 128])
        ots = [C.sb([128, 8, 128]) for _ in range(2)]
        pss, b_pss = C.ps([128, 128])
        P.dma(lambda e: e.dma_start(out=gt[:], in_=gfin), writes=[b_g])
        P.dve(lambda e: e.memset(ones[:], 1.0), writes=[b_ones])
        epsc, b_epsc = C.sb([128, 1])
        P.dve(lambda e: e.memset(epsc[:], EPS), writes=[b_epsc])
        ov = outT.rearrange("(kc p) n -> p kc n", p=128)
    x1v = x1T.rearrange("(kc p) n -> p kc n", p=128)
    x2v = x2T.rearrange("(kc p) n -> p kc n", p=128)
    ng = 0
    rc = {}
    ixs = [C.sb([128, 1], I32) for _ in range(4)]
    for j in range(NJ):
        acc, b_acc = accs[j % 2]
        x1t, b_x1 = x1s[j % 2]
        x2t, b_x2 = x2s[j % 2]
        cs_ = slice(j * 128, (j + 1) * 128)
        P.dma(lambda e, x1t=x1t, cs_=cs_: e.dma_start(out=x1t[:], in_=x1v[:, :, cs_]), writes=[b_x1])
        for ex in range(16):
            G_, b_G = Gs[ng % 2]
            ix, b_ix = ixs[ng % 4]
            ng += 1
            P.act(lambda e, ix=ix, ex=ex, j=j: e.activation(out=ix[:], in_=pi[:, ex, j:j + 1], func=AF.Copy), reads=[b_pi], writes=[b_ix])
            P.dma(lambda e, G_=G_, ex=ex, ix=ix: e.indirect_dma_start(out=G_[:, :], out_offset=None, in_=ye[ex][:, :],
                                                                      in_offset=bass.IndirectOffsetOnAxis(ap=ix[:, :], axis=0), bounds_check=_breg(e, rc, CAP - 1), oob_is_err=False),
                  reads=[b_ix], writes=[b_G], q="pool")
            if ex == 0:
                P.dve(lambda e, acc=acc, G_=G_, ex=ex, j=j: e.tensor_scalar(out=acc[:], in0=G_[:], scalar1=gm[:, ex, j:j + 1], scalar2=None, op0=ALU.mult), reads=[b_G, b_gm], writes=[b_acc])
            else:
                P.dve(lambda e, acc=acc, G_=G_, ex=ex, j=j: e.scalar_tensor_tensor(out=acc[:], in0=G_[:], scalar=gm[:, ex, j:j + 1], in1=acc[:], op0=ALU.mult, op1=ALU.add),
                      reads=[b_G, b_gm, b_acc], writes=[b_acc])
        for kc in range(8):
            ptr, b_ptr = ptrs[kc % 2]
            P.pe(lambda e, ptr=ptr, acc=acc, kc=kc: e.transpose(ptr[:], acc[:, kc * 128:(kc + 1) * 128], idt[:]), reads=[b_acc, b_id], writes=[b_ptr])
            P.dve(lambda e, ptr=ptr, x2t=x2t, x1t=x1t, kc=kc: e.tensor_tensor(out=x2t[:, kc, :], in0=ptr[:], in1=x1t[:, kc, :], op=ALU.add), reads=[b_ptr, b_x1], writes=[b_x2])
        P.dma(lambda e, x2t=x2t, cs_=cs_: e.dma_start(out=x2v[:, :, cs_], in_=x2t[:]), reads=[b_x2], writes=[Buf()])
        if outT is not None:
            ot, b_ot = ots[j % 2]
            P.pool(lambda e, x2t=x2t: e.tensor_tensor(out=sq[:], in0=x2t[:], in1=x2t[:], op=ALU.mult), reads=[b_x2], writes=[b_sq])
            for kc in range(8):
                P.pe(lambda e, kc=kc: e.matmul(pss[:], ones[:], sq[:, kc, :], start=(kc == 0), stop=(kc == 7)), reads=[b_sq, b_ones], writes=[b_pss])
            P.act(lambda e: e.activation(out=rs[:], in_=pss[:], func=AF.Ln, scale=1.0 / D, bias=epsc[:, 0:1]), reads=[b_pss, b_epsc], writes=[b_rs])
            P.act(lambda e: e.activation(out=rs[:], in_=rs[:], func=AF.Exp, scale=-0.5), reads=[b_rs], writes=[b_rs])
            for kc in range(8):
                P.dve(lambda e, ot=ot, x2t=x2t, kc=kc: e.scalar_tensor_tensor(out=ot[:, kc, :], in0=x2t[:, kc, :], scalar=gt[:, kc:kc + 1], in1=rs[:], op0=ALU.mult, op1=ALU.mult),
                      reads=[b_x2, b_g, b_rs], writes=[b_ot])
            P.dma(lambda e, ot=ot, cs_=cs_: e.dma_start(out=ov[:, :, cs_], in_=ot[:]), reads=[b_ot], writes=[Buf()])
    C.end()


DILS = (1, 4, 16)
DSPAN = (1, 2, 8)


def dil_masks():
    kk = np.arange(128)[:, None]
    qq = np.arange(128)[None, :]
    ms = []
    for g, d in enumerate(DILS):
        for dl in range(-DSPAN[g], DSPAN[g] + 1):
            rel = 128 * dl + kk - qq
            ms.append(((rel % d == 0) & (np.abs(rel) <= 64 * d)).astype(np.float32))
    return np.stack(ms)


def stage_vprep(C, pT, ident, vtok, L, v_row):
    P = C.P
    C.begin()
    TB = min(512, L)
    NT = TB // 128
    idt, b_id = C.sb([128, 128])
    P.dma(lambda e: e.dma_start(out=idt[:], in_=ident), writes=[b_id])
    vts = [C.sb([64, TB]) for _ in range(2)]
    vos = [C.sb([128, NT, 66], BF16) for _ in range(2)]
    for vo, b_vo in vos:
        P.dve(lambda e, vo=vo: e.memset(vo[:], 1.0), writes=[b_vo])
    ptrs = [C.ps([128, 64]) for _ in range(2)]
    it = 0
    for hd in range(12):
        for tb in range(L // TB):
            vt, b_vt = vts[it % 2]
            vo, b_vo = vos[it % 2]
            it += 1
            P.dma(lambda e, vt=vt, hd=hd, tb=tb: e.dma_start(out=vt[:], in_=pT[v_row + hd * 64:v_row + (hd + 1) * 64, tb * TB:(tb + 1) * TB]), writes=[b_vt])
            for t in range(NT):
                ptr, b_ptr = ptrs[t % 2]
                P.pe(lambda e, ptr=ptr, vt=vt, t=t: e.transpose(ptr[:], vt[:, t * 128:(t + 1) * 128], idt[0:64, 0:64]), reads=[b_vt, b_id], writes=[b_ptr])
                if t % 2 == 0:
                    P.act(lambda e, ptr=ptr, vo=vo, t=t: e.activation(out=vo[:, t, 0:64], in_=ptr[:], func=AF.Copy), reads=[b_ptr], writes=[b_vo])
                else:
                    P.dve(lambda e, ptr=ptr, vo=vo, t=t: e.tensor_copy(out=vo[:, t, 0:64], in_=ptr[:]), reads=[b_ptr], writes=[b_vo])
            P.dma(lambda e, vo=vo, hd=hd, tb=tb: e.dma_start(out=vtok[hd][tb * TB:(tb + 1) * TB, :].rearrange("(t p) c -> p t c", p=128), in_=vo[:]), reads=[b_vo], writes=[Buf()])
    C.end()


def stage_qk16(C, pT, qk16, L, nrows):
    P = C.P
    C.begin()
    TB = min(512, L)
    xs = [C.sb([128, TB]) for _ in range(3)]
    ys = [C.sb([128, TB], BF16) for _ in range(3)]
    it = 0
    for rt in range(nrows // 128):
        for tb in range(L // TB):
            x_, b_x = xs[it % 3]
            y_, b_y = ys[it % 3]
            it += 1
            P.dma(lambda e, x_=x_, rt=rt, tb=tb: e.dma_start(out=x_[:], in_=pT[rt * 128:(rt + 1) * 128, tb * TB:(tb + 1) * TB]), writes=[b_x])
            if it % 2 == 0:
                P.act(lambda e, x_=x_, y_=y_: e.activation(out=y_[:], in_=x_[:], func=AF.Copy), reads=[b_x], writes=[b_y])
            else:
                P.dve(lambda e, x_=x_, y_=y_: e.tensor_copy(out=y_[:], in_=x_[:]), reads=[b_x], writes=[b_y])
            P.dma(lambda e, y_=y_, rt=rt, tb=tb: e.dma_start(out=qk16[rt * 128:(rt + 1) * 128, tb * TB:(tb + 1) * TB], in_=y_[:]), reads=[b_y], writes=[Buf()])
    C.end()


def stage_dil(C, pT, ident, masks, vtok, mixT, L, q_row, k_row, out_row):
    P = C.P
    NCH = L // 128
    for i in range(4):
        C.begin()
        idt, b_id = C.sb([128, 128])
        mk, b_mk = C.sb([128, 25, 128], BF16)
        P.dma(lambda e: e.dma_start(out=idt[:], in_=ident), writes=[b_id])
        for m0_ in range(0, 25, 5):
            P.dma(lambda e, m0_=m0_: e.dma_start(out=mk[:, m0_:m0_ + 5, :], in_=masks[m0_:m0_ + 5].rearrange("m p n -> p m n")), writes=[b_mk], q="pool")
        qs = [[C.sb([64, 128], BF16) for _ in range(2)] for _ in range(3)]
        ks = [[C.sb([64, (2 * DSPAN[g] + 1) * 128], BF16) for _ in range(2)] for g in range(3)]
        vs = [[C.sb([128, 2 * DSPAN[g] + 1, 66], BF16) for _ in range(2)] for g in range(3)]
        pss = [C.ps([128, 512]) for _ in range(2)]
        pacc = [C.ps([128, 65]) for _ in range(2)]
        ptr, b_ptr = C.ps([64, 128])
        es = [C.sb([128, 512], BF16) for _ in range(3)]
        pms = [C.sb([128, 512], BF16) for _ in range(3)]
        rd, b_rd = C.sb([128, 1])
        ots = [C.sb([128, 64]) for _ in range(2)]
        oTs = [C.sb([64, 128]) for _ in range(2)]
        moff = (0, 3, 8)
        ngrp = 0
        for n in range(NCH):
            pa, b_pa = pacc[n % 2]
            first = True
            plan = []
            for g in range(3):
                hd = 4 * g + i
                D_ = DSPAN[g]
                m0 = max(0, n - D_)
                m1 = min(NCH - 1, n + D_)
                q_, b_q = qs[g][n % 2]
                k_, b_k = ks[g][n % 2]
                v_, b_v = vs[g][n % 2]
                nm = m1 - m0 + 1
                P.dma(lambda e, q_=q_, hd=hd, n=n: e.dma_start(out=q_[:], in_=pT[q_row + hd * 64:q_row + (hd + 1) * 64, n * 128:(n + 1) * 128]), writes=[b_q])
                P.dma(lambda e, k_=k_, hd=hd, m0=m0, nm=nm: e.dma_start(out=k_[:, 0:nm * 128], in_=pT[k_row + hd * 64:k_row + (hd + 1) * 64, m0 * 128:(m0 + nm) * 128]), writes=[b_k])
                P.dma(lambda e, v_=v_, hd=hd, m0=m0, nm=nm: e.dma_start(out=v_[:, 0:nm, :], in_=vtok[hd][m0 * 128:(m0 + nm) * 128, :].rearrange("(t p) c -> p t c", p=128)), writes=[b_v])
                tiles = [(g, m, m - m0, moff[g] + (m - n) + D_) for m in range(m0, m1 + 1)]
                for c0 in range(0, len(tiles), 4):
                    plan.append((g, tiles[c0:c0 + 4], (q_, b_q), (k_, b_k), (v_, b_v)))
            total = sum(len(p[1]) for p in plan)
            done = 0
            for (g, tl, (q_, b_q), (k_, b_k), (v_, b_v)) in plan:
                ps_, b_ps = pss[ngrp % 2]
                e_, b_e = es[ngrp % 3]
                pm, b_pm = pms[ngrp % 3]
                nt = len(tl)
                for a, (_, m, ml, mi) in enumerate(tl):
                    P.pe(lambda e, ps_=ps_, k_=k_, q_=q_, a=a, ml=ml: e.matmul(ps_[:, a * 128:(a + 1) * 128], k_[:, ml * 128:(ml + 1) * 128], q_[:], start=True, stop=True),
                         reads=[b_k, b_q], writes=[b_ps])
                P.act(lambda e, e_=e_, ps_=ps_, nt=nt: e.activation(out=e_[:, 0:nt * 128], in_=ps_[:, 0:nt * 128], func=AF.Exp, scale=0.125), reads=[b_ps], writes=[b_e])
                mi0 = tl[0][3]
                eng = P.dve if ngrp % 2 == 0 else P.pool
                eng(lambda e, pm=pm, e_=e_, nt=nt, mi0=mi0: e.tensor_tensor(out=pm[:, 0:nt * 128], in0=e_[:, 0:nt * 128], in1=mk[:, mi0:mi0 + nt, :].rearrange("p m n -> p (m n)"), op=ALU.mult),
                    reads=[b_e, b_mk], writes=[b_pm])
                for a, (_, m, ml, mi) in enumerate(tl):
                    done += 1
                    P.pe(lambda e, pa=pa, pm=pm, v_=v_, a=a, ml=ml, st=(done == 1), sp=(done == total): e.matmul(pa[:], pm[:, a * 128:(a + 1) * 128], v_[:, ml, 0:65], start=st, stop=sp),
                         reads=[b_pm, b_v], writes=[b_pa])
                ngrp += 1
            ot, b_ot = ots[n % 2]
            oT, b_oT = oTs[n % 2]
            P.dve(lambda e, pa=pa: e.reciprocal(out=rd[:], in_=pa[:, 64:65]), reads=[b_pa], writes=[b_rd])
            P.dve(lambda e, ot=ot, pa=pa: e.tensor_scalar(out=ot[:], in0=pa[:, 0:64], scalar1=rd[:, 0:1], scalar2=None, op0=ALU.mult), reads=[b_pa, b_rd], writes=[b_ot])
            P.pe(lambda e, ot=ot: e.transpose(ptr[:], ot[:], idt[:]), reads=[b_ot, b_id], writes=[b_ptr])
            P.act(lambda e, oT=oT: e.activation(out=oT[:], in_=ptr[:], func=AF.Copy), reads=[b_ptr], writes=[b_oT])
            P.dma(lambda e, oT=oT, n=n: e.dma_start(out=mixT[out_row + i * 64:out_row + (i + 1) * 64, n * 128:(n + 1) * 128], in_=oT[:]), reads=[b_oT], writes=[Buf()])
        C.end()


def rope_tables(L, half, period):
    inv = (10000.0 ** (-np.arange(half, dtype=np.float32) / half)).astype(np.float32)
    ang = (np.arange(L, dtype=np.float32)[None, :] * inv[:, None]).astype(np.float32)
    cos = np.cos(ang).astype(np.float32)
    sin = np.sin(ang).astype(np.float32)
    rows = np.arange(128) % period
    c = cos[rows % half]
    s = np.where((rows < half)[:, None], -sin[rows % half], sin[rows % half])
    return np.ascontiguousarray(c, np.float32), np.ascontiguousarray(s, np.float32)


def swap_cols(w, ncols, period):
    half = period // 2
    idx = np.arange(ncols)
    src = (idx // period) * period + (idx % period + half) % period
    return np.ascontiguousarray(w[:, src])


def ret_tables(L):
    NCH = L // 128
    s = 128.0 ** -0.5
    tab = np.zeros((128, 24, NCH), np.float64)
    t = np.arange(128, dtype=np.float64)
    for h in range(4):
        lg = np.log1p(-(2.0 ** (-5.0 - h)))
        for dr in range(2):
            e = (t + 1) if dr == 0 else (128 - t)
            j0 = (h * 2 + dr) * 3
            tab[:, j0, :] = np.exp(lg * e)[:, None]
            tab[:, j0 + 1, :] = (np.exp(-lg * e) * s)[:, None]
            tab[:, j0 + 2, :] = np.exp(lg * 128)
    return tab.astype(np.float32)


def la_masks():
    j = np.arange(128)[:, None]
    i = np.arange(128)[None, :]
    return np.stack([(j <= i), (j > i), (j >= i)]).astype(np.float32)


def g8(v):
    return np.ascontiguousarray(v.reshape(8, 128).T)


def s5_host(d, T):
    a_re, a_im = d['s5_a_re'][0], d['s5_a_im'][0]
    are2 = np.concatenate([a_re.reshape(64, 64).T] * 2, 0)
    aim2 = np.concatenate([a_im.reshape(64, 64).T] * 2, 0)
    lstep = np.tile(d['s5_log_step'][0].reshape(1, 64), (128, 1))
    dsk = d['s5_d'][0].reshape(4, 128).T
    prm = {"are2": are2, "aim2": aim2, "lstep": lstep, "dsk": dsk, "b_re": d['s5_b_re'][0], "b_im": d['s5_b_im'][0],
           "c_re": d['s5_c_re'][0], "c_im": d['s5_c_im'][0]}
    psw = np.zeros((128, 128), np.float32)
    for k in range(128):
        psw[k, (k + 64) % 128] = 1
    ksel = np.zeros((128, 3), np.float32)
    ksel[:64, 0] = 1; ksel[64:, 1] = 1; ksel[:64, 2] = 1; ksel[64:, 2] = -1
    tau = np.tile(np.arange(1, T + 1, dtype=np.float32)[None, :], (128, 1))
    consts = {"psw": psw, "ksel": ksel, "tau": tau}
    return {k: np.ascontiguousarray(v, np.float32) for k, v in prm.items()}, consts


def route_consts(L):
    NJ = L // 128
    q = np.arange(128)[:, None]; p = np.arange(128)[None, :]
    us = (q < p).astype(np.float32)
    rm = np.ones((16, NJ), np.float32); rm[:, 0] = 0
    rm = np.tile(rm.reshape(1, -1), (128, 1))
    return us, np.ascontiguousarray(rm)


class RowSplit:
    def __init__(self, C, name, rows, L, chunk=1024):
        self.chunk = chunk
        self.parts = [C.dscr("%s_%d" % (name, k), [min(chunk, rows - k * chunk), L]) for k in range(-(-rows // chunk))]

    def __getitem__(self, key):
        rs, cs = key
        k = rs.start // self.chunk
        assert (rs.stop - 1) // self.chunk == k
        return self.parts[k][rs.start - k * self.chunk:rs.stop - k * self.chunk, cs]


def build_program(L, FF):
    NCH = L // 128
    NJ = L // 128
    CAP = L // 8
    T = min(512, L)
    C = Ctx()
    i = {}
    def din(name, shape, dt=F32):
        i[name] = C.din(name, shape, dt)
        return i[name]
    xT = din("xT", [D, L])
    w_in = [din("w_in0", [D, 2560]), din("w_in1", [D, 4480])]
    wsw = [din("wsw0", [D, 1024]), din("wsw1", [D, 1536])]
    w_out = [din("w_out0", [1024, 1024]), din("w_out1", [768, 1024])]
    gmix = [din("gmix0", [128, 8]), din("gmix1", [128, 8])]
    gffn = [din("gffn0", [128, 8]), din("gffn1", [128, 8])]
    gfin = din("gfin", [128, 8])
    cos = [din("cos0", [128, L]), din("cos1", [128, L])]
    sin = [din("sin0", [128, L]), din("sin1", [128, L])]
    ident = din("ident", [128, 128])
    lam = din("lamask", [3, 128, 128])
    rtab = din("rtab", [128, 24, NCH])
    prm_shapes = {"are2": [128, 64], "aim2": [128, 64], "lstep": [128, 64], "dsk": [128, 4], "b_re": [2, 32, 64, 16], "b_im": [2, 32, 64, 16],
                  "c_re": [2, 32, 16, 64], "c_im": [2, 32, 16, 64]}
    prm = {}
    for k, shp in prm_shapes.items():
        a = din("s5_" + k, shp)
        prm[k] = a[:, :] if len(shp) == 2 else a
    consts = {"ident": ident[:, :], "psw": din("c_psw", [128, 128])[:, :], "ksel": din("c_ksel", [128, 3])[:, :], "tau": din("c_tau", [128, T])[:, :]}
    gluw = din("gluw", [512, 512])
    glub = din("glub", [128, 4])
    router = [din("router0", [1024, 16]), din("router1", [1024, 16])]
    ustrict = din("ustrict", [128, 128])
    rmask = din("rmask", [128, 16 * NJ])
    mlb = din("mlb", [128, 16])
    dmasks = din("dmasks", [25, 128, 128])
    w1 = [din("w1_0", [16, 1024, FF]), din("w1_1", [16, 1024, FF])]
    w3 = [din("w3_0", [16, 1024, FF]), din("w3_1", [16, 1024, FF])]
    w2 = [din("w2_0", [16, FF, 1024]), din("w2_1", [16, FF, 1024])]
    outT = C.dout("outT", [D, L])
    pT = RowSplit(C, "pT", 4480, L)
    mixT = C.dscr("mixT", [1024, L])
    o1 = C.dscr("o1", [4, L, 128])
    yf = C.dscr("yf", [512, L])
    x1T = C.dscr("x1T", [1024, L])
    x2T = C.dscr("x2T", [1024, L])
    x3T = C.dscr("x3T", [1024, L])
    htok = C.dscr("htok", [L, 1024])
    aff = C.dscr("aff", [L, 16])
    posd = C.dscr("posd", [128, 16 * NJ], I32)
    gmd = C.dscr("gmd", [128, 16 * NJ])
    xe = [C.dscr("xe%d" % e, [CAP, 1024]) for e in range(16)]
    ye = [C.dscr("ye%d" % e, [CAP, 1024]) for e in range(16)]
    tabd = C.dscr("tabd", [128, 24, NCH])
    vtok = [C.dscr("vtok%d" % h, [L, 66], BF16) for h in range(12)]
    qk16 = C.dscr("qk16", [1536, L], BF16)

    def moe(layer, xin1T, xoutT, final):
        stage_route(C, aff, ustrict[:, :], rmask[:, :], posd[:, :], gmd[:, :], L)
        stage_dispatch(C, htok, posd[:, :], xe, L)
        stage_ffn(C, xe, w1[layer], w3[layer], w2[layer], ye, ident[:, :], L, FF)
        stage_combine(C, ye, posd[:, :], gmd[:, :], xin1T, ident[:, :], xoutT, outT if final else None, gfin[:, :], L)

    stage_proj(C, xT, w_in[0], wsw[0], gmix[0][:, :], cos[0], sin[0], pT, L, 20, 8)
    stage_linattn(C, pT, rtab[:, :, :], ident[:, :], lam, o1, mixT, L, 0, 512, 1024, 1536, 0, False)
    stage_s5(C, pT, prm, consts, yf, mixT, L, 2048, 512)
    stage_out(C, xT, mixT, w_out[0], gluw, glub[:, :], gffn[0][:, :], router[0], ident[:, :], x1T, htok, aff, L, True)
    moe(0, x1T, x2T, False)
    stage_proj(C, x2T, w_in[1], wsw[1], gmix[1][:, :], cos[1], sin[1], pT, L, 35, 12)
    stage_gates(C, pT, mlb[:, :], ident[:, :], tabd, L, 4352)
    stage_linattn(C, pT, tabd[:, :, :], ident[:, :], lam, o1, mixT, L, 2304, 2816, 3328, 3840, 256, True)
    stage_vprep(C, pT, ident[:, :], vtok, L, 1536)
    stage_qk16(C, pT, qk16, L, 1536)
    stage_dil(C, qk16, ident[:, :], dmasks, vtok, mixT, L, 0, 768, 0)
    stage_out(C, x2T, mixT, w_out[1], None, None, gffn[1][:, :], router[1], ident[:, :], x1T, htok, aff, L, False)
    moe(1, x1T, x3T, True)
    ninst = C.P.ninst
    return C.finish(), ninst


def host_inputs(inp, b, L, FF):
    f = lambda a: np.ascontiguousarray(a, np.float32)
    T = min(512, L)
    im = {"xT": f(inp["x"][b].T)}
    W0 = inp["ev_w_in"][0]
    W1 = np.zeros((D, 4480), np.float32)
    W1[:, :4368] = inp["od_w_in"][0]
    im["w_in0"] = f(W0); im["w_in1"] = W1
    im["wsw0"] = swap_cols(W0[:, :1024], 1024, 128); im["wsw1"] = swap_cols(W1[:, :1536], 1536, 64)
    im["w_out0"] = f(inp["ev_w_out"][0]); im["w_out1"] = f(inp["od_w_out"][0])
    for l in range(2):
        im["gmix%d" % l] = g8(inp["norm_mix_g"][l]); im["gffn%d" % l] = g8(inp["norm_ffn_g"][l])
        im["router%d" % l] = f(inp["moe_router"][l])
        im["w1_%d" % l] = f(inp["moe_w1"][l]); im["w3_%d" % l] = f(inp["moe_w3"][l]); im["w2_%d" % l] = f(inp["moe_w2"][l])
    im["gfin"] = g8(inp["final_g"])
    im["cos0"], im["sin0"] = rope_tables(L, 64, 128)
    im["cos1"], im["sin1"] = rope_tables(L, 32, 64)
    im["ident"] = np.eye(128, dtype=np.float32)
    im["lamask"] = la_masks()
    im["rtab"] = ret_tables(L)
    prm, consts = s5_host({k: inp[k] for k in ("s5_a_re", "s5_a_im", "s5_b_re", "s5_b_im", "s5_c_re", "s5_c_im", "s5_log_step", "s5_d")}, T)
    for k, v in prm.items():
        im["s5_" + k] = v
    for k, v in consts.items():
        im["c_" + k] = f(v)
    im["gluw"] = f(inp["s5_glu_w"][0]); im["glub"] = f(inp["s5_glu_b"][0].reshape(4, 128).T)
    im["ustrict"], im["rmask"] = route_consts(L)
    im["mlb"] = f(np.tile(np.concatenate([inp["ml_i_bias"][0].reshape(-1), inp["ml_f_bias"][0].reshape(-1)])[None, :], (128, 1)))
    im["dmasks"] = dil_masks()
    return im


def run_model(inp, batches):
    L = inp["x"].shape[1]
    FF = inp["moe_w1"].shape[-1]
    nc, ninst = build_program(L, FF)
    ims = [host_inputs(inp, b, L, FF) for b in batches]
    res = run_bass_kernel_spmd(nc, ims, core_ids=list(range(len(batches))))
    return [np.ascontiguousarray(r["outT"].T) for r in res.results]


def kernel(**inputs):
    inp = {k: np.asarray(v) for k, v in inputs.items()}
    B = inp["x"].shape[0]
    outs = run_model(inp, list(range(B)))
    return np.stack(outs, 0).astype(np.float32)
```

```python
import contextlib
import numpy as np
import concourse.bass as bass
import concourse.mybir as mybir
from concourse.bass_utils import run_bass_kernel_spmd

F32 = mybir.dt.float32
BF16 = mybir.dt.bfloat16
I32 = mybir.dt.int32
AF = mybir.ActivationFunctionType
ALU = mybir.AluOpType
AX = mybir.AxisListType
D = 1024
EPS = 1e-6
ENGS = ("pe", "act", "dve", "pool", "sp")
ENGOBJ = {"pe": "tensor", "act": "scalar", "dve": "vector", "pool": "gpsimd", "sp": "sync"}


class Buf:
    __slots__ = ("last_w", "readers")

    def __init__(self):
        self.last_w = None
        self.readers = []


class Op:
    __slots__ = ("eng", "fn", "idx", "deps", "dma", "signal", "cnt", "sem", "semval", "stage")


class Prog:
    ND = {"sp": 10, "act": 2, "pool": 2}

    def __init__(self, nc, st):
        self.nc = nc
        self.esem = {e: st.enter_context(nc.semaphore("s_" + e)) for e in ENGS}
        self.dsem = {(q, k): st.enter_context(nc.semaphore("d_%s%d" % (q, k))) for q in self.ND for k in range(self.ND[q])}
        self.base = {e: 0 for e in ENGS}
        self.dk = {q: 0 for q in self.ND}
        self.dval = {k: 0 for k in self.dsem}
        self.waited = {e: {} for e in ENGS}
        self.stage = 0
        self.ops = {e: [] for e in ENGS}
        self.ninst = 0

    def op(self, eng, fn, reads=(), writes=(), dma=False):
        o = Op()
        o.eng, o.fn, o.idx, o.dma, o.signal, o.cnt, o.stage = eng, fn, len(self.ops[eng]), dma, False, 0, self.stage
        o.sem, o.semval = None, 0
        deps = {}
        for b in reads:
            if b.last_w is not None and b.last_w.stage == self.stage:
                deps[id(b.last_w)] = b.last_w
        for b in writes:
            if b.last_w is not None and b.last_w.stage == self.stage:
                deps[id(b.last_w)] = b.last_w
            for r in b.readers:
                if r.stage == self.stage:
                    deps[id(r)] = r
        o.deps = list(deps.values())
        for b in reads:
            b.readers.append(o)
        for b in writes:
            b.last_w = o
            b.readers = []
        if dma:
            k = self.dk[eng]
            self.dk[eng] += 1
            nd = self.ND[eng]
            o.sem = (eng, k % nd)
            o.semval = 16 * (k // nd + 1)
            self.dval[o.sem] = o.semval
        self.ops[eng].append(o)
        self.ninst += 1
        return o

    def pe(self, fn, reads=(), writes=()):
        return self.op("pe", fn, reads, writes)

    def act(self, fn, reads=(), writes=()):
        return self.op("act", fn, reads, writes)

    def dve(self, fn, reads=(), writes=()):
        return self.op("dve", fn, reads, writes)

    def pool(self, fn, reads=(), writes=()):
        return self.op("pool", fn, reads, writes)

    def dma(self, fn, reads=(), writes=(), q="sp"):
        return self.op(q, fn, reads, writes, dma=True)

    def end_stage(self):
        nc = self.nc
        for e in ENGS:
            comp = [o for o in self.ops[e] if not o.dma]
            if comp:
                comp[-1].signal = True
            for o in self.ops[e]:
                for d in o.deps:
                    if d.dma:
                        continue
                    if d.eng == o.eng:
                        if e == "pe":
                            continue
                        if o.idx - d.idx > 2 and not o.dma:
                            continue
                    d.signal = True
        final = {}
        for e in ENGS:
            c = self.base[e]
            for o in self.ops[e]:
                if o.dma:
                    continue
                if o.signal:
                    c += 1
                o.cnt = c
            final[e] = c
        with nc.Block() as block:
            def run_engine(e, eng):
                waited = self.waited[e]

                def need(sem, key, val):
                    if waited.get(key, 0) < val:
                        eng.wait_ge(sem, val)
                        waited[key] = val

                for o in self.ops[e]:
                    for d in o.deps:
                        if d.dma:
                            need(self.dsem[d.sem], d.sem, d.semval)
                        else:
                            if d.eng == e:
                                if e == "pe":
                                    continue
                                if o.idx - d.idx > 2 and not o.dma:
                                    continue
                            need(self.esem[d.eng], d.eng, d.cnt)
                    if o.dma:
                        if o.semval > 16:
                            need(self.dsem[o.sem], o.sem, o.semval - 16)
                        inst = o.fn(eng)
                        inst.then_inc(self.dsem[o.sem], 16)
                    else:
                        inst = o.fn(eng)
                        if o.signal:
                            inst.then_inc(self.esem[e], 1)
                for e2 in ENGS:
                    if e2 != e and final[e2] > 0:
                        need(self.esem[e2], e2, final[e2])
                for k, v in self.dval.items():
                    if v > 0:
                        need(self.dsem[k], k, v)

            for e in ENGS:
                def _f(eng, e=e):
                    run_engine(e, eng)
                getattr(block, ENGOBJ[e])(_f)
        self.base = final
        self.stage += 1
        self.ops = {e: [] for e in ENGS}


class Ctx:
    def __init__(self):
        self.nc = bass.Bass("TRN2", target_bir_lowering=False)
        self.gst = contextlib.ExitStack()
        self.P = Prog(self.nc, self.gst)
        self.st = None
        self.n = 0
        self.dbg = {}

    def din(self, name, shape, dt=F32):
        return self.nc.dram_tensor(name, list(shape), dt, kind="ExternalInput").ap()

    def dout(self, name, shape, dt=F32):
        return self.nc.dram_tensor(name, list(shape), dt, kind="ExternalOutput").ap()

    def dscr(self, name, shape, dt=F32, debug=False):
        if debug:
            return self.dout(name, shape, dt)
        return self.nc.dram_tensor(name, list(shape), dt, kind="Internal").ap()

    def begin(self):
        self.st = contextlib.ExitStack()

    def end(self):
        self.P.end_stage()
        self.st.close()
        self.st = None

    def sb(self, shape, dt=F32):
        self.n += 1
        t = self.st.enter_context(self.nc.sbuf_tensor("t%d" % self.n, list(shape), dt))
        return t, Buf()

    def ps(self, shape, dt=F32):
        self.n += 1
        t = self.st.enter_context(self.nc.psum_tensor("p%d" % self.n, list(shape), dt))
        return t, Buf()

    def finish(self):
        self.gst.close()
        return self.nc


def stage_proj(C, xT, w, wsw, g8, cos, sin, pT, L, NCT, NRT):
    P = C.P
    C.begin()
    TB = min(512, L)
    NB = L // TB
    wb, _ = C.sb([128, 8, NCT * 128], BF16)
    wswb, _ = C.sb([128, 8, NRT * 128], BF16)
    b_w = [Buf() for _ in range(8)]
    b_ws = [Buf() for _ in range(8)]
    gt, b_g = C.sb([128, 8])
    ones, b_ones = C.sb([128, 128], BF16)
    P.dma(lambda e: e.dma_start(out=gt[:], in_=g8), writes=[b_g])
    P.dve(lambda e: e.memset(ones[:], 1.0), writes=[b_ones])
    epsc, b_epsc = C.sb([128, 1])
    P.dve(lambda e: e.memset(epsc[:], EPS), writes=[b_epsc])
    CH = 640
    for kc in range(8):
        for c0 in range(0, NCT * 128, CH):
            P.dma(lambda e, kc=kc, c0=c0: e.dma_start(out=wb[:, kc, c0:c0 + CH], in_=w[kc * 128:(kc + 1) * 128, c0:c0 + CH]),
                  writes=[b_w[kc]], q="pool")
        for c0 in range(0, NRT * 128, 512):
            P.dma(lambda e, kc=kc, c0=c0: e.dma_start(out=wswb[:, kc, c0:c0 + 512], in_=wsw[kc * 128:(kc + 1) * 128, c0:c0 + 512]),
                  writes=[b_ws[kc]], q="pool")
    nxb = 2 if NCT <= 24 else 1
    xts = [C.sb([128, 8, TB]) for _ in range(nxb)]
    xbs = [C.sb([128, 8, TB], BF16) for _ in range(nxb)]
    sq, b_sq = C.sb([128, 8, TB], BF16)
    css = [C.sb([128, TB]) for _ in range(2)]
    sns = [C.sb([128, TB]) for _ in range(2)]
    rss = [C.sb([128, TB]) for _ in range(2)]
    pss, b_pss = C.ps([128, TB])
    ps_a = [C.ps([128, TB]) for _ in range(2)]
    ps_b = [C.ps([128, TB]) for _ in range(2)]
    t1s = [C.sb([128, TB]) for _ in range(2)]
    t2s = [C.sb([128, TB]) for _ in range(2)]
    os_ = [C.sb([128, TB]) for _ in range(4)]
    b_out = Buf()
    xv = xT.rearrange("(kc p) n -> p kc n", p=128)
    no = 0
    na = 0
    for tb in range(NB):
        sl = slice(tb * TB, (tb + 1) * TB)
        xt, b_x = xts[tb % nxb]
        xb, b_xb = xbs[tb % nxb]
        cs, b_cs = css[tb % 2]
        sn, b_sn = sns[tb % 2]
        rs, b_rs = rss[tb % 2]
        P.dma(lambda e, xt=xt, sl=sl: e.dma_start(out=xt[:], in_=xv[:, :, sl]), writes=[b_x])
        if NRT:
            P.dma(lambda e, cs=cs, sl=sl: e.dma_start(out=cs[:], in_=cos[:, sl]), writes=[b_cs])
            P.dma(lambda e, sn=sn, sl=sl: e.dma_start(out=sn[:], in_=sin[:, sl]), writes=[b_sn])
        P.pool(lambda e, xt=xt: e.tensor_tensor(out=sq[:], in0=xt[:], in1=xt[:], op=ALU.mult), reads=[b_x], writes=[b_sq])
        for kc in range(8):
            eng = P.dve if kc % 2 == 0 else P.pool
            eng(lambda e, kc=kc, xt=xt, xb=xb: e.tensor_scalar(out=xb[:, kc, :], in0=xt[:, kc, :], scalar1=gt[:, kc:kc + 1], scalar2=None, op0=ALU.mult),
                reads=[b_x, b_g], writes=[b_xb])
        for kc in range(8):
            P.pe(lambda e, kc=kc: e.matmul(pss[:], ones[:], sq[:, kc, :], start=(kc == 0), stop=(kc == 7)),
                 reads=[b_sq, b_ones], writes=[b_pss])
        P.act(lambda e, rs=rs: e.activation(out=rs[:], in_=pss[:], func=AF.Ln, scale=1.0 / D, bias=epsc[:, 0:1]), reads=[b_pss, b_epsc], writes=[b_rs])
        P.act(lambda e, rs=rs: e.activation(out=rs[:], in_=rs[:], func=AF.Exp, scale=-0.5), reads=[b_rs], writes=[b_rs])
        if NRT:
            P.pool(lambda e, cs=cs, rs=rs: e.tensor_tensor(out=cs[:], in0=cs[:], in1=rs[:], op=ALU.mult), reads=[b_cs, b_rs], writes=[b_cs])
            P.pool(lambda e, sn=sn, rs=rs: e.tensor_tensor(out=sn[:], in0=sn[:], in1=rs[:], op=ALU.mult), reads=[b_sn, b_rs], writes=[b_sn])
        for ct in range(NCT):
            pa, b_pa = ps_a[na % 2]
            pb, b_pb = ps_b[na % 2]
            na += 1
            o, b_o = os_[no % 4]
            no += 1
            for kc in range(8):
                P.pe(lambda e, kc=kc, ct=ct, pa=pa, xb=xb: e.matmul(pa[:], wb[:, kc, ct * 128:(ct + 1) * 128], xb[:, kc, :], start=(kc == 0), stop=(kc == 7)),
                     reads=[b_w[kc], b_xb], writes=[b_pa])
            if ct < NRT:
                for kc in range(8):
                    P.pe(lambda e, kc=kc, ct=ct, pb=pb, xb=xb: e.matmul(pb[:], wswb[:, kc, ct * 128:(ct + 1) * 128], xb[:, kc, :], start=(kc == 0), stop=(kc == 7)),
                         reads=[b_ws[kc], b_xb], writes=[b_pb])
                t1, b_t1 = t1s[ct % 2]
                t2, b_t2 = t2s[ct % 2]
                P.dve(lambda e, t1=t1, pa=pa, cs=cs: e.tensor_tensor(out=t1[:], in0=pa[:], in1=cs[:], op=ALU.mult), reads=[b_pa, b_cs], writes=[b_t1])
                P.dve(lambda e, t2=t2, pb=pb, sn=sn: e.tensor_tensor(out=t2[:], in0=pb[:], in1=sn[:], op=ALU.mult), reads=[b_pb, b_sn], writes=[b_t2])
                P.pool(lambda e, o=o, t1=t1, t2=t2: e.tensor_tensor(out=o[:], in0=t1[:], in1=t2[:], op=ALU.add), reads=[b_t1, b_t2], writes=[b_o])
            else:
                P.dve(lambda e, o=o, pa=pa, rs=rs: e.tensor_tensor(out=o[:], in0=pa[:], in1=rs[:], op=ALU.mult), reads=[b_pa, b_rs], writes=[b_o])
            P.dma(lambda e, o=o, ct=ct, sl=sl: e.dma_start(out=pT[ct * 128:(ct + 1) * 128, sl], in_=o[:]), reads=[b_o], writes=[Buf()])
    C.end()


def stage_gates(C, pT, mlb, ident, tabd, L, mg_row):
    P = C.P
    C.begin()
    NCH = L // 128
    s = 128.0 ** -0.5
    idt, b_id = C.sb([128, 128])
    mb, b_mb = C.sb([128, 16])
    nfb, b_nfb = C.sb([128, 8])
    lns, b_lns = C.sb([128, 1])
    onesq, b_onesq = C.sb([128, 128])
    P.dma(lambda e: e.dma_start(out=idt[:], in_=ident), writes=[b_id])
    P.dma(lambda e: e.dma_start(out=mb[:], in_=mlb), writes=[b_mb])
    P.dve(lambda e: e.tensor_scalar(out=nfb[:], in0=mb[:, 8:16], scalar1=-1.0, scalar2=None, op0=ALU.mult), reads=[b_mb], writes=[b_nfb])
    P.dve(lambda e: e.memset(lns[:], float(np.log(s))), writes=[b_lns])
    P.dve(lambda e: e.memset(onesq[:], 1.0), writes=[b_onesq])
    b_out = Buf()
    gps = [C.ps([128, 128]) for _ in range(3)]
    for h in range(4):
        for dr in range(2):
            gi, b_gi = C.sb([128, 128])
            gf, b_gf = C.sb([128, 128])
            ri = mg_row + dr * 4 + h
            rf = mg_row + 8 + dr * 4 + h
            P.dma(lambda e, gi=gi, ri=ri: e.dma_start(out=gi[0:NCH, :], in_=pT[ri:ri + 1, :].rearrange("o (c t) -> (o c) t", t=128)), writes=[b_gi])
            P.dma(lambda e, gf=gf, rf=rf: e.dma_start(out=gf[0:NCH, :], in_=pT[rf:rf + 1, :].rearrange("o (c t) -> (o c) t", t=128)), writes=[b_gf])
            sp, b_sp = C.sb([128, 128])
            csp, b_csp = C.sb([128, 128])
            col = dr * 4 + h
            P.act(lambda e, sp=sp, gf=gf, col=col: e.activation(out=sp[0:NCH, :], in_=gf[0:NCH, :], func=AF.Exp, scale=-1.0, bias=nfb[0:NCH, col:col + 1]),
                  reads=[b_gf, b_nfb], writes=[b_sp])
            P.act(lambda e, sp=sp: e.activation(out=sp[0:NCH, :], in_=sp[0:NCH, :], func=AF.Ln, scale=1.0, bias=1.0), reads=[b_sp], writes=[b_sp])
            P.dve(lambda e, csp=csp, sp=sp: e.tensor_tensor_scan(out=csp[0:NCH, :], data0=onesq[0:NCH, :], data1=sp[0:NCH, :], initial=0.0, op0=ALU.mult, op1=ALU.add),
                  reads=[b_sp, b_onesq], writes=[b_csp])
            ex, b_ex = C.sb([128, 128])
            if dr == 0:
                P.dve(lambda e, ex=ex, csp=csp: e.tensor_copy(out=ex[0:NCH, :], in_=csp[0:NCH, :]), reads=[b_csp], writes=[b_ex])
            else:
                P.dve(lambda e, ex=ex, sp=sp, csp=csp: e.tensor_tensor(out=ex[0:NCH, :], in0=sp[0:NCH, :], in1=csp[0:NCH, :], op=ALU.subtract), reads=[b_sp, b_csp], writes=[b_ex])
                P.dve(lambda e, ex=ex, csp=csp: e.tensor_scalar(out=ex[0:NCH, :], in0=ex[0:NCH, :], scalar1=csp[0:NCH, 127:128], scalar2=None, op0=ALU.add), reads=[b_ex, b_csp], writes=[b_ex])
            av, b_av = C.sb([128, 128])
            cv, b_cv = C.sb([128, 128])
            ebc, b_ebc = C.sb([128, 1])
            P.act(lambda e, av=av, ex=ex: e.activation(out=av[0:NCH, :], in_=ex[0:NCH, :], func=AF.Exp, scale=-1.0), reads=[b_ex], writes=[b_av])
            P.dve(lambda e, cv=cv, gi=gi, ex=ex: e.tensor_tensor(out=cv[0:NCH, :], in0=gi[0:NCH, :], in1=ex[0:NCH, :], op=ALU.add), reads=[b_gi, b_ex], writes=[b_cv])
            P.dve(lambda e, cv=cv, col=col: e.tensor_scalar(out=cv[0:NCH, :], in0=cv[0:NCH, :], scalar1=mb[0:NCH, col:col + 1], scalar2=lns[0:NCH, 0:1], op0=ALU.add, op1=ALU.add),
                  reads=[b_cv, b_mb, b_lns], writes=[b_cv])
            P.act(lambda e, cv=cv: e.activation(out=cv[0:NCH, :], in_=cv[0:NCH, :], func=AF.Exp), reads=[b_cv], writes=[b_cv])
            P.act(lambda e, ebc=ebc, csp=csp: e.activation(out=ebc[0:NCH, :], in_=csp[0:NCH, 127:128], func=AF.Exp, scale=-1.0), reads=[b_csp], writes=[b_ebc])
            tb_, b_tb = C.sb([128, 3, NCH])
            for k, (src, b_src) in enumerate(((av, b_av), (cv, b_cv))):
                pt, b_pt = gps[k]
                P.pe(lambda e, pt=pt, src=src: e.transpose(pt[:, 0:NCH], src[0:NCH, :], idt[0:NCH, 0:NCH]), reads=[b_src, b_id], writes=[b_pt])
                P.act(lambda e, pt=pt, k=k, tb_=tb_: e.activation(out=tb_[:, k, :], in_=pt[:, 0:NCH], func=AF.Copy), reads=[b_pt], writes=[b_tb])
            tm, b_tm = C.sb([128, 128])
            P.dve(lambda e, tm=tm, ebc=ebc: e.tensor_scalar(out=tm[0:NCH, :], in0=onesq[0:NCH, :], scalar1=ebc[0:NCH, 0:1], scalar2=None, op0=ALU.mult),
                  reads=[b_onesq, b_ebc], writes=[b_tm])
            pt, b_pt = gps[2]
            P.pe(lambda e, pt=pt, tm=tm: e.matmul(pt[:, 0:NCH], tm[0:NCH, :], idt[0:NCH, 0:NCH], start=True, stop=True), reads=[b_tm, b_id], writes=[b_pt])
            P.act(lambda e, pt=pt, tb_=tb_: e.activation(out=tb_[:, 2, :], in_=pt[:, 0:NCH], func=AF.Copy), reads=[b_pt], writes=[b_tb])
            j0 = (h * 2 + dr) * 3
            P.dma(lambda e, tb_=tb_, j0=j0: e.dma_start(out=tabd[:, j0:j0 + 3, :], in_=tb_[:]), reads=[b_tb], writes=[Buf()])
    C.end()


def stage_linattn(C, pT, tab, ident, masks, o1, mixT, L, q_row, k_row, v_row, g_row, out_row, mlstm):
    P = C.P
    C.begin()
    NCH = L // 128
    NV = 129 if mlstm else 128
    idt, b_id = C.sb([128, 128])
    mk, b_mk = C.sb([128, 3, 128])
    tb_, b_tb = C.sb([128, 24, NCH])
    P.dma(lambda e: e.dma_start(out=idt[:], in_=ident), writes=[b_id])
    P.dma(lambda e: e.dma_start(out=mk[:], in_=masks.rearrange("m p n -> p m n")), writes=[b_mk])
    P.dma(lambda e: e.dma_start(out=tb_[:], in_=tab), writes=[b_tb])
    epsc, b_epsc = C.sb([128, 1])
    P.dve(lambda e: e.memset(epsc[:], EPS), writes=[b_epsc])
    NBUF = 4
    qTs = [C.sb([128, 128]) for _ in range(NBUF)]
    kTs = [C.sb([128, 128]) for _ in range(NBUF)]
    vTs = [C.sb([128, 128]) for _ in range(NBUF)]
    gTs = [C.sb([128, 128]) for _ in range(NBUF)]
    hfs = [C.sb([128, 128]) for _ in range(NBUF)]
    ktoks = [C.sb([128, 128]) for _ in range(NBUF)]
    vpps = [C.sb([128, NV]) for _ in range(NBUF)]
    sms = [C.sb([128, 128]) for _ in range(NBUF)]
    os_ = [C.sb([128, NV]) for _ in range(NBUF)]
    hs = [C.sb([128, 128]) for _ in range(NBUF)]
    gas = [C.sb([128, 128]) for _ in range(NBUF)]
    outs = [C.sb([128, 128]) for _ in range(NBUF)]
    p_kt = [C.ps([128, 128]) for _ in range(1)]
    p_vt = [C.ps([128, 128]) for _ in range(1)]
    p_s = [C.ps([128, 128]) for _ in range(2)]
    p_o = [C.ps([128, NV]) for _ in range(2)]
    p_kv = [C.ps([128, NV]) for _ in range(1)]
    p_tr = [C.ps([128, 128]) for _ in range(1)]
    cst, b_cst = C.sb([128, NV])
    tmpc, b_tmpc = C.sb([128, NV])
    st6, b_st6 = C.sb([128, 6])
    mv, b_mv = C.sb([128, 2])
    rsd, b_rsd = C.sb([128, 1])
    dn, b_dn = C.sb([128, 1])
    b_o1 = [[Buf() for _ in range(NCH)] for _ in range(4)]
    csts = [(cst, b_cst)] + [C.sb([128, NV]) for _ in range(3)]
    SK = 2
    for dr in range(2):
        mi = 0 if dr == 0 else (2 if mlstm else 1)
        for h in range(4):
            P.dve(lambda e, c_=csts[h][0]: e.memset(c_[:], 0.0), writes=[csts[h][1]])
        order = list(range(NCH)) if dr == 0 else list(range(NCH - 1, -1, -1))
        items = [(n, h) for n in order for h in range(4)]

        def phaseA(k, dr=dr, mi=mi, items=items):
            n, h = items[k]
            j0 = (h * 2 + dr) * 3
            i = k % NBUF
            cs_ = slice(n * 128, (n + 1) * 128)
            qT, b_q = qTs[i]
            kT, b_k = kTs[i]
            vT, b_v = vTs[i]
            P.dma(lambda e: e.dma_start(out=qT[:], in_=pT[q_row + h * 128:q_row + (h + 1) * 128, cs_]), writes=[b_q])
            P.dma(lambda e: e.dma_start(out=kT[:], in_=pT[k_row + h * 128:k_row + (h + 1) * 128, cs_]), writes=[b_k])
            P.dma(lambda e: e.dma_start(out=vT[:], in_=pT[v_row + h * 128:v_row + (h + 1) * 128, cs_]), writes=[b_v])
            pk, b_pk = p_kt[0]
            pv, b_pv = p_vt[0]
            ktok, b_kt = ktoks[i]
            vpp, b_vp = vpps[i]
            P.pe(lambda e: e.transpose(pk[:], kT[:], idt[:]), reads=[b_k, b_id], writes=[b_pk])
            P.pe(lambda e: e.transpose(pv[:], vT[:], idt[:]), reads=[b_v, b_id], writes=[b_pv])
            P.act(lambda e: e.activation(out=ktok[:], in_=pk[:], func=AF.Copy), reads=[b_pk], writes=[b_kt])
            P.dve(lambda e: e.tensor_scalar(out=vpp[:, 0:128], in0=pv[:], scalar1=tb_[:, j0 + 1, n:n + 1], scalar2=None, op0=ALU.mult), reads=[b_pv, b_tb], writes=[b_vp])
            if mlstm:
                P.act(lambda e: e.activation(out=vpp[:, 128:129], in_=tb_[:, j0 + 1, n:n + 1], func=AF.Copy), reads=[b_tb], writes=[b_vp])
            ps_, b_ps = p_s[k % 2]
            sm, b_sm = sms[i]
            P.pe(lambda e: e.matmul(ps_[:], kT[:], qT[:], start=True, stop=True), reads=[b_k, b_q], writes=[b_ps])
            P.dve(lambda e: e.tensor_tensor(out=sm[:], in0=ps_[:], in1=mk[:, mi, :], op=ALU.mult), reads=[b_ps, b_mk], writes=[b_sm])
            if dr == 1:
                hf, b_hf = hfs[i]
                gT, b_g = gTs[i]
                ga, b_ga = gas[i]
                P.dma(lambda e: e.dma_start(out=hf[:], in_=o1[h, cs_, :]), reads=[b_o1[h][n]], writes=[b_hf])
                P.dma(lambda e: e.dma_start(out=gT[:], in_=pT[g_row + h * 128:g_row + (h + 1) * 128, cs_]), writes=[b_g])
                P.act(lambda e: e.activation(out=ga[:], in_=gT[:], func=AF.Exp, scale=-1.0), reads=[b_g], writes=[b_ga])
                P.pool(lambda e: e.tensor_scalar(out=ga[:], in0=ga[:], scalar1=1.0, scalar2=None, op0=ALU.add), reads=[b_ga], writes=[b_ga])
                P.dve(lambda e: e.reciprocal(out=ga[:], in_=ga[:]), reads=[b_ga], writes=[b_ga])
                if not mlstm:
                    P.pool(lambda e: e.tensor_tensor(out=ga[:], in0=ga[:], in1=gT[:], op=ALU.mult), reads=[b_ga, b_g], writes=[b_ga])

        def phaseB(k, dr=dr, items=items):
            n, h = items[k]
            j0 = (h * 2 + dr) * 3
            i = k % NBUF
            cs_ = slice(n * 128, (n + 1) * 128)
            cst, b_cst = csts[h]
            qT, b_q = qTs[i]
            ktok, b_kt = ktoks[i]
            vpp, b_vp = vpps[i]
            sm, b_sm = sms[i]
            po, b_po = p_o[k % 2]
            P.pe(lambda e: e.matmul(po[:], sm[:], vpp[:], start=True, stop=False), reads=[b_sm, b_vp], writes=[b_po])
            P.pe(lambda e: e.matmul(po[:], qT[:], cst[:], start=False, stop=True), reads=[b_q, b_cst], writes=[b_po])
            o_, b_o = os_[i]
            P.act(lambda e: e.activation(out=o_[:], in_=po[:], func=AF.Copy, scale=tb_[:, j0, n:n + 1]), reads=[b_po, b_tb], writes=[b_o])
            pkv, b_pkv = p_kv[0]
            P.pe(lambda e: e.matmul(pkv[:], ktok[:], vpp[:], start=True, stop=True), reads=[b_kt, b_vp], writes=[b_pkv])
            P.dve(lambda e: e.tensor_tensor(out=tmpc[:], in0=pkv[:], in1=cst[:], op=ALU.add), reads=[b_pkv, b_cst], writes=[b_tmpc])
            P.dve(lambda e: e.tensor_scalar(out=cst[:], in0=tmpc[:], scalar1=tb_[:, j0 + 2, n:n + 1], scalar2=None, op0=ALU.mult), reads=[b_tmpc, b_tb], writes=[b_cst])
            hh, b_h = hs[i]
            if mlstm:
                P.dve(lambda e: e.tensor_scalar(out=dn[:], in0=o_[:, 128:129], scalar1=-1.0, scalar2=1.0, op0=ALU.mult, op1=ALU.max), reads=[b_o], writes=[b_dn])
                P.dve(lambda e: e.tensor_tensor(out=dn[:], in0=dn[:], in1=o_[:, 128:129], op=ALU.max), reads=[b_dn, b_o], writes=[b_dn])
                P.dve(lambda e: e.reciprocal(out=dn[:], in_=dn[:]), reads=[b_dn], writes=[b_dn])
                P.dve(lambda e: e.tensor_scalar(out=hh[:], in0=o_[:, 0:128], scalar1=dn[:, 0:1], scalar2=None, op0=ALU.mult), reads=[b_o, b_dn], writes=[b_h])
                src, b_src = hh, b_h
            else:
                src, b_src = o_, b_o
            if dr == 0:
                P.dma(lambda e: e.dma_start(out=o1[h, cs_, :], in_=src[:, 0:128]), reads=[b_src], writes=[b_o1[h][n]])
            else:
                hf, b_hf = hfs[i]
                ga, b_ga = gas[i]
                P.pool(lambda e: e.tensor_tensor(out=hf[:], in0=hf[:], in1=src[:, 0:128], op=ALU.add), reads=[b_hf, b_src], writes=[b_hf])
                P.dve(lambda e: e.bn_stats(out=st6[:], in_=hf[:]), reads=[b_hf], writes=[b_st6])
                P.dve(lambda e: e.bn_aggr(out=mv[:], in_=st6[:]), reads=[b_st6], writes=[b_mv])
                P.act(lambda e: e.activation(out=rsd[:], in_=mv[:, 1:2], func=AF.Ln, scale=1.0, bias=epsc[:, 0:1]), reads=[b_mv, b_epsc], writes=[b_rsd])
                P.act(lambda e: e.activation(out=rsd[:], in_=rsd[:], func=AF.Exp, scale=-0.5), reads=[b_rsd], writes=[b_rsd])
                P.dve(lambda e: e.tensor_scalar(out=hf[:], in0=hf[:], scalar1=mv[:, 0:1], scalar2=rsd[:, 0:1], op0=ALU.subtract, op1=ALU.mult), reads=[b_hf, b_mv, b_rsd], writes=[b_hf])
                ptr, b_ptr = p_tr[0]
                P.pe(lambda e: e.transpose(ptr[:], hf[:], idt[:]), reads=[b_hf, b_id], writes=[b_ptr])
                ot, b_ot = outs[i]
                P.dve(lambda e: e.tensor_tensor(out=ot[:], in0=ptr[:], in1=ga[:], op=ALU.mult), reads=[b_ptr, b_ga], writes=[b_ot])
                P.dma(lambda e: e.dma_start(out=mixT[out_row + h * 128:out_row + (h + 1) * 128, cs_], in_=ot[:]), reads=[b_ot], writes=[Buf()])

        for k in range(len(items) + SK):
            if k < len(items):
                phaseA(k)
            if k - SK >= 0:
                phaseB(k - SK)
    C.end()


PI = float(np.pi)


def _wrap(P, C, x, b_x, shape, add=0.0, scr=None, key="a"):
    if scr is not None and ("u" + key) in scr:
        (u, b_u), (ki, b_ki), (kf, b_kf), (y, b_y), (m, b_m) = [scr[n + key] for n in "uikym"]
    else:
        u, b_u = C.sb(shape)
        ki, b_ki = C.sb(shape, I32)
        kf, b_kf = C.sb(shape)
        y, b_y = C.sb(shape)
        m, b_m = C.sb(shape)
        if scr is not None:
            for n, v in zip("uikym", ((u, b_u), (ki, b_ki), (kf, b_kf), (y, b_y), (m, b_m))):
                scr[n + key] = v
    P.dve(lambda e: e.tensor_scalar(out=u[:], in0=x[:], scalar1=add, scalar2=1.0 / (2 * PI), op0=ALU.add, op1=ALU.mult), reads=[b_x], writes=[b_u])
    P.dve(lambda e: e.tensor_copy(out=ki[:], in_=u[:]), reads=[b_u], writes=[b_ki])
    P.dve(lambda e: e.tensor_copy(out=kf[:], in_=ki[:]), reads=[b_ki], writes=[b_kf])
    P.dve(lambda e: e.tensor_scalar(out=u[:], in0=x[:], scalar1=add, scalar2=None, op0=ALU.add), reads=[b_x, b_kf], writes=[b_u])
    P.dve(lambda e: e.scalar_tensor_tensor(out=y[:], in0=kf[:], scalar=-2 * PI, in1=u[:], op0=ALU.mult, op1=ALU.add), reads=[b_kf, b_u], writes=[b_y])
    P.dve(lambda e: e.tensor_scalar(out=m[:], in0=y[:], scalar1=PI, scalar2=-2 * PI, op0=ALU.is_gt, op1=ALU.mult), reads=[b_y], writes=[b_m])
    P.dve(lambda e: e.tensor_tensor(out=y[:], in0=y[:], in1=m[:], op=ALU.add), reads=[b_y, b_m], writes=[b_y])
    P.dve(lambda e: e.tensor_scalar(out=m[:], in0=y[:], scalar1=-PI, scalar2=2 * PI, op0=ALU.is_lt, op1=ALU.mult), reads=[b_y], writes=[b_m])
    P.dve(lambda e: e.tensor_tensor(out=y[:], in0=y[:], in1=m[:], op=ALU.add), reads=[b_y, b_m], writes=[b_y])
    P.dve(lambda e: e.tensor_scalar(out=y[:], in0=y[:], scalar1=-PI, scalar2=PI, op0=ALU.max, op1=ALU.min), reads=[b_y], writes=[b_y])
    return y, b_y


def stage_s5(C, pT, prm, consts, yf, mixT, L, u_row, out_row):
    P = C.P
    T = min(512, L)
    NBK = L // T
    for dr in range(2):
        for ct in range(4):
            C.begin()
            idt, b_id = C.sb([128, 128])
            psw, b_psw = C.sb([128, 128])
            tau, b_tau = C.sb([128, T])
            ks, b_ks = C.sb([128, 3])
            lst, b_lst = C.sb([128, 64])
            dsk, b_dsk = C.sb([128, 4])
            onesT, b_onesT = C.sb([128, T])
            P.dma(lambda e: e.dma_start(out=idt[:], in_=consts["ident"]), writes=[b_id])
            P.dma(lambda e: e.dma_start(out=psw[:], in_=consts["psw"]), writes=[b_psw])
            P.dma(lambda e: e.dma_start(out=tau[:], in_=consts["tau"]), writes=[b_tau])
            P.dma(lambda e: e.dma_start(out=ks[:], in_=consts["ksel"]), writes=[b_ks])
            P.dma(lambda e: e.dma_start(out=lst[:], in_=prm["lstep"]), writes=[b_lst])
            P.dma(lambda e: e.dma_start(out=dsk[:], in_=prm["dsk"]), writes=[b_dsk])
            P.dve(lambda e: e.memset(onesT[:], 1.0), writes=[b_onesT])
            G = []
            scr = {}
            ang, b_ang = C.sb([128, T])
            are2, b_are2 = C.sb([128, 64])
            aim2, b_aim2 = C.sb([128, 64])
            P.dma(lambda e: e.dma_start(out=are2[:], in_=prm['are2']), writes=[b_are2])
            P.dma(lambda e: e.dma_start(out=aim2[:], in_=prm['aim2']), writes=[b_aim2])
            ptr = [C.ps([128, 128]) for _ in range(2)]
            for gp in range(8):
                g = ct * 8 + gp
                are, b_are = C.sb([128, 1])
                aim, b_aim = C.sb([128, 1])
                P.dve(lambda e, are=are, cg=dr * 32 + g: e.tensor_copy(out=are[:], in_=are2[:, cg:cg + 1]), reads=[b_are2], writes=[b_are])
                P.dve(lambda e, aim=aim, cg=dr * 32 + g: e.tensor_copy(out=aim[:], in_=aim2[:, cg:cg + 1]), reads=[b_aim2], writes=[b_aim])
                dl, b_dl = C.sb([128, 1])
                r, b_r = C.sb([128, 1])
                th, b_th = C.sb([128, 1])
                col = dr * 32 + g
                P.act(lambda e, dl=dl, col=col: e.activation(out=dl[:], in_=lst[:, col:col + 1], func=AF.Exp), reads=[b_lst], writes=[b_dl])
                P.act(lambda e, r=r, are=are, dl=dl: e.activation(out=r[:], in_=are[:], func=AF.Exp, scale=dl[:, 0:1]), reads=[b_are, b_dl], writes=[b_r])
                P.dve(lambda e, th=th, aim=aim, dl=dl: e.tensor_tensor(out=th[:], in0=aim[:], in1=dl[:], op=ALU.mult), reads=[b_aim, b_dl], writes=[b_th])
                thr0, b_thr0 = _wrap(P, C, th, b_th, [128, 1], scr=scr, key='c')
                thr, b_thr = C.sb([128, 1])
                P.dve(lambda e, thr=thr, thr0=thr0: e.tensor_copy(out=thr[:], in_=thr0[:]), reads=[b_thr0], writes=[b_thr])
                thc, b_thc = _wrap(P, C, thr, b_thr, [128, 1], add=PI / 2, scr=scr, key='d')
                s0, b_s0 = C.sb([128, 1])
                c0, b_c0 = C.sb([128, 1])
                P.act(lambda e, s0=s0, thr=thr: e.activation(out=s0[:], in_=thr[:], func=AF.Sin), reads=[b_thr], writes=[b_s0])
                P.act(lambda e, c0=c0, thc=thc: e.activation(out=c0[:], in_=thc[:], func=AF.Sin), reads=[b_thc], writes=[b_c0])
                nre, b_nre = C.sb([128, 1])
                nim, b_nim = C.sb([128, 1])
                den, b_den = C.sb([128, 1])
                t0, b_t0 = C.sb([128, 1])
                kre, b_kre = C.sb([128, 1])
                kim, b_kim = C.sb([128, 1])
                P.dve(lambda e, nre=nre, r=r, c0=c0: e.tensor_tensor(out=nre[:], in0=r[:], in1=c0[:], op=ALU.mult), reads=[b_r, b_c0], writes=[b_nre])
                P.dve(lambda e, nre=nre: e.tensor_scalar(out=nre[:], in0=nre[:], scalar1=-1.0, scalar2=None, op0=ALU.add), reads=[b_nre], writes=[b_nre])
                P.dve(lambda e, nim=nim, r=r, s0=s0: e.tensor_tensor(out=nim[:], in0=r[:], in1=s0[:], op=ALU.mult), reads=[b_r, b_s0], writes=[b_nim])
                P.dve(lambda e, den=den, are=are: e.tensor_tensor(out=den[:], in0=are[:], in1=are[:], op=ALU.mult), reads=[b_are], writes=[b_den])
                P.dve(lambda e, den=den, aim=aim: e.scalar_tensor_tensor(out=den[:], in0=aim[:], scalar=aim[:, 0:1], in1=den[:], op0=ALU.mult, op1=ALU.add), reads=[b_aim, b_den], writes=[b_den])
                P.dve(lambda e, den=den: e.reciprocal(out=den[:], in_=den[:]), reads=[b_den], writes=[b_den])
                P.dve(lambda e, t0=t0, nre=nre, are=are: e.tensor_tensor(out=t0[:], in0=nre[:], in1=are[:], op=ALU.mult), reads=[b_nre, b_are], writes=[b_t0])
                P.dve(lambda e, kre=kre, nim=nim, aim=aim, t0=t0: e.scalar_tensor_tensor(out=kre[:], in0=nim[:], scalar=aim[:, 0:1], in1=t0[:], op0=ALU.mult, op1=ALU.add), reads=[b_nim, b_aim, b_t0], writes=[b_kre])
                P.dve(lambda e, kre=kre, den=den: e.tensor_tensor(out=kre[:], in0=kre[:], in1=den[:], op=ALU.mult), reads=[b_kre, b_den], writes=[b_kre])
                P.dve(lambda e, t0=t0, nre=nre, aim=aim: e.tensor_tensor(out=t0[:], in0=nre[:], in1=aim[:], op=ALU.mult), reads=[b_nre, b_aim], writes=[b_t0])
                P.dve(lambda e, kim=kim, nim=nim, are=are, t0=t0: e.scalar_tensor_tensor(out=kim[:], in0=nim[:], scalar=are[:, 0:1], in1=t0[:], op0=ALU.mult, op1=ALU.subtract), reads=[b_nim, b_are, b_t0], writes=[b_kim])
                P.dve(lambda e, kim=kim, den=den: e.tensor_tensor(out=kim[:], in0=kim[:], in1=den[:], op=ALU.mult), reads=[b_kim, b_den], writes=[b_kim])
                cA, b_cA = C.sb([128, 1])
                cB, b_cB = C.sb([128, 1])
                cC, b_cC = C.sb([128, 1])
                P.dve(lambda e, cA=cA, kre=kre: e.tensor_tensor(out=cA[:], in0=kre[:], in1=ks[:, 0:1], op=ALU.mult), reads=[b_kre, b_ks], writes=[b_cA])
                P.dve(lambda e, cA=cA, kim=kim: e.scalar_tensor_tensor(out=cA[:], in0=kim[:], scalar=ks[:, 1:2], in1=cA[:], op0=ALU.mult, op1=ALU.add), reads=[b_kim, b_ks, b_cA], writes=[b_cA])
                P.dve(lambda e, cB=cB, kre=kre: e.tensor_tensor(out=cB[:], in0=kre[:], in1=ks[:, 1:2], op=ALU.mult), reads=[b_kre, b_ks], writes=[b_cB])
                P.dve(lambda e, cB=cB, kim=kim: e.scalar_tensor_tensor(out=cB[:], in0=kim[:], scalar=ks[:, 0:1], in1=cB[:], op0=ALU.mult, op1=ALU.subtract), reads=[b_kim, b_ks, b_cB], writes=[b_cB])
                P.dve(lambda e, cC=cC, cB=cB: e.tensor_copy(out=cC[:], in_=cB[:]), reads=[b_cB], writes=[b_cC])
                P.dve(lambda e, cB=cB, cC=cC: e.tensor_scalar(out=cB[:], in0=cC[:], scalar1=-1.0, scalar2=None, op0=ALU.mult), reads=[b_cC], writes=[b_cB])
                P.dve(lambda e, thr=thr: e.tensor_scalar(out=ang[:], in0=tau[:], scalar1=thr[:, 0:1], scalar2=None, op0=ALU.mult), reads=[b_tau, b_thr], writes=[b_ang])
                aw, b_aw = _wrap(P, C, ang, b_ang, [128, T], scr=scr, key='A')
                ac, b_ac = _wrap(P, C, aw, b_aw, [128, T], add=PI / 2, scr=scr, key='B')
                St, b_St = C.sb([128, T])
                Ct, b_Ct = C.sb([128, T])
                Rb, b_Rb = C.sb([128, T])
                P.act(lambda e, St=St, aw=aw: e.activation(out=St[:], in_=aw[:], func=AF.Sin), reads=[b_aw], writes=[b_St])
                P.act(lambda e, Ct=Ct, ac=ac: e.activation(out=Ct[:], in_=ac[:], func=AF.Sin), reads=[b_ac], writes=[b_Ct])
                P.dve(lambda e, Rb=Rb, r=r: e.tensor_scalar(out=Rb[:], in0=onesT[:], scalar1=r[:, 0:1], scalar2=None, op0=ALU.mult), reads=[b_onesT, b_r], writes=[b_Rb])
                bre, b_bre = C.sb([128, 16])
                bim, b_bim = C.sb([128, 16])
                for half in range(2):
                    P.dma(lambda e, half=half, bre=bre, g=g: e.dma_start(out=bre[half * 64:(half + 1) * 64, :], in_=prm["b_re"][dr, g, :, :]), writes=[b_bre])
                    P.dma(lambda e, half=half, bim=bim, g=g: e.dma_start(out=bim[half * 64:(half + 1) * 64, :], in_=prm["b_im"][dr, g, :, :]), writes=[b_bim])
                BT = []
                for (c1, b_c1, c2, b_c2) in ((cA, b_cA, cB, b_cB), (cC, b_cC, cA, b_cA)):
                    bp, b_bp = C.sb([128, 128])
                    tt, b_tt = C.sb([128, 16])
                    P.pool(lambda e, bp=bp: e.memset(bp[:], 0.0), writes=[b_bp])
                    P.dve(lambda e, tt=tt, c1=c1, bre=bre: e.tensor_scalar(out=tt[:], in0=bre[:], scalar1=c1[:, 0:1], scalar2=None, op0=ALU.mult), reads=[b_bre, b_c1], writes=[b_tt])
                    P.dve(lambda e, bp=bp, tt=tt, c2=c2, gp=gp, bim=bim: e.scalar_tensor_tensor(out=bp[:, gp * 16:(gp + 1) * 16], in0=bim[:], scalar=c2[:, 0:1], in1=tt[:], op0=ALU.mult, op1=ALU.add),
                          reads=[b_bim, b_c2, b_tt, b_bp], writes=[b_bp])
                    pt, b_pt = ptr[0]
                    bT, b_bT = C.sb([128, 128], BF16)
                    P.pe(lambda e, pt=pt, bp=bp: e.transpose(pt[:], bp[:], idt[:]), reads=[b_bp, b_id], writes=[b_pt])
                    P.act(lambda e, bT=bT, pt=pt: e.activation(out=bT[:], in_=pt[:], func=AF.Copy), reads=[b_pt], writes=[b_bT])
                    BT.append((bT, b_bT))
                cc1, b_cc1 = C.sb([16, 128])
                cc2, b_cc2 = C.sb([16, 128])
                P.dma(lambda e, cc1=cc1, g=g: e.dma_start(out=cc1[:, 0:64], in_=prm["c_re"][dr, g, :, :]), writes=[b_cc1])
                P.dma(lambda e, cc1=cc1, g=g: e.dma_start(out=cc1[:, 64:128], in_=prm["c_im"][dr, g, :, :]), writes=[b_cc1])
                P.dma(lambda e, cc2=cc2, g=g: e.dma_start(out=cc2[:, 0:64], in_=prm["c_im"][dr, g, :, :]), writes=[b_cc2])
                P.dma(lambda e, cc2=cc2, g=g: e.dma_start(out=cc2[:, 64:128], in_=prm["c_re"][dr, g, :, :]), writes=[b_cc2])
                cm1, b_cm1 = C.sb([128, 128], BF16)
                cm2, b_cm2 = C.sb([128, 128], BF16)
                P.pool(lambda e, cm1=cm1: e.memset(cm1[:], 0.0), writes=[b_cm1])
                P.pool(lambda e, cm2=cm2: e.memset(cm2[:], 0.0), writes=[b_cm2])
                pt, b_pt = ptr[1]
                P.pe(lambda e, pt=pt, cc1=cc1: e.transpose(pt[:, 0:16], cc1[:], idt[0:16, 0:16]), reads=[b_cc1, b_id], writes=[b_pt])
                P.dve(lambda e, cm1=cm1, pt=pt, gp=gp: e.tensor_scalar(out=cm1[:, gp * 16:(gp + 1) * 16], in0=pt[:, 0:16], scalar1=ks[:, 2:3], scalar2=None, op0=ALU.mult),
                      reads=[b_pt, b_ks, b_cm1], writes=[b_cm1])
                P.pe(lambda e, pt=pt, cc2=cc2: e.transpose(pt[:, 0:16], cc2[:], idt[0:16, 0:16]), reads=[b_cc2, b_id], writes=[b_pt])
                P.dve(lambda e, cm2=cm2, pt=pt, gp=gp: e.tensor_scalar(out=cm2[:, gp * 16:(gp + 1) * 16], in0=pt[:, 0:16], scalar1=-1.0, scalar2=None, op0=ALU.mult),
                      reads=[b_pt, b_cm2], writes=[b_cm2])
                q_, b_q = C.sb([128, 1])
                rt, b_rt = C.sb([128, 128])
                P.dve(lambda e, q_=q_, St=St: e.tensor_tensor(out=q_[:], in0=St[:, T - 1:T], in1=ks[:, 2:3], op=ALU.mult), reads=[b_St, b_ks], writes=[b_q])
                P.dve(lambda e, rt=rt, Ct=Ct: e.tensor_scalar(out=rt[:], in0=idt[:], scalar1=Ct[:, T - 1:T], scalar2=None, op0=ALU.mult), reads=[b_id, b_Ct], writes=[b_rt])
                P.dve(lambda e, rt=rt, q_=q_: e.scalar_tensor_tensor(out=rt[:], in0=psw[:], scalar=q_[:, 0:1], in1=rt[:], op0=ALU.mult, op1=ALU.add), reads=[b_psw, b_q, b_rt], writes=[b_rt])
                carry, b_carry = C.sb([128, 1])
                P.dve(lambda e, carry=carry: e.memset(carry[:], 0.0), writes=[b_carry])
                G.append(dict(St=(St, b_St), Ct=(Ct, b_Ct), Rb=(Rb, b_Rb), B1=BT[0], B2=BT[1], C1=(cm1, b_cm1), C2=(cm2, b_cm2), RT=(rt, b_rt), carry=(carry, b_carry)))
            uts = [C.sb([128, T]) for _ in range(2)]
            utbs = [C.sb([128, T], BF16) for _ in range(2)]
            pbs = [C.ps([128, T]) for _ in range(2)]
            pss_ = [C.ps([128, T]) for _ in range(2)]
            py, b_py = C.ps([128, T])
            pc, b_pc = C.ps([128, 1])
            NB3 = 3
            t1s = [C.sb([128, T]) for _ in range(NB3)]
            t2s = [C.sb([128, T]) for _ in range(NB3)]
            bps = [C.sb([128, T]) for _ in range(NB3)]
            ws_ = [C.sb([128, T]) for _ in range(NB3)]
            wcs = [C.sb([128, T], BF16) for _ in range(NB3)]
            wss = [C.sb([128, T], BF16) for _ in range(NB3)]
            yos = [C.sb([128, T]) for _ in range(2)]
            yfs = [C.sb([128, T]) for _ in range(2)]
            blocks = list(range(NBK)) if dr == 0 else list(range(NBK - 1, -1, -1))
            rows = slice(u_row + ct * 128, u_row + (ct + 1) * 128)
            rv = (lambda a: a[:, ::-1]) if dr == 1 else (lambda a: a[:])
            items = [(bi, bk, gp) for bi, bk in enumerate(blocks) for gp in range(8)]
            st_ = {}
            SK = 2
            NB4 = 4
            bps = [C.sb([128, T]) for _ in range(NB4)]

            def phaseA(k):
                bi, bk, gp = items[k]
                cs_ = slice(bk * T, (bk + 1) * T)
                if gp == 0:
                    ut, b_ut = uts[bi % 2]
                    utb, b_utb = utbs[bi % 2]
                    P.dma(lambda e, ut=ut, cs_=cs_: e.dma_start(out=ut[:], in_=pT[rows, cs_]), writes=[b_ut])
                    P.act(lambda e, ut=ut, utb=utb: e.activation(out=utb[:], in_=ut[:], func=AF.Copy), reads=[b_ut], writes=[b_utb])
                utb, b_utb = utbs[bi % 2]
                gd = G[gp]
                pb, b_pb = pbs[k % 2]
                pq, b_pq = pss_[k % 2]
                t1, b_t1 = t1s[k % 2]
                t2, b_t2 = t2s[k % 2]
                bp, b_bp = bps[k % NB4]
                P.pe(lambda e, pb=pb, gd=gd, utb=utb: e.matmul(pb[:], gd["B1"][0][:], utb[:], start=True, stop=True), reads=[gd["B1"][1], b_utb], writes=[b_pb])
                P.pe(lambda e, pq=pq, gd=gd, utb=utb: e.matmul(pq[:], gd["B2"][0][:], utb[:], start=True, stop=True), reads=[gd["B2"][1], b_utb], writes=[b_pq])
                P.dve(lambda e, t1=t1, pb=pb, gd=gd: e.tensor_tensor(out=t1[:], in0=rv(pb), in1=gd["Ct"][0][:], op=ALU.mult), reads=[b_pb, gd["Ct"][1]], writes=[b_t1])
                P.dve(lambda e, t2=t2, pq=pq, gd=gd: e.tensor_tensor(out=t2[:], in0=rv(pq), in1=gd["St"][0][:], op=ALU.mult), reads=[b_pq, gd["St"][1]], writes=[b_t2])
                P.pool(lambda e, bp=bp, t1=t1, t2=t2: e.tensor_tensor(out=bp[:], in0=t1[:], in1=t2[:], op=ALU.add), reads=[b_t1, b_t2], writes=[b_bp])

            def phaseB(k):
                bi, bk, gp = items[k]
                cs_ = slice(bk * T, (bk + 1) * T)
                gd = G[gp]
                bp, b_bp = bps[k % NB4]
                w_, b_w = ws_[k % 3]
                wc, b_wc = wcs[k % 3]
                wsn, b_wsn = wss[k % 3]
                P.dve(lambda e, w_=w_, bp=bp, gd=gd: e.tensor_tensor_scan(out=w_[:], data0=gd["Rb"][0][:], data1=bp[:], initial=gd["carry"][0][:, 0:1], op0=ALU.mult, op1=ALU.add),
                      reads=[gd["Rb"][1], b_bp, gd["carry"][1]], writes=[b_w])
                P.pool(lambda e, wc=wc, w_=w_, gd=gd: e.tensor_tensor(out=wc[:], in0=w_[:], in1=gd["Ct"][0][:], op=ALU.mult), reads=[b_w, gd["Ct"][1]], writes=[b_wc])
                P.dve(lambda e, wsn=wsn, w_=w_, gd=gd: e.tensor_tensor(out=wsn[:], in0=w_[:], in1=gd["St"][0][:], op=ALU.mult), reads=[b_w, gd["St"][1]], writes=[b_wsn])
                P.pe(lambda e, gd=gd, wc=wc, gp=gp: e.matmul(py[:], gd["C1"][0][:], wc[:], start=(gp == 0), stop=False), reads=[gd["C1"][1], b_wc], writes=[b_py])
                P.pe(lambda e, gd=gd, wsn=wsn, gp=gp: e.matmul(py[:], gd["C2"][0][:], wsn[:], start=False, stop=(gp == 7)), reads=[gd["C2"][1], b_wsn], writes=[b_py])
                P.pe(lambda e, gd=gd, w_=w_: e.matmul(pc[:], gd["RT"][0][:], w_[:, T - 1:T], start=True, stop=True), reads=[gd["RT"][1], b_w], writes=[b_pc])
                P.act(lambda e, gd=gd: e.activation(out=gd["carry"][0][:], in_=pc[:], func=AF.Copy), reads=[b_pc], writes=[gd["carry"][1]])
                if gp == 7:
                    ut, b_ut = uts[bi % 2]
                    yo, b_yo = yos[bi % 2]
                    if dr == 0:
                        P.act(lambda e, yo=yo: e.activation(out=yo[:], in_=py[:], func=AF.Copy), reads=[b_py], writes=[b_yo])
                        P.dma(lambda e, yo=yo, cs_=cs_: e.dma_start(out=yf[ct * 128:(ct + 1) * 128, cs_], in_=yo[:]), reads=[b_yo], writes=[Buf()])
                    else:
                        yft, b_yft = yfs[bi % 2]
                        P.dma(lambda e, yft=yft, cs_=cs_: e.dma_start(out=yft[:], in_=yf[ct * 128:(ct + 1) * 128, cs_]), writes=[b_yft])
                        P.dve(lambda e, yo=yo, yft=yft: e.tensor_tensor(out=yo[:], in0=py[:, ::-1], in1=yft[:], op=ALU.add), reads=[b_py, b_yft], writes=[b_yo])
                        P.dve(lambda e, yo=yo, ut=ut: e.scalar_tensor_tensor(out=yo[:], in0=ut[:], scalar=dsk[:, ct:ct + 1], in1=yo[:], op0=ALU.mult, op1=ALU.add), reads=[b_ut, b_dsk, b_yo], writes=[b_yo])
                        P.dma(lambda e, yo=yo, cs_=cs_: e.dma_start(out=mixT[out_row + ct * 128:out_row + (ct + 1) * 128, cs_], in_=yo[:]), reads=[b_yo], writes=[Buf()])

            for k in range(len(items) + SK):
                if k < len(items):
                    phaseA(k)
                if k - SK >= 0:
                    phaseB(k - SK)
            C.end()


def stage_out(C, xT, mixT, w_out, gluw, glub, g8, router, ident, x1T, htok, aff, L, even):
    P = C.P
    C.begin()
    TB = min(512, L)
    NB = L // TB
    NTS = TB // 128
    KC = 8 if even else 6
    woutb, _ = C.sb([128, KC, 1024], BF16)
    b_wo = [Buf() for _ in range(KC)]
    for kc in range(KC):
        for c0 in range(0, 1024, 512):
            P.dma(lambda e, kc=kc, c0=c0: e.dma_start(out=woutb[:, kc, c0:c0 + 512], in_=w_out[kc * 128:(kc + 1) * 128, c0:c0 + 512]), writes=[b_wo[kc]], q="pool")
    if even:
        gluwb, b_gw = C.sb([128, 4, 512], BF16)
        for kc in range(4):
            P.dma(lambda e, kc=kc: e.dma_start(out=gluwb[:, kc, :], in_=gluw[kc * 128:(kc + 1) * 128, :]), writes=[b_gw], q="pool")
        gb, b_gb = C.sb([128, 4])
        P.dma(lambda e: e.dma_start(out=gb[:], in_=glub), writes=[b_gb])
        ngb, b_ngb = C.sb([128, 4])
        P.dve(lambda e: e.tensor_scalar(out=ngb[:], in0=gb[:], scalar1=-1.0, scalar2=None, op0=ALU.mult), reads=[b_gb], writes=[b_ngb])
    gt, b_g = C.sb([128, 8])
    wr, b_wr = C.sb([128, 8, 16])
    idt, b_id = C.sb([128, 128])
    ones, b_ones = C.sb([128, 128], BF16)
    P.dma(lambda e: e.dma_start(out=gt[:], in_=g8), writes=[b_g])
    P.dma(lambda e: e.dma_start(out=wr[:], in_=router.rearrange("(kc p) n -> p kc n", p=128)), writes=[b_wr])
    P.dma(lambda e: e.dma_start(out=idt[:], in_=ident), writes=[b_id])
    P.dve(lambda e: e.memset(ones[:], 1.0), writes=[b_ones])
    epsc, b_epsc = C.sb([128, 1])
    P.dve(lambda e: e.memset(epsc[:], EPS), writes=[b_epsc])
    mix, b_mix = C.sb([128, KC, TB])
    mixb, b_mixb = C.sb([128, KC, TB], BF16)
    xt, b_xt = C.sb([128, 8, TB])
    x1t, b_x1 = C.sb([128, 8, TB])
    ht, b_ht = C.sb([128, 8, TB])
    sq, b_sq = C.sb([128, 8, TB], BF16)
    rs, b_rs = C.sb([128, TB])
    yg, b_yg = C.sb([128, 4, TB])
    ygb, b_ygb = C.sb([128, 4, TB], BF16)
    sg, b_sg = C.sb([128, TB])
    hrows = [C.sb([128, 1024]) for _ in range(2)]
    afts = [C.sb([128, 16]) for _ in range(2)]
    ex, b_ex = C.sb([128, 16])
    mx, b_mx = C.sb([128, 1])
    sm, b_sm = C.sb([128, 1])
    pxs = [C.ps([128, TB]) for _ in range(2)]
    pss, b_pss = C.ps([128, TB])
    pz, b_pz = C.ps([128, TB])
    pl, b_pl = C.ps([128, 16])
    ptrs = [C.ps([128, 128]) for _ in range(2)]
    xv = xT.rearrange("(kc p) n -> p kc n", p=128)
    mv = mixT.rearrange("(kc p) n -> p kc n", p=128)
    x1v = x1T.rearrange("(kc p) n -> p kc n", p=128)
    nt = 0
    for tb in range(NB):
        sl = slice(tb * TB, (tb + 1) * TB)
        P.dma(lambda e, sl=sl: e.dma_start(out=mix[:], in_=mv[:, 0:KC, sl]), writes=[b_mix])
        P.dma(lambda e, sl=sl: e.dma_start(out=xt[:], in_=xv[:, :, sl]), writes=[b_xt])
        if even:
            P.act(lambda e: e.activation(out=mixb[:, 0:4, :], in_=mix[:, 0:4, :], func=AF.Copy), reads=[b_mix], writes=[b_mixb])
            P.act(lambda e: e.activation(out=yg[:], in_=mix[:, 4:8, :], func=AF.Gelu), reads=[b_mix], writes=[b_yg])
            P.dve(lambda e: e.tensor_copy(out=ygb[:], in_=yg[:]), reads=[b_yg], writes=[b_ygb])
            for ct in range(4):
                for kc in range(4):
                    P.pe(lambda e, ct=ct, kc=kc: e.matmul(pz[:], gluwb[:, kc, ct * 128:(ct + 1) * 128], ygb[:, kc, :], start=(kc == 0), stop=(kc == 3)),
                         reads=[b_gw, b_ygb], writes=[b_pz])
                P.act(lambda e, ct=ct: e.activation(out=sg[:], in_=pz[:], func=AF.Exp, scale=-1.0, bias=ngb[:, ct:ct + 1]), reads=[b_pz, b_ngb], writes=[b_sg])
                P.pool(lambda e: e.tensor_scalar(out=sg[:], in0=sg[:], scalar1=1.0, scalar2=None, op0=ALU.add), reads=[b_sg], writes=[b_sg])
                P.dve(lambda e: e.reciprocal(out=sg[:], in_=sg[:]), reads=[b_sg], writes=[b_sg])
                P.dve(lambda e, ct=ct: e.tensor_tensor(out=mixb[:, 4 + ct, :], in0=yg[:, ct, :], in1=sg[:], op=ALU.mult), reads=[b_yg, b_sg], writes=[b_mixb])
        else:
            P.act(lambda e: e.activation(out=mixb[:], in_=mix[:], func=AF.Copy), reads=[b_mix], writes=[b_mixb])
        for dt in range(8):
            px, b_px = pxs[dt % 2]
            for kc in range(KC):
                P.pe(lambda e, px=px, dt=dt, kc=kc: e.matmul(px[:], woutb[:, kc, dt * 128:(dt + 1) * 128], mixb[:, kc, :], start=(kc == 0), stop=(kc == KC - 1)),
                     reads=[b_wo[kc], b_mixb], writes=[b_px])
            P.dve(lambda e, px=px, dt=dt: e.tensor_tensor(out=x1t[:, dt, :], in0=px[:], in1=xt[:, dt, :], op=ALU.add), reads=[b_px, b_xt], writes=[b_x1])
        P.dma(lambda e, sl=sl: e.dma_start(out=x1v[:, :, sl], in_=x1t[:]), reads=[b_x1], writes=[Buf()])
        P.pool(lambda e: e.tensor_tensor(out=sq[:], in0=x1t[:], in1=x1t[:], op=ALU.mult), reads=[b_x1], writes=[b_sq])
        for kc in range(8):
            P.pe(lambda e, kc=kc: e.matmul(pss[:], ones[:], sq[:, kc, :], start=(kc == 0), stop=(kc == 7)), reads=[b_sq, b_ones], writes=[b_pss])
        P.act(lambda e: e.activation(out=rs[:], in_=pss[:], func=AF.Ln, scale=1.0 / D, bias=epsc[:, 0:1]), reads=[b_pss, b_epsc], writes=[b_rs])
        P.act(lambda e: e.activation(out=rs[:], in_=rs[:], func=AF.Exp, scale=-0.5), reads=[b_rs], writes=[b_rs])
        for dt in range(8):
            P.dve(lambda e, dt=dt: e.scalar_tensor_tensor(out=ht[:, dt, :], in0=x1t[:, dt, :], scalar=gt[:, dt:dt + 1], in1=rs[:], op0=ALU.mult, op1=ALU.mult),
                  reads=[b_x1, b_g, b_rs], writes=[b_ht])
        for ts in range(NTS):
            tsl = slice(ts * 128, (ts + 1) * 128)
            r0 = tb * TB + ts * 128
            for kc in range(8):
                P.pe(lambda e, kc=kc, tsl=tsl: e.matmul(pl[:], ht[:, kc, tsl], wr[:, kc, :], start=(kc == 0), stop=(kc == 7)), reads=[b_ht, b_wr], writes=[b_pl])
            aft, b_aft = afts[nt % 2]
            hrow, b_hrow = hrows[nt % 2]
            nt += 1
            P.dve(lambda e: e.reduce_max(out=mx[:], in_=pl[:], axis=AX.X), reads=[b_pl], writes=[b_mx])
            P.dve(lambda e: e.tensor_scalar(out=mx[:], in0=mx[:], scalar1=-1.0, scalar2=None, op0=ALU.mult), reads=[b_mx], writes=[b_mx])
            P.act(lambda e: e.activation(out=ex[:], in_=pl[:], func=AF.Exp, bias=mx[:, 0:1], accum_out=sm[:]), reads=[b_pl, b_mx], writes=[b_ex, b_sm])
            P.dve(lambda e: e.reciprocal(out=sm[:], in_=sm[:]), reads=[b_sm], writes=[b_sm])
            P.dve(lambda e, aft=aft: e.tensor_scalar(out=aft[:], in0=ex[:], scalar1=sm[:, 0:1], scalar2=None, op0=ALU.mult), reads=[b_ex, b_sm], writes=[b_aft])
            P.dma(lambda e, aft=aft, r0=r0: e.dma_start(out=aff[r0:r0 + 128, :], in_=aft[:]), reads=[b_aft], writes=[Buf()])
            for kc in range(8):
                ptr, b_ptr = ptrs[kc % 2]
                P.pe(lambda e, ptr=ptr, kc=kc, tsl=tsl: e.transpose(ptr[:], ht[:, kc, tsl], idt[:]), reads=[b_ht, b_id], writes=[b_ptr])
                if kc % 2 == 0:
                    P.act(lambda e, ptr=ptr, kc=kc, hrow=hrow: e.activation(out=hrow[:, kc * 128:(kc + 1) * 128], in_=ptr[:], func=AF.Copy), reads=[b_ptr], writes=[b_hrow])
                else:
                    P.dve(lambda e, ptr=ptr, kc=kc, hrow=hrow: e.tensor_copy(out=hrow[:, kc * 128:(kc + 1) * 128], in_=ptr[:]), reads=[b_ptr], writes=[b_hrow])
            P.dma(lambda e, hrow=hrow, r0=r0: e.dma_start(out=htok[r0:r0 + 128, :], in_=hrow[:]), reads=[b_hrow], writes=[Buf()])
    C.end()


BIG = 1.0e6


def _breg(e, rc, val):
    if 'r' not in rc:
        rc['r'] = e.to_reg(val)
    return rc['r']


def stage_route(C, aff, ustrict, rmask, posd, gmd, L):
    P = C.P
    C.begin()
    NJ = L // 128
    CAP = L // 8
    NCOL = 16 * NJ
    A, b_A = C.sb([128, NJ, 16])
    Ae, b_Ae = C.sb([128, 16, NJ])
    for jc in range(0, NJ, 16):
        je = min(NJ, jc + 16)
        P.dma(lambda e, jc=jc, je=je: e.dma_start(out=A[:, jc:je, :], in_=aff[jc * 128:je * 128, :].rearrange("(j p) e -> p j e", p=128)), writes=[b_A])
    P.dve(lambda e: e.tensor_copy(out=Ae[:], in_=A[:].rearrange("p j e -> p e j")), reads=[b_A], writes=[b_Ae])
    us, b_us = C.sb([128, 128])
    rm, b_rm = C.sb([128, NCOL])
    onesf, b_of = C.sb([128, 128])
    P.dma(lambda e: e.dma_start(out=us[:], in_=ustrict), writes=[b_us])
    P.dma(lambda e: e.dma_start(out=rm[:], in_=rmask), writes=[b_rm])
    P.dve(lambda e: e.memset(onesf[:], 1.0), writes=[b_of])
    lo, b_lo = C.sb([128, 16])
    hi, b_hi = C.sb([128, 16])
    mid, b_mid = C.sb([128, 16])
    cnt, b_cnt = C.sb([128, 16])
    ge, b_ge = C.sb([128, 16])
    d1, b_d1 = C.sb([128, 16])
    cmps = [C.sb([128, NJ]) for _ in range(2)]
    ptot, b_ptot = C.ps([128, 16])
    P.dve(lambda e: e.memset(lo[:], 0.0), writes=[b_lo])
    P.dve(lambda e: e.memset(hi[:], 2.0), writes=[b_hi])
    for it in range(34):
        P.dve(lambda e: e.tensor_tensor(out=mid[:], in0=lo[:], in1=hi[:], op=ALU.add), reads=[b_lo, b_hi], writes=[b_mid])
        P.dve(lambda e: e.tensor_scalar(out=mid[:], in0=mid[:], scalar1=0.5, scalar2=None, op0=ALU.mult), reads=[b_mid], writes=[b_mid])
        for ex in range(16):
            cm, b_cm = cmps[ex % 2]
            P.dve(lambda e, ex=ex, cm=cm: e.tensor_scalar(out=cm[:], in0=Ae[:, ex, :], scalar1=mid[:, ex:ex + 1], scalar2=None, op0=ALU.is_ge, op1=ALU.add, accum_out=cnt[:, ex:ex + 1]),
                  reads=[b_Ae, b_mid], writes=[b_cm, b_cnt])
        P.pe(lambda e: e.matmul(ptot[:], onesf[:], cnt[:], start=True, stop=True), reads=[b_of, b_cnt], writes=[b_ptot])
        P.dve(lambda e: e.tensor_scalar(out=ge[:], in0=ptot[:], scalar1=float(CAP) - 0.5, scalar2=None, op0=ALU.is_ge), reads=[b_ptot], writes=[b_ge])
        P.dve(lambda e: e.tensor_tensor(out=d1[:], in0=mid[:], in1=lo[:], op=ALU.subtract), reads=[b_mid, b_lo], writes=[b_d1])
        P.dve(lambda e: e.tensor_tensor(out=d1[:], in0=d1[:], in1=ge[:], op=ALU.mult), reads=[b_d1, b_ge], writes=[b_d1])
        P.dve(lambda e: e.tensor_tensor(out=lo[:], in0=lo[:], in1=d1[:], op=ALU.add), reads=[b_lo, b_d1], writes=[b_lo])
        P.dve(lambda e: e.tensor_tensor(out=d1[:], in0=hi[:], in1=mid[:], op=ALU.subtract), reads=[b_hi, b_mid], writes=[b_d1])
        P.dve(lambda e: e.tensor_tensor(out=d1[:], in0=d1[:], in1=ge[:], op=ALU.mult), reads=[b_d1, b_ge], writes=[b_d1])
        P.dve(lambda e: e.tensor_tensor(out=hi[:], in0=mid[:], in1=d1[:], op=ALU.add), reads=[b_mid, b_d1], writes=[b_hi])
    Me, b_Me = C.sb([128, 16, NJ])
    gm, b_gm = C.sb([128, 16, NJ])
    for ex in range(16):
        P.dve(lambda e, ex=ex: e.tensor_scalar(out=Me[:, ex, :], in0=Ae[:, ex, :], scalar1=lo[:, ex:ex + 1], scalar2=None, op0=ALU.is_ge), reads=[b_Ae, b_lo], writes=[b_Me])
    P.dve(lambda e: e.tensor_tensor(out=gm[:], in0=Ae[:], in1=Me[:], op=ALU.mult), reads=[b_Ae, b_Me], writes=[b_gm])
    Mf = Me[:].rearrange("p e j -> p (e j)")
    pre, b_pre = C.sb([128, NCOL])
    cn, b_cn = C.sb([128, NCOL])
    off, b_off = C.sb([128, NCOL])
    pp, b_pp = C.ps([128, min(512, NCOL)])
    pc, b_pc = C.ps([128, min(512, NCOL)])
    CW = min(512, NCOL)
    for c0 in range(0, NCOL, CW):
        P.pe(lambda e, c0=c0: e.matmul(pp[:], us[:], Mf[:, c0:c0 + CW], start=True, stop=True), reads=[b_us, b_Me], writes=[b_pp])
        P.act(lambda e, c0=c0: e.activation(out=pre[:, c0:c0 + CW], in_=pp[:], func=AF.Copy), reads=[b_pp], writes=[b_pre])
        P.pe(lambda e, c0=c0: e.matmul(pc[:], onesf[:], Mf[:, c0:c0 + CW], start=True, stop=True), reads=[b_of, b_Me], writes=[b_pc])
        P.act(lambda e, c0=c0: e.activation(out=cn[:, c0:c0 + CW], in_=pc[:], func=AF.Copy), reads=[b_pc], writes=[b_cn])
    P.dve(lambda e: e.tensor_tensor_scan(out=off[:], data0=rm[:], data1=cn[:], initial=0.0, op0=ALU.mult, op1=ALU.add), reads=[b_rm, b_cn], writes=[b_off])
    P.dve(lambda e: e.tensor_tensor(out=off[:], in0=off[:], in1=cn[:], op=ALU.subtract), reads=[b_off, b_cn], writes=[b_off])
    P.dve(lambda e: e.tensor_tensor(out=pre[:], in0=pre[:], in1=off[:], op=ALU.add), reads=[b_pre, b_off], writes=[b_pre])
    P.dve(lambda e: e.tensor_scalar(out=pre[:], in0=pre[:], scalar1=-BIG, scalar2=None, op0=ALU.add), reads=[b_pre], writes=[b_pre])
    P.dve(lambda e: e.tensor_tensor(out=pre[:], in0=pre[:], in1=Mf, op=ALU.mult), reads=[b_pre, b_Me], writes=[b_pre])
    P.dve(lambda e: e.tensor_scalar(out=pre[:], in0=pre[:], scalar1=BIG, scalar2=None, op0=ALU.add), reads=[b_pre], writes=[b_pre])
    pi, b_pi = C.sb([128, NCOL], I32)
    P.dve(lambda e: e.tensor_copy(out=pi[:], in_=pre[:]), reads=[b_pre], writes=[b_pi])
    P.dma(lambda e: e.dma_start(out=posd, in_=pi[:]), reads=[b_pi], writes=[Buf()])
    P.dma(lambda e: e.dma_start(out=gmd, in_=gm[:].rearrange("p e j -> p (e j)")), reads=[b_gm], writes=[Buf()])
    C.end()


def stage_dispatch(C, htok, posd, xe, L):
    P = C.P
    C.begin()
    NJ = L // 128
    CAP = L // 8
    pi, b_pi = C.sb([128, 16, NJ], I32)
    P.dma(lambda e: e.dma_start(out=pi[:].rearrange("p e j -> p (e j)"), in_=posd), writes=[b_pi])
    rc = {}
    hts = [C.sb([128, 1024]) for _ in range(2)]
    ixs = [C.sb([128, 1], I32) for _ in range(4)]
    ni = 0
    for j in range(NJ):
        ht, b_ht = hts[j % 2]
        P.dma(lambda e, ht=ht, j=j: e.dma_start(out=ht[:], in_=htok[j * 128:(j + 1) * 128, :]), writes=[b_ht])
        for ex in range(16):
            ix, b_ix = ixs[ni % 4]
            ni += 1
            P.dve(lambda e, ix=ix, ex=ex, j=j: e.tensor_copy(out=ix[:], in_=pi[:, ex, j:j + 1]), reads=[b_pi], writes=[b_ix])
            P.dma(lambda e, ht=ht, ix=ix, ex=ex: e.indirect_dma_start(out=xe[ex][:, :], out_offset=bass.IndirectOffsetOnAxis(ap=ix[:, :], axis=0),
                                                                      in_=ht[:, :], in_offset=None, bounds_check=_breg(e, rc, CAP - 1), oob_is_err=False),
                  reads=[b_ht, b_ix], writes=[Buf()], q="pool")
    C.end()


def stage_ffn(C, xe, w1, w3, w2, ye, ident, L, FF):
    P = C.P
    C.begin()
    CAP = L // 8
    SB = min(512, CAP)
    NSB = CAP // SB
    NST = SB // 128
    NF = FF // 128
    idt, b_id = C.sb([128, 128])
    P.dma(lambda e: e.dma_start(out=idt[:], in_=ident), writes=[b_id])
    w1b, _ = C.sb([128, 8, FF], BF16)
    w3b, _ = C.sb([128, 8, FF], BF16)
    w2b, _ = C.sb([128, NF, 1024], BF16)
    b_w1 = [Buf() for _ in range(8)]
    b_w3 = [Buf() for _ in range(8)]
    b_w2 = [Buf() for _ in range(NF)]
    xrs = [C.sb([128, 1024]) for _ in range(2)]
    xeT, b_xeT = C.sb([128, 8, SB], BF16)
    hid, b_hid = C.sb([128, NF, SB], BF16)
    sas = [C.sb([128, SB]) for _ in range(2)]
    yrows = [C.sb([128, 1024]) for _ in range(2)]
    ptrs = [C.ps([128, 128]) for _ in range(2)]
    pas = [C.ps([128, SB]) for _ in range(2)]
    pbs = [C.ps([128, SB]) for _ in range(2)]
    pys = [C.ps([128, 512]) for _ in range(2)]
    nx = 0
    ny = 0
    NSTG = 3
    stg = [C.sb([128, max(FF, 1024)]) for _ in range(NSTG)]
    ns = 0
    for ex in range(16):
        jobs = []
        for kc in range(8):
            jobs.append((w1[ex, kc * 128:(kc + 1) * 128, :], FF, w1b, kc, b_w1[kc]))
            jobs.append((w3[ex, kc * 128:(kc + 1) * 128, :], FF, w3b, kc, b_w3[kc]))
        for fc in range(NF):
            jobs.append((w2[ex, fc * 128:(fc + 1) * 128, :], 1024, w2b, fc, b_w2[fc]))
        for (src, width, dstt, di, b_dst) in jobs:
            sg_, b_sg = stg[ns % NSTG]
            ns += 1
            P.dma(lambda e, sg_=sg_, src=src, width=width: e.dma_start(out=sg_[:, 0:width], in_=src), writes=[b_sg])
            P.act(lambda e, sg_=sg_, dstt=dstt, di=di, width=width: e.activation(out=dstt[:, di, :], in_=sg_[:, 0:width], func=AF.Copy), reads=[b_sg], writes=[b_dst])
        for sb_ in range(NSB):
            for st in range(NST):
                r0 = sb_ * SB + st * 128
                xr, b_xr = xrs[nx % 2]
                nx += 1
                P.dma(lambda e, xr=xr, ex=ex, r0=r0: e.dma_start(out=xr[:], in_=xe[ex][r0:r0 + 128, :]), writes=[b_xr], q="pool")
                for kc in range(8):
                    ptr, b_ptr = ptrs[kc % 2]
                    P.pe(lambda e, ptr=ptr, xr=xr, kc=kc: e.transpose(ptr[:], xr[:, kc * 128:(kc + 1) * 128], idt[:]), reads=[b_xr, b_id], writes=[b_ptr])
                    if kc % 2 == 0:
                        P.act(lambda e, ptr=ptr, kc=kc, st=st: e.activation(out=xeT[:, kc, st * 128:(st + 1) * 128], in_=ptr[:], func=AF.Copy), reads=[b_ptr], writes=[b_xeT])
                    else:
                        P.dve(lambda e, ptr=ptr, kc=kc, st=st: e.tensor_copy(out=xeT[:, kc, st * 128:(st + 1) * 128], in_=ptr[:]), reads=[b_ptr], writes=[b_xeT])
            for ft in range(NF):
                pa, b_pa = pas[ft % 2]
                pb, b_pb = pbs[ft % 2]
                sa, b_sa = sas[ft % 2]
                for kc in range(8):
                    P.pe(lambda e, pa=pa, kc=kc, ft=ft: e.matmul(pa[:], w1b[:, kc, ft * 128:(ft + 1) * 128], xeT[:, kc, :], start=(kc == 0), stop=(kc == 7)), reads=[b_w1[kc], b_xeT], writes=[b_pa])
                for kc in range(8):
                    P.pe(lambda e, pb=pb, kc=kc, ft=ft: e.matmul(pb[:], w3b[:, kc, ft * 128:(ft + 1) * 128], xeT[:, kc, :], start=(kc == 0), stop=(kc == 7)), reads=[b_w3[kc], b_xeT], writes=[b_pb])
                P.act(lambda e, sa=sa, pa=pa: e.activation(out=sa[:], in_=pa[:], func=AF.Exp, scale=-1.0), reads=[b_pa], writes=[b_sa])
                P.dve(lambda e, sa=sa: e.tensor_scalar(out=sa[:], in0=sa[:], scalar1=1.0, scalar2=None, op0=ALU.add), reads=[b_sa], writes=[b_sa])
                P.dve(lambda e, sa=sa: e.reciprocal(out=sa[:], in_=sa[:]), reads=[b_sa], writes=[b_sa])
                P.dve(lambda e, sa=sa, pa=pa: e.tensor_tensor(out=sa[:], in0=pa[:], in1=sa[:], op=ALU.mult), reads=[b_pa, b_sa], writes=[b_sa])
                P.dve(lambda e, sa=sa, pb=pb, ft=ft: e.tensor_tensor(out=hid[:, ft, :], in0=pb[:], in1=sa[:], op=ALU.mult), reads=[b_pb, b_sa], writes=[b_hid])
            for st in range(NST):
                r0 = sb_ * SB + st * 128
                yrow, b_yrow = yrows[ny % 2]
                ny += 1
                for dh in range(2):
                    py, b_py = pys[dh]
                    for fc in range(NF):
                        P.pe(lambda e, py=py, fc=fc, st=st, dh=dh: e.matmul(py[:], hid[:, fc, st * 128:(st + 1) * 128], w2b[:, fc, dh * 512:(dh + 1) * 512], start=(fc == 0), stop=(fc == NF - 1)),
                             reads=[b_hid, b_w2[fc]], writes=[b_py])
                    if dh == 0:
                        P.act(lambda e, py=py, yrow=yrow: e.activation(out=yrow[:, 0:512], in_=py[:], func=AF.Copy), reads=[b_py], writes=[b_yrow])
                    else:
                        P.dve(lambda e, py=py, yrow=yrow: e.tensor_copy(out=yrow[:, 512:1024], in_=py[:]), reads=[b_py], writes=[b_yrow])
                P.dma(lambda e, yrow=yrow, ex=ex, r0=r0: e.dma_start(out=ye[ex][r0:r0 + 128, :], in_=yrow[:]), reads=[b_yrow], writes=[Buf()], q="pool")
    C.end()


def stage_combine(C, ye, posd, gmd, x1T, ident, x2T, outT, gfin, L):
    P = C.P
    C.begin()
    NJ = L // 128
    CAP = L // 8
    idt, b_id = C.sb([128, 128])
    P.dma(lambda e: e.dma_start(out=idt[:], in_=ident), writes=[b_id])
    pi, b_pi = C.sb([128, 16, NJ], I32)
    gm, b_gm = C.sb([128, 16, NJ])
    P.dma(lambda e: e.dma_start(out=pi[:].rearrange("p e j -> p (e j)"), in_=posd), writes=[b_pi])
    P.dma(lambda e: e.dma_start(out=gm[:].rearrange("p e j -> p (e j)"), in_=gmd), writes=[b_gm])
    Gs = [C.sb([128, 1024]) for _ in range(2)]
    for G_, b_G in Gs:
        P.dve(lambda e, G_=G_: e.memset(G_[:], 0.0), writes=[b_G])
    accs = [C.sb([128, 1024]) for _ in range(2)]
    x1s = [C.sb([128, 8, 128]) for _ in range(2)]
    x2s = [C.sb([128, 8, 128]) for _ in range(2)]
    ptrs = [C.ps([128, 128]) for _ in range(2)]
    if outT is not None:
        gt, b_g = C.sb([128, 8])
        ones, b_ones = C.sb([128, 128], BF16)
        sq, b_sq = C.sb([128, 8, 128], BF16)
        rs, b_rs = C.sb([128, 128])
        ots = [C.sb([128, 8, 128]) for _ in range(2)]
        pss, b_pss = C.ps([128, 128])
        P.dma(lambda e: e.dma_start(out=gt[:], in_=gfin), writes=[b_g])
        P.dve(lambda e: e.memset(ones[:], 1.0), writes=[b_ones])
        epsc, b_epsc = C.sb([128, 1])
        P.dve(lambda e: e.memset(epsc[:], EPS), writes=[b_epsc])
        ov = outT.rearrange("(kc p) n -> p kc n", p=128)
    x1v = x1T.rearrange("(kc p) n -> p kc n", p=128)
    x2v = x2T.rearrange("(kc p) n -> p kc n", p=128)
    ng = 0
    rc = {}
    ixs = [C.sb([128, 1], I32) for _ in range(4)]
    for j in range(NJ):
        acc, b_acc = accs[j % 2]
        x1t, b_x1 = x1s[j % 2]
        x2t, b_x2 = x2s[j % 2]
        cs_ = slice(j * 128, (j + 1) * 128)
        P.dma(lambda e, x1t=x1t, cs_=cs_: e.dma_start(out=x1t[:], in_=x1v[:, :, cs_]), writes=[b_x1])
        for ex in range(16):
            G_, b_G = Gs[ng % 2]
            ix, b_ix = ixs[ng % 4]
            ng += 1
            P.act(lambda e, ix=ix, ex=ex, j=j: e.activation(out=ix[:], in_=pi[:, ex, j:j + 1], func=AF.Copy), reads=[b_pi], writes=[b_ix])
            P.dma(lambda e, G_=G_, ex=ex, ix=ix: e.indirect_dma_start(out=G_[:, :], out_offset=None, in_=ye[ex][:, :],
                                                                      in_offset=bass.IndirectOffsetOnAxis(ap=ix[:, :], axis=0), bounds_check=_breg(e, rc, CAP - 1), oob_is_err=False),
                  reads=[b_ix], writes=[b_G], q="pool")
            if ex == 0:
                P.dve(lambda e, acc=acc, G_=G_, ex=ex, j=j: e.tensor_scalar(out=acc[:], in0=G_[:], scalar1=gm[:, ex, j:j + 1], scalar2=None, op0=ALU.mult), reads=[b_G, b_gm], writes=[b_acc])
            else:
                P.dve(lambda e, acc=acc, G_=G_, ex=ex, j=j: e.scalar_tensor_tensor(out=acc[:], in0=G_[:], scalar=gm[:, ex, j:j + 1], in1=acc[:], op0=ALU.mult, op1=ALU.add),
                      reads=[b_G, b_gm, b_acc], writes=[b_acc])
        for kc in range(8):
            ptr, b_ptr = ptrs[kc % 2]
            P.pe(lambda e, ptr=ptr, acc=acc, kc=kc: e.transpose(ptr[:], acc[:, kc * 128:(kc + 1) * 128], idt[:]), reads=[b_acc, b_id], writes=[b_ptr])
            P.dve(lambda e, ptr=ptr, x2t=x2t, x1t=x1t, kc=kc: e.tensor_tensor(out=x2t[:, kc, :], in0=ptr[:], in1=x1t[:, kc, :], op=ALU.add), reads=[b_ptr, b_x1], writes=[b_x2])
        P.dma(lambda e, x2t=x2t, cs_=cs_: e.dma_start(out=x2v[:, :, cs_], in_=x2t[:]), reads=[b_x2], writes=[Buf()])
        if outT is not None:
            ot, b_ot = ots[j % 2]
            P.pool(lambda e, x2t=x2t: e.tensor_tensor(out=sq[:], in0=x2t[:], in1=x2t[:], op=ALU.mult), reads=[b_x2], writes=[b_sq])
            for kc in range(8):
                P.pe(lambda e, kc=kc: e.matmul(pss[:], ones[:], sq[:, kc, :], start=(kc == 0), stop=(kc == 7)), reads=[b_sq, b_ones], writes=[b_pss])
            P.act(lambda e: e.activation(out=rs[:], in_=pss[:], func=AF.Ln, scale=1.0 / D, bias=epsc[:, 0:1]), reads=[b_pss, b_epsc], writes=[b_rs])
            P.act(lambda e: e.activation(out=rs[:], in_=rs[:], func=AF.Exp, scale=-0.5), reads=[b_rs], writes=[b_rs])
            for kc in range(8):
                P.dve(lambda e, ot=ot, x2t=x2t, kc=kc: e.scalar_tensor_tensor(out=ot[:, kc, :], in0=x2t[:, kc, :], scalar=gt[:, kc:kc + 1], in1=rs[:], op0=ALU.mult, op1=ALU.mult),
                      reads=[b_x2, b_g, b_rs], writes=[b_ot])
            P.dma(lambda e, ot=ot, cs_=cs_: e.dma_start(out=ov[:, :, cs_], in_=ot[:]), reads=[b_ot], writes=[Buf()])
    C.end()


DILS = (1, 4, 16)
DSPAN = (1, 2, 8)


def dil_masks():
    kk = np.arange(128)[:, None]
    qq = np.arange(128)[None, :]
    ms = []
    for g, d in enumerate(DILS):
        for dl in range(-DSPAN[g], DSPAN[g] + 1):
            rel = 128 * dl + kk - qq
            ms.append(((rel % d == 0) & (np.abs(rel) <= 64 * d)).astype(np.float32))
    return np.stack(ms)


def stage_vprep(C, pT, ident, vtok, L, v_row):
    P = C.P
    C.begin()
    TB = min(512, L)
    NT = TB // 128
    idt, b_id = C.sb([128, 128])
    P.dma(lambda e: e.dma_start(out=idt[:], in_=ident), writes=[b_id])
    vts = [C.sb([64, TB]) for _ in range(2)]
    vos = [C.sb([128, NT, 66], BF16) for _ in range(2)]
    for vo, b_vo in vos:
        P.dve(lambda e, vo=vo: e.memset(vo[:], 1.0), writes=[b_vo])
    ptrs = [C.ps([128, 64]) for _ in range(2)]
    it = 0
    for hd in range(12):
        for tb in range(L // TB):
            vt, b_vt = vts[it % 2]
            vo, b_vo = vos[it % 2]
            it += 1
            P.dma(lambda e, vt=vt, hd=hd, tb=tb: e.dma_start(out=vt[:], in_=pT[v_row + hd * 64:v_row + (hd + 1) * 64, tb * TB:(tb + 1) * TB]), writes=[b_vt])
            for t in range(NT):
                ptr, b_ptr = ptrs[t % 2]
                P.pe(lambda e, ptr=ptr, vt=vt, t=t: e.transpose(ptr[:], vt[:, t * 128:(t + 1) * 128], idt[0:64, 0:64]), reads=[b_vt, b_id], writes=[b_ptr])
                if t % 2 == 0:
                    P.act(lambda e, ptr=ptr, vo=vo, t=t: e.activation(out=vo[:, t, 0:64], in_=ptr[:], func=AF.Copy), reads=[b_ptr], writes=[b_vo])
                else:
                    P.dve(lambda e, ptr=ptr, vo=vo, t=t: e.tensor_copy(out=vo[:, t, 0:64], in_=ptr[:]), reads=[b_ptr], writes=[b_vo])
            P.dma(lambda e, vo=vo, hd=hd, tb=tb: e.dma_start(out=vtok[hd][tb * TB:(tb + 1) * TB, :].rearrange("(t p) c -> p t c", p=128), in_=vo[:]), reads=[b_vo], writes=[Buf()])
    C.end()


def stage_qk16(C, pT, qk16, L, nrows):
    P = C.P
    C.begin()
    TB = min(512, L)
    xs = [C.sb([128, TB]) for _ in range(3)]
    ys = [C.sb([128, TB], BF16) for _ in range(3)]
    it = 0
    for rt in range(nrows // 128):
        for tb in range(L // TB):
            x_, b_x = xs[it % 3]
            y_, b_y = ys[it % 3]
            it += 1
            P.dma(lambda e, x_=x_, rt=rt, tb=tb: e.dma_start(out=x_[:], in_=pT[rt * 128:(rt + 1) * 128, tb * TB:(tb + 1) * TB]), writes=[b_x])
            if it % 2 == 0:
                P.act(lambda e, x_=x_, y_=y_: e.activation(out=y_[:], in_=x_[:], func=AF.Copy), reads=[b_x], writes=[b_y])
            else:
                P.dve(lambda e, x_=x_, y_=y_: e.tensor_copy(out=y_[:], in_=x_[:]), reads=[b_x], writes=[b_y])
            P.dma(lambda e, y_=y_, rt=rt, tb=tb: e.dma_start(out=qk16[rt * 128:(rt + 1) * 128, tb * TB:(tb + 1) * TB], in_=y_[:]), reads=[b_y], writes=[Buf()])
    C.end()


def stage_dil(C, pT, ident, masks, vtok, mixT, L, q_row, k_row, out_row):
    P = C.P
    NCH = L // 128
    for i in range(4):
        C.begin()
        idt, b_id = C.sb([128, 128])
        mk, b_mk = C.sb([128, 25, 128], BF16)
        P.dma(lambda e: e.dma_start(out=idt[:], in_=ident), writes=[b_id])
        for m0_ in range(0, 25, 5):
            P.dma(lambda e, m0_=m0_: e.dma_start(out=mk[:, m0_:m0_ + 5, :], in_=masks[m0_:m0_ + 5].rearrange("m p n -> p m n")), writes=[b_mk], q="pool")
        qs = [[C.sb([64, 128], BF16) for _ in range(2)] for _ in range(3)]
        ks = [[C.sb([64, (2 * DSPAN[g] + 1) * 128], BF16) for _ in range(2)] for g in range(3)]
        vs = [[C.sb([128, 2 * DSPAN[g] + 1, 66], BF16) for _ in range(2)] for g in range(3)]
        pss = [C.ps([128, 512]) for _ in range(2)]
        pacc = [C.ps([128, 65]) for _ in range(2)]
        ptr, b_ptr = C.ps([64, 128])
        es = [C.sb([128, 512], BF16) for _ in range(3)]
        pms = [C.sb([128, 512], BF16) for _ in range(3)]
        rd, b_rd = C.sb([128, 1])
        ots = [C.sb([128, 64]) for _ in range(2)]
        oTs = [C.sb([64, 128]) for _ in range(2)]
        moff = (0, 3, 8)
        ngrp = 0
        for n in range(NCH):
            pa, b_pa = pacc[n % 2]
            first = True
            plan = []
            for g in range(3):
                hd = 4 * g + i
                D_ = DSPAN[g]
                m0 = max(0, n - D_)
                m1 = min(NCH - 1, n + D_)
                q_, b_q = qs[g][n % 2]
                k_, b_k = ks[g][n % 2]
                v_, b_v = vs[g][n % 2]
                nm = m1 - m0 + 1
                P.dma(lambda e, q_=q_, hd=hd, n=n: e.dma_start(out=q_[:], in_=pT[q_row + hd * 64:q_row + (hd + 1) * 64, n * 128:(n + 1) * 128]), writes=[b_q])
                P.dma(lambda e, k_=k_, hd=hd, m0=m0, nm=nm: e.dma_start(out=k_[:, 0:nm * 128], in_=pT[k_row + hd * 64:k_row + (hd + 1) * 64, m0 * 128:(m0 + nm) * 128]), writes=[b_k])
                P.dma(lambda e, v_=v_, hd=hd, m0=m0, nm=nm: e.dma_start(out=v_[:, 0:nm, :], in_=vtok[hd][m0 * 128:(m0 + nm) * 128, :].rearrange("(t p) c -> p t c", p=128)), writes=[b_v])
                tiles = [(g, m, m - m0, moff[g] + (m - n) + D_) for m in range(m0, m1 + 1)]
                for c0 in range(0, len(tiles), 4):
                    plan.append((g, tiles[c0:c0 + 4], (q_, b_q), (k_, b_k), (v_, b_v)))
            total = sum(len(p[1]) for p in plan)
            done = 0
            for (g, tl, (q_, b_q), (k_, b_k), (v_, b_v)) in plan:
                ps_, b_ps = pss[ngrp % 2]
                e_, b_e = es[ngrp % 3]
                pm, b_pm = pms[ngrp % 3]
                nt = len(tl)
                for a, (_, m, ml, mi) in enumerate(tl):
                    P.pe(lambda e, ps_=ps_, k_=k_, q_=q_, a=a, ml=ml: e.matmul(ps_[:, a * 128:(a + 1) * 128], k_[:, ml * 128:(ml + 1) * 128], q_[:], start=True, stop=True),
                         reads=[b_k, b_q], writes=[b_ps])
                P.act(lambda e, e_=e_, ps_=ps_, nt=nt: e.activation(out=e_[:, 0:nt * 128], in_=ps_[:, 0:nt * 128], func=AF.Exp, scale=0.125), reads=[b_ps], writes=[b_e])
                mi0 = tl[0][3]
                eng = P.dve if ngrp % 2 == 0 else P.pool
                eng(lambda e, pm=pm, e_=e_, nt=nt, mi0=mi0: e.tensor_tensor(out=pm[:, 0:nt * 128], in0=e_[:, 0:nt * 128], in1=mk[:, mi0:mi0 + nt, :].rearrange("p m n -> p (m n)"), op=ALU.mult),
                    reads=[b_e, b_mk], writes=[b_pm])
                for a, (_, m, ml, mi) in enumerate(tl):
                    done += 1
                    P.pe(lambda e, pa=pa, pm=pm, v_=v_, a=a, ml=ml, st=(done == 1), sp=(done == total): e.matmul(pa[:], pm[:, a * 128:(a + 1) * 128], v_[:, ml, 0:65], start=st, stop=sp),
                         reads=[b_pm, b_v], writes=[b_pa])
                ngrp += 1
            ot, b_ot = ots[n % 2]
            oT, b_oT = oTs[n % 2]
            P.dve(lambda e, pa=pa: e.reciprocal(out=rd[:], in_=pa[:, 64:65]), reads=[b_pa], writes=[b_rd])
            P.dve(lambda e, ot=ot, pa=pa: e.tensor_scalar(out=ot[:], in0=pa[:, 0:64], scalar1=rd[:, 0:1], scalar2=None, op0=ALU.mult), reads=[b_pa, b_rd], writes=[b_ot])
            P.pe(lambda e, ot=ot: e.transpose(ptr[:], ot[:], idt[:]), reads=[b_ot, b_id], writes=[b_ptr])
            P.act(lambda e, oT=oT: e.activation(out=oT[:], in_=ptr[:], func=AF.Copy), reads=[b_ptr], writes=[b_oT])
            P.dma(lambda e, oT=oT, n=n: e.dma_start(out=mixT[out_row + i * 64:out_row + (i + 1) * 64, n * 128:(n + 1) * 128], in_=oT[:]), reads=[b_oT], writes=[Buf()])
        C.end()


def rope_tables(L, half, period):
    inv = (10000.0 ** (-np.arange(half, dtype=np.float32) / half)).astype(np.float32)
    ang = (np.arange(L, dtype=np.float32)[None, :] * inv[:, None]).astype(np.float32)
    cos = np.cos(ang).astype(np.float32)
    sin = np.sin(ang).astype(np.float32)
    rows = np.arange(128) % period
    c = cos[rows % half]
    s = np.where((rows < half)[:, None], -sin[rows % half], sin[rows % half])
    return np.ascontiguousarray(c, np.float32), np.ascontiguousarray(s, np.float32)


def swap_cols(w, ncols, period):
    half = period // 2
    idx = np.arange(ncols)
    src = (idx // period) * period + (idx % period + half) % period
    return np.ascontiguousarray(w[:, src])


def ret_tables(L):
    NCH = L // 128
    s = 128.0 ** -0.5
    tab = np.zeros((128, 24, NCH), np.float64)
    t = np.arange(128, dtype=np.float64)
    for h in range(4):
        lg = np.log1p(-(2.0 ** (-5.0 - h)))
        for dr in range(2):
            e = (t + 1) if dr == 0 else (128 - t)
            j0 = (h * 2 + dr) * 3
            tab[:, j0, :] = np.exp(lg * e)[:, None]
            tab[:, j0 + 1, :] = (np.exp(-lg * e) * s)[:, None]
            tab[:, j0 + 2, :] = np.exp(lg * 128)
    return tab.astype(np.float32)


def la_masks():
    j = np.arange(128)[:, None]
    i = np.arange(128)[None, :]
    return np.stack([(j <= i), (j > i), (j >= i)]).astype(np.float32)


def g8(v):
    return np.ascontiguousarray(v.reshape(8, 128).T)


def s5_host(d, T):
    a_re, a_im = d['s5_a_re'][0], d['s5_a_im'][0]
    are2 = np.concatenate([a_re.reshape(64, 64).T] * 2, 0)
    aim2 = np.concatenate([a_im.reshape(64, 64).T] * 2, 0)
    lstep = np.tile(d['s5_log_step'][0].reshape(1, 64), (128, 1))
    dsk = d['s5_d'][0].reshape(4, 128).T
    prm = {"are2": are2, "aim2": aim2, "lstep": lstep, "dsk": dsk, "b_re": d['s5_b_re'][0], "b_im": d['s5_b_im'][0],
           "c_re": d['s5_c_re'][0], "c_im": d['s5_c_im'][0]}
    psw = np.zeros((128, 128), np.float32)
    for k in range(128):
        psw[k, (k + 64) % 128] = 1
    ksel = np.zeros((128, 3), np.float32)
    ksel[:64, 0] = 1; ksel[64:, 1] = 1; ksel[:64, 2] = 1; ksel[64:, 2] = -1
    tau = np.tile(np.arange(1, T + 1, dtype=np.float32)[None, :], (128, 1))
    consts = {"psw": psw, "ksel": ksel, "tau": tau}
    return {k: np.ascontiguousarray(v, np.float32) for k, v in prm.items()}, consts


def route_consts(L):
    NJ = L // 128
    q = np.arange(128)[:, None]; p = np.arange(128)[None, :]
    us = (q < p).astype(np.float32)
    rm = np.ones((16, NJ), np.float32); rm[:, 0] = 0
    rm = np.tile(rm.reshape(1, -1), (128, 1))
    return us, np.ascontiguousarray(rm)


class RowSplit:
    def __init__(self, C, name, rows, L, chunk=1024):
        self.chunk = chunk
        self.parts = [C.dscr("%s_%d" % (name, k), [min(chunk, rows - k * chunk), L]) for k in range(-(-rows // chunk))]

    def __getitem__(self, key):
        rs, cs = key
        k = rs.start // self.chunk
        assert (rs.stop - 1) // self.chunk == k
        return self.parts[k][rs.start - k * self.chunk:rs.stop - k * self.chunk, cs]


def build_program(L, FF):
    NCH = L // 128
    NJ = L // 128
    CAP = L // 8
    T = min(512, L)
    C = Ctx()
    i = {}
    def din(name, shape, dt=F32):
        i[name] = C.din(name, shape, dt)
        return i[name]
    xT = din("xT", [D, L])
    w_in = [din("w_in0", [D, 2560]), din("w_in1", [D, 4480])]
    wsw = [din("wsw0", [D, 1024]), din("wsw1", [D, 1536])]
    w_out = [din("w_out0", [1024, 1024]), din("w_out1", [768, 1024])]
    gmix = [din("gmix0", [128, 8]), din("gmix1", [128, 8])]
    gffn = [din("gffn0", [128, 8]), din("gffn1", [128, 8])]
    gfin = din("gfin", [128, 8])
    cos = [din("cos0", [128, L]), din("cos1", [128, L])]
    sin = [din("sin0", [128, L]), din("sin1", [128, L])]
    ident = din("ident", [128, 128])
    lam = din("lamask", [3, 128, 128])
    rtab = din("rtab", [128, 24, NCH])
    prm_shapes = {"are2": [128, 64], "aim2": [128, 64], "lstep": [128, 64], "dsk": [128, 4], "b_re": [2, 32, 64, 16], "b_im": [2, 32, 64, 16],
                  "c_re": [2, 32, 16, 64], "c_im": [2, 32, 16, 64]}
    prm = {}
    for k, shp in prm_shapes.items():
        a = din("s5_" + k, shp)
        prm[k] = a[:, :] if len(shp) == 2 else a
    consts = {"ident": ident[:, :], "psw": din("c_psw", [128, 128])[:, :], "ksel": din("c_ksel", [128, 3])[:, :], "tau": din("c_tau", [128, T])[:, :]}
    gluw = din("gluw", [512, 512])
    glub = din("glub", [128, 4])
    router = [din("router0", [1024, 16]), din("router1", [1024, 16])]
    ustrict = din("ustrict", [128, 128])
    rmask = din("rmask", [128, 16 * NJ])
    mlb = din("mlb", [128, 16])
    dmasks = din("dmasks", [25, 128, 128])
    w1 = [din("w1_0", [16, 1024, FF]), din("w1_1", [16, 1024, FF])]
    w3 = [din("w3_0", [16, 1024, FF]), din("w3_1", [16, 1024, FF])]
    w2 = [din("w2_0", [16, FF, 1024]), din("w2_1", [16, FF, 1024])]
    outT = C.dout("outT", [D, L])
    pT = RowSplit(C, "pT", 4480, L)
    mixT = C.dscr("mixT", [1024, L])
    o1 = C.dscr("o1", [4, L, 128])
    yf = C.dscr("yf", [512, L])
    x1T = C.dscr("x1T", [1024, L])
    x2T = C.dscr("x2T", [1024, L])
    x3T = C.dscr("x3T", [1024, L])
    htok = C.dscr("htok", [L, 1024])
    aff = C.dscr("aff", [L, 16])
    posd = C.dscr("posd", [128, 16 * NJ], I32)
    gmd = C.dscr("gmd", [128, 16 * NJ])
    xe = [C.dscr("xe%d" % e, [CAP, 1024]) for e in range(16)]
    ye = [C.dscr("ye%d" % e, [CAP, 1024]) for e in range(16)]
    tabd = C.dscr("tabd", [128, 24, NCH])
    vtok = [C.dscr("vtok%d" % h, [L, 66], BF16) for h in range(12)]
    qk16 = C.dscr("qk16", [1536, L], BF16)

    def moe(layer, xin1T, xoutT, final):
        stage_route(C, aff, ustrict[:, :], rmask[:, :], posd[:, :], gmd[:, :], L)
        stage_dispatch(C, htok, posd[:, :], xe, L)
        stage_ffn(C, xe, w1[layer], w3[layer], w2[layer], ye, ident[:, :], L, FF)
        stage_combine(C, ye, posd[:, :], gmd[:, :], xin1T, ident[:, :], xoutT, outT if final else None, gfin[:, :], L)

    stage_proj(C, xT, w_in[0], wsw[0], gmix[0][:, :], cos[0], sin[0], pT, L, 20, 8)
    stage_linattn(C, pT, rtab[:, :, :], ident[:, :], lam, o1, mixT, L, 0, 512, 1024, 1536, 0, False)
    stage_s5(C, pT, prm, consts, yf, mixT, L, 2048, 512)
    stage_out(C, xT, mixT, w_out[0], gluw, glub[:, :], gffn[0][:, :], router[0], ident[:, :], x1T, htok, aff, L, True)
    moe(0, x1T, x2T, False)
    stage_proj(C, x2T, w_in[1], wsw[1], gmix[1][:, :], cos[1], sin[1], pT, L, 35, 12)
    stage_gates(C, pT, mlb[:, :], ident[:, :], tabd, L, 4352)
    stage_linattn(C, pT, tabd[:, :, :], ident[:, :], lam, o1, mixT, L, 2304, 2816, 3328, 3840, 256, True)
    stage_vprep(C, pT, ident[:, :], vtok, L, 1536)
    stage_qk16(C, pT, qk16, L, 1536)
    stage_dil(C, qk16, ident[:, :], dmasks, vtok, mixT, L, 0, 768, 0)
    stage_out(C, x2T, mixT, w_out[1], None, None, gffn[1][:, :], router[1], ident[:, :], x1T, htok, aff, L, False)
    moe(1, x1T, x3T, True)
    ninst = C.P.ninst
    return C.finish(), ninst


def host_inputs(inp, b, L, FF):
    f = lambda a: np.ascontiguousarray(a, np.float32)
    T = min(512, L)
    im = {"xT": f(inp["x"][b].T)}
    W0 = inp["ev_w_in"][0]
    W1 = np.zeros((D, 4480), np.float32)
    W1[:, :4368] = inp["od_w_in"][0]
    im["w_in0"] = f(W0); im["w_in1"] = W1
    im["wsw0"] = swap_cols(W0[:, :1024], 1024, 128); im["wsw1"] = swap_cols(W1[:, :1536], 1536, 64)
    im["w_out0"] = f(inp["ev_w_out"][0]); im["w_out1"] = f(inp["od_w_out"][0])
    for l in range(2):
        im["gmix%d" % l] = g8(inp["norm_mix_g"][l]); im["gffn%d" % l] = g8(inp["norm_ffn_g"][l])
        im["router%d" % l] = f(inp["moe_router"][l])
        im["w1_%d" % l] = f(inp["moe_w1"][l]); im["w3_%d" % l] = f(inp["moe_w3"][l]); im["w2_%d" % l] = f(inp["moe_w2"][l])
    im["gfin"] = g8(inp["final_g"])
    im["cos0"], im["sin0"] = rope_tables(L, 64, 128)
    im["cos1"], im["sin1"] = rope_tables(L, 32, 64)
    im["ident"] = np.eye(128, dtype=np.float32)
    im["lamask"] = la_masks()
    im["rtab"] = ret_tables(L)
    prm, consts = s5_host({k: inp[k] for k in ("s5_a_re", "s5_a_im", "s5_b_re", "s5_b_im", "s5_c_re", "s5_c_im", "s5_log_step", "s5_d")}, T)
    for k, v in prm.items():
        im["s5_" + k] = v
    for k, v in consts.items():
        im["c_" + k] = f(v)
    im["gluw"] = f(inp["s5_glu_w"][0]); im["glub"] = f(inp["s5_glu_b"][0].reshape(4, 128).T)
    im["ustrict"], im["rmask"] = route_consts(L)
    im["mlb"] = f(np.tile(np.concatenate([inp["ml_i_bias"][0].reshape(-1), inp["ml_f_bias"][0].reshape(-1)])[None, :], (128, 1)))
    im["dmasks"] = dil_masks()
    return im


def run_model(inp, batches):
    L = inp["x"].shape[1]
    FF = inp["moe_w1"].shape[-1]
    nc, ninst = build_program(L, FF)
    ims = [host_inputs(inp, b, L, FF) for b in batches]
    res = run_bass_kernel_spmd(nc, ims, core_ids=list(range(len(batches))))
    return [np.ascontiguousarray(r["outT"].T) for r in res.results]


def kernel(**inputs):
    inp = {k: np.asarray(v) for k, v in inputs.items()}
    B = inp["x"].shape[0]
    outs = run_model(inp, list(range(B)))
    return np.stack(outs, 0).astype(np.float32)
```

```python
import contextlib
import numpy as np
import concourse.bass as bass
import concourse.mybir as mybir
from concourse.bass_utils import run_bass_kernel_spmd

F32 = mybir.dt.float32
BF16 = mybir.dt.bfloat16
I32 = mybir.dt.int32
AF = mybir.ActivationFunctionType
ALU = mybir.AluOpType
AX = mybir.AxisListType
D = 1024
EPS = 1e-6
ENGS = ("pe", "act", "dve", "pool", "sp")
ENGOBJ = {"pe": "tensor", "act": "scalar", "dve": "vector", "pool": "gpsimd", "sp": "sync"}


class Buf:
    __slots__ = ("last_w", "readers")

    def __init__(self):
        self.last_w = None
        self.readers = []


class Op:
    __slots__ = ("eng", "fn", "idx", "deps", "dma", "signal", "cnt", "sem", "semval", "stage")


class Prog:
    ND = {"sp": 10, "act": 2, "pool": 4}

    def __init__(self, nc, st):
        self.nc = nc
        self.esem = {e: st.enter_context(nc.semaphore("s_" + e)) for e in ENGS}
        self.dsem = {(q, k): st.enter_context(nc.semaphore("d_%s%d" % (q, k))) for q in self.ND for k in range(self.ND[q])}
        self.base = {e: 0 for e in ENGS}
        self.dk = {q: 0 for q in self.ND}
        self.dval = {k: 0 for k in self.dsem}
        self.waited = {e: {} for e in ENGS}
        self.stage = 0
        self.ops = {e: [] for e in ENGS}
        self.ninst = 0

    def op(self, eng, fn, reads=(), writes=(), dma=False):
        o = Op()
        o.eng, o.fn, o.idx, o.dma, o.signal, o.cnt, o.stage = eng, fn, len(self.ops[eng]), dma, False, 0, self.stage
        o.sem, o.semval = None, 0
        deps = {}
        for b in reads:
            if b.last_w is not None and b.last_w.stage == self.stage:
                deps[id(b.last_w)] = b.last_w
        for b in writes:
            if b.last_w is not None and b.last_w.stage == self.stage:
                deps[id(b.last_w)] = b.last_w
            for r in b.readers:
                if r.stage == self.stage:
                    deps[id(r)] = r
        o.deps = list(deps.values())
        for b in reads:
            b.readers.append(o)
        for b in writes:
            b.last_w = o
            b.readers = []
        if dma:
            k = self.dk[eng]
            self.dk[eng] += 1
            nd = self.ND[eng]
            o.sem = (eng, k % nd)
            o.semval = 16 * (k // nd + 1)
            self.dval[o.sem] = o.semval
        self.ops[eng].append(o)
        self.ninst += 1
        return o

    def pe(self, fn, reads=(), writes=()):
        return self.op("pe", fn, reads, writes)

    def act(self, fn, reads=(), writes=()):
        return self.op("act", fn, reads, writes)

    def dve(self, fn, reads=(), writes=()):
        return self.op("dve", fn, reads, writes)

    def pool(self, fn, reads=(), writes=()):
        return self.op("pool", fn, reads, writes)

    def dma(self, fn, reads=(), writes=(), q="sp"):
        return self.op(q, fn, reads, writes, dma=True)

    def end_stage(self):
        nc = self.nc
        for e in ENGS:
            comp = [o for o in self.ops[e] if not o.dma]
            if comp:
                comp[-1].signal = True
            for o in self.ops[e]:
                for d in o.deps:
                    if d.dma:
                        continue
                    if d.eng == o.eng:
                        if e == "pe":
                            continue
                        if o.idx - d.idx > 2 and not o.dma:
                            continue
                    d.signal = True
        final = {}
        for e in ENGS:
            c = self.base[e]
            for o in self.ops[e]:
                if o.dma:
                    continue
                if o.signal:
                    c += 1
                o.cnt = c
            final[e] = c
        with nc.Block() as block:
            def run_engine(e, eng):
                waited = self.waited[e]

                def need(sem, key, val):
                    if waited.get(key, 0) < val:
                        eng.wait_ge(sem, val)
                        waited[key] = val

                for o in self.ops[e]:
                    for d in o.deps:
                        if d.dma:
                            need(self.dsem[d.sem], d.sem, d.semval)
                        else:
                            if d.eng == e:
                                if e == "pe":
                                    continue
                                if o.idx - d.idx > 2 and not o.dma:
                                    continue
                            need(self.esem[d.eng], d.eng, d.cnt)
                    if o.dma:
                        if o.semval > 16:
                            need(self.dsem[o.sem], o.sem, o.semval - 16)
                        inst = o.fn(eng)
                        inst.then_inc(self.dsem[o.sem], 16)
                    else:
                        inst = o.fn(eng)
                        if o.signal:
                            inst.then_inc(self.esem[e], 1)
                for e2 in ENGS:
                    if e2 != e and final[e2] > 0:
                        need(self.esem[e2], e2, final[e2])
                for k, v in self.dval.items():
                    if v > 0:
                        need(self.dsem[k], k, v)

            for e in ENGS:
                def _f(eng, e=e):
                    run_engine(e, eng)
                getattr(block, ENGOBJ[e])(_f)
        self.base = final
        self.stage += 1
        self.ops = {e: [] for e in ENGS}


class Ctx:
    def __init__(self):
        self.nc = bass.Bass("TRN2", target_bir_lowering=False)
        self.gst = contextlib.ExitStack()
        self.P = Prog(self.nc, self.gst)
        self.st = None
        self.n = 0
        self.dbg = {}

    def din(self, name, shape, dt=F32):
        return self.nc.dram_tensor(name, list(shape), dt, kind="ExternalInput").ap()

    def dout(self, name, shape, dt=F32):
        return self.nc.dram_tensor(name, list(shape), dt, kind="ExternalOutput").ap()

    def dscr(self, name, shape, dt=F32, debug=False):
        if debug:
            return self.dout(name, shape, dt)
        return self.nc.dram_tensor(name, list(shape), dt, kind="Internal").ap()

    def begin(self):
        self.st = contextlib.ExitStack()

    def end(self):
        self.P.end_stage()
        self.st.close()
        self.st = None

    def sb(self, shape, dt=F32):
        self.n += 1
        t = self.st.enter_context(self.nc.sbuf_tensor("t%d" % self.n, list(shape), dt))
        return t, Buf()

    def ps(self, shape, dt=F32):
        self.n += 1
        t = self.st.enter_context(self.nc.psum_tensor("p%d" % self.n, list(shape), dt))
        return t, Buf()

    def finish(self):
        self.gst.close()
        return self.nc


def stage_proj(C, xT, w, wsw, g8, cos, sin, pT, L, NCT, NRT):
    P = C.P
    C.begin()
    TB = min(512, L)
    NB = L // TB
    wb, _ = C.sb([128, 8, NCT * 128], BF16)
    wswb, _ = C.sb([128, 8, NRT * 128], BF16)
    b_w = [Buf() for _ in range(8)]
    b_ws = [Buf() for _ in range(8)]
    gt, b_g = C.sb([128, 8])
    ones, b_ones = C.sb([128, 128], BF16)
    P.dma(lambda e: e.dma_start(out=gt[:], in_=g8), writes=[b_g])
    P.dve(lambda e: e.memset(ones[:], 1.0), writes=[b_ones])
    epsc, b_epsc = C.sb([128, 1])
    P.dve(lambda e: e.memset(epsc[:], EPS), writes=[b_epsc])
    CH = 640
    for kc in range(8):
        for c0 in range(0, NCT * 128, CH):
            P.dma(lambda e, kc=kc, c0=c0: e.dma_start(out=wb[:, kc, c0:c0 + CH], in_=w[kc * 128:(kc + 1) * 128, c0:c0 + CH]),
                  writes=[b_w[kc]], q="pool")
        for c0 in range(0, NRT * 128, 512):
            P.dma(lambda e, kc=kc, c0=c0: e.dma_start(out=wswb[:, kc, c0:c0 + 512], in_=wsw[kc * 128:(kc + 1) * 128, c0:c0 + 512]),
                  writes=[b_ws[kc]], q="pool")
    nxb = 2 if NCT <= 24 else 1
    xts = [C.sb([128, 8, TB]) for _ in range(nxb)]
    xbs = [C.sb([128, 8, TB], BF16) for _ in range(nxb)]
    sq, b_sq = C.sb([128, 8, TB], BF16)
    css = [C.sb([128, TB]) for _ in range(2)]
    sns = [C.sb([128, TB]) for _ in range(2)]
    rss = [C.sb([128, TB]) for _ in range(2)]
    pss, b_pss = C.ps([128, TB])
    ps_a = [C.ps([128, TB]) for _ in range(2)]
    ps_b = [C.ps([128, TB]) for _ in range(2)]
    t1s = [C.sb([128, TB]) for _ in range(2)]
    t2s = [C.sb([128, TB]) for _ in range(2)]
    os_ = [C.sb([128, TB]) for _ in range(4)]
    b_out = Buf()
    xv = xT.rearrange("(kc p) n -> p kc n", p=128)
    no = 0
    na = 0
    for tb in range(NB):
        sl = slice(tb * TB, (tb + 1) * TB)
        xt, b_x = xts[tb % nxb]
        xb, b_xb = xbs[tb % nxb]
        cs, b_cs = css[tb % 2]
        sn, b_sn = sns[tb % 2]
        rs, b_rs = rss[tb % 2]
        P.dma(lambda e, xt=xt, sl=sl: e.dma_start(out=xt[:], in_=xv[:, :, sl]), writes=[b_x])
        if NRT:
            P.dma(lambda e, cs=cs, sl=sl: e.dma_start(out=cs[:], in_=cos[:, sl]), writes=[b_cs])
            P.dma(lambda e, sn=sn, sl=sl: e.dma_start(out=sn[:], in_=sin[:, sl]), writes=[b_sn])
        P.pool(lambda e, xt=xt: e.tensor_tensor(out=sq[:], in0=xt[:], in1=xt[:], op=ALU.mult), reads=[b_x], writes=[b_sq])
        for kc in range(8):
            eng = P.dve if kc % 2 == 0 else P.pool
            eng(lambda e, kc=kc, xt=xt, xb=xb: e.tensor_scalar(out=xb[:, kc, :], in0=xt[:, kc, :], scalar1=gt[:, kc:kc + 1], scalar2=None, op0=ALU.mult),
                reads=[b_x, b_g], writes=[b_xb])
        for kc in range(8):
            P.pe(lambda e, kc=kc: e.matmul(pss[:], ones[:], sq[:, kc, :], start=(kc == 0), stop=(kc == 7)),
                 reads=[b_sq, b_ones], writes=[b_pss])
        P.act(lambda e, rs=rs: e.activation(out=rs[:], in_=pss[:], func=AF.Ln, scale=1.0 / D, bias=epsc[:, 0:1]), reads=[b_pss, b_epsc], writes=[b_rs])
        P.act(lambda e, rs=rs: e.activation(out=rs[:], in_=rs[:], func=AF.Exp, scale=-0.5), reads=[b_rs], writes=[b_rs])
        if NRT:
            P.pool(lambda e, cs=cs, rs=rs: e.tensor_tensor(out=cs[:], in0=cs[:], in1=rs[:], op=ALU.mult), reads=[b_cs, b_rs], writes=[b_cs])
            P.pool(lambda e, sn=sn, rs=rs: e.tensor_tensor(out=sn[:], in0=sn[:], in1=rs[:], op=ALU.mult), reads=[b_sn, b_rs], writes=[b_sn])
        for ct in range(NCT):
            pa, b_pa = ps_a[na % 2]
            pb, b_pb = ps_b[na % 2]
            na += 1
            o, b_o = os_[no % 4]
            no += 1
            for kc in range(8):
                P.pe(lambda e, kc=kc, ct=ct, pa=pa, xb=xb: e.matmul(pa[:], wb[:, kc, ct * 128:(ct + 1) * 128], xb[:, kc, :], start=(kc == 0), stop=(kc == 7)),
                     reads=[b_w[kc], b_xb], writes=[b_pa])
            if ct < NRT:
                for kc in range(8):
                    P.pe(lambda e, kc=kc, ct=ct, pb=pb, xb=xb: e.matmul(pb[:], wswb[:, kc, ct * 128:(ct + 1) * 128], xb[:, kc, :], start=(kc == 0), stop=(kc == 7)),
                         reads=[b_ws[kc], b_xb], writes=[b_pb])
                t1, b_t1 = t1s[ct % 2]
                t2, b_t2 = t2s[ct % 2]
                P.dve(lambda e, t1=t1, pa=pa, cs=cs: e.tensor_tensor(out=t1[:], in0=pa[:], in1=cs[:], op=ALU.mult), reads=[b_pa, b_cs], writes=[b_t1])
                P.dve(lambda e, t2=t2, pb=pb, sn=sn: e.tensor_tensor(out=t2[:], in0=pb[:], in1=sn[:], op=ALU.mult), reads=[b_pb, b_sn], writes=[b_t2])
                P.pool(lambda e, o=o, t1=t1, t2=t2: e.tensor_tensor(out=o[:], in0=t1[:], in1=t2[:], op=ALU.add), reads=[b_t1, b_t2], writes=[b_o])
            else:
                P.dve(lambda e, o=o, pa=pa, rs=rs: e.tensor_tensor(out=o[:], in0=pa[:], in1=rs[:], op=ALU.mult), reads=[b_pa, b_rs], writes=[b_o])
            P.dma(lambda e, o=o, ct=ct, sl=sl: e.dma_start(out=pT[ct * 128:(ct + 1) * 128, sl], in_=o[:]), reads=[b_o], writes=[Buf()])
    C.end()


def stage_gates(C, pT, mlb, ident, tabd, L, mg_row):
    P = C.P
    C.begin()
    NCH = L // 128
    s = 128.0 ** -0.5
    idt, b_id = C.sb([128, 128])
    mb, b_mb = C.sb([128, 16])
    nfb, b_nfb = C.sb([128, 8])
    lns, b_lns = C.sb([128, 1])
    onesq, b_onesq = C.sb([128, 128])
    P.dma(lambda e: e.dma_start(out=idt[:], in_=ident), writes=[b_id])
    P.dma(lambda e: e.dma_start(out=mb[:], in_=mlb), writes=[b_mb])
    P.dve(lambda e: e.tensor_scalar(out=nfb[:], in0=mb[:, 8:16], scalar1=-1.0, scalar2=None, op0=ALU.mult), reads=[b_mb], writes=[b_nfb])
    P.dve(lambda e: e.memset(lns[:], float(np.log(s))), writes=[b_lns])
    P.dve(lambda e: e.memset(onesq[:], 1.0), writes=[b_onesq])
    b_out = Buf()
    gps = [C.ps([128, 128]) for _ in range(3)]
    for h in range(4):
        for dr in range(2):
            gi, b_gi = C.sb([128, 128])
            gf, b_gf = C.sb([128, 128])
            ri = mg_row + dr * 4 + h
            rf = mg_row + 8 + dr * 4 + h
            P.dma(lambda e, gi=gi, ri=ri: e.dma_start(out=gi[0:NCH, :], in_=pT[ri:ri + 1, :].rearrange("o (c t) -> (o c) t", t=128)), writes=[b_gi])
            P.dma(lambda e, gf=gf, rf=rf: e.dma_start(out=gf[0:NCH, :], in_=pT[rf:rf + 1, :].rearrange("o (c t) -> (o c) t", t=128)), writes=[b_gf])
            sp, b_sp = C.sb([128, 128])
            csp, b_csp = C.sb([128, 128])
            col = dr * 4 + h
            P.act(lambda e, sp=sp, gf=gf, col=col: e.activation(out=sp[0:NCH, :], in_=gf[0:NCH, :], func=AF.Exp, scale=-1.0, bias=nfb[0:NCH, col:col + 1]),
                  reads=[b_gf, b_nfb], writes=[b_sp])
            P.act(lambda e, sp=sp: e.activation(out=sp[0:NCH, :], in_=sp[0:NCH, :], func=AF.Ln, scale=1.0, bias=1.0), reads=[b_sp], writes=[b_sp])
            P.dve(lambda e, csp=csp, sp=sp: e.tensor_tensor_scan(out=csp[0:NCH, :], data0=onesq[0:NCH, :], data1=sp[0:NCH, :], initial=0.0, op0=ALU.mult, op1=ALU.add),
                  reads=[b_sp, b_onesq], writes=[b_csp])
            ex, b_ex = C.sb([128, 128])
            if dr == 0:
                P.dve(lambda e, ex=ex, csp=csp: e.tensor_copy(out=ex[0:NCH, :], in_=csp[0:NCH, :]), reads=[b_csp], writes=[b_ex])
            else:
                P.dve(lambda e, ex=ex, sp=sp, csp=csp: e.tensor_tensor(out=ex[0:NCH, :], in0=sp[0:NCH, :], in1=csp[0:NCH, :], op=ALU.subtract), reads=[b_sp, b_csp], writes=[b_ex])
                P.dve(lambda e, ex=ex, csp=csp: e.tensor_scalar(out=ex[0:NCH, :], in0=ex[0:NCH, :], scalar1=csp[0:NCH, 127:128], scalar2=None, op0=ALU.add), reads=[b_ex, b_csp], writes=[b_ex])
            av, b_av = C.sb([128, 128])
            cv, b_cv = C.sb([128, 128])
            ebc, b_ebc = C.sb([128, 1])
            P.act(lambda e, av=av, ex=ex: e.activation(out=av[0:NCH, :], in_=ex[0:NCH, :], func=AF.Exp, scale=-1.0), reads=[b_ex], writes=[b_av])
            P.dve(lambda e, cv=cv, gi=gi, ex=ex: e.tensor_tensor(out=cv[0:NCH, :], in0=gi[0:NCH, :], in1=ex[0:NCH, :], op=ALU.add), reads=[b_gi, b_ex], writes=[b_cv])
            P.dve(lambda e, cv=cv, col=col: e.tensor_scalar(out=cv[0:NCH, :], in0=cv[0:NCH, :], scalar1=mb[0:NCH, col:col + 1], scalar2=lns[0:NCH, 0:1], op0=ALU.add, op1=ALU.add),
                  reads=[b_cv, b_mb, b_lns], writes=[b_cv])
            P.act(lambda e, cv=cv: e.activation(out=cv[0:NCH, :], in_=cv[0:NCH, :], func=AF.Exp), reads=[b_cv], writes=[b_cv])
            P.act(lambda e, ebc=ebc, csp=csp: e.activation(out=ebc[0:NCH, :], in_=csp[0:NCH, 127:128], func=AF.Exp, scale=-1.0), reads=[b_csp], writes=[b_ebc])
            tb_, b_tb = C.sb([128, 3, NCH])
            for k, (src, b_src) in enumerate(((av, b_av), (cv, b_cv))):
                pt, b_pt = gps[k]
                P.pe(lambda e, pt=pt, src=src: e.transpose(pt[:, 0:NCH], src[0:NCH, :], idt[0:NCH, 0:NCH]), reads=[b_src, b_id], writes=[b_pt])
                P.act(lambda e, pt=pt, k=k, tb_=tb_: e.activation(out=tb_[:, k, :], in_=pt[:, 0:NCH], func=AF.Copy), reads=[b_pt], writes=[b_tb])
            tm, b_tm = C.sb([128, 128])
            P.dve(lambda e, tm=tm, ebc=ebc: e.tensor_scalar(out=tm[0:NCH, :], in0=onesq[0:NCH, :], scalar1=ebc[0:NCH, 0:1], scalar2=None, op0=ALU.mult),
                  reads=[b_onesq, b_ebc], writes=[b_tm])
            pt, b_pt = gps[2]
            P.pe(lambda e, pt=pt, tm=tm: e.matmul(pt[:, 0:NCH], tm[0:NCH, :], idt[0:NCH, 0:NCH], start=True, stop=True), reads=[b_tm, b_id], writes=[b_pt])
            P.act(lambda e, pt=pt, tb_=tb_: e.activation(out=tb_[:, 2, :], in_=pt[:, 0:NCH], func=AF.Copy), reads=[b_pt], writes=[b_tb])
            j0 = (h * 2 + dr) * 3
            P.dma(lambda e, tb_=tb_, j0=j0: e.dma_start(out=tabd[:, j0:j0 + 3, :], in_=tb_[:]), reads=[b_tb], writes=[Buf()])
    C.end()


def stage_linattn(C, pT, tab, ident, masks, o1, mixT, L, q_row, k_row, v_row, g_row, out_row, mlstm):
    P = C.P
    C.begin()
    NCH = L // 128
    NV = 129 if mlstm else 128
    idt, b_id = C.sb([128, 128])
    mk, b_mk = C.sb([128, 3, 128])
    tb_, b_tb = C.sb([128, 24, NCH])
    P.dma(lambda e: e.dma_start(out=idt[:], in_=ident), writes=[b_id])
    P.dma(lambda e: e.dma_start(out=mk[:], in_=masks.rearrange("m p n -> p m n")), writes=[b_mk])
    P.dma(lambda e: e.dma_start(out=tb_[:], in_=tab), writes=[b_tb])
    epsc, b_epsc = C.sb([128, 1])
    P.dve(lambda e: e.memset(epsc[:], EPS), writes=[b_epsc])
    NBUF = 4
    qTs = [C.sb([128, 128]) for _ in range(NBUF)]
    kTs = [C.sb([128, 128]) for _ in range(NBUF)]
    vTs = [C.sb([128, 128]) for _ in range(NBUF)]
    gTs = [C.sb([128, 128]) for _ in range(NBUF)]
    hfs = [C.sb([128, 128]) for _ in range(NBUF)]
    ktoks = [C.sb([128, 128]) for _ in range(NBUF)]
    vpps = [C.sb([128, NV]) for _ in range(NBUF)]
    sms = [C.sb([128, 128]) for _ in range(NBUF)]
    os_ = [C.sb([128, NV]) for _ in range(NBUF)]
    hs = [C.sb([128, 128]) for _ in range(NBUF)]
    gas = [C.sb([128, 128]) for _ in range(NBUF)]
    outs = [C.sb([128, 128]) for _ in range(NBUF)]
    p_kt = [C.ps([128, 128]) for _ in range(1)]
    p_vt = [C.ps([128, 128]) for _ in range(1)]
    p_s = [C.ps([128, 128]) for _ in range(2)]
    p_o = [C.ps([128, NV]) for _ in range(2)]
    p_kv = [C.ps([128, NV]) for _ in range(1)]
    p_tr = [C.ps([128, 128]) for _ in range(1)]
    cst, b_cst = C.sb([128, NV])
    tmpc, b_tmpc = C.sb([128, NV])
    st6, b_st6 = C.sb([128, 6])
    mv, b_mv = C.sb([128, 2])
    rsd, b_rsd = C.sb([128, 1])
    dn, b_dn = C.sb([128, 1])
    b_o1 = [[Buf() for _ in range(NCH)] for _ in range(4)]
    csts = [(cst, b_cst)] + [C.sb([128, NV]) for _ in range(3)]
    SK = 2
    for dr in range(2):
        mi = 0 if dr == 0 else (2 if mlstm else 1)
        for h in range(4):
            P.dve(lambda e, c_=csts[h][0]: e.memset(c_[:], 0.0), writes=[csts[h][1]])
        order = list(range(NCH)) if dr == 0 else list(range(NCH - 1, -1, -1))
        items = [(n, h) for n in order for h in range(4)]

        def phaseA(k, dr=dr, mi=mi, items=items):
            n, h = items[k]
            j0 = (h * 2 + dr) * 3
            i = k % NBUF
            cs_ = slice(n * 128, (n + 1) * 128)
            qT, b_q = qTs[i]
            kT, b_k = kTs[i]
            vT, b_v = vTs[i]
            P.dma(lambda e: e.dma_start(out=qT[:], in_=pT[q_row + h * 128:q_row + (h + 1) * 128, cs_]), writes=[b_q])
            P.dma(lambda e: e.dma_start(out=kT[:], in_=pT[k_row + h * 128:k_row + (h + 1) * 128, cs_]), writes=[b_k])
            P.dma(lambda e: e.dma_start(out=vT[:], in_=pT[v_row + h * 128:v_row + (h + 1) * 128, cs_]), writes=[b_v])
            pk, b_pk = p_kt[0]
            pv, b_pv = p_vt[0]
            ktok, b_kt = ktoks[i]
            vpp, b_vp = vpps[i]
            P.pe(lambda e: e.transpose(pk[:], kT[:], idt[:]), reads=[b_k, b_id], writes=[b_pk])
            P.pe(lambda e: e.transpose(pv[:], vT[:], idt[:]), reads=[b_v, b_id], writes=[b_pv])
            P.act(lambda e: e.activation(out=ktok[:], in_=pk[:], func=AF.Copy), reads=[b_pk], writes=[b_kt])
            P.dve(lambda e: e.tensor_scalar(out=vpp[:, 0:128], in0=pv[:], scalar1=tb_[:, j0 + 1, n:n + 1], scalar2=None, op0=ALU.mult), reads=[b_pv, b_tb], writes=[b_vp])
            if mlstm:
                P.act(lambda e: e.activation(out=vpp[:, 128:129], in_=tb_[:, j0 + 1, n:n + 1], func=AF.Copy), reads=[b_tb], writes=[b_vp])
            ps_, b_ps = p_s[k % 2]
            sm, b_sm = sms[i]
            P.pe(lambda e: e.matmul(ps_[:], kT[:], qT[:], start=True, stop=True), reads=[b_k, b_q], writes=[b_ps])
            P.dve(lambda e: e.tensor_tensor(out=sm[:], in0=ps_[:], in1=mk[:, mi, :], op=ALU.mult), reads=[b_ps, b_mk], writes=[b_sm])
            if dr == 1:
                hf, b_hf = hfs[i]
                gT, b_g = gTs[i]
                ga, b_ga = gas[i]
                P.dma(lambda e: e.dma_start(out=hf[:], in_=o1[h, cs_, :]), reads=[b_o1[h][n]], writes=[b_hf])
                P.dma(lambda e: e.dma_start(out=gT[:], in_=pT[g_row + h * 128:g_row + (h + 1) * 128, cs_]), writes=[b_g])
                P.act(lambda e: e.activation(out=ga[:], in_=gT[:], func=AF.Exp, scale=-1.0), reads=[b_g], writes=[b_ga])
                P.pool(lambda e: e.tensor_scalar(out=ga[:], in0=ga[:], scalar1=1.0, scalar2=None, op0=ALU.add), reads=[b_ga], writes=[b_ga])
                P.dve(lambda e: e.reciprocal(out=ga[:], in_=ga[:]), reads=[b_ga], writes=[b_ga])
                if not mlstm:
                    P.pool(lambda e: e.tensor_tensor(out=ga[:], in0=ga[:], in1=gT[:], op=ALU.mult), reads=[b_ga, b_g], writes=[b_ga])

        def phaseB(k, dr=dr, items=items):
            n, h = items[k]
            j0 = (h * 2 + dr) * 3
            i = k % NBUF
            cs_ = slice(n * 128, (n + 1) * 128)
            cst, b_cst = csts[h]
            qT, b_q = qTs[i]
            ktok, b_kt = ktoks[i]
            vpp, b_vp = vpps[i]
            sm, b_sm = sms[i]
            po, b_po = p_o[k % 2]
            P.pe(lambda e: e.matmul(po[:], sm[:], vpp[:], start=True, stop=False), reads=[b_sm, b_vp], writes=[b_po])
            P.pe(lambda e: e.matmul(po[:], qT[:], cst[:], start=False, stop=True), reads=[b_q, b_cst], writes=[b_po])
            o_, b_o = os_[i]
            P.act(lambda e: e.activation(out=o_[:], in_=po[:], func=AF.Copy, scale=tb_[:, j0, n:n + 1]), reads=[b_po, b_tb], writes=[b_o])
            pkv, b_pkv = p_kv[0]
            P.pe(lambda e: e.matmul(pkv[:], ktok[:], vpp[:], start=True, stop=True), reads=[b_kt, b_vp], writes=[b_pkv])
            P.dve(lambda e: e.tensor_tensor(out=tmpc[:], in0=pkv[:], in1=cst[:], op=ALU.add), reads=[b_pkv, b_cst], writes=[b_tmpc])
            P.dve(lambda e: e.tensor_scalar(out=cst[:], in0=tmpc[:], scalar1=tb_[:, j0 + 2, n:n + 1], scalar2=None, op0=ALU.mult), reads=[b_tmpc, b_tb], writes=[b_cst])
            hh, b_h = hs[i]
            if mlstm:
                P.dve(lambda e: e.tensor_scalar(out=dn[:], in0=o_[:, 128:129], scalar1=-1.0, scalar2=1.0, op0=ALU.mult, op1=ALU.max), reads=[b_o], writes=[b_dn])
                P.dve(lambda e: e.tensor_tensor(out=dn[:], in0=dn[:], in1=o_[:, 128:129], op=ALU.max), reads=[b_dn, b_o], writes=[b_dn])
                P.dve(lambda e: e.reciprocal(out=dn[:], in_=dn[:]), reads=[b_dn], writes=[b_dn])
                P.dve(lambda e: e.tensor_scalar(out=hh[:], in0=o_[:, 0:128], scalar1=dn[:, 0:1], scalar2=None, op0=ALU.mult), reads=[b_o, b_dn], writes=[b_h])
                src, b_src = hh, b_h
            else:
                src, b_src = o_, b_o
            if dr == 0:
                P.dma(lambda e: e.dma_start(out=o1[h, cs_, :], in_=src[:, 0:128]), reads=[b_src], writes=[b_o1[h][n]])
            else:
                hf, b_hf = hfs[i]
                ga, b_ga = gas[i]
                P.pool(lambda e: e.tensor_tensor(out=hf[:], in0=hf[:], in1=src[:, 0:128], op=ALU.add), reads=[b_hf, b_src], writes=[b_hf])
                P.dve(lambda e: e.bn_stats(out=st6[:], in_=hf[:]), reads=[b_hf], writes=[b_st6])
                P.dve(lambda e: e.bn_aggr(out=mv[:], in_=st6[:]), reads=[b_st6], writes=[b_mv])
                P.act(lambda e: e.activation(out=rsd[:], in_=mv[:, 1:2], func=AF.Ln, scale=1.0, bias=epsc[:, 0:1]), reads=[b_mv, b_epsc], writes=[b_rsd])
                P.act(lambda e: e.activation(out=rsd[:], in_=rsd[:], func=AF.Exp, scale=-0.5), reads=[b_rsd], writes=[b_rsd])
                P.dve(lambda e: e.tensor_scalar(out=hf[:], in0=hf[:], scalar1=mv[:, 0:1], scalar2=rsd[:, 0:1], op0=ALU.subtract, op1=ALU.mult), reads=[b_hf, b_mv, b_rsd], writes=[b_hf])
                ptr, b_ptr = p_tr[0]
                P.pe(lambda e: e.transpose(ptr[:], hf[:], idt[:]), reads=[b_hf, b_id], writes=[b_ptr])
                ot, b_ot = outs[i]
                P.dve(lambda e: e.tensor_tensor(out=ot[:], in0=ptr[:], in1=ga[:], op=ALU.mult), reads=[b_ptr, b_ga], writes=[b_ot])
                P.dma(lambda e: e.dma_start(out=mixT[out_row + h * 128:out_row + (h + 1) * 128, cs_], in_=ot[:]), reads=[b_ot], writes=[Buf()])

        for k in range(len(items) + SK):
            if k < len(items):
                phaseA(k)
            if k - SK >= 0:
                phaseB(k - SK)
    C.end()


PI = float(np.pi)


def _wrap(P, C, x, b_x, shape, add=0.0, scr=None, key="a"):
    if scr is not None and ("u" + key) in scr:
        (u, b_u), (ki, b_ki), (kf, b_kf), (y, b_y), (m, b_m) = [scr[n + key] for n in "uikym"]
    else:
        u, b_u = C.sb(shape)
        ki, b_ki = C.sb(shape, I32)
        kf, b_kf = C.sb(shape)
        y, b_y = C.sb(shape)
        m, b_m = C.sb(shape)
        if scr is not None:
            for n, v in zip("uikym", ((u, b_u), (ki, b_ki), (kf, b_kf), (y, b_y), (m, b_m))):
                scr[n + key] = v
    P.dve(lambda e: e.tensor_scalar(out=u[:], in0=x[:], scalar1=add, scalar2=1.0 / (2 * PI), op0=ALU.add, op1=ALU.mult), reads=[b_x], writes=[b_u])
    P.dve(lambda e: e.tensor_copy(out=ki[:], in_=u[:]), reads=[b_u], writes=[b_ki])
    P.dve(lambda e: e.tensor_copy(out=kf[:], in_=ki[:]), reads=[b_ki], writes=[b_kf])
    P.dve(lambda e: e.tensor_scalar(out=u[:], in0=x[:], scalar1=add, scalar2=None, op0=ALU.add), reads=[b_x, b_kf], writes=[b_u])
    P.dve(lambda e: e.scalar_tensor_tensor(out=y[:], in0=kf[:], scalar=-2 * PI, in1=u[:], op0=ALU.mult, op1=ALU.add), reads=[b_kf, b_u], writes=[b_y])
    P.dve(lambda e: e.tensor_scalar(out=m[:], in0=y[:], scalar1=PI, scalar2=-2 * PI, op0=ALU.is_gt, op1=ALU.mult), reads=[b_y], writes=[b_m])
    P.dve(lambda e: e.tensor_tensor(out=y[:], in0=y[:], in1=m[:], op=ALU.add), reads=[b_y, b_m], writes=[b_y])
    P.dve(lambda e: e.tensor_scalar(out=m[:], in0=y[:], scalar1=-PI, scalar2=2 * PI, op0=ALU.is_lt, op1=ALU.mult), reads=[b_y], writes=[b_m])
    P.dve(lambda e: e.tensor_tensor(out=y[:], in0=y[:], in1=m[:], op=ALU.add), reads=[b_y, b_m], writes=[b_y])
    P.dve(lambda e: e.tensor_scalar(out=y[:], in0=y[:], scalar1=-PI, scalar2=PI, op0=ALU.max, op1=ALU.min), reads=[b_y], writes=[b_y])
    return y, b_y


def stage_s5(C, pT, prm, consts, yf, mixT, L, u_row, out_row):
    P = C.P
    T = min(512, L)
    NBK = L // T
    for dr in range(2):
        for ct in range(4):
            C.begin()
            idt, b_id = C.sb([128, 128])
            psw, b_psw = C.sb([128, 128])
            tau, b_tau = C.sb([128, T])
            ks, b_ks = C.sb([128, 3])
            lst, b_lst = C.sb([128, 64])
            dsk, b_dsk = C.sb([128, 4])
            onesT, b_onesT = C.sb([128, T])
            P.dma(lambda e: e.dma_start(out=idt[:], in_=consts["ident"]), writes=[b_id])
            P.dma(lambda e: e.dma_start(out=psw[:], in_=consts["psw"]), writes=[b_psw])
            P.dma(lambda e: e.dma_start(out=tau[:], in_=consts["tau"]), writes=[b_tau])
            P.dma(lambda e: e.dma_start(out=ks[:], in_=consts["ksel"]), writes=[b_ks])
            P.dma(lambda e: e.dma_start(out=lst[:], in_=prm["lstep"]), writes=[b_lst])
            P.dma(lambda e: e.dma_start(out=dsk[:], in_=prm["dsk"]), writes=[b_dsk])
            P.dve(lambda e: e.memset(onesT[:], 1.0), writes=[b_onesT])
            G = []
            scr = {}
            ang, b_ang = C.sb([128, T])
            are2, b_are2 = C.sb([128, 64])
            aim2, b_aim2 = C.sb([128, 64])
            P.dma(lambda e: e.dma_start(out=are2[:], in_=prm['are2']), writes=[b_are2])
            P.dma(lambda e: e.dma_start(out=aim2[:], in_=prm['aim2']), writes=[b_aim2])
            ptr = [C.ps([128, 128]) for _ in range(2)]
            for gp in range(8):
                g = ct * 8 + gp
                are, b_are = C.sb([128, 1])
                aim, b_aim = C.sb([128, 1])
                P.dve(lambda e, are=are, cg=dr * 32 + g: e.tensor_copy(out=are[:], in_=are2[:, cg:cg + 1]), reads=[b_are2], writes=[b_are])
                P.dve(lambda e, aim=aim, cg=dr * 32 + g: e.tensor_copy(out=aim[:], in_=aim2[:, cg:cg + 1]), reads=[b_aim2], writes=[b_aim])
                dl, b_dl = C.sb([128, 1])
                r, b_r = C.sb([128, 1])
                th, b_th = C.sb([128, 1])
                col = dr * 32 + g
                P.act(lambda e, dl=dl, col=col: e.activation(out=dl[:], in_=lst[:, col:col + 1], func=AF.Exp), reads=[b_lst], writes=[b_dl])
                P.act(lambda e, r=r, are=are, dl=dl: e.activation(out=r[:], in_=are[:], func=AF.Exp, scale=dl[:, 0:1]), reads=[b_are, b_dl], writes=[b_r])
                P.dve(lambda e, th=th, aim=aim, dl=dl: e.tensor_tensor(out=th[:], in0=aim[:], in1=dl[:], op=ALU.mult), reads=[b_aim, b_dl], writes=[b_th])
                thr0, b_thr0 = _wrap(P, C, th, b_th, [128, 1], scr=scr, key='c')
                thr, b_thr = C.sb([128, 1])
                P.dve(lambda e, thr=thr, thr0=thr0: e.tensor_copy(out=thr[:], in_=thr0[:]), reads=[b_thr0], writes=[b_thr])
                thc, b_thc = _wrap(P, C, thr, b_thr, [128, 1], add=PI / 2, scr=scr, key='d')
                s0, b_s0 = C.sb([128, 1])
                c0, b_c0 = C.sb([128, 1])
                P.act(lambda e, s0=s0, thr=thr: e.activation(out=s0[:], in_=thr[:], func=AF.Sin), reads=[b_thr], writes=[b_s0])
                P.act(lambda e, c0=c0, thc=thc: e.activation(out=c0[:], in_=thc[:], func=AF.Sin), reads=[b_thc], writes=[b_c0])
                nre, b_nre = C.sb([128, 1])
                nim, b_nim = C.sb([128, 1])
                den, b_den = C.sb([128, 1])
                t0, b_t0 = C.sb([128, 1])
                kre, b_kre = C.sb([128, 1])
                kim, b_kim = C.sb([128, 1])
                P.dve(lambda e, nre=nre, r=r, c0=c0: e.tensor_tensor(out=nre[:], in0=r[:], in1=c0[:], op=ALU.mult), reads=[b_r, b_c0], writes=[b_nre])
                P.dve(lambda e, nre=nre: e.tensor_scalar(out=nre[:], in0=nre[:], scalar1=-1.0, scalar2=None, op0=ALU.add), reads=[b_nre], writes=[b_nre])
                P.dve(lambda e, nim=nim, r=r, s0=s0: e.tensor_tensor(out=nim[:], in0=r[:], in1=s0[:], op=ALU.mult), reads=[b_r, b_s0], writes=[b_nim])
                P.dve(lambda e, den=den, are=are: e.tensor_tensor(out=den[:], in0=are[:], in1=are[:], op=ALU.mult), reads=[b_are], writes=[b_den])
                P.dve(lambda e, den=den, aim=aim: e.scalar_tensor_tensor(out=den[:], in0=aim[:], scalar=aim[:, 0:1], in1=den[:], op0=ALU.mult, op1=ALU.add), reads=[b_aim, b_den], writes=[b_den])
                P.dve(lambda e, den=den: e.reciprocal(out=den[:], in_=den[:]), reads=[b_den], writes=[b_den])
                P.dve(lambda e, t0=t0, nre=nre, are=are: e.tensor_tensor(out=t0[:], in0=nre[:], in1=are[:], op=ALU.mult), reads=[b_nre, b_are], writes=[b_t0])
                P.dve(lambda e, kre=kre, nim=nim, aim=aim, t0=t0: e.scalar_tensor_tensor(out=kre[:], in0=nim[:], scalar=aim[:, 0:1], in1=t0[:], op0=ALU.mult, op1=ALU.add), reads=[b_nim, b_aim, b_t0], writes=[b_kre])
                P.dve(lambda e, kre=kre, den=den: e.tensor_tensor(out=kre[:], in0=kre[:], in1=den[:], op=ALU.mult), reads=[b_kre, b_den], writes=[b_kre])
                P.dve(lambda e, t0=t0, nre=nre, aim=aim: e.tensor_tensor(out=t0[:], in0=nre[:], in1=aim[:], op=ALU.mult), reads=[b_nre, b_aim], writes=[b_t0])
                P.dve(lambda e, kim=kim, nim=nim, are=are, t0=t0: e.scalar_tensor_tensor(out=kim[:], in0=nim[:], scalar=are[:, 0:1], in1=t0[:], op0=ALU.mult, op1=ALU.subtract), reads=[b_nim, b_are, b_t0], writes=[b_kim])
                P.dve(lambda e, kim=kim, den=den: e.tensor_tensor(out=kim[:], in0=kim[:], in1=den[:], op=ALU.mult), reads=[b_kim, b_den], writes=[b_kim])
                cA, b_cA = C.sb([128, 1])
                cB, b_cB = C.sb([128, 1])
                cC, b_cC = C.sb([128, 1])
                P.dve(lambda e, cA=cA, kre=kre: e.tensor_tensor(out=cA[:], in0=kre[:], in1=ks[:, 0:1], op=ALU.mult), reads=[b_kre, b_ks], writes=[b_cA])
                P.dve(lambda e, cA=cA, kim=kim: e.scalar_tensor_tensor(out=cA[:], in0=kim[:], scalar=ks[:, 1:2], in1=cA[:], op0=ALU.mult, op1=ALU.add), reads=[b_kim, b_ks, b_cA], writes=[b_cA])
                P.dve(lambda e, cB=cB, kre=kre: e.tensor_tensor(out=cB[:], in0=kre[:], in1=ks[:, 1:2], op=ALU.mult), reads=[b_kre, b_ks], writes=[b_cB])
                P.dve(lambda e, cB=cB, kim=kim: e.scalar_tensor_tensor(out=cB[:], in0=kim[:], scalar=ks[:, 0:1], in1=cB[:], op0=ALU.mult, op1=ALU.subtract), reads=[b_kim, b_ks, b_cB], writes=[b_cB])
                P.dve(lambda e, cC=cC, cB=cB: e.tensor_copy(out=cC[:], in_=cB[:]), reads=[b_cB], writes=[b_cC])
                P.dve(lambda e, cB=cB, cC=cC: e.tensor_scalar(out=cB[:], in0=cC[:], scalar1=-1.0, scalar2=None, op0=ALU.mult), reads=[b_cC], writes=[b_cB])
                P.dve(lambda e, thr=thr: e.tensor_scalar(out=ang[:], in0=tau[:], scalar1=thr[:, 0:1], scalar2=None, op0=ALU.mult), reads=[b_tau, b_thr], writes=[b_ang])
                aw, b_aw = _wrap(P, C, ang, b_ang, [128, T], scr=scr, key='A')
                ac, b_ac = _wrap(P, C, aw, b_aw, [128, T], add=PI / 2, scr=scr, key='B')
                St, b_St = C.sb([128, T])
                Ct, b_Ct = C.sb([128, T])
                Rb, b_Rb = C.sb([128, T])
                P.act(lambda e, St=St, aw=aw: e.activation(out=St[:], in_=aw[:], func=AF.Sin), reads=[b_aw], writes=[b_St])
                P.act(lambda e, Ct=Ct, ac=ac: e.activation(out=Ct[:], in_=ac[:], func=AF.Sin), reads=[b_ac], writes=[b_Ct])
                P.dve(lambda e, Rb=Rb, r=r: e.tensor_scalar(out=Rb[:], in0=onesT[:], scalar1=r[:, 0:1], scalar2=None, op0=ALU.mult), reads=[b_onesT, b_r], writes=[b_Rb])
                bre, b_bre = C.sb([128, 16])
                bim, b_bim = C.sb([128, 16])
                for half in range(2):
                    P.dma(lambda e, half=half, bre=bre, g=g: e.dma_start(out=bre[half * 64:(half + 1) * 64, :], in_=prm["b_re"][dr, g, :, :]), writes=[b_bre])
                    P.dma(lambda e, half=half, bim=bim, g=g: e.dma_start(out=bim[half * 64:(half + 1) * 64, :], in_=prm["b_im"][dr, g, :, :]), writes=[b_bim])
                BT = []
                for (c1, b_c1, c2, b_c2) in ((cA, b_cA, cB, b_cB), (cC, b_cC, cA, b_cA)):
                    bp, b_bp = C.sb([128, 128])
                    tt, b_tt = C.sb([128, 16])
                    P.pool(lambda e, bp=bp: e.memset(bp[:], 0.0), writes=[b_bp])
                    P.dve(lambda e, tt=tt, c1=c1, bre=bre: e.tensor_scalar(out=tt[:], in0=bre[:], scalar1=c1[:, 0:1], scalar2=None, op0=ALU.mult), reads=[b_bre, b_c1], writes=[b_tt])
                    P.dve(lambda e, bp=bp, tt=tt, c2=c2, gp=gp, bim=bim: e.scalar_tensor_tensor(out=bp[:, gp * 16:(gp + 1) * 16], in0=bim[:], scalar=c2[:, 0:1], in1=tt[:], op0=ALU.mult, op1=ALU.add),
                          reads=[b_bim, b_c2, b_tt, b_bp], writes=[b_bp])
                    pt, b_pt = ptr[0]
                    bT, b_bT = C.sb([128, 128], BF16)
                    P.pe(lambda e, pt=pt, bp=bp: e.transpose(pt[:], bp[:], idt[:]), reads=[b_bp, b_id], writes=[b_pt])
                    P.act(lambda e, bT=bT, pt=pt: e.activation(out=bT[:], in_=pt[:], func=AF.Copy), reads=[b_pt], writes=[b_bT])
                    BT.append((bT, b_bT))
                cc1, b_cc1 = C.sb([16, 128])
                cc2, b_cc2 = C.sb([16, 128])
                P.dma(lambda e, cc1=cc1, g=g: e.dma_start(out=cc1[:, 0:64], in_=prm["c_re"][dr, g, :, :]), writes=[b_cc1])
                P.dma(lambda e, cc1=cc1, g=g: e.dma_start(out=cc1[:, 64:128], in_=prm["c_im"][dr, g, :, :]), writes=[b_cc1])
                P.dma(lambda e, cc2=cc2, g=g: e.dma_start(out=cc2[:, 0:64], in_=prm["c_im"][dr, g, :, :]), writes=[b_cc2])
                P.dma(lambda e, cc2=cc2, g=g: e.dma_start(out=cc2[:, 64:128], in_=prm["c_re"][dr, g, :, :]), writes=[b_cc2])
                cm1, b_cm1 = C.sb([128, 128], BF16)
                cm2, b_cm2 = C.sb([128, 128], BF16)
                P.pool(lambda e, cm1=cm1: e.memset(cm1[:], 0.0), writes=[b_cm1])
                P.pool(lambda e, cm2=cm2: e.memset(cm2[:], 0.0), writes=[b_cm2])
                pt, b_pt = ptr[1]
                P.pe(lambda e, pt=pt, cc1=cc1: e.transpose(pt[:, 0:16], cc1[:], idt[0:16, 0:16]), reads=[b_cc1, b_id], writes=[b_pt])
                P.dve(lambda e, cm1=cm1, pt=pt, gp=gp: e.tensor_scalar(out=cm1[:, gp * 16:(gp + 1) * 16], in0=pt[:, 0:16], scalar1=ks[:, 2:3], scalar2=None, op0=ALU.mult),
                      reads=[b_pt, b_ks, b_cm1], writes=[b_cm1])
                P.pe(lambda e, pt=pt, cc2=cc2: e.transpose(pt[:, 0:16], cc2[:], idt[0:16, 0:16]), reads=[b_cc2, b_id], writes=[b_pt])
                P.dve(lambda e, cm2=cm2, pt=pt, gp=gp: e.tensor_scalar(out=cm2[:, gp * 16:(gp + 1) * 16], in0=pt[:, 0:16], scalar1=-1.0, scalar2=None, op0=ALU.mult),
                      reads=[b_pt, b_cm2], writes=[b_cm2])
                q_, b_q = C.sb([128, 1])
                rt, b_rt = C.sb([128, 128])
                P.dve(lambda e, q_=q_, St=St: e.tensor_tensor(out=q_[:], in0=St[:, T - 1:T], in1=ks[:, 2:3], op=ALU.mult), reads=[b_St, b_ks], writes=[b_q])
                P.dve(lambda e, rt=rt, Ct=Ct: e.tensor_scalar(out=rt[:], in0=idt[:], scalar1=Ct[:, T - 1:T], scalar2=None, op0=ALU.mult), reads=[b_id, b_Ct], writes=[b_rt])
                P.dve(lambda e, rt=rt, q_=q_: e.scalar_tensor_tensor(out=rt[:], in0=psw[:], scalar=q_[:, 0:1], in1=rt[:], op0=ALU.mult, op1=ALU.add), reads=[b_psw, b_q, b_rt], writes=[b_rt])
                carry, b_carry = C.sb([128, 1])
                P.dve(lambda e, carry=carry: e.memset(carry[:], 0.0), writes=[b_carry])
                G.append(dict(St=(St, b_St), Ct=(Ct, b_Ct), Rb=(Rb, b_Rb), B1=BT[0], B2=BT[1], C1=(cm1, b_cm1), C2=(cm2, b_cm2), RT=(rt, b_rt), carry=(carry, b_carry)))
            uts = [C.sb([128, T]) for _ in range(2)]
            utbs = [C.sb([128, T], BF16) for _ in range(2)]
            pbs = [C.ps([128, T]) for _ in range(2)]
            pss_ = [C.ps([128, T]) for _ in range(2)]
            py, b_py = C.ps([128, T])
            pc, b_pc = C.ps([128, 1])
            NB3 = 3
            t1s = [C.sb([128, T]) for _ in range(NB3)]
            t2s = [C.sb([128, T]) for _ in range(NB3)]
            bps = [C.sb([128, T]) for _ in range(NB3)]
            ws_ = [C.sb([128, T]) for _ in range(NB3)]
            wcs = [C.sb([128, T], BF16) for _ in range(NB3)]
            wss = [C.sb([128, T], BF16) for _ in range(NB3)]
            yos = [C.sb([128, T]) for _ in range(2)]
            yfs = [C.sb([128, T]) for _ in range(2)]
            blocks = list(range(NBK)) if dr == 0 else list(range(NBK - 1, -1, -1))
            rows = slice(u_row + ct * 128, u_row + (ct + 1) * 128)
            rv = (lambda a: a[:, ::-1]) if dr == 1 else (lambda a: a[:])
            items = [(bi, bk, gp) for bi, bk in enumerate(blocks) for gp in range(8)]
            st_ = {}
            SK = 2
            NB4 = 4
            bps = [C.sb([128, T]) for _ in range(NB4)]

            def phaseA(k):
                bi, bk, gp = items[k]
                cs_ = slice(bk * T, (bk + 1) * T)
                if gp == 0:
                    ut, b_ut = uts[bi % 2]
                    utb, b_utb = utbs[bi % 2]
                    P.dma(lambda e, ut=ut, cs_=cs_: e.dma_start(out=ut[:], in_=pT[rows, cs_]), writes=[b_ut])
                    P.act(lambda e, ut=ut, utb=utb: e.activation(out=utb[:], in_=ut[:], func=AF.Copy), reads=[b_ut], writes=[b_utb])
                utb, b_utb = utbs[bi % 2]
                gd = G[gp]
                pb, b_pb = pbs[k % 2]
                pq, b_pq = pss_[k % 2]
                t1, b_t1 = t1s[k % 2]
                t2, b_t2 = t2s[k % 2]
                bp, b_bp = bps[k % NB4]
                P.pe(lambda e, pb=pb, gd=gd, utb=utb: e.matmul(pb[:], gd["B1"][0][:], utb[:], start=True, stop=True), reads=[gd["B1"][1], b_utb], writes=[b_pb])
                P.pe(lambda e, pq=pq, gd=gd, utb=utb: e.matmul(pq[:], gd["B2"][0][:], utb[:], start=True, stop=True), reads=[gd["B2"][1], b_utb], writes=[b_pq])
                P.dve(lambda e, t1=t1, pb=pb, gd=gd: e.tensor_tensor(out=t1[:], in0=rv(pb), in1=gd["Ct"][0][:], op=ALU.mult), reads=[b_pb, gd["Ct"][1]], writes=[b_t1])
                P.dve(lambda e, t2=t2, pq=pq, gd=gd: e.tensor_tensor(out=t2[:], in0=rv(pq), in1=gd["St"][0][:], op=ALU.mult), reads=[b_pq, gd["St"][1]], writes=[b_t2])
                P.pool(lambda e, bp=bp, t1=t1, t2=t2: e.tensor_tensor(out=bp[:], in0=t1[:], in1=t2[:], op=ALU.add), reads=[b_t1, b_t2], writes=[b_bp])

            def phaseB(k):
                bi, bk, gp = items[k]
                cs_ = slice(bk * T, (bk + 1) * T)
                gd = G[gp]
                bp, b_bp = bps[k % NB4]
                w_, b_w = ws_[k % 3]
                wc, b_wc = wcs[k % 3]
                wsn, b_wsn = wss[k % 3]
                P.dve(lambda e, w_=w_, bp=bp, gd=gd: e.tensor_tensor_scan(out=w_[:], data0=gd["Rb"][0][:], data1=bp[:], initial=gd["carry"][0][:, 0:1], op0=ALU.mult, op1=ALU.add),
                      reads=[gd["Rb"][1], b_bp, gd["carry"][1]], writes=[b_w])
                P.pool(lambda e, wc=wc, w_=w_, gd=gd: e.tensor_tensor(out=wc[:], in0=w_[:], in1=gd["Ct"][0][:], op=ALU.mult), reads=[b_w, gd["Ct"][1]], writes=[b_wc])
                P.dve(lambda e, wsn=wsn, w_=w_, gd=gd: e.tensor_tensor(out=wsn[:], in0=w_[:], in1=gd["St"][0][:], op=ALU.mult), reads=[b_w, gd["St"][1]], writes=[b_wsn])
                P.pe(lambda e, gd=gd, wc=wc, gp=gp: e.matmul(py[:], gd["C1"][0][:], wc[:], start=(gp == 0), stop=False), reads=[gd["C1"][1], b_wc], writes=[b_py])
                P.pe(lambda e, gd=gd, wsn=wsn, gp=gp: e.matmul(py[:], gd["C2"][0][:], wsn[:], start=False, stop=(gp == 7)), reads=[gd["C2"][1], b_wsn], writes=[b_py])
                P.pe(lambda e, gd=gd, w_=w_: e.matmul(pc[:], gd["RT"][0][:], w_[:, T - 1:T], start=True, stop=True), reads=[gd["RT"][1], b_w], writes=[b_pc])
                P.act(lambda e, gd=gd: e.activation(out=gd["carry"][0][:], in_=pc[:], func=AF.Copy), reads=[b_pc], writes=[gd["carry"][1]])
                if gp == 7:
                    ut, b_ut = uts[bi % 2]
                    yo, b_yo = yos[bi % 2]
                    if dr == 0:
                        P.act(lambda e, yo=yo: e.activation(out=yo[:], in_=py[:], func=AF.Copy), reads=[b_py], writes=[b_yo])
                        P.dma(lambda e, yo=yo, cs_=cs_: e.dma_start(out=yf[ct * 128:(ct + 1) * 128, cs_], in_=yo[:]), reads=[b_yo], writes=[Buf()])
                    else:
                        yft, b_yft = yfs[bi % 2]
                        P.dma(lambda e, yft=yft, cs_=cs_: e.dma_start(out=yft[:], in_=yf[ct * 128:(ct + 1) * 128, cs_]), writes=[b_yft])
                        P.dve(lambda e, yo=yo, yft=yft: e.tensor_tensor(out=yo[:], in0=py[:, ::-1], in1=yft[:], op=ALU.add), reads=[b_py, b_yft], writes=[b_yo])
                        P.dve(lambda e, yo=yo, ut=ut: e.scalar_tensor_tensor(out=yo[:], in0=ut[:], scalar=dsk[:, ct:ct + 1], in1=yo[:], op0=ALU.mult, op1=ALU.add), reads=[b_ut, b_dsk, b_yo], writes=[b_yo])
                        P.dma(lambda e, yo=yo, cs_=cs_: e.dma_start(out=mixT[out_row + ct * 128:out_row + (ct + 1) * 128, cs_], in_=yo[:]), reads=[b_yo], writes=[Buf()])

            for k in range(len(items) + SK):
                if k < len(items):
                    phaseA(k)
                if k - SK >= 0:
                    phaseB(k - SK)
            C.end()


def stage_out(C, xT, mixT, w_out, gluw, glub, g8, router, ident, x1T, htok, aff, L, even):
    P = C.P
    C.begin()
    TB = min(512, L)
    NB = L // TB
    NTS = TB // 128
    KC = 8 if even else 6
    woutb, _ = C.sb([128, KC, 1024], BF16)
    b_wo = [Buf() for _ in range(KC)]
    for kc in range(KC):
        for c0 in range(0, 1024, 512):
            P.dma(lambda e, kc=kc, c0=c0: e.dma_start(out=woutb[:, kc, c0:c0 + 512], in_=w_out[kc * 128:(kc + 1) * 128, c0:c0 + 512]), writes=[b_wo[kc]], q="pool")
    if even:
        gluwb, b_gw = C.sb([128, 4, 512], BF16)
        for kc in range(4):
            P.dma(lambda e, kc=kc: e.dma_start(out=gluwb[:, kc, :], in_=gluw[kc * 128:(kc + 1) * 128, :]), writes=[b_gw], q="pool")
        gb, b_gb = C.sb([128, 4])
        P.dma(lambda e: e.dma_start(out=gb[:], in_=glub), writes=[b_gb])
        ngb, b_ngb = C.sb([128, 4])
        P.dve(lambda e: e.tensor_scalar(out=ngb[:], in0=gb[:], scalar1=-1.0, scalar2=None, op0=ALU.mult), reads=[b_gb], writes=[b_ngb])
    gt, b_g = C.sb([128, 8])
    wr, b_wr = C.sb([128, 8, 16])
    idt, b_id = C.sb([128, 128])
    ones, b_ones = C.sb([128, 128], BF16)
    P.dma(lambda e: e.dma_start(out=gt[:], in_=g8), writes=[b_g])
    P.dma(lambda e: e.dma_start(out=wr[:], in_=router.rearrange("(kc p) n -> p kc n", p=128)), writes=[b_wr])
    P.dma(lambda e: e.dma_start(out=idt[:], in_=ident), writes=[b_id])
    P.dve(lambda e: e.memset(ones[:], 1.0), writes=[b_ones])
    epsc, b_epsc = C.sb([128, 1])
    P.dve(lambda e: e.memset(epsc[:], EPS), writes=[b_epsc])
    mix, b_mix = C.sb([128, KC, TB])
    mixb, b_mixb = C.sb([128, KC, TB], BF16)
    xt, b_xt = C.sb([128, 8, TB])
    x1t, b_x1 = C.sb([128, 8, TB])
    ht, b_ht = C.sb([128, 8, TB])
    sq, b_sq = C.sb([128, 8, TB], BF16)
    rs, b_rs = C.sb([128, TB])
    yg, b_yg = C.sb([128, 4, TB])
    ygb, b_ygb = C.sb([128, 4, TB], BF16)
    sg, b_sg = C.sb([128, TB])
    hrows = [C.sb([128, 1024]) for _ in range(2)]
    afts = [C.sb([128, 16]) for _ in range(2)]
    ex, b_ex = C.sb([128, 16])
    mx, b_mx = C.sb([128, 1])
    sm, b_sm = C.sb([128, 1])
    pxs = [C.ps([128, TB]) for _ in range(2)]
    pss, b_pss = C.ps([128, TB])
    pz, b_pz = C.ps([128, TB])
    pl, b_pl = C.ps([128, 16])
    ptrs = [C.ps([128, 128]) for _ in range(2)]
    xv = xT.rearrange("(kc p) n -> p kc n", p=128)
    mv = mixT.rearrange("(kc p) n -> p kc n", p=128)
    x1v = x1T.rearrange("(kc p) n -> p kc n", p=128)
    nt = 0
    for tb in range(NB):
        sl = slice(tb * TB, (tb + 1) * TB)
        P.dma(lambda e, sl=sl: e.dma_start(out=mix[:], in_=mv[:, 0:KC, sl]), writes=[b_mix])
        P.dma(lambda e, sl=sl: e.dma_start(out=xt[:], in_=xv[:, :, sl]), writes=[b_xt])
        if even:
            P.act(lambda e: e.activation(out=mixb[:, 0:4, :], in_=mix[:, 0:4, :], func=AF.Copy), reads=[b_mix], writes=[b_mixb])
            P.act(lambda e: e.activation(out=yg[:], in_=mix[:, 4:8, :], func=AF.Gelu), reads=[b_mix], writes=[b_yg])
            P.dve(lambda e: e.tensor_copy(out=ygb[:], in_=yg[:]), reads=[b_yg], writes=[b_ygb])
            for ct in range(4):
                for kc in range(4):
                    P.pe(lambda e, ct=ct, kc=kc: e.matmul(pz[:], gluwb[:, kc, ct * 128:(ct + 1) * 128], ygb[:, kc, :], start=(kc == 0), stop=(kc == 3)),
                         reads=[b_gw, b_ygb], writes=[b_pz])
                P.act(lambda e, ct=ct: e.activation(out=sg[:], in_=pz[:], func=AF.Exp, scale=-1.0, bias=ngb[:, ct:ct + 1]), reads=[b_pz, b_ngb], writes=[b_sg])
                P.pool(lambda e: e.tensor_scalar(out=sg[:], in0=sg[:], scalar1=1.0, scalar2=None, op0=ALU.add), reads=[b_sg], writes=[b_sg])
                P.dve(lambda e: e.reciprocal(out=sg[:], in_=sg[:]), reads=[b_sg], writes=[b_sg])
                P.dve(lambda e, ct=ct: e.tensor_tensor(out=mixb[:, 4 + ct, :], in0=yg[:, ct, :], in1=sg[:], op=ALU.mult), reads=[b_yg, b_sg], writes=[b_mixb])
        else:
            P.act(lambda e: e.activation(out=mixb[:], in_=mix[:], func=AF.Copy), reads=[b_mix], writes=[b_mixb])
        for dt in range(8):
            px, b_px = pxs[dt % 2]
            for kc in range(KC):
                P.pe(lambda e, px=px, dt=dt, kc=kc: e.matmul(px[:], woutb[:, kc, dt * 128:(dt + 1) * 128], mixb[:, kc, :], start=(kc == 0), stop=(kc == KC - 1)),
                     reads=[b_wo[kc], b_mixb], writes=[b_px])
            P.dve(lambda e, px=px, dt=dt: e.tensor_tensor(out=x1t[:, dt, :], in0=px[:], in1=xt[:, dt, :], op=ALU.add), reads=[b_px, b_xt], writes=[b_x1])
        P.dma(lambda e, sl=sl: e.dma_start(out=x1v[:, :, sl], in_=x1t[:]), reads=[b_x1], writes=[Buf()])
        P.pool(lambda e: e.tensor_tensor(out=sq[:], in0=x1t[:], in1=x1t[:], op=ALU.mult), reads=[b_x1], writes=[b_sq])
        for kc in range(8):
            P.pe(lambda e, kc=kc: e.matmul(pss[:], ones[:], sq[:, kc, :], start=(kc == 0), stop=(kc == 7)), reads=[b_sq, b_ones], writes=[b_pss])
        P.act(lambda e: e.activation(out=rs[:], in_=pss[:], func=AF.Ln, scale=1.0 / D, bias=epsc[:, 0:1]), reads=[b_pss, b_epsc], writes=[b_rs])
        P.act(lambda e: e.activation(out=rs[:], in_=rs[:], func=AF.Exp, scale=-0.5), reads=[b_rs], writes=[b_rs])
        for dt in range(8):
            P.dve(lambda e, dt=dt: e.scalar_tensor_tensor(out=ht[:, dt, :], in0=x1t[:, dt, :], scalar=gt[:, dt:dt + 1], in1=rs[:], op0=ALU.mult, op1=ALU.mult),
                  reads=[b_x1, b_g, b_rs], writes=[b_ht])
        for ts in range(NTS):
            tsl = slice(ts * 128, (ts + 1) * 128)
            r0 = tb * TB + ts * 128
            for kc in range(8):
                P.pe(lambda e, kc=kc, tsl=tsl: e.matmul(pl[:], ht[:, kc, tsl], wr[:, kc, :], start=(kc == 0), stop=(kc == 7)), reads=[b_ht, b_wr], writes=[b_pl])
            aft, b_aft = afts[nt % 2]
            hrow, b_hrow = hrows[nt % 2]
            nt += 1
            P.dve(lambda e: e.reduce_max(out=mx[:], in_=pl[:], axis=AX.X), reads=[b_pl], writes=[b_mx])
            P.dve(lambda e: e.tensor_scalar(out=mx[:], in0=mx[:], scalar1=-1.0, scalar2=None, op0=ALU.mult), reads=[b_mx], writes=[b_mx])
            P.act(lambda e: e.activation(out=ex[:], in_=pl[:], func=AF.Exp, bias=mx[:, 0:1], accum_out=sm[:]), reads=[b_pl, b_mx], writes=[b_ex, b_sm])
            P.dve(lambda e: e.reciprocal(out=sm[:], in_=sm[:]), reads=[b_sm], writes=[b_sm])
            P.dve(lambda e, aft=aft: e.tensor_scalar(out=aft[:], in0=ex[:], scalar1=sm[:, 0:1], scalar2=None, op0=ALU.mult), reads=[b_ex, b_sm], writes=[b_aft])
            P.dma(lambda e, aft=aft, r0=r0: e.dma_start(out=aff[r0:r0 + 128, :], in_=aft[:]), reads=[b_aft], writes=[Buf()])
            for kc in range(8):
                ptr, b_ptr = ptrs[kc % 2]
                P.pe(lambda e, ptr=ptr, kc=kc, tsl=tsl: e.transpose(ptr[:], ht[:, kc, tsl], idt[:]), reads=[b_ht, b_id], writes=[b_ptr])
                if kc % 2 == 0:
                    P.act(lambda e, ptr=ptr, kc=kc, hrow=hrow: e.activation(out=hrow[:, kc * 128:(kc + 1) * 128], in_=ptr[:], func=AF.Copy), reads=[b_ptr], writes=[b_hrow])
                else:
                    P.dve(lambda e, ptr=ptr, kc=kc, hrow=hrow: e.tensor_copy(out=hrow[:, kc * 128:(kc + 1) * 128], in_=ptr[:]), reads=[b_ptr], writes=[b_hrow])
            P.dma(lambda e, hrow=hrow, r0=r0: e.dma_start(out=htok[r0:r0 + 128, :], in_=hrow[:]), reads=[b_hrow], writes=[Buf()])
    C.end()


BIG = 1.0e6


def _breg(e, rc, val):
    if 'r' not in rc:
        rc['r'] = e.to_reg(val)
    return rc['r']


def stage_route(C, aff, ustrict, rmask, posd, gmd, L):
    P = C.P
    C.begin()
    NJ = L // 128
    CAP = L // 8
    NCOL = 16 * NJ
    A, b_A = C.sb([128, NJ, 16])
    Ae, b_Ae = C.sb([128, 16, NJ])
    for jc in range(0, NJ, 16):
        je = min(NJ, jc + 16)
        P.dma(lambda e, jc=jc, je=je: e.dma_start(out=A[:, jc:je, :], in_=aff[jc * 128:je * 128, :].rearrange("(j p) e -> p j e", p=128)), writes=[b_A])
    P.dve(lambda e: e.tensor_copy(out=Ae[:], in_=A[:].rearrange("p j e -> p e j")), reads=[b_A], writes=[b_Ae])
    us, b_us = C.sb([128, 128])
    rm, b_rm = C.sb([128, NCOL])
    onesf, b_of = C.sb([128, 128])
    P.dma(lambda e: e.dma_start(out=us[:], in_=ustrict), writes=[b_us])
    P.dma(lambda e: e.dma_start(out=rm[:], in_=rmask), writes=[b_rm])
    P.dve(lambda e: e.memset(onesf[:], 1.0), writes=[b_of])
    lo, b_lo = C.sb([128, 16])
    hi, b_hi = C.sb([128, 16])
    mid, b_mid = C.sb([128, 16])
    cnt, b_cnt = C.sb([128, 16])
    ge, b_ge = C.sb([128, 16])
    d1, b_d1 = C.sb([128, 16])
    cmps = [C.sb([128, NJ]) for _ in range(2)]
    ptot, b_ptot = C.ps([128, 16])
    P.dve(lambda e: e.memset(lo[:], 0.0), writes=[b_lo])
    P.dve(lambda e: e.memset(hi[:], 2.0), writes=[b_hi])
    for it in range(34):
        P.dve(lambda e: e.tensor_tensor(out=mid[:], in0=lo[:], in1=hi[:], op=ALU.add), reads=[b_lo, b_hi], writes=[b_mid])
        P.dve(lambda e: e.tensor_scalar(out=mid[:], in0=mid[:], scalar1=0.5, scalar2=None, op0=ALU.mult), reads=[b_mid], writes=[b_mid])
        for ex in range(16):
            cm, b_cm = cmps[ex % 2]
            P.dve(lambda e, ex=ex, cm=cm: e.tensor_scalar(out=cm[:], in0=Ae[:, ex, :], scalar1=mid[:, ex:ex + 1], scalar2=None, op0=ALU.is_ge, op1=ALU.add, accum_out=cnt[:, ex:ex + 1]),
                  reads=[b_Ae, b_mid], writes=[b_cm, b_cnt])
        P.pe(lambda e: e.matmul(ptot[:], onesf[:], cnt[:], start=True, stop=True), reads=[b_of, b_cnt], writes=[b_ptot])
        P.dve(lambda e: e.tensor_scalar(out=ge[:], in0=ptot[:], scalar1=float(CAP) - 0.5, scalar2=None, op0=ALU.is_ge), reads=[b_ptot], writes=[b_ge])
        P.dve(lambda e: e.tensor_tensor(out=d1[:], in0=mid[:], in1=lo[:], op=ALU.subtract), reads=[b_mid, b_lo], writes=[b_d1])
        P.dve(lambda e: e.tensor_tensor(out=d1[:], in0=d1[:], in1=ge[:], op=ALU.mult), reads=[b_d1, b_ge], writes=[b_d1])
        P.dve(lambda e: e.tensor_tensor(out=lo[:], in0=lo[:], in1=d1[:], op=ALU.add), reads=[b_lo, b_d1], writes=[b_lo])
        P.dve(lambda e: e.tensor_tensor(out=d1[:], in0=hi[:], in1=mid[:], op=ALU.subtract), reads=[b_hi, b_mid], writes=[b_d1])
        P.dve(lambda e: e.tensor_tensor(out=d1[:], in0=d1[:], in1=ge[:], op=ALU.mult), reads=[b_d1, b_ge], writes=[b_d1])
        P.dve(lambda e: e.tensor_tensor(out=hi[:], in0=mid[:], in1=d1[:], op=ALU.add), reads=[b_mid, b_d1], writes=[b_hi])
    Me, b_Me = C.sb([128, 16, NJ])
    gm, b_gm = C.sb([128, 16, NJ])
    for ex in range(16):
        P.dve(lambda e, ex=ex: e.tensor_scalar(out=Me[:, ex, :], in0=Ae[:, ex, :], scalar1=lo[:, ex:ex + 1], scalar2=None, op0=ALU.is_ge), reads=[b_Ae, b_lo], writes=[b_Me])
    P.dve(lambda e: e.tensor_tensor(out=gm[:], in0=Ae[:], in1=Me[:], op=ALU.mult), reads=[b_Ae, b_Me], writes=[b_gm])
    Mf = Me[:].rearrange("p e j -> p (e j)")
    pre, b_pre = C.sb([128, NCOL])
    cn, b_cn = C.sb([128, NCOL])
    off, b_off = C.sb([128, NCOL])
    pp, b_pp = C.ps([128, min(512, NCOL)])
    pc, b_pc = C.ps([128, min(512, NCOL)])
    CW = min(512, NCOL)
    for c0 in range(0, NCOL, CW):
        P.pe(lambda e, c0=c0: e.matmul(pp[:], us[:], Mf[:, c0:c0 + CW], start=True, stop=True), reads=[b_us, b_Me], writes=[b_pp])
        P.act(lambda e, c0=c0: e.activation(out=pre[:, c0:c0 + CW], in_=pp[:], func=AF.Copy), reads=[b_pp], writes=[b_pre])
        P.pe(lambda e, c0=c0: e.matmul(pc[:], onesf[:], Mf[:, c0:c0 + CW], start=True, stop=True), reads=[b_of, b_Me], writes=[b_pc])
        P.act(lambda e, c0=c0: e.activation(out=cn[:, c0:c0 + CW], in_=pc[:], func=AF.Copy), reads=[b_pc], writes=[b_cn])
    P.dve(lambda e: e.tensor_tensor_scan(out=off[:], data0=rm[:], data1=cn[:], initial=0.0, op0=ALU.mult, op1=ALU.add), reads=[b_rm, b_cn], writes=[b_off])
    P.dve(lambda e: e.tensor_tensor(out=off[:], in0=off[:], in1=cn[:], op=ALU.subtract), reads=[b_off, b_cn], writes=[b_off])
    P.dve(lambda e: e.tensor_tensor(out=pre[:], in0=pre[:], in1=off[:], op=ALU.add), reads=[b_pre, b_off], writes=[b_pre])
    P.dve(lambda e: e.tensor_scalar(out=pre[:], in0=pre[:], scalar1=-BIG, scalar2=None, op0=ALU.add), reads=[b_pre], writes=[b_pre])
    P.dve(lambda e: e.tensor_tensor(out=pre[:], in0=pre[:], in1=Mf, op=ALU.mult), reads=[b_pre, b_Me], writes=[b_pre])
    P.dve(lambda e: e.tensor_scalar(out=pre[:], in0=pre[:], scalar1=BIG, scalar2=None, op0=ALU.add), reads=[b_pre], writes=[b_pre])
    pi, b_pi = C.sb([128, NCOL], I32)
    P.dve(lambda e: e.tensor_copy(out=pi[:], in_=pre[:]), reads=[b_pre], writes=[b_pi])
    P.dma(lambda e: e.dma_start(out=posd, in_=pi[:]), reads=[b_pi], writes=[Buf()])
    P.dma(lambda e: e.dma_start(out=gmd, in_=gm[:].rearrange("p e j -> p (e j)")), reads=[b_gm], writes=[Buf()])
    C.end()


def stage_dispatch(C, htok, posd, xe, L):
    P = C.P
    C.begin()
    NJ = L // 128
    CAP = L // 8
    pi, b_pi = C.sb([128, 16, NJ], I32)
    P.dma(lambda e: e.dma_start(out=pi[:].rearrange("p e j -> p (e j)"), in_=posd), writes=[b_pi])
    rc = {}
    hts = [C.sb([128, 1024]) for _ in range(2)]
    ixs = [C.sb([128, 1], I32) for _ in range(8)]
    ni = 0
    for j in range(NJ):
        ht, b_ht = hts[j % 2]
        P.dma(lambda e, ht=ht, j=j: e.dma_start(out=ht[:], in_=htok[j * 128:(j + 1) * 128, :]), writes=[b_ht])
        for ex in range(16):
            ix, b_ix = ixs[ni % 8]
            ni += 1
            P.dve(lambda e, ix=ix, ex=ex, j=j: e.tensor_copy(out=ix[:], in_=pi[:, ex, j:j + 1]), reads=[b_pi], writes=[b_ix])
            P.dma(lambda e, ht=ht, ix=ix, ex=ex: e.indirect_dma_start(out=xe[ex][:, :], out_offset=bass.IndirectOffsetOnAxis(ap=ix[:, :], axis=0),
                                                                      in_=ht[:, :], in_offset=None, bounds_check=_breg(e, rc, CAP - 1), oob_is_err=False),
                  reads=[b_ht, b_ix], writes=[Buf()], q="pool")
    C.end()


def stage_ffn(C, xe, w1, w3, w2, ye, ident, L, FF):
    P = C.P
    C.begin()
    CAP = L // 8
    SB = min(512, CAP)
    NSB = CAP // SB
    NST = SB // 128
    NF = FF // 128
    idt, b_id = C.sb([128, 128])
    P.dma(lambda e: e.dma_start(out=idt[:], in_=ident), writes=[b_id])
    w1b, _ = C.sb([128, 8, FF], BF16)
    w3b, _ = C.sb([128, 8, FF], BF16)
    w2b, _ = C.sb([128, NF, 1024], BF16)
    b_w1 = [Buf() for _ in range(8)]
    b_w3 = [Buf() for _ in range(8)]
    b_w2 = [Buf() for _ in range(NF)]
    xrs = [C.sb([128, 1024]) for _ in range(2)]
    xeT, b_xeT = C.sb([128, 8, SB], BF16)
    hid, b_hid = C.sb([128, NF, SB], BF16)
    sas = [C.sb([128, SB]) for _ in range(2)]
    yrows = [C.sb([128, 1024]) for _ in range(2)]
    ptrs = [C.ps([128, 128]) for _ in range(2)]
    pas = [C.ps([128, SB]) for _ in range(2)]
    pbs = [C.ps([128, SB]) for _ in range(2)]
    pys = [C.ps([128, 512]) for _ in range(2)]
    nx = 0
    ny = 0
    NSTG = 3
    stg = [C.sb([128, max(FF, 1024)]) for _ in range(NSTG)]
    ns = 0
    for ex in range(16):
        jobs = []
        for kc in range(8):
            jobs.append((w1[ex, kc * 128:(kc + 1) * 128, :], FF, w1b, kc, b_w1[kc]))
            jobs.append((w3[ex, kc * 128:(kc + 1) * 128, :], FF, w3b, kc, b_w3[kc]))
        for fc in range(NF):
            jobs.append((w2[ex, fc * 128:(fc + 1) * 128, :], 1024, w2b, fc, b_w2[fc]))
        for (src, width, dstt, di, b_dst) in jobs:
            sg_, b_sg = stg[ns % NSTG]
            ns += 1
            P.dma(lambda e, sg_=sg_, src=src, width=width: e.dma_start(out=sg_[:, 0:width], in_=src), writes=[b_sg])
            P.act(lambda e, sg_=sg_, dstt=dstt, di=di, width=width: e.activation(out=dstt[:, di, :], in_=sg_[:, 0:width], func=AF.Copy), reads=[b_sg], writes=[b_dst])
        for sb_ in range(NSB):
            for st in range(NST):
                r0 = sb_ * SB + st * 128
                xr, b_xr = xrs[nx % 2]
                nx += 1
                P.dma(lambda e, xr=xr, ex=ex, r0=r0: e.dma_start(out=xr[:], in_=xe[ex][r0:r0 + 128, :]), writes=[b_xr], q="pool")
                for kc in range(8):
                    ptr, b_ptr = ptrs[kc % 2]
                    P.pe(lambda e, ptr=ptr, xr=xr, kc=kc: e.transpose(ptr[:], xr[:, kc * 128:(kc + 1) * 128], idt[:]), reads=[b_xr, b_id], writes=[b_ptr])
                    if kc % 2 == 0:
                        P.act(lambda e, ptr=ptr, kc=kc, st=st: e.activation(out=xeT[:, kc, st * 128:(st + 1) * 128], in_=ptr[:], func=AF.Copy), reads=[b_ptr], writes=[b_xeT])
                    else:
                        P.dve(lambda e, ptr=ptr, kc=kc, st=st: e.tensor_copy(out=xeT[:, kc, st * 128:(st + 1) * 128], in_=ptr[:]), reads=[b_ptr], writes=[b_xeT])
            for ft in range(NF):
                pa, b_pa = pas[ft % 2]
                pb, b_pb = pbs[ft % 2]
                sa, b_sa = sas[ft % 2]
                for kc in range(8):
                    P.pe(lambda e, pa=pa, kc=kc, ft=ft: e.matmul(pa[:], w1b[:, kc, ft * 128:(ft + 1) * 128], xeT[:, kc, :], start=(kc == 0), stop=(kc == 7)), reads=[b_w1[kc], b_xeT], writes=[b_pa])
                for kc in range(8):
                    P.pe(lambda e, pb=pb, kc=kc, ft=ft: e.matmul(pb[:], w3b[:, kc, ft * 128:(ft + 1) * 128], xeT[:, kc, :], start=(kc == 0), stop=(kc == 7)), reads=[b_w3[kc], b_xeT], writes=[b_pb])
                P.act(lambda e, sa=sa, pa=pa: e.activation(out=sa[:], in_=pa[:], func=AF.Exp, scale=-1.0), reads=[b_pa], writes=[b_sa])
                P.dve(lambda e, sa=sa: e.tensor_scalar(out=sa[:], in0=sa[:], scalar1=1.0, scalar2=None, op0=ALU.add), reads=[b_sa], writes=[b_sa])
                P.dve(lambda e, sa=sa: e.reciprocal(out=sa[:], in_=sa[:]), reads=[b_sa], writes=[b_sa])
                P.dve(lambda e, sa=sa, pa=pa: e.tensor_tensor(out=sa[:], in0=pa[:], in1=sa[:], op=ALU.mult), reads=[b_pa, b_sa], writes=[b_sa])
                P.dve(lambda e, sa=sa, pb=pb, ft=ft: e.tensor_tensor(out=hid[:, ft, :], in0=pb[:], in1=sa[:], op=ALU.mult), reads=[b_pb, b_sa], writes=[b_hid])
            for st in range(NST):
                r0 = sb_ * SB + st * 128
                yrow, b_yrow = yrows[ny % 2]
                ny += 1
                for dh in range(2):
                    py, b_py = pys[dh]
                    for fc in range(NF):
                        P.pe(lambda e, py=py, fc=fc, st=st, dh=dh: e.matmul(py[:], hid[:, fc, st * 128:(st + 1) * 128], w2b[:, fc, dh * 512:(dh + 1) * 512], start=(fc == 0), stop=(fc == NF - 1)),
                             reads=[b_hid, b_w2[fc]], writes=[b_py])
                    if dh == 0:
                        P.act(lambda e, py=py, yrow=yrow: e.activation(out=yrow[:, 0:512], in_=py[:], func=AF.Copy), reads=[b_py], writes=[b_yrow])
                    else:
                        P.dve(lambda e, py=py, yrow=yrow: e.tensor_copy(out=yrow[:, 512:1024], in_=py[:]), reads=[b_py], writes=[b_yrow])
                P.dma(lambda e, yrow=yrow, ex=ex, r0=r0: e.dma_start(out=ye[ex][r0:r0 + 128, :], in_=yrow[:]), reads=[b_yrow], writes=[Buf()], q="pool")
    C.end()


def stage_combine(C, ye, posd, gmd, x1T, ident, x2T, outT, gfin, L):
    P = C.P
    C.begin()
    NJ = L // 128
    CAP = L // 8
    idt, b_id = C.sb([128, 128])
    P.dma(lambda e: e.dma_start(out=idt[:], in_=ident), writes=[b_id])
    pi, b_pi = C.sb([128, 16, NJ], I32)
    gm, b_gm = C.sb([128, 16, NJ])
    P.dma(lambda e: e.dma_start(out=pi[:].rearrange("p e j -> p (e j)"), in_=posd), writes=[b_pi])
    P.dma(lambda e: e.dma_start(out=gm[:].rearrange("p e j -> p (e j)"), in_=gmd), writes=[b_gm])
    Gs = [C.sb([128, 1024]) for _ in range(4)]
    for G_, b_G in Gs:
        P.dve(lambda e, G_=G_: e.memset(G_[:], 0.0), writes=[b_G])
    accs = [C.sb([128, 1024]) for _ in range(2)]
    x1s = [C.sb([128, 8, 128]) for _ in range(2)]
    x2s = [C.sb([128, 8, 128]) for _ in range(2)]
    ptrs = [C.ps([128, 128]) for _ in range(2)]
    if outT is not None:
        gt, b_g = C.sb([128, 8])
        ones, b_ones = C.sb([128, 128], BF16)
        sq, b_sq = C.sb([128, 8, 128], BF16)
        rs, b_rs = C.sb([128, 128])
        ots = [C.sb([128, 8, 128]) for _ in range(2)]
        pss, b_pss = C.ps([128, 128])
        P.dma(lambda e: e.dma_start(out=gt[:], in_=gfin), writes=[b_g])
        P.dve(lambda e: e.memset(ones[:], 1.0), writes=[b_ones])
        epsc, b_epsc = C.sb([128, 1])
        P.dve(lambda e: e.memset(epsc[:], EPS), writes=[b_epsc])
        ov = outT.rearrange("(kc p) n -> p kc n", p=128)
    x1v = x1T.rearrange("(kc p) n -> p kc n", p=128)
    x2v = x2T.rearrange("(kc p) n -> p kc n", p=128)
    ng = 0
    rc = {}
    ixs = [C.sb([128, 1], I32) for _ in range(8)]
    for j in range(NJ):
        acc, b_acc = accs[j % 2]
        x1t, b_x1 = x1s[j % 2]
        x2t, b_x2 = x2s[j % 2]
        cs_ = slice(j * 128, (j + 1) * 128)
        P.dma(lambda e, x1t=x1t, cs_=cs_: e.dma_start(out=x1t[:], in_=x1v[:, :, cs_]), writes=[b_x1])
        for ex in range(16):
            G_, b_G = Gs[ng % 4]
            ix, b_ix = ixs[ng % 8]
            ng += 1
            P.act(lambda e, ix=ix, ex=ex, j=j: e.activation(out=ix[:], in_=pi[:, ex, j:j + 1], func=AF.Copy), reads=[b_pi], writes=[b_ix])
            P.dma(lambda e, G_=G_, ex=ex, ix=ix: e.indirect_dma_start(out=G_[:, :], out_offset=None, in_=ye[ex][:, :],
                                                                      in_offset=bass.IndirectOffsetOnAxis(ap=ix[:, :], axis=0), bounds_check=_breg(e, rc, CAP - 1), oob_is_err=False),
                  reads=[b_ix], writes=[b_G], q="pool")
            if ex == 0:
                P.dve(lambda e, acc=acc, G_=G_, ex=ex, j=j: e.tensor_scalar(out=acc[:], in0=G_[:], scalar1=gm[:, ex, j:j + 1], scalar2=None, op0=ALU.mult), reads=[b_G, b_gm], writes=[b_acc])
            else:
                P.dve(lambda e, acc=acc, G_=G_, ex=ex, j=j: e.scalar_tensor_tensor(out=acc[:], in0=G_[:], scalar=gm[:, ex, j:j + 1], in1=acc[:], op0=ALU.mult, op1=ALU.add),
                      reads=[b_G, b_gm, b_acc], writes=[b_acc])
        for kc in range(8):
            ptr, b_ptr = ptrs[kc % 2]
            P.pe(lambda e, ptr=ptr, acc=acc, kc=kc: e.transpose(ptr[:], acc[:, kc * 128:(kc + 1) * 128], idt[:]), reads=[b_acc, b_id], writes=[b_ptr])
            P.dve(lambda e, ptr=ptr, x2t=x2t, x1t=x1t, kc=kc: e.tensor_tensor(out=x2t[:, kc, :], in0=ptr[:], in1=x1t[:, kc, :], op=ALU.add), reads=[b_ptr, b_x1], writes=[b_x2])
        P.dma(lambda e, x2t=x2t, cs_=cs_: e.dma_start(out=x2v[:, :, cs_], in_=x2t[:]), reads=[b_x2], writes=[Buf()])
        if outT is not None:
            ot, b_ot = ots[j % 2]
            P.pool(lambda e, x2t=x2t: e.tensor_tensor(out=sq[:], in0=x2t[:], in1=x2t[:], op=ALU.mult), reads=[b_x2], writes=[b_sq])
            for kc in range(8):
                P.pe(lambda e, kc=kc: e.matmul(pss[:], ones[:], sq[:, kc, :], start=(kc == 0), stop=(kc == 7)), reads=[b_sq, b_ones], writes=[b_pss])
            P.act(lambda e: e.activation(out=rs[:], in_=pss[:], func=AF.Ln, scale=1.0 / D, bias=epsc[:, 0:1]), reads=[b_pss, b_epsc], writes=[b_rs])
            P.act(lambda e: e.activation(out=rs[:], in_=rs[:], func=AF.Exp, scale=-0.5), reads=[b_rs], writes=[b_rs])
            for kc in range(8):
                P.dve(lambda e, ot=ot, x2t=x2t, kc=kc: e.scalar_tensor_tensor(out=ot[:, kc, :], in0=x2t[:, kc, :], scalar=gt[:, kc:kc + 1], in1=rs[:], op0=ALU.mult, op1=ALU.mult),
                      reads=[b_x2, b_g, b_rs], writes=[b_ot])
            P.dma(lambda e, ot=ot, cs_=cs_: e.dma_start(out=ov[:, :, cs_], in_=ot[:]), reads=[b_ot], writes=[Buf()])
    C.end()


DILS = (1, 4, 16)
DSPAN = (1, 2, 8)


def dil_masks():
    kk = np.arange(128)[:, None]
    qq = np.arange(128)[None, :]
    ms = []
    for g, d in enumerate(DILS):
        for dl in range(-DSPAN[g], DSPAN[g] + 1):
            rel = 128 * dl + kk - qq
            ms.append(((rel % d == 0) & (np.abs(rel) <= 64 * d)).astype(np.float32))
    return np.stack(ms)


def stage_vprep(C, pT, ident, vtok, L, v_row):
    P = C.P
    C.begin()
    TB = min(512, L)
    NT = TB // 128
    idt, b_id = C.sb([128, 128])
    P.dma(lambda e: e.dma_start(out=idt[:], in_=ident), writes=[b_id])
    vts = [C.sb([64, TB]) for _ in range(2)]
    vos = [C.sb([128, NT, 66], BF16) for _ in range(2)]
    for vo, b_vo in vos:
        P.dve(lambda e, vo=vo: e.memset(vo[:], 1.0), writes=[b_vo])
    ptrs = [C.ps([128, 64]) for _ in range(2)]
    it = 0
    for hd in range(12):
        for tb in range(L // TB):
            vt, b_vt = vts[it % 2]
            vo, b_vo = vos[it % 2]
            it += 1
            P.dma(lambda e, vt=vt, hd=hd, tb=tb: e.dma_start(out=vt[:], in_=pT[v_row + hd * 64:v_row + (hd + 1) * 64, tb * TB:(tb + 1) * TB]), writes=[b_vt])
            for t in range(NT):
                ptr, b_ptr = ptrs[t % 2]
                P.pe(lambda e, ptr=ptr, vt=vt, t=t: e.transpose(ptr[:], vt[:, t * 128:(t + 1) * 128], idt[0:64, 0:64]), reads=[b_vt, b_id], writes=[b_ptr])
                if t % 2 == 0:
                    P.act(lambda e, ptr=ptr, vo=vo, t=t: e.activation(out=vo[:, t, 0:64], in_=ptr[:], func=AF.Copy), reads=[b_ptr], writes=[b_vo])
                else:
                    P.dve(lambda e, ptr=ptr, vo=vo, t=t: e.tensor_copy(out=vo[:, t, 0:64], in_=ptr[:]), reads=[b_ptr], writes=[b_vo])
            P.dma(lambda e, vo=vo, hd=hd, tb=tb: e.dma_start(out=vtok[hd][tb * TB:(tb + 1) * TB, :].rearrange("(t p) c -> p t c", p=128), in_=vo[:]), reads=[b_vo], writes=[Buf()])
    C.end()


def stage_qk16(C, pT, qk16, L, nrows):
    P = C.P
    C.begin()
    TB = min(512, L)
    xs = [C.sb([128, TB]) for _ in range(3)]
    ys = [C.sb([128, TB], BF16) for _ in range(3)]
    it = 0
    for rt in range(nrows // 128):
        for tb in range(L // TB):
            x_, b_x = xs[it % 3]
            y_, b_y = ys[it % 3]
            it += 1
            P.dma(lambda e, x_=x_, rt=rt, tb=tb: e.dma_start(out=x_[:], in_=pT[rt * 128:(rt + 1) * 128, tb * TB:(tb + 1) * TB]), writes=[b_x])
            if it % 2 == 0:
                P.act(lambda e, x_=x_, y_=y_: e.activation(out=y_[:], in_=x_[:], func=AF.Copy), reads=[b_x], writes=[b_y])
            else:
                P.dve(lambda e, x_=x_, y_=y_: e.tensor_copy(out=y_[:], in_=x_[:]), reads=[b_x], writes=[b_y])
            P.dma(lambda e, y_=y_, rt=rt, tb=tb: e.dma_start(out=qk16[rt * 128:(rt + 1) * 128, tb * TB:(tb + 1) * TB], in_=y_[:]), reads=[b_y], writes=[Buf()])
    C.end()


def stage_dil(C, pT, ident, masks, vtok, mixT, L, q_row, k_row, out_row):
    P = C.P
    NCH = L // 128
    for i in range(4):
        C.begin()
        idt, b_id = C.sb([128, 128])
        mk, b_mk = C.sb([128, 25, 128], BF16)
        P.dma(lambda e: e.dma_start(out=idt[:], in_=ident), writes=[b_id])
        for m0_ in range(0, 25, 5):
            P.dma(lambda e, m0_=m0_: e.dma_start(out=mk[:, m0_:m0_ + 5, :], in_=masks[m0_:m0_ + 5].rearrange("m p n -> p m n")), writes=[b_mk], q="pool")
        qs = [[C.sb([64, 128], BF16) for _ in range(2)] for _ in range(3)]
        ks = [[C.sb([64, (2 * DSPAN[g] + 1) * 128], BF16) for _ in range(2)] for g in range(3)]
        vs = [[C.sb([128, 2 * DSPAN[g] + 1, 66], BF16) for _ in range(2)] for g in range(3)]
        pss = [C.ps([128, 512]) for _ in range(2)]
        pacc = [C.ps([128, 65]) for _ in range(2)]
        ptr, b_ptr = C.ps([64, 128])
        es = [C.sb([128, 512], BF16) for _ in range(3)]
        pms = [C.sb([128, 512], BF16) for _ in range(3)]
        rd, b_rd = C.sb([128, 1])
        ots = [C.sb([128, 64]) for _ in range(2)]
        oTs = [C.sb([64, 128]) for _ in range(2)]
        moff = (0, 3, 8)
        ngrp = 0
        for n in range(NCH):
            pa, b_pa = pacc[n % 2]
            first = True
            plan = []
            for g in range(3):
                hd = 4 * g + i
                D_ = DSPAN[g]
                m0 = max(0, n - D_)
                m1 = min(NCH - 1, n + D_)
                q_, b_q = qs[g][n % 2]
                k_, b_k = ks[g][n % 2]
                v_, b_v = vs[g][n % 2]
                nm = m1 - m0 + 1
                P.dma(lambda e, q_=q_, hd=hd, n=n: e.dma_start(out=q_[:], in_=pT[q_row + hd * 64:q_row + (hd + 1) * 64, n * 128:(n + 1) * 128]), writes=[b_q])
                P.dma(lambda e, k_=k_, hd=hd, m0=m0, nm=nm: e.dma_start(out=k_[:, 0:nm * 128], in_=pT[k_row + hd * 64:k_row + (hd + 1) * 64, m0 * 128:(m0 + nm) * 128]), writes=[b_k])
                P.dma(lambda e, v_=v_, hd=hd, m0=m0, nm=nm: e.dma_start(out=v_[:, 0:nm, :], in_=vtok[hd][m0 * 128:(m0 + nm) * 128, :].rearrange("(t p) c -> p t c", p=128)), writes=[b_v])
                tiles = [(g, m, m - m0, moff[g] + (m - n) + D_) for m in range(m0, m1 + 1)]
                for c0 in range(0, len(tiles), 4):
                    plan.append((g, tiles[c0:c0 + 4], (q_, b_q), (k_, b_k), (v_, b_v)))
            total = sum(len(p[1]) for p in plan)
            done = 0
            for (g, tl, (q_, b_q), (k_, b_k), (v_, b_v)) in plan:
                ps_, b_ps = pss[ngrp % 2]
                e_, b_e = es[ngrp % 3]
                pm, b_pm = pms[ngrp % 3]
                nt = len(tl)
                for a, (_, m, ml, mi) in enumerate(tl):
                    P.pe(lambda e, ps_=ps_, k_=k_, q_=q_, a=a, ml=ml: e.matmul(ps_[:, a * 128:(a + 1) * 128], k_[:, ml * 128:(ml + 1) * 128], q_[:], start=True, stop=True),
                         reads=[b_k, b_q], writes=[b_ps])
                P.act(lambda e, e_=e_, ps_=ps_, nt=nt: e.activation(out=e_[:, 0:nt * 128], in_=ps_[:, 0:nt * 128], func=AF.Exp, scale=0.125), reads=[b_ps], writes=[b_e])
                mi0 = tl[0][3]
                eng = P.dve if ngrp % 2 == 0 else P.pool
                eng(lambda e, pm=pm, e_=e_, nt=nt, mi0=mi0: e.tensor_tensor(out=pm[:, 0:nt * 128], in0=e_[:, 0:nt * 128], in1=mk[:, mi0:mi0 + nt, :].rearrange("p m n -> p (m n)"), op=ALU.mult),
                    reads=[b_e, b_mk], writes=[b_pm])
                for a, (_, m, ml, mi) in enumerate(tl):
                    done += 1
                    P.pe(lambda e, pa=pa, pm=pm, v_=v_, a=a, ml=ml, st=(done == 1), sp=(done == total): e.matmul(pa[:], pm[:, a * 128:(a + 1) * 128], v_[:, ml, 0:65], start=st, stop=sp),
                         reads=[b_pm, b_v], writes=[b_pa])
                ngrp += 1
            ot, b_ot = ots[n % 2]
            oT, b_oT = oTs[n % 2]
            P.dve(lambda e, pa=pa: e.reciprocal(out=rd[:], in_=pa[:, 64:65]), reads=[b_pa], writes=[b_rd])
            P.dve(lambda e, ot=ot, pa=pa: e.tensor_scalar(out=ot[:], in0=pa[:, 0:64], scalar1=rd[:, 0:1], scalar2=None, op0=ALU.mult), reads=[b_pa, b_rd], writes=[b_ot])
            P.pe(lambda e, ot=ot: e.transpose(ptr[:], ot[:], idt[:]), reads=[b_ot, b_id], writes=[b_ptr])
            P.act(lambda e, oT=oT: e.activation(out=oT[:], in_=ptr[:], func=AF.Copy), reads=[b_ptr], writes=[b_oT])
            P.dma(lambda e, oT=oT, n=n: e.dma_start(out=mixT[out_row + i * 64:out_row + (i + 1) * 64, n * 128:(n + 1) * 128], in_=oT[:]), reads=[b_oT], writes=[Buf()])
        C.end()


def rope_tables(L, half, period):
    inv = (10000.0 ** (-np.arange(half, dtype=np.float32) / half)).astype(np.float32)
    ang = (np.arange(L, dtype=np.float32)[None, :] * inv[:, None]).astype(np.float32)
    cos = np.cos(ang).astype(np.float32)
    sin = np.sin(ang).astype(np.float32)
    rows = np.arange(128) % period
    c = cos[rows % half]
    s = np.where((rows < half)[:, None], -sin[rows % half], sin[rows % half])
    return np.ascontiguousarray(c, np.float32), np.ascontiguousarray(s, np.float32)


def swap_cols(w, ncols, period):
    half = period // 2
    idx = np.arange(ncols)
    src = (idx // period) * period + (idx % period + half) % period
    return np.ascontiguousarray(w[:, src])


def ret_tables(L):
    NCH = L // 128
    s = 128.0 ** -0.5
    tab = np.zeros((128, 24, NCH), np.float64)
    t = np.arange(128, dtype=np.float64)
    for h in range(4):
        lg = np.log1p(-(2.0 ** (-5.0 - h)))
        for dr in range(2):
            e = (t + 1) if dr == 0 else (128 - t)
            j0 = (h * 2 + dr) * 3
            tab[:, j0, :] = np.exp(lg * e)[:, None]
            tab[:, j0 + 1, :] = (np.exp(-lg * e) * s)[:, None]
            tab[:, j0 + 2, :] = np.exp(lg * 128)
    return tab.astype(np.float32)


def la_masks():
    j = np.arange(128)[:, None]
    i = np.arange(128)[None, :]
    return np.stack([(j <= i), (j > i), (j >= i)]).astype(np.float32)


def g8(v):
    return np.ascontiguousarray(v.reshape(8, 128).T)


def s5_host(d, T):
    a_re, a_im = d['s5_a_re'][0], d['s5_a_im'][0]
    are2 = np.concatenate([a_re.reshape(64, 64).T] * 2, 0)
    aim2 = np.concatenate([a_im.reshape(64, 64).T] * 2, 0)
    lstep = np.tile(d['s5_log_step'][0].reshape(1, 64), (128, 1))
    dsk = d['s5_d'][0].reshape(4, 128).T
    prm = {"are2": are2, "aim2": aim2, "lstep": lstep, "dsk": dsk, "b_re": d['s5_b_re'][0], "b_im": d['s5_b_im'][0],
           "c_re": d['s5_c_re'][0], "c_im": d['s5_c_im'][0]}
    psw = np.zeros((128, 128), np.float32)
    for k in range(128):
        psw[k, (k + 64) % 128] = 1
    ksel = np.zeros((128, 3), np.float32)
    ksel[:64, 0] = 1; ksel[64:, 1] = 1; ksel[:64, 2] = 1; ksel[64:, 2] = -1
    tau = np.tile(np.arange(1, T + 1, dtype=np.float32)[None, :], (128, 1))
    consts = {"psw": psw, "ksel": ksel, "tau": tau}
    return {k: np.ascontiguousarray(v, np.float32) for k, v in prm.items()}, consts


def route_consts(L):
    NJ = L // 128
    q = np.arange(128)[:, None]; p = np.arange(128)[None, :]
    us = (q < p).astype(np.float32)
    rm = np.ones((16, NJ), np.float32); rm[:, 0] = 0
    rm = np.tile(rm.reshape(1, -1), (128, 1))
    return us, np.ascontiguousarray(rm)


class RowSplit:
    def __init__(self, C, name, rows, L, chunk=1024):
        self.chunk = chunk
        self.parts = [C.dscr("%s_%d" % (name, k), [min(chunk, rows - k * chunk), L]) for k in range(-(-rows // chunk))]

    def __getitem__(self, key):
        rs, cs = key
        k = rs.start // self.chunk
        assert (rs.stop - 1) // self.chunk == k
        return self.parts[k][rs.start - k * self.chunk:rs.stop - k * self.chunk, cs]


def build_program(L, FF):
    NCH = L // 128
    NJ = L // 128
    CAP = L // 8
    T = min(512, L)
    C = Ctx()
    i = {}
    def din(name, shape, dt=F32):
        i[name] = C.din(name, shape, dt)
        return i[name]
    xT = din("xT", [D, L])
    w_in = [din("w_in0", [D, 2560]), din("w_in1", [D, 4480])]
    wsw = [din("wsw0", [D, 1024]), din("wsw1", [D, 1536])]
    w_out = [din("w_out0", [1024, 1024]), din("w_out1", [768, 1024])]
    gmix = [din("gmix0", [128, 8]), din("gmix1", [128, 8])]
    gffn = [din("gffn0", [128, 8]), din("gffn1", [128, 8])]
    gfin = din("gfin", [128, 8])
    cos = [din("cos0", [128, L]), din("cos1", [128, L])]
    sin = [din("sin0", [128, L]), din("sin1", [128, L])]
    ident = din("ident", [128, 128])
    lam = din("lamask", [3, 128, 128])
    rtab = din("rtab", [128, 24, NCH])
    prm_shapes = {"are2": [128, 64], "aim2": [128, 64], "lstep": [128, 64], "dsk": [128, 4], "b_re": [2, 32, 64, 16], "b_im": [2, 32, 64, 16],
                  "c_re": [2, 32, 16, 64], "c_im": [2, 32, 16, 64]}
    prm = {}
    for k, shp in prm_shapes.items():
        a = din("s5_" + k, shp)
        prm[k] = a[:, :] if len(shp) == 2 else a
    consts = {"ident": ident[:, :], "psw": din("c_psw", [128, 128])[:, :], "ksel": din("c_ksel", [128, 3])[:, :], "tau": din("c_tau", [128, T])[:, :]}
    gluw = din("gluw", [512, 512])
    glub = din("glub", [128, 4])
    router = [din("router0", [1024, 16]), din("router1", [1024, 16])]
    ustrict = din("ustrict", [128, 128])
    rmask = din("rmask", [128, 16 * NJ])
    mlb = din("mlb", [128, 16])
    dmasks = din("dmasks", [25, 128, 128])
    w1 = [din("w1_0", [16, 1024, FF]), din("w1_1", [16, 1024, FF])]
    w3 = [din("w3_0", [16, 1024, FF]), din("w3_1", [16, 1024, FF])]
    w2 = [din("w2_0", [16, FF, 1024]), din("w2_1", [16, FF, 1024])]
    outT = C.dout("outT", [D, L])
    pT = RowSplit(C, "pT", 4480, L)
    mixT = C.dscr("mixT", [1024, L])
    o1 = C.dscr("o1", [4, L, 128])
    yf = C.dscr("yf", [512, L])
    x1T = C.dscr("x1T", [1024, L])
    x2T = C.dscr("x2T", [1024, L])
    x3T = C.dscr("x3T", [1024, L])
    htok = C.dscr("htok", [L, 1024])
    aff = C.dscr("aff", [L, 16])
    posd = C.dscr("posd", [128, 16 * NJ], I32)
    gmd = C.dscr("gmd", [128, 16 * NJ])
    xe = [C.dscr("xe%d" % e, [CAP, 1024]) for e in range(16)]
    ye = [C.dscr("ye%d" % e, [CAP, 1024]) for e in range(16)]
    tabd = C.dscr("tabd", [128, 24, NCH])
    vtok = [C.dscr("vtok%d" % h, [L, 66], BF16) for h in range(12)]
    qk16 = C.dscr("qk16", [1536, L], BF16)

    def moe(layer, xin1T, xoutT, final):
        stage_route(C, aff, ustrict[:, :], rmask[:, :], posd[:, :], gmd[:, :], L)
        stage_dispatch(C, htok, posd[:, :], xe, L)
        stage_ffn(C, xe, w1[layer], w3[layer], w2[layer], ye, ident[:, :], L, FF)
        stage_combine(C, ye, posd[:, :], gmd[:, :], xin1T, ident[:, :], xoutT, outT if final else None, gfin[:, :], L)

    stage_proj(C, xT, w_in[0], wsw[0], gmix[0][:, :], cos[0], sin[0], pT, L, 20, 8)
    stage_linattn(C, pT, rtab[:, :, :], ident[:, :], lam, o1, mixT, L, 0, 512, 1024, 1536, 0, False)
    stage_s5(C, pT, prm, consts, yf, mixT, L, 2048, 512)
    stage_out(C, xT, mixT, w_out[0], gluw, glub[:, :], gffn[0][:, :], router[0], ident[:, :], x1T, htok, aff, L, True)
    moe(0, x1T, x2T, False)
    stage_proj(C, x2T, w_in[1], wsw[1], gmix[1][:, :], cos[1], sin[1], pT, L, 35, 12)
    stage_gates(C, pT, mlb[:, :], ident[:, :], tabd, L, 4352)
    stage_linattn(C, pT, tabd[:, :, :], ident[:, :], lam, o1, mixT, L, 2304, 2816, 3328, 3840, 256, True)
    stage_vprep(C, pT, ident[:, :], vtok, L, 1536)
    stage_qk16(C, pT, qk16, L, 1536)
    stage_dil(C, qk16, ident[:, :], dmasks, vtok, mixT, L, 0, 768, 0)
    stage_out(C, x2T, mixT, w_out[1], None, None, gffn[1][:, :], router[1], ident[:, :], x1T, htok, aff, L, False)
    moe(1, x1T, x3T, True)
    ninst = C.P.ninst
    return C.finish(), ninst


def host_inputs(inp, b, L, FF):
    f = lambda a: np.ascontiguousarray(a, np.float32)
    T = min(512, L)
    im = {"xT": f(inp["x"][b].T)}
    W0 = inp["ev_w_in"][0]
    W1 = np.zeros((D, 4480), np.float32)
    W1[:, :4368] = inp["od_w_in"][0]
    im["w_in0"] = f(W0); im["w_in1"] = W1
    im["wsw0"] = swap_cols(W0[:, :1024], 1024, 128); im["wsw1"] = swap_cols(W1[:, :1536], 1536, 64)
    im["w_out0"] = f(inp["ev_w_out"][0]); im["w_out1"] = f(inp["od_w_out"][0])
    for l in range(2):
        im["gmix%d" % l] = g8(inp["norm_mix_g"][l]); im["gffn%d" % l] = g8(inp["norm_ffn_g"][l])
        im["router%d" % l] = f(inp["moe_router"][l])
        im["w1_%d" % l] = f(inp["moe_w1"][l]); im["w3_%d" % l] = f(inp["moe_w3"][l]); im["w2_%d" % l] = f(inp["moe_w2"][l])
    im["gfin"] = g8(inp["final_g"])
    im["cos0"], im["sin0"] = rope_tables(L, 64, 128)
    im["cos1"], im["sin1"] = rope_tables(L, 32, 64)
    im["ident"] = np.eye(128, dtype=np.float32)
    im["lamask"] = la_masks()
    im["rtab"] = ret_tables(L)
    prm, consts = s5_host({k: inp[k] for k in ("s5_a_re", "s5_a_im", "s5_b_re", "s5_b_im", "s5_c_re", "s5_c_im", "s5_log_step", "s5_d")}, T)
    for k, v in prm.items():
        im["s5_" + k] = v
    for k, v in consts.items():
        im["c_" + k] = f(v)
    im["gluw"] = f(inp["s5_glu_w"][0]); im["glub"] = f(inp["s5_glu_b"][0].reshape(4, 128).T)
    im["ustrict"], im["rmask"] = route_consts(L)
    im["mlb"] = f(np.tile(np.concatenate([inp["ml_i_bias"][0].reshape(-1), inp["ml_f_bias"][0].reshape(-1)])[None, :], (128, 1)))
    im["dmasks"] = dil_masks()
    return im


def run_model(inp, batches):
    L = inp["x"].shape[1]
    FF = inp["moe_w1"].shape[-1]
    nc, ninst = build_program(L, FF)
    ims = [host_inputs(inp, b, L, FF) for b in batches]
    res = run_bass_kernel_spmd(nc, ims, core_ids=list(range(len(batches))))
    return [np.ascontiguousarray(r["outT"].T) for r in res.results]


def kernel(**inputs):
    inp = {k: np.asarray(v) for k, v in inputs.items()}
    B = inp["x"].shape[0]
    outs = run_model(inp, list(range(B)))
    return np.stack(outs, 0).astype(np.float32)
```

```python
import contextlib
import numpy as np
import concourse.bass as bass
import concourse.mybir as mybir
from concourse.bass_utils import run_bass_kernel_spmd

F32 = mybir.dt.float32
BF16 = mybir.dt.bfloat16
I32 = mybir.dt.int32
AF = mybir.ActivationFunctionType
ALU = mybir.AluOpType
AX = mybir.AxisListType
D = 1024
EPS = 1e-6
ENGS = ("pe", "act", "dve", "pool", "sp")
ENGOBJ = {"pe": "tensor", "act": "scalar", "dve": "vector", "pool": "gpsimd", "sp": "sync"}


class Buf:
    __slots__ = ("last_w", "readers")

    def __init__(self):
        self.last_w = None
        self.readers = []


class Op:
    __slots__ = ("eng", "fn", "idx", "deps", "dma", "signal", "cnt", "sem", "semval", "stage")


class Prog:
    ND = {"sp": 10, "act": 2, "pool": 6}

    def __init__(self, nc, st):
        self.nc = nc
        self.esem = {e: st.enter_context(nc.semaphore("s_" + e)) for e in ENGS}
        self.dsem = {(q, k): st.enter_context(nc.semaphore("d_%s%d" % (q, k))) for q in self.ND for k in range(self.ND[q])}
        self.base = {e: 0 for e in ENGS}
        self.dk = {q: 0 for q in self.ND}
        self.dval = {k: 0 for k in self.dsem}
        self.waited = {e: {} for e in ENGS}
        self.stage = 0
        self.ops = {e: [] for e in ENGS}
        self.ninst = 0

    def op(self, eng, fn, reads=(), writes=(), dma=False):
        o = Op()
        o.eng, o.fn, o.idx, o.dma, o.signal, o.cnt, o.stage = eng, fn, len(self.ops[eng]), dma, False, 0, self.stage
        o.sem, o.semval = None, 0
        deps = {}
        for b in reads:
            if b.last_w is not None and b.last_w.stage == self.stage:
                deps[id(b.last_w)] = b.last_w
        for b in writes:
            if b.last_w is not None and b.last_w.stage == self.stage:
                deps[id(b.last_w)] = b.last_w
            for r in b.readers:
                if r.stage == self.stage:
                    deps[id(r)] = r
        o.deps = list(deps.values())
        for b in reads:
            b.readers.append(o)
        for b in writes:
            b.last_w = o
            b.readers = []
        if dma:
            k = self.dk[eng]
            self.dk[eng] += 1
            nd = self.ND[eng]
            o.sem = (eng, k % nd)
            o.semval = 16 * (k // nd + 1)
            self.dval[o.sem] = o.semval
        self.ops[eng].append(o)
        self.ninst += 1
        return o

    def pe(self, fn, reads=(), writes=()):
        return self.op("pe", fn, reads, writes)

    def act(self, fn, reads=(), writes=()):
        return self.op("act", fn, reads, writes)

    def dve(self, fn, reads=(), writes=()):
        return self.op("dve", fn, reads, writes)

    def pool(self, fn, reads=(), writes=()):
        return self.op("pool", fn, reads, writes)

    def dma(self, fn, reads=(), writes=(), q="sp"):
        return self.op(q, fn, reads, writes, dma=True)

    def end_stage(self):
        nc = self.nc
        for e in ENGS:
            comp = [o for o in self.ops[e] if not o.dma]
            if comp:
                comp[-1].signal = True
            for o in self.ops[e]:
                for d in o.deps:
                    if d.dma:
                        continue
                    if d.eng == o.eng:
                        if e == "pe":
                            continue
                        if o.idx - d.idx > 2 and not o.dma:
                            continue
                    d.signal = True
        final = {}
        for e in ENGS:
            c = self.base[e]
            for o in self.ops[e]:
                if o.dma:
                    continue
                if o.signal:
                    c += 1
                o.cnt = c
            final[e] = c
        with nc.Block() as block:
            def run_engine(e, eng):
                waited = self.waited[e]

                def need(sem, key, val):
                    if waited.get(key, 0) < val:
                        eng.wait_ge(sem, val)
                        waited[key] = val

                for o in self.ops[e]:
                    for d in o.deps:
                        if d.dma:
                            need(self.dsem[d.sem], d.sem, d.semval)
                        else:
                            if d.eng == e:
                                if e == "pe":
                                    continue
                                if o.idx - d.idx > 2 and not o.dma:
                                    continue
                            need(self.esem[d.eng], d.eng, d.cnt)
                    if o.dma:
                        if o.semval > 16:
                            need(self.dsem[o.sem], o.sem, o.semval - 16)
                        inst = o.fn(eng)
                        inst.then_inc(self.dsem[o.sem], 16)
                    else:
                        inst = o.fn(eng)
                        if o.signal:
                            inst.then_inc(self.esem[e], 1)
                for e2 in ENGS:
                    if e2 != e and final[e2] > 0:
                        need(self.esem[e2], e2, final[e2])
                for k, v in self.dval.items():
                    if v > 0:
                        need(self.dsem[k], k, v)

            for e in ENGS:
                def _f(eng, e=e):
                    run_engine(e, eng)
                getattr(block, ENGOBJ[e])(_f)
        self.base = final
        self.stage += 1
        self.ops = {e: [] for e in ENGS}


class Ctx:
    def __init__(self):
        self.nc = bass.Bass("TRN2", target_bir_lowering=False)
        self.gst = contextlib.ExitStack()
        self.P = Prog(self.nc, self.gst)
        self.st = None
        self.n = 0
        self.dbg = {}

    def din(self, name, shape, dt=F32):
        return self.nc.dram_tensor(name, list(shape), dt, kind="ExternalInput").ap()

    def dout(self, name, shape, dt=F32):
        return self.nc.dram_tensor(name, list(shape), dt, kind="ExternalOutput").ap()

    def dscr(self, name, shape, dt=F32, debug=False):
        if debug:
            return self.dout(name, shape, dt)
        return self.nc.dram_tensor(name, list(shape), dt, kind="Internal").ap()

    def begin(self):
        self.st = contextlib.ExitStack()

    def end(self):
        self.P.end_stage()
        self.st.close()
        self.st = None

    def sb(self, shape, dt=F32):
        self.n += 1
        t = self.st.enter_context(self.nc.sbuf_tensor("t%d" % self.n, list(shape), dt))
        return t, Buf()

    def ps(self, shape, dt=F32):
        self.n += 1
        t = self.st.enter_context(self.nc.psum_tensor("p%d" % self.n, list(shape), dt))
        return t, Buf()

    def finish(self):
        self.gst.close()
        return self.nc


def stage_proj(C, xT, w, wsw, g8, cos, sin, pT, L, NCT, NRT):
    P = C.P
    C.begin()
    TB = min(512, L)
    NB = L // TB
    wb, _ = C.sb([128, 8, NCT * 128], BF16)
    wswb, _ = C.sb([128, 8, NRT * 128], BF16)
    b_w = [Buf() for _ in range(8)]
    b_ws = [Buf() for _ in range(8)]
    gt, b_g = C.sb([128, 8])
    ones, b_ones = C.sb([128, 128], BF16)
    P.dma(lambda e: e.dma_start(out=gt[:], in_=g8), writes=[b_g])
    P.dve(lambda e: e.memset(ones[:], 1.0), writes=[b_ones])
    epsc, b_epsc = C.sb([128, 1])
    P.dve(lambda e: e.memset(epsc[:], EPS), writes=[b_epsc])
    CH = 640
    for kc in range(8):
        for c0 in range(0, NCT * 128, CH):
            P.dma(lambda e, kc=kc, c0=c0: e.dma_start(out=wb[:, kc, c0:c0 + CH], in_=w[kc * 128:(kc + 1) * 128, c0:c0 + CH]),
                  writes=[b_w[kc]], q="pool")
        for c0 in range(0, NRT * 128, 512):
            P.dma(lambda e, kc=kc, c0=c0: e.dma_start(out=wswb[:, kc, c0:c0 + 512], in_=wsw[kc * 128:(kc + 1) * 128, c0:c0 + 512]),
                  writes=[b_ws[kc]], q="pool")
    nxb = 2 if NCT <= 24 else 1
    xts = [C.sb([128, 8, TB]) for _ in range(nxb)]
    xbs = [C.sb([128, 8, TB], BF16) for _ in range(nxb)]
    sq, b_sq = C.sb([128, 8, TB], BF16)
    css = [C.sb([128, TB]) for _ in range(2)]
    sns = [C.sb([128, TB]) for _ in range(2)]
    rss = [C.sb([128, TB]) for _ in range(2)]
    pss, b_pss = C.ps([128, TB])
    ps_a = [C.ps([128, TB]) for _ in range(2)]
    ps_b = [C.ps([128, TB]) for _ in range(2)]
    t1s = [C.sb([128, TB]) for _ in range(2)]
    t2s = [C.sb([128, TB]) for _ in range(2)]
    os_ = [C.sb([128, TB]) for _ in range(4)]
    b_out = Buf()
    xv = xT.rearrange("(kc p) n -> p kc n", p=128)
    no = 0
    na = 0
    for tb in range(NB):
        sl = slice(tb * TB, (tb + 1) * TB)
        xt, b_x = xts[tb % nxb]
        xb, b_xb = xbs[tb % nxb]
        cs, b_cs = css[tb % 2]
        sn, b_sn = sns[tb % 2]
        rs, b_rs = rss[tb % 2]
        P.dma(lambda e, xt=xt, sl=sl: e.dma_start(out=xt[:], in_=xv[:, :, sl]), writes=[b_x])
        if NRT:
            P.dma(lambda e, cs=cs, sl=sl: e.dma_start(out=cs[:], in_=cos[:, sl]), writes=[b_cs])
            P.dma(lambda e, sn=sn, sl=sl: e.dma_start(out=sn[:], in_=sin[:, sl]), writes=[b_sn])
        P.pool(lambda e, xt=xt: e.tensor_tensor(out=sq[:], in0=xt[:], in1=xt[:], op=ALU.mult), reads=[b_x], writes=[b_sq])
        for kc in range(8):
            eng = P.dve if kc % 2 == 0 else P.pool
            eng(lambda e, kc=kc, xt=xt, xb=xb: e.tensor_scalar(out=xb[:, kc, :], in0=xt[:, kc, :], scalar1=gt[:, kc:kc + 1], scalar2=None, op0=ALU.mult),
                reads=[b_x, b_g], writes=[b_xb])
        for kc in range(8):
            P.pe(lambda e, kc=kc: e.matmul(pss[:], ones[:], sq[:, kc, :], start=(kc == 0), stop=(kc == 7)),
                 reads=[b_sq, b_ones], writes=[b_pss])
        P.act(lambda e, rs=rs: e.activation(out=rs[:], in_=pss[:], func=AF.Ln, scale=1.0 / D, bias=epsc[:, 0:1]), reads=[b_pss, b_epsc], writes=[b_rs])
        P.act(lambda e, rs=rs: e.activation(out=rs[:], in_=rs[:], func=AF.Exp, scale=-0.5), reads=[b_rs], writes=[b_rs])
        if NRT:
            P.pool(lambda e, cs=cs, rs=rs: e.tensor_tensor(out=cs[:], in0=cs[:], in1=rs[:], op=ALU.mult), reads=[b_cs, b_rs], writes=[b_cs])
            P.pool(lambda e, sn=sn, rs=rs: e.tensor_tensor(out=sn[:], in0=sn[:], in1=rs[:], op=ALU.mult), reads=[b_sn, b_rs], writes=[b_sn])
        for ct in range(NCT):
            pa, b_pa = ps_a[na % 2]
            pb, b_pb = ps_b[na % 2]
            na += 1
            o, b_o = os_[no % 4]
            no += 1
            for kc in range(8):
                P.pe(lambda e, kc=kc, ct=ct, pa=pa, xb=xb: e.matmul(pa[:], wb[:, kc, ct * 128:(ct + 1) * 128], xb[:, kc, :], start=(kc == 0), stop=(kc == 7)),
                     reads=[b_w[kc], b_xb], writes=[b_pa])
            if ct < NRT:
                for kc in range(8):
                    P.pe(lambda e, kc=kc, ct=ct, pb=pb, xb=xb: e.matmul(pb[:], wswb[:, kc, ct * 128:(ct + 1) * 128], xb[:, kc, :], start=(kc == 0), stop=(kc == 7)),
                         reads=[b_ws[kc], b_xb], writes=[b_pb])
                t1, b_t1 = t1s[ct % 2]
                t2, b_t2 = t2s[ct % 2]
                P.dve(lambda e, t1=t1, pa=pa, cs=cs: e.tensor_tensor(out=t1[:], in0=pa[:], in1=cs[:], op=ALU.mult), reads=[b_pa, b_cs], writes=[b_t1])
                P.dve(lambda e, t2=t2, pb=pb, sn=sn: e.tensor_tensor(out=t2[:], in0=pb[:], in1=sn[:], op=ALU.mult), reads=[b_pb, b_sn], writes=[b_t2])
                P.pool(lambda e, o=o, t1=t1, t2=t2: e.tensor_tensor(out=o[:], in0=t1[:], in1=t2[:], op=ALU.add), reads=[b_t1, b_t2], writes=[b_o])
            else:
                P.dve(lambda e, o=o, pa=pa, rs=rs: e.tensor_tensor(out=o[:], in0=pa[:], in1=rs[:], op=ALU.mult), reads=[b_pa, b_rs], writes=[b_o])
            P.dma(lambda e, o=o, ct=ct, sl=sl: e.dma_start(out=pT[ct * 128:(ct + 1) * 128, sl], in_=o[:]), reads=[b_o], writes=[Buf()])
    C.end()


def stage_gates(C, pT, mlb, ident, tabd, L, mg_row):
    P = C.P
    C.begin()
    NCH = L // 128
    s = 128.0 ** -0.5
    idt, b_id = C.sb([128, 128])
    mb, b_mb = C.sb([128, 16])
    nfb, b_nfb = C.sb([128, 8])
    lns, b_lns = C.sb([128, 1])
    onesq, b_onesq = C.sb([128, 128])
    P.dma(lambda e: e.dma_start(out=idt[:], in_=ident), writes=[b_id])
    P.dma(lambda e: e.dma_start(out=mb[:], in_=mlb), writes=[b_mb])
    P.dve(lambda e: e.tensor_scalar(out=nfb[:], in0=mb[:, 8:16], scalar1=-1.0, scalar2=None, op0=ALU.mult), reads=[b_mb], writes=[b_nfb])
    P.dve(lambda e: e.memset(lns[:], float(np.log(s))), writes=[b_lns])
    P.dve(lambda e: e.memset(onesq[:], 1.0), writes=[b_onesq])
    b_out = Buf()
    gps = [C.ps([128, 128]) for _ in range(3)]
    for h in range(4):
        for dr in range(2):
            gi, b_gi = C.sb([128, 128])
            gf, b_gf = C.sb([128, 128])
            ri = mg_row + dr * 4 + h
            rf = mg_row + 8 + dr * 4 + h
            P.dma(lambda e, gi=gi, ri=ri: e.dma_start(out=gi[0:NCH, :], in_=pT[ri:ri + 1, :].rearrange("o (c t) -> (o c) t", t=128)), writes=[b_gi])
            P.dma(lambda e, gf=gf, rf=rf: e.dma_start(out=gf[0:NCH, :], in_=pT[rf:rf + 1, :].rearrange("o (c t) -> (o c) t", t=128)), writes=[b_gf])
            sp, b_sp = C.sb([128, 128])
            csp, b_csp = C.sb([128, 128])
            col = dr * 4 + h
            P.act(lambda e, sp=sp, gf=gf, col=col: e.activation(out=sp[0:NCH, :], in_=gf[0:NCH, :], func=AF.Exp, scale=-1.0, bias=nfb[0:NCH, col:col + 1]),
                  reads=[b_gf, b_nfb], writes=[b_sp])
            P.act(lambda e, sp=sp: e.activation(out=sp[0:NCH, :], in_=sp[0:NCH, :], func=AF.Ln, scale=1.0, bias=1.0), reads=[b_sp], writes=[b_sp])
            P.dve(lambda e, csp=csp, sp=sp: e.tensor_tensor_scan(out=csp[0:NCH, :], data0=onesq[0:NCH, :], data1=sp[0:NCH, :], initial=0.0, op0=ALU.mult, op1=ALU.add),
                  reads=[b_sp, b_onesq], writes=[b_csp])
            ex, b_ex = C.sb([128, 128])
            if dr == 0:
                P.dve(lambda e, ex=ex, csp=csp: e.tensor_copy(out=ex[0:NCH, :], in_=csp[0:NCH, :]), reads=[b_csp], writes=[b_ex])
            else:
                P.dve(lambda e, ex=ex, sp=sp, csp=csp: e.tensor_tensor(out=ex[0:NCH, :], in0=sp[0:NCH, :], in1=csp[0:NCH, :], op=ALU.subtract), reads=[b_sp, b_csp], writes=[b_ex])
                P.dve(lambda e, ex=ex, csp=csp: e.tensor_scalar(out=ex[0:NCH, :], in0=ex[0:NCH, :], scalar1=csp[0:NCH, 127:128], scalar2=None, op0=ALU.add), reads=[b_ex, b_csp], writes=[b_ex])
            av, b_av = C.sb([128, 128])
            cv, b_cv = C.sb([128, 128])
            ebc, b_ebc = C.sb([128, 1])
            P.act(lambda e, av=av, ex=ex: e.activation(out=av[0:NCH, :], in_=ex[0:NCH, :], func=AF.Exp, scale=-1.0), reads=[b_ex], writes=[b_av])
            P.dve(lambda e, cv=cv, gi=gi, ex=ex: e.tensor_tensor(out=cv[0:NCH, :], in0=gi[0:NCH, :], in1=ex[0:NCH, :], op=ALU.add), reads=[b_gi, b_ex], writes=[b_cv])
            P.dve(lambda e, cv=cv, col=col: e.tensor_scalar(out=cv[0:NCH, :], in0=cv[0:NCH, :], scalar1=mb[0:NCH, col:col + 1], scalar2=lns[0:NCH, 0:1], op0=ALU.add, op1=ALU.add),
                  reads=[b_cv, b_mb, b_lns], writes=[b_cv])
            P.act(lambda e, cv=cv: e.activation(out=cv[0:NCH, :], in_=cv[0:NCH, :], func=AF.Exp), reads=[b_cv], writes=[b_cv])
            P.act(lambda e, ebc=ebc, csp=csp: e.activation(out=ebc[0:NCH, :], in_=csp[0:NCH, 127:128], func=AF.Exp, scale=-1.0), reads=[b_csp], writes=[b_ebc])
            tb_, b_tb = C.sb([128, 3, NCH])
            for k, (src, b_src) in enumerate(((av, b_av), (cv, b_cv))):
                pt, b_pt = gps[k]
                P.pe(lambda e, pt=pt, src=src: e.transpose(pt[:, 0:NCH], src[0:NCH, :], idt[0:NCH, 0:NCH]), reads=[b_src, b_id], writes=[b_pt])
                P.act(lambda e, pt=pt, k=k, tb_=tb_: e.activation(out=tb_[:, k, :], in_=pt[:, 0:NCH], func=AF.Copy), reads=[b_pt], writes=[b_tb])
            tm, b_tm = C.sb([128, 128])
            P.dve(lambda e, tm=tm, ebc=ebc: e.tensor_scalar(out=tm[0:NCH, :], in0=onesq[0:NCH, :], scalar1=ebc[0:NCH, 0:1], scalar2=None, op0=ALU.mult),
                  reads=[b_onesq, b_ebc], writes=[b_tm])
            pt, b_pt = gps[2]
            P.pe(lambda e, pt=pt, tm=tm: e.matmul(pt[:, 0:NCH], tm[0:NCH, :], idt[0:NCH, 0:NCH], start=True, stop=True), reads=[b_tm, b_id], writes=[b_pt])
            P.act(lambda e, pt=pt, tb_=tb_: e.activation(out=tb_[:, 2, :], in_=pt[:, 0:NCH], func=AF.Copy), reads=[b_pt], writes=[b_tb])
            j0 = (h * 2 + dr) * 3
            P.dma(lambda e, tb_=tb_, j0=j0: e.dma_start(out=tabd[:, j0:j0 + 3, :], in_=tb_[:]), reads=[b_tb], writes=[Buf()])
    C.end()


def stage_linattn(C, pT, tab, ident, masks, o1, mixT, L, q_row, k_row, v_row, g_row, out_row, mlstm):
    P = C.P
    C.begin()
    NCH = L // 128
    NV = 129 if mlstm else 128
    idt, b_id = C.sb([128, 128])
    mk, b_mk = C.sb([128, 3, 128])
    tb_, b_tb = C.sb([128, 24, NCH])
    P.dma(lambda e: e.dma_start(out=idt[:], in_=ident), writes=[b_id])
    P.dma(lambda e: e.dma_start(out=mk[:], in_=masks.rearrange("m p n -> p m n")), writes=[b_mk])
    P.dma(lambda e: e.dma_start(out=tb_[:], in_=tab), writes=[b_tb])
    epsc, b_epsc = C.sb([128, 1])
    P.dve(lambda e: e.memset(epsc[:], EPS), writes=[b_epsc])
    NBUF = 4
    qTs = [C.sb([128, 128]) for _ in range(NBUF)]
    kTs = [C.sb([128, 128]) for _ in range(NBUF)]
    vTs = [C.sb([128, 128]) for _ in range(NBUF)]
    gTs = [C.sb([128, 128]) for _ in range(NBUF)]
    hfs = [C.sb([128, 128]) for _ in range(NBUF)]
    ktoks = [C.sb([128, 128]) for _ in range(NBUF)]
    vpps = [C.sb([128, NV]) for _ in range(NBUF)]
    sms = [C.sb([128, 128]) for _ in range(NBUF)]
    os_ = [C.sb([128, NV]) for _ in range(NBUF)]
    hs = [C.sb([128, 128]) for _ in range(NBUF)]
    gas = [C.sb([128, 128]) for _ in range(NBUF)]
    outs = [C.sb([128, 128]) for _ in range(NBUF)]
    p_kt = [C.ps([128, 128]) for _ in range(1)]
    p_vt = [C.ps([128, 128]) for _ in range(1)]
    p_s = [C.ps([128, 128]) for _ in range(2)]
    p_o = [C.ps([128, NV]) for _ in range(2)]
    p_kv = [C.ps([128, NV]) for _ in range(1)]
    p_tr = [C.ps([128, 128]) for _ in range(1)]
    cst, b_cst = C.sb([128, NV])
    tmpc, b_tmpc = C.sb([128, NV])
    st6, b_st6 = C.sb([128, 6])
    mv, b_mv = C.sb([128, 2])
    rsd, b_rsd = C.sb([128, 1])
    dn, b_dn = C.sb([128, 1])
    b_o1 = [[Buf() for _ in range(NCH)] for _ in range(4)]
    csts = [(cst, b_cst)] + [C.sb([128, NV]) for _ in range(3)]
    SK = 2
    for dr in range(2):
        mi = 0 if dr == 0 else (2 if mlstm else 1)
        for h in range(4):
            P.dve(lambda e, c_=csts[h][0]: e.memset(c_[:], 0.0), writes=[csts[h][1]])
        order = list(range(NCH)) if dr == 0 else list(range(NCH - 1, -1, -1))
        items = [(n, h) for n in order for h in range(4)]

        def phaseA(k, dr=dr, mi=mi, items=items):
            n, h = items[k]
            j0 = (h * 2 + dr) * 3
            i = k % NBUF
            cs_ = slice(n * 128, (n + 1) * 128)
            qT, b_q = qTs[i]
            kT, b_k = kTs[i]
            vT, b_v = vTs[i]
            P.dma(lambda e: e.dma_start(out=qT[:], in_=pT[q_row + h * 128:q_row + (h + 1) * 128, cs_]), writes=[b_q])
            P.dma(lambda e: e.dma_start(out=kT[:], in_=pT[k_row + h * 128:k_row + (h + 1) * 128, cs_]), writes=[b_k])
            P.dma(lambda e: e.dma_start(out=vT[:], in_=pT[v_row + h * 128:v_row + (h + 1) * 128, cs_]), writes=[b_v])
            pk, b_pk = p_kt[0]
            pv, b_pv = p_vt[0]
            ktok, b_kt = ktoks[i]
            vpp, b_vp = vpps[i]
            P.pe(lambda e: e.transpose(pk[:], kT[:], idt[:]), reads=[b_k, b_id], writes=[b_pk])
            P.pe(lambda e: e.transpose(pv[:], vT[:], idt[:]), reads=[b_v, b_id], writes=[b_pv])
            P.act(lambda e: e.activation(out=ktok[:], in_=pk[:], func=AF.Copy), reads=[b_pk], writes=[b_kt])
            P.dve(lambda e: e.tensor_scalar(out=vpp[:, 0:128], in0=pv[:], scalar1=tb_[:, j0 + 1, n:n + 1], scalar2=None, op0=ALU.mult), reads=[b_pv, b_tb], writes=[b_vp])
            if mlstm:
                P.act(lambda e: e.activation(out=vpp[:, 128:129], in_=tb_[:, j0 + 1, n:n + 1], func=AF.Copy), reads=[b_tb], writes=[b_vp])
            ps_, b_ps = p_s[k % 2]
            sm, b_sm = sms[i]
            P.pe(lambda e: e.matmul(ps_[:], kT[:], qT[:], start=True, stop=True), reads=[b_k, b_q], writes=[b_ps])
            P.dve(lambda e: e.tensor_tensor(out=sm[:], in0=ps_[:], in1=mk[:, mi, :], op=ALU.mult), reads=[b_ps, b_mk], writes=[b_sm])
            if dr == 1:
                hf, b_hf = hfs[i]
                gT, b_g = gTs[i]
                ga, b_ga = gas[i]
                P.dma(lambda e: e.dma_start(out=hf[:], in_=o1[h, cs_, :]), reads=[b_o1[h][n]], writes=[b_hf])
                P.dma(lambda e: e.dma_start(out=gT[:], in_=pT[g_row + h * 128:g_row + (h + 1) * 128, cs_]), writes=[b_g])
                P.act(lambda e: e.activation(out=ga[:], in_=gT[:], func=AF.Exp, scale=-1.0), reads=[b_g], writes=[b_ga])
                P.pool(lambda e: e.tensor_scalar(out=ga[:], in0=ga[:], scalar1=1.0, scalar2=None, op0=ALU.add), reads=[b_ga], writes=[b_ga])
                P.dve(lambda e: e.reciprocal(out=ga[:], in_=ga[:]), reads=[b_ga], writes=[b_ga])
                if not mlstm:
                    P.pool(lambda e: e.tensor_tensor(out=ga[:], in0=ga[:], in1=gT[:], op=ALU.mult), reads=[b_ga, b_g], writes=[b_ga])

        def phaseB(k, dr=dr, items=items):
            n, h = items[k]
            j0 = (h * 2 + dr) * 3
            i = k % NBUF
            cs_ = slice(n * 128, (n + 1) * 128)
            cst, b_cst = csts[h]
            qT, b_q = qTs[i]
            ktok, b_kt = ktoks[i]
            vpp, b_vp = vpps[i]
            sm, b_sm = sms[i]
            po, b_po = p_o[k % 2]
            P.pe(lambda e: e.matmul(po[:], sm[:], vpp[:], start=True, stop=False), reads=[b_sm, b_vp], writes=[b_po])
            P.pe(lambda e: e.matmul(po[:], qT[:], cst[:], start=False, stop=True), reads=[b_q, b_cst], writes=[b_po])
            o_, b_o = os_[i]
            P.act(lambda e: e.activation(out=o_[:], in_=po[:], func=AF.Copy, scale=tb_[:, j0, n:n + 1]), reads=[b_po, b_tb], writes=[b_o])
            pkv, b_pkv = p_kv[0]
            P.pe(lambda e: e.matmul(pkv[:], ktok[:], vpp[:], start=True, stop=True), reads=[b_kt, b_vp], writes=[b_pkv])
            P.dve(lambda e: e.tensor_tensor(out=tmpc[:], in0=pkv[:], in1=cst[:], op=ALU.add), reads=[b_pkv, b_cst], writes=[b_tmpc])
            P.dve(lambda e: e.tensor_scalar(out=cst[:], in0=tmpc[:], scalar1=tb_[:, j0 + 2, n:n + 1], scalar2=None, op0=ALU.mult), reads=[b_tmpc, b_tb], writes=[b_cst])
            hh, b_h = hs[i]
            if mlstm:
                P.dve(lambda e: e.tensor_scalar(out=dn[:], in0=o_[:, 128:129], scalar1=-1.0, scalar2=1.0, op0=ALU.mult, op1=ALU.max), reads=[b_o], writes=[b_dn])
                P.dve(lambda e: e.tensor_tensor(out=dn[:], in0=dn[:], in1=o_[:, 128:129], op=ALU.max), reads=[b_dn, b_o], writes=[b_dn])
                P.dve(lambda e: e.reciprocal(out=dn[:], in_=dn[:]), reads=[b_dn], writes=[b_dn])
                P.dve(lambda e: e.tensor_scalar(out=hh[:], in0=o_[:, 0:128], scalar1=dn[:, 0:1], scalar2=None, op0=ALU.mult), reads=[b_o, b_dn], writes=[b_h])
                src, b_src = hh, b_h
            else:
                src, b_src = o_, b_o
            if dr == 0:
                P.dma(lambda e: e.dma_start(out=o1[h, cs_, :], in_=src[:, 0:128]), reads=[b_src], writes=[b_o1[h][n]])
            else:
                hf, b_hf = hfs[i]
                ga, b_ga = gas[i]
                P.pool(lambda e: e.tensor_tensor(out=hf[:], in0=hf[:], in1=src[:, 0:128], op=ALU.add), reads=[b_hf, b_src], writes=[b_hf])
                P.dve(lambda e: e.bn_stats(out=st6[:], in_=hf[:]), reads=[b_hf], writes=[b_st6])
                P.dve(lambda e: e.bn_aggr(out=mv[:], in_=st6[:]), reads=[b_st6], writes=[b_mv])
                P.act(lambda e: e.activation(out=rsd[:], in_=mv[:, 1:2], func=AF.Ln, scale=1.0, bias=epsc[:, 0:1]), reads=[b_mv, b_epsc], writes=[b_rsd])
                P.act(lambda e: e.activation(out=rsd[:], in_=rsd[:], func=AF.Exp, scale=-0.5), reads=[b_rsd], writes=[b_rsd])
                P.dve(lambda e: e.tensor_scalar(out=hf[:], in0=hf[:], scalar1=mv[:, 0:1], scalar2=rsd[:, 0:1], op0=ALU.subtract, op1=ALU.mult), reads=[b_hf, b_mv, b_rsd], writes=[b_hf])
                ptr, b_ptr = p_tr[0]
                P.pe(lambda e: e.transpose(ptr[:], hf[:], idt[:]), reads=[b_hf, b_id], writes=[b_ptr])
                ot, b_ot = outs[i]
                P.dve(lambda e: e.tensor_tensor(out=ot[:], in0=ptr[:], in1=ga[:], op=ALU.mult), reads=[b_ptr, b_ga], writes=[b_ot])
                P.dma(lambda e: e.dma_start(out=mixT[out_row + h * 128:out_row + (h + 1) * 128, cs_], in_=ot[:]), reads=[b_ot], writes=[Buf()])

        for k in range(len(items) + SK):
            if k < len(items):
                phaseA(k)
            if k - SK >= 0:
                phaseB(k - SK)
    C.end()


PI = float(np.pi)


def _wrap(P, C, x, b_x, shape, add=0.0, scr=None, key="a"):
    if scr is not None and ("u" + key) in scr:
        (u, b_u), (ki, b_ki), (kf, b_kf), (y, b_y), (m, b_m) = [scr[n + key] for n in "uikym"]
    else:
        u, b_u = C.sb(shape)
        ki, b_ki = C.sb(shape, I32)
        kf, b_kf = C.sb(shape)
        y, b_y = C.sb(shape)
        m, b_m = C.sb(shape)
        if scr is not None:
            for n, v in zip("uikym", ((u, b_u), (ki, b_ki), (kf, b_kf), (y, b_y), (m, b_m))):
                scr[n + key] = v
    P.dve(lambda e: e.tensor_scalar(out=u[:], in0=x[:], scalar1=add, scalar2=1.0 / (2 * PI), op0=ALU.add, op1=ALU.mult), reads=[b_x], writes=[b_u])
    P.dve(lambda e: e.tensor_copy(out=ki[:], in_=u[:]), reads=[b_u], writes=[b_ki])
    P.dve(lambda e: e.tensor_copy(out=kf[:], in_=ki[:]), reads=[b_ki], writes=[b_kf])
    P.dve(lambda e: e.tensor_scalar(out=u[:], in0=x[:], scalar1=add, scalar2=None, op0=ALU.add), reads=[b_x, b_kf], writes=[b_u])
    P.dve(lambda e: e.scalar_tensor_tensor(out=y[:], in0=kf[:], scalar=-2 * PI, in1=u[:], op0=ALU.mult, op1=ALU.add), reads=[b_kf, b_u], writes=[b_y])
    P.dve(lambda e: e.tensor_scalar(out=m[:], in0=y[:], scalar1=PI, scalar2=-2 * PI, op0=ALU.is_gt, op1=ALU.mult), reads=[b_y], writes=[b_m])
    P.dve(lambda e: e.tensor_tensor(out=y[:], in0=y[:], in1=m[:], op=ALU.add), reads=[b_y, b_m], writes=[b_y])
    P.dve(lambda e: e.tensor_scalar(out=m[:], in0=y[:], scalar1=-PI, scalar2=2 * PI, op0=ALU.is_lt, op1=ALU.mult), reads=[b_y], writes=[b_m])
    P.dve(lambda e: e.tensor_tensor(out=y[:], in0=y[:], in1=m[:], op=ALU.add), reads=[b_y, b_m], writes=[b_y])
    P.dve(lambda e: e.tensor_scalar(out=y[:], in0=y[:], scalar1=-PI, scalar2=PI, op0=ALU.max, op1=ALU.min), reads=[b_y], writes=[b_y])
    return y, b_y


def stage_s5(C, pT, prm, consts, yf, mixT, L, u_row, out_row):
    P = C.P
    T = min(512, L)
    NBK = L // T
    for dr in range(2):
        for ct in range(4):
            C.begin()
            idt, b_id = C.sb([128, 128])
            psw, b_psw = C.sb([128, 128])
            tau, b_tau = C.sb([128, T])
            ks, b_ks = C.sb([128, 3])
            lst, b_lst = C.sb([128, 64])
            dsk, b_dsk = C.sb([128, 4])
            onesT, b_onesT = C.sb([128, T])
            P.dma(lambda e: e.dma_start(out=idt[:], in_=consts["ident"]), writes=[b_id])
            P.dma(lambda e: e.dma_start(out=psw[:], in_=consts["psw"]), writes=[b_psw])
            P.dma(lambda e: e.dma_start(out=tau[:], in_=consts["tau"]), writes=[b_tau])
            P.dma(lambda e: e.dma_start(out=ks[:], in_=consts["ksel"]), writes=[b_ks])
            P.dma(lambda e: e.dma_start(out=lst[:], in_=prm["lstep"]), writes=[b_lst])
            P.dma(lambda e: e.dma_start(out=dsk[:], in_=prm["dsk"]), writes=[b_dsk])
            P.dve(lambda e: e.memset(onesT[:], 1.0), writes=[b_onesT])
            G = []
            scr = {}
            ang, b_ang = C.sb([128, T])
            are2, b_are2 = C.sb([128, 64])
            aim2, b_aim2 = C.sb([128, 64])
            P.dma(lambda e: e.dma_start(out=are2[:], in_=prm['are2']), writes=[b_are2])
            P.dma(lambda e: e.dma_start(out=aim2[:], in_=prm['aim2']), writes=[b_aim2])
            ptr = [C.ps([128, 128]) for _ in range(2)]
            for gp in range(8):
                g = ct * 8 + gp
                are, b_are = C.sb([128, 1])
                aim, b_aim = C.sb([128, 1])
                P.dve(lambda e, are=are, cg=dr * 32 + g: e.tensor_copy(out=are[:], in_=are2[:, cg:cg + 1]), reads=[b_are2], writes=[b_are])
                P.dve(lambda e, aim=aim, cg=dr * 32 + g: e.tensor_copy(out=aim[:], in_=aim2[:, cg:cg + 1]), reads=[b_aim2], writes=[b_aim])
                dl, b_dl = C.sb([128, 1])
                r, b_r = C.sb([128, 1])
                th, b_th = C.sb([128, 1])
                col = dr * 32 + g
                P.act(lambda e, dl=dl, col=col: e.activation(out=dl[:], in_=lst[:, col:col + 1], func=AF.Exp), reads=[b_lst], writes=[b_dl])
                P.act(lambda e, r=r, are=are, dl=dl: e.activation(out=r[:], in_=are[:], func=AF.Exp, scale=dl[:, 0:1]), reads=[b_are, b_dl], writes=[b_r])
                P.dve(lambda e, th=th, aim=aim, dl=dl: e.tensor_tensor(out=th[:], in0=aim[:], in1=dl[:], op=ALU.mult), reads=[b_aim, b_dl], writes=[b_th])
                thr0, b_thr0 = _wrap(P, C, th, b_th, [128, 1], scr=scr, key='c')
                thr, b_thr = C.sb([128, 1])
                P.dve(lambda e, thr=thr, thr0=thr0: e.tensor_copy(out=thr[:], in_=thr0[:]), reads=[b_thr0], writes=[b_thr])
                thc, b_thc = _wrap(P, C, thr, b_thr, [128, 1], add=PI / 2, scr=scr, key='d')
                s0, b_s0 = C.sb([128, 1])
                c0, b_c0 = C.sb([128, 1])
                P.act(lambda e, s0=s0, thr=thr: e.activation(out=s0[:], in_=thr[:], func=AF.Sin), reads=[b_thr], writes=[b_s0])
                P.act(lambda e, c0=c0, thc=thc: e.activation(out=c0[:], in_=thc[:], func=AF.Sin), reads=[b_thc], writes=[b_c0])
                nre, b_nre = C.sb([128, 1])
                nim, b_nim = C.sb([128, 1])
                den, b_den = C.sb([128, 1])
                t0, b_t0 = C.sb([128, 1])
                kre, b_kre = C.sb([128, 1])
                kim, b_kim = C.sb([128, 1])
                P.dve(lambda e, nre=nre, r=r, c0=c0: e.tensor_tensor(out=nre[:], in0=r[:], in1=c0[:], op=ALU.mult), reads=[b_r, b_c0], writes=[b_nre])
                P.dve(lambda e, nre=nre: e.tensor_scalar(out=nre[:], in0=nre[:], scalar1=-1.0, scalar2=None, op0=ALU.add), reads=[b_nre], writes=[b_nre])
                P.dve(lambda e, nim=nim, r=r, s0=s0: e.tensor_tensor(out=nim[:], in0=r[:], in1=s0[:], op=ALU.mult), reads=[b_r, b_s0], writes=[b_nim])
                P.dve(lambda e, den=den, are=are: e.tensor_tensor(out=den[:], in0=are[:], in1=are[:], op=ALU.mult), reads=[b_are], writes=[b_den])
                P.dve(lambda e, den=den, aim=aim: e.scalar_tensor_tensor(out=den[:], in0=aim[:], scalar=aim[:, 0:1], in1=den[:], op0=ALU.mult, op1=ALU.add), reads=[b_aim, b_den], writes=[b_den])
                P.dve(lambda e, den=den: e.reciprocal(out=den[:], in_=den[:]), reads=[b_den], writes=[b_den])
                P.dve(lambda e, t0=t0, nre=nre, are=are: e.tensor_tensor(out=t0[:], in0=nre[:], in1=are[:], op=ALU.mult), reads=[b_nre, b_are], writes=[b_t0])
                P.dve(lambda e, kre=kre, nim=nim, aim=aim, t0=t0: e.scalar_tensor_tensor(out=kre[:], in0=nim[:], scalar=aim[:, 0:1], in1=t0[:], op0=ALU.mult, op1=ALU.add), reads=[b_nim, b_aim, b_t0], writes=[b_kre])
                P.dve(lambda e, kre=kre, den=den: e.tensor_tensor(out=kre[:], in0=kre[:], in1=den[:], op=ALU.mult), reads=[b_kre, b_den], writes=[b_kre])
                P.dve(lambda e, t0=t0, nre=nre, aim=aim: e.tensor_tensor(out=t0[:], in0=nre[:], in1=aim[:], op=ALU.mult), reads=[b_nre, b_aim], writes=[b_t0])
                P.dve(lambda e, kim=kim, nim=nim, are=are, t0=t0: e.scalar_tensor_tensor(out=kim[:], in0=nim[:], scalar=are[:, 0:1], in1=t0[:], op0=ALU.mult, op1=ALU.subtract), reads=[b_nim, b_are, b_t0], writes=[b_kim])
                P.dve(lambda e, kim=kim, den=den: e.tensor_tensor(out=kim[:], in0=kim[:], in1=den[:], op=ALU.mult), reads=[b_kim, b_den], writes=[b_kim])
                cA, b_cA = C.sb([128, 1])
                cB, b_cB = C.sb([128, 1])
                cC, b_cC = C.sb([128, 1])
                P.dve(lambda e, cA=cA, kre=kre: e.tensor_tensor(out=cA[:], in0=kre[:], in1=ks[:, 0:1], op=ALU.mult), reads=[b_kre, b_ks], writes=[b_cA])
                P.dve(lambda e, cA=cA, kim=kim: e.scalar_tensor_tensor(out=cA[:], in0=kim[:], scalar=ks[:, 1:2], in1=cA[:], op0=ALU.mult, op1=ALU.add), reads=[b_kim, b_ks, b_cA], writes=[b_cA])
                P.dve(lambda e, cB=cB, kre=kre: e.tensor_tensor(out=cB[:], in0=kre[:], in1=ks[:, 1:2], op=ALU.mult), reads=[b_kre, b_ks], writes=[b_cB])
                P.dve(lambda e, cB=cB, kim=kim: e.scalar_tensor_tensor(out=cB[:], in0=kim[:], scalar=ks[:, 0:1], in1=cB[:], op0=ALU.mult, op1=ALU.subtract), reads=[b_kim, b_ks, b_cB], writes=[b_cB])
                P.dve(lambda e, cC=cC, cB=cB: e.tensor_copy(out=cC[:], in_=cB[:]), reads=[b_cB], writes=[b_cC])
                P.dve(lambda e, cB=cB, cC=cC: e.tensor_scalar(out=cB[:], in0=cC[:], scalar1=-1.0, scalar2=None, op0=ALU.mult), reads=[b_cC], writes=[b_cB])
                P.dve(lambda e, thr=thr: e.tensor_scalar(out=ang[:], in0=tau[:], scalar1=thr[:, 0:1], scalar2=None, op0=ALU.mult), reads=[b_tau, b_thr], writes=[b_ang])
                aw, b_aw = _wrap(P, C, ang, b_ang, [128, T], scr=scr, key='A')
                ac, b_ac = _wrap(P, C, aw, b_aw, [128, T], add=PI / 2, scr=scr, key='B')
                St, b_St = C.sb([128, T])
                Ct, b_Ct = C.sb([128, T])
                Rb, b_Rb = C.sb([128, T])
                P.act(lambda e, St=St, aw=aw: e.activation(out=St[:], in_=aw[:], func=AF.Sin), reads=[b_aw], writes=[b_St])
                P.act(lambda e, Ct=Ct, ac=ac: e.activation(out=Ct[:], in_=ac[:], func=AF.Sin), reads=[b_ac], writes=[b_Ct])
                P.dve(lambda e, Rb=Rb, r=r: e.tensor_scalar(out=Rb[:], in0=onesT[:], scalar1=r[:, 0:1], scalar2=None, op0=ALU.mult), reads=[b_onesT, b_r], writes=[b_Rb])
                bre, b_bre = C.sb([128, 16])
                bim, b_bim = C.sb([128, 16])
                for half in range(2):
                    P.dma(lambda e, half=half, bre=bre, g=g: e.dma_start(out=bre[half * 64:(half + 1) * 64, :], in_=prm["b_re"][dr, g, :, :]), writes=[b_bre])
                    P.dma(lambda e, half=half, bim=bim, g=g: e.dma_start(out=bim[half * 64:(half + 1) * 64, :], in_=prm["b_im"][dr, g, :, :]), writes=[b_bim])
                BT = []
                for (c1, b_c1, c2, b_c2) in ((cA, b_cA, cB, b_cB), (cC, b_cC, cA, b_cA)):
                    bp, b_bp = C.sb([128, 128])
                    tt, b_tt = C.sb([128, 16])
                    P.pool(lambda e, bp=bp: e.memset(bp[:], 0.0), writes=[b_bp])
                    P.dve(lambda e, tt=tt, c1=c1, bre=bre: e.tensor_scalar(out=tt[:], in0=bre[:], scalar1=c1[:, 0:1], scalar2=None, op0=ALU.mult), reads=[b_bre, b_c1], writes=[b_tt])
                    P.dve(lambda e, bp=bp, tt=tt, c2=c2, gp=gp, bim=bim: e.scalar_tensor_tensor(out=bp[:, gp * 16:(gp + 1) * 16], in0=bim[:], scalar=c2[:, 0:1], in1=tt[:], op0=ALU.mult, op1=ALU.add),
                          reads=[b_bim, b_c2, b_tt, b_bp], writes=[b_bp])
                    pt, b_pt = ptr[0]
                    bT, b_bT = C.sb([128, 128], BF16)
                    P.pe(lambda e, pt=pt, bp=bp: e.transpose(pt[:], bp[:], idt[:]), reads=[b_bp, b_id], writes=[b_pt])
                    P.act(lambda e, bT=bT, pt=pt: e.activation(out=bT[:], in_=pt[:], func=AF.Copy), reads=[b_pt], writes=[b_bT])
                    BT.append((bT, b_bT))
                cc1, b_cc1 = C.sb([16, 128])
                cc2, b_cc2 = C.sb([16, 128])
                P.dma(lambda e, cc1=cc1, g=g: e.dma_start(out=cc1[:, 0:64], in_=prm["c_re"][dr, g, :, :]), writes=[b_cc1])
                P.dma(lambda e, cc1=cc1, g=g: e.dma_start(out=cc1[:, 64:128], in_=prm["c_im"][dr, g, :, :]), writes=[b_cc1])
                P.dma(lambda e, cc2=cc2, g=g: e.dma_start(out=cc2[:, 0:64], in_=prm["c_im"][dr, g, :, :]), writes=[b_cc2])
                P.dma(lambda e, cc2=cc2, g=g: e.dma_start(out=cc2[:, 64:128], in_=prm["c_re"][dr, g, :, :]), writes=[b_cc2])
                cm1, b_cm1 = C.sb([128, 128], BF16)
                cm2, b_cm2 = C.sb([128, 128], BF16)
                P.pool(lambda e, cm1=cm1: e.memset(cm1[:], 0.0), writes=[b_cm1])
                P.pool(lambda e, cm2=cm2: e.memset(cm2[:], 0.0), writes=[b_cm2])
                pt, b_pt = ptr[1]
                P.pe(lambda e, pt=pt, cc1=cc1: e.transpose(pt[:, 0:16], cc1[:], idt[0:16, 0:16]), reads=[b_cc1, b_id], writes=[b_pt])
                P.dve(lambda e, cm1=cm1, pt=pt, gp=gp: e.tensor_scalar(out=cm1[:, gp * 16:(gp + 1) * 16], in0=pt[:, 0:16], scalar1=ks[:, 2:3], scalar2=None, op0=ALU.mult),
                      reads=[b_pt, b_ks, b_cm1], writes=[b_cm1])
                P.pe(lambda e, pt=pt, cc2=cc2: e.transpose(pt[:, 0:16], cc2[:], idt[0:16, 0:16]), reads=[b_cc2, b_id], writes=[b_pt])
                P.dve(lambda e, cm2=cm2, pt=pt, gp=gp: e.tensor_scalar(out=cm2[:, gp * 16:(gp + 1) * 16], in0=pt[:, 0:16], scalar1=-1.0, scalar2=None, op0=ALU.mult),
                      reads=[b_pt, b_cm2], writes=[b_cm2])
                q_, b_q = C.sb([128, 1])
                rt, b_rt = C.sb([128, 128])
                P.dve(lambda e, q_=q_, St=St: e.tensor_tensor(out=q_[:], in0=St[:, T - 1:T], in1=ks[:, 2:3], op=ALU.mult), reads=[b_St, b_ks], writes=[b_q])
                P.dve(lambda e, rt=rt, Ct=Ct: e.tensor_scalar(out=rt[:], in0=idt[:], scalar1=Ct[:, T - 1:T], scalar2=None, op0=ALU.mult), reads=[b_id, b_Ct], writes=[b_rt])
                P.dve(lambda e, rt=rt, q_=q_: e.scalar_tensor_tensor(out=rt[:], in0=psw[:], scalar=q_[:, 0:1], in1=rt[:], op0=ALU.mult, op1=ALU.add), reads=[b_psw, b_q, b_rt], writes=[b_rt])
                carry, b_carry = C.sb([128, 1])
                P.dve(lambda e, carry=carry: e.memset(carry[:], 0.0), writes=[b_carry])
                G.append(dict(St=(St, b_St), Ct=(Ct, b_Ct), Rb=(Rb, b_Rb), B1=BT[0], B2=BT[1], C1=(cm1, b_cm1), C2=(cm2, b_cm2), RT=(rt, b_rt), carry=(carry, b_carry)))
            uts = [C.sb([128, T]) for _ in range(2)]
            utbs = [C.sb([128, T], BF16) for _ in range(2)]
            pbs = [C.ps([128, T]) for _ in range(2)]
            pss_ = [C.ps([128, T]) for _ in range(2)]
            py, b_py = C.ps([128, T])
            pc, b_pc = C.ps([128, 1])
            NB3 = 3
            t1s = [C.sb([128, T]) for _ in range(NB3)]
            t2s = [C.sb([128, T]) for _ in range(NB3)]
            bps = [C.sb([128, T]) for _ in range(NB3)]
            ws_ = [C.sb([128, T]) for _ in range(NB3)]
            wcs = [C.sb([128, T], BF16) for _ in range(NB3)]
            wss = [C.sb([128, T], BF16) for _ in range(NB3)]
            yos = [C.sb([128, T]) for _ in range(2)]
            yfs = [C.sb([128, T]) for _ in range(2)]
            blocks = list(range(NBK)) if dr == 0 else list(range(NBK - 1, -1, -1))
            rows = slice(u_row + ct * 128, u_row + (ct + 1) * 128)
            rv = (lambda a: a[:, ::-1]) if dr == 1 else (lambda a: a[:])
            items = [(bi, bk, gp) for bi, bk in enumerate(blocks) for gp in range(8)]
            st_ = {}
            SK = 2
            NB4 = 4
            bps = [C.sb([128, T]) for _ in range(NB4)]

            def phaseA(k):
                bi, bk, gp = items[k]
                cs_ = slice(bk * T, (bk + 1) * T)
                if gp == 0:
                    ut, b_ut = uts[bi % 2]
                    utb, b_utb = utbs[bi % 2]
                    P.dma(lambda e, ut=ut, cs_=cs_: e.dma_start(out=ut[:], in_=pT[rows, cs_]), writes=[b_ut])
                    P.act(lambda e, ut=ut, utb=utb: e.activation(out=utb[:], in_=ut[:], func=AF.Copy), reads=[b_ut], writes=[b_utb])
                utb, b_utb = utbs[bi % 2]
                gd = G[gp]
                pb, b_pb = pbs[k % 2]
                pq, b_pq = pss_[k % 2]
                t1, b_t1 = t1s[k % 2]
                t2, b_t2 = t2s[k % 2]
                bp, b_bp = bps[k % NB4]
                P.pe(lambda e, pb=pb, gd=gd, utb=utb: e.matmul(pb[:], gd["B1"][0][:], utb[:], start=True, stop=True), reads=[gd["B1"][1], b_utb], writes=[b_pb])
                P.pe(lambda e, pq=pq, gd=gd, utb=utb: e.matmul(pq[:], gd["B2"][0][:], utb[:], start=True, stop=True), reads=[gd["B2"][1], b_utb], writes=[b_pq])
                P.dve(lambda e, t1=t1, pb=pb, gd=gd: e.tensor_tensor(out=t1[:], in0=rv(pb), in1=gd["Ct"][0][:], op=ALU.mult), reads=[b_pb, gd["Ct"][1]], writes=[b_t1])
                P.dve(lambda e, t2=t2, pq=pq, gd=gd: e.tensor_tensor(out=t2[:], in0=rv(pq), in1=gd["St"][0][:], op=ALU.mult), reads=[b_pq, gd["St"][1]], writes=[b_t2])
                P.pool(lambda e, bp=bp, t1=t1, t2=t2: e.tensor_tensor(out=bp[:], in0=t1[:], in1=t2[:], op=ALU.add), reads=[b_t1, b_t2], writes=[b_bp])

            def phaseB(k):
                bi, bk, gp = items[k]
                cs_ = slice(bk * T, (bk + 1) * T)
                gd = G[gp]
                bp, b_bp = bps[k % NB4]
                w_, b_w = ws_[k % 3]
                wc, b_wc = wcs[k % 3]
                wsn, b_wsn = wss[k % 3]
                P.dve(lambda e, w_=w_, bp=bp, gd=gd: e.tensor_tensor_scan(out=w_[:], data0=gd["Rb"][0][:], data1=bp[:], initial=gd["carry"][0][:, 0:1], op0=ALU.mult, op1=ALU.add),
                      reads=[gd["Rb"][1], b_bp, gd["carry"][1]], writes=[b_w])
                P.pool(lambda e, wc=wc, w_=w_, gd=gd: e.tensor_tensor(out=wc[:], in0=w_[:], in1=gd["Ct"][0][:], op=ALU.mult), reads=[b_w, gd["Ct"][1]], writes=[b_wc])
                P.dve(lambda e, wsn=wsn, w_=w_, gd=gd: e.tensor_tensor(out=wsn[:], in0=w_[:], in1=gd["St"][0][:], op=ALU.mult), reads=[b_w, gd["St"][1]], writes=[b_wsn])
                P.pe(lambda e, gd=gd, wc=wc, gp=gp: e.matmul(py[:], gd["C1"][0][:], wc[:], start=(gp == 0), stop=False), reads=[gd["C1"][1], b_wc], writes=[b_py])
                P.pe(lambda e, gd=gd, wsn=wsn, gp=gp: e.matmul(py[:], gd["C2"][0][:], wsn[:], start=False, stop=(gp == 7)), reads=[gd["C2"][1], b_wsn], writes=[b_py])
                P.pe(lambda e, gd=gd, w_=w_: e.matmul(pc[:], gd["RT"][0][:], w_[:, T - 1:T], start=True, stop=True), reads=[gd["RT"][1], b_w], writes=[b_pc])
                P.act(lambda e, gd=gd: e.activation(out=gd["carry"][0][:], in_=pc[:], func=AF.Copy), reads=[b_pc], writes=[gd["carry"][1]])
                if gp == 7:
                    ut, b_ut = uts[bi % 2]
                    yo, b_yo = yos[bi % 2]
                    if dr == 0:
                        P.act(lambda e, yo=yo: e.activation(out=yo[:], in_=py[:], func=AF.Copy), reads=[b_py], writes=[b_yo])
                        P.dma(lambda e, yo=yo, cs_=cs_: e.dma_start(out=yf[ct * 128:(ct + 1) * 128, cs_], in_=yo[:]), reads=[b_yo], writes=[Buf()])
                    else:
                        yft, b_yft = yfs[bi % 2]
                        P.dma(lambda e, yft=yft, cs_=cs_: e.dma_start(out=yft[:], in_=yf[ct * 128:(ct + 1) * 128, cs_]), writes=[b_yft])
                        P.dve(lambda e, yo=yo, yft=yft: e.tensor_tensor(out=yo[:], in0=py[:, ::-1], in1=yft[:], op=ALU.add), reads=[b_py, b_yft], writes=[b_yo])
                        P.dve(lambda e, yo=yo, ut=ut: e.scalar_tensor_tensor(out=yo[:], in0=ut[:], scalar=dsk[:, ct:ct + 1], in1=yo[:], op0=ALU.mult, op1=ALU.add), reads=[b_ut, b_dsk, b_yo], writes=[b_yo])
                        P.dma(lambda e, yo=yo, cs_=cs_: e.dma_start(out=mixT[out_row + ct * 128:out_row + (ct + 1) * 128, cs_], in_=yo[:]), reads=[b_yo], writes=[Buf()])

            for k in range(len(items) + SK):
                if k < len(items):
                    phaseA(k)
                if k - SK >= 0:
                    phaseB(k - SK)
            C.end()


def stage_out(C, xT, mixT, w_out, gluw, glub, g8, router, ident, x1T, htok, aff, L, even):
    P = C.P
    C.begin()
    TB = min(512, L)
    NB = L // TB
    NTS = TB // 128
    KC = 8 if even else 6
    woutb, _ = C.sb([128, KC, 1024], BF16)
    b_wo = [Buf() for _ in range(KC)]
    for kc in range(KC):
        for c0 in range(0, 1024, 512):
            P.dma(lambda e, kc=kc, c0=c0: e.dma_start(out=woutb[:, kc, c0:c0 + 512], in_=w_out[kc * 128:(kc + 1) * 128, c0:c0 + 512]), writes=[b_wo[kc]], q="pool")
    if even:
        gluwb, b_gw = C.sb([128, 4, 512], BF16)
        for kc in range(4):
            P.dma(lambda e, kc=kc: e.dma_start(out=gluwb[:, kc, :], in_=gluw[kc * 128:(kc + 1) * 128, :]), writes=[b_gw], q="pool")
        gb, b_gb = C.sb([128, 4])
        P.dma(lambda e: e.dma_start(out=gb[:], in_=glub), writes=[b_gb])
        ngb, b_ngb = C.sb([128, 4])
        P.dve(lambda e: e.tensor_scalar(out=ngb[:], in0=gb[:], scalar1=-1.0, scalar2=None, op0=ALU.mult), reads=[b_gb], writes=[b_ngb])
    gt, b_g = C.sb([128, 8])
    wr, b_wr = C.sb([128, 8, 16])
    idt, b_id = C.sb([128, 128])
    ones, b_ones = C.sb([128, 128], BF16)
    P.dma(lambda e: e.dma_start(out=gt[:], in_=g8), writes=[b_g])
    P.dma(lambda e: e.dma_start(out=wr[:], in_=router.rearrange("(kc p) n -> p kc n", p=128)), writes=[b_wr])
    P.dma(lambda e: e.dma_start(out=idt[:], in_=ident), writes=[b_id])
    P.dve(lambda e: e.memset(ones[:], 1.0), writes=[b_ones])
    epsc, b_epsc = C.sb([128, 1])
    P.dve(lambda e: e.memset(epsc[:], EPS), writes=[b_epsc])
    mix, b_mix = C.sb([128, KC, TB])
    mixb, b_mixb = C.sb([128, KC, TB], BF16)
    xt, b_xt = C.sb([128, 8, TB])
    x1t, b_x1 = C.sb([128, 8, TB])
    ht, b_ht = C.sb([128, 8, TB])
    sq, b_sq = C.sb([128, 8, TB], BF16)
    rs, b_rs = C.sb([128, TB])
    yg, b_yg = C.sb([128, 4, TB])
    ygb, b_ygb = C.sb([128, 4, TB], BF16)
    sg, b_sg = C.sb([128, TB])
    hrows = [C.sb([128, 1024]) for _ in range(2)]
    afts = [C.sb([128, 16]) for _ in range(2)]
    ex, b_ex = C.sb([128, 16])
    mx, b_mx = C.sb([128, 1])
    sm, b_sm = C.sb([128, 1])
    pxs = [C.ps([128, TB]) for _ in range(2)]
    pss, b_pss = C.ps([128, TB])
    pz, b_pz = C.ps([128, TB])
    pl, b_pl = C.ps([128, 16])
    ptrs = [C.ps([128, 128]) for _ in range(2)]
    xv = xT.rearrange("(kc p) n -> p kc n", p=128)
    mv = mixT.rearrange("(kc p) n -> p kc n", p=128)
    x1v = x1T.rearrange("(kc p) n -> p kc n", p=128)
    nt = 0
    for tb in range(NB):
        sl = slice(tb * TB, (tb + 1) * TB)
        P.dma(lambda e, sl=sl: e.dma_start(out=mix[:], in_=mv[:, 0:KC, sl]), writes=[b_mix])
        P.dma(lambda e, sl=sl: e.dma_start(out=xt[:], in_=xv[:, :, sl]), writes=[b_xt])
        if even:
            P.act(lambda e: e.activation(out=mixb[:, 0:4, :], in_=mix[:, 0:4, :], func=AF.Copy), reads=[b_mix], writes=[b_mixb])
            P.act(lambda e: e.activation(out=yg[:], in_=mix[:, 4:8, :], func=AF.Gelu), reads=[b_mix], writes=[b_yg])
            P.dve(lambda e: e.tensor_copy(out=ygb[:], in_=yg[:]), reads=[b_yg], writes=[b_ygb])
            for ct in range(4):
                for kc in range(4):
                    P.pe(lambda e, ct=ct, kc=kc: e.matmul(pz[:], gluwb[:, kc, ct * 128:(ct + 1) * 128], ygb[:, kc, :], start=(kc == 0), stop=(kc == 3)),
                         reads=[b_gw, b_ygb], writes=[b_pz])
                P.act(lambda e, ct=ct: e.activation(out=sg[:], in_=pz[:], func=AF.Exp, scale=-1.0, bias=ngb[:, ct:ct + 1]), reads=[b_pz, b_ngb], writes=[b_sg])
                P.pool(lambda e: e.tensor_scalar(out=sg[:], in0=sg[:], scalar1=1.0, scalar2=None, op0=ALU.add), reads=[b_sg], writes=[b_sg])
                P.dve(lambda e: e.reciprocal(out=sg[:], in_=sg[:]), reads=[b_sg], writes=[b_sg])
                P.dve(lambda e, ct=ct: e.tensor_tensor(out=mixb[:, 4 + ct, :], in0=yg[:, ct, :], in1=sg[:], op=ALU.mult), reads=[b_yg, b_sg], writes=[b_mixb])
        else:
            P.act(lambda e: e.activation(out=mixb[:], in_=mix[:], func=AF.Copy), reads=[b_mix], writes=[b_mixb])
        for dt in range(8):
            px, b_px = pxs[dt % 2]
            for kc in range(KC):
                P.pe(lambda e, px=px, dt=dt, kc=kc: e.matmul(px[:], woutb[:, kc, dt * 128:(dt + 1) * 128], mixb[:, kc, :], start=(kc == 0), stop=(kc == KC - 1)),
                     reads=[b_wo[kc], b_mixb], writes=[b_px])
            P.dve(lambda e, px=px, dt=dt: e.tensor_tensor(out=x1t[:, dt, :], in0=px[:], in1=xt[:, dt, :], op=ALU.add), reads=[b_px, b_xt], writes=[b_x1])
        P.dma(lambda e, sl=sl: e.dma_start(out=x1v[:, :, sl], in_=x1t[:]), reads=[b_x1], writes=[Buf()])
        P.pool(lambda e: e.tensor_tensor(out=sq[:], in0=x1t[:], in1=x1t[:], op=ALU.mult), reads=[b_x1], writes=[b_sq])
        for kc in range(8):
            P.pe(lambda e, kc=kc: e.matmul(pss[:], ones[:], sq[:, kc, :], start=(kc == 0), stop=(kc == 7)), reads=[b_sq, b_ones], writes=[b_pss])
        P.act(lambda e: e.activation(out=rs[:], in_=pss[:], func=AF.Ln, scale=1.0 / D, bias=epsc[:, 0:1]), reads=[b_pss, b_epsc], writes=[b_rs])
        P.act(lambda e: e.activation(out=rs[:], in_=rs[:], func=AF.Exp, scale=-0.5), reads=[b_rs], writes=[b_rs])
        for dt in range(8):
            P.dve(lambda e, dt=dt: e.scalar_tensor_tensor(out=ht[:, dt, :], in0=x1t[:, dt, :], scalar=gt[:, dt:dt + 1], in1=rs[:], op0=ALU.mult, op1=ALU.mult),
                  reads=[b_x1, b_g, b_rs], writes=[b_ht])
        for ts in range(NTS):
            tsl = slice(ts * 128, (ts + 1) * 128)
            r0 = tb * TB + ts * 128
            for kc in range(8):
                P.pe(lambda e, kc=kc, tsl=tsl: e.matmul(pl[:], ht[:, kc, tsl], wr[:, kc, :], start=(kc == 0), stop=(kc == 7)), reads=[b_ht, b_wr], writes=[b_pl])
            aft, b_aft = afts[nt % 2]
            hrow, b_hrow = hrows[nt % 2]
            nt += 1
            P.dve(lambda e: e.reduce_max(out=mx[:], in_=pl[:], axis=AX.X), reads=[b_pl], writes=[b_mx])
            P.dve(lambda e: e.tensor_scalar(out=mx[:], in0=mx[:], scalar1=-1.0, scalar2=None, op0=ALU.mult), reads=[b_mx], writes=[b_mx])
            P.act(lambda e: e.activation(out=ex[:], in_=pl[:], func=AF.Exp, bias=mx[:, 0:1], accum_out=sm[:]), reads=[b_pl, b_mx], writes=[b_ex, b_sm])
            P.dve(lambda e: e.reciprocal(out=sm[:], in_=sm[:]), reads=[b_sm], writes=[b_sm])
            P.dve(lambda e, aft=aft: e.tensor_scalar(out=aft[:], in0=ex[:], scalar1=sm[:, 0:1], scalar2=None, op0=ALU.mult), reads=[b_ex, b_sm], writes=[b_aft])
            P.dma(lambda e, aft=aft, r0=r0: e.dma_start(out=aff[r0:r0 + 128, :], in_=aft[:]), reads=[b_aft], writes=[Buf()])
            for kc in range(8):
                ptr, b_ptr = ptrs[kc % 2]
                P.pe(lambda e, ptr=ptr, kc=kc, tsl=tsl: e.transpose(ptr[:], ht[:, kc, tsl], idt[:]), reads=[b_ht, b_id], writes=[b_ptr])
                if kc % 2 == 0:
                    P.act(lambda e, ptr=ptr, kc=kc, hrow=hrow: e.activation(out=hrow[:, kc * 128:(kc + 1) * 128], in_=ptr[:], func=AF.Copy), reads=[b_ptr], writes=[b_hrow])
                else:
                    P.dve(lambda e, ptr=ptr, kc=kc, hrow=hrow: e.tensor_copy(out=hrow[:, kc * 128:(kc + 1) * 128], in_=ptr[:]), reads=[b_ptr], writes=[b_hrow])
            P.dma(lambda e, hrow=hrow, r0=r0: e.dma_start(out=htok[r0:r0 + 128, :], in_=hrow[:]), reads=[b_hrow], writes=[Buf()])
    C.end()


BIG = 1.0e6


def _breg(e, rc, val):
    if 'r' not in rc:
        rc['r'] = e.to_reg(val)
    return rc['r']


def stage_route(C, aff, ustrict, rmask, posd, gmd, L):
    P = C.P
    C.begin()
    NJ = L // 128
    CAP = L // 8
    NCOL = 16 * NJ
    A, b_A = C.sb([128, NJ, 16])
    Ae, b_Ae = C.sb([128, 16, NJ])
    for jc in range(0, NJ, 16):
        je = min(NJ, jc + 16)
        P.dma(lambda e, jc=jc, je=je: e.dma_start(out=A[:, jc:je, :], in_=aff[jc * 128:je * 128, :].rearrange("(j p) e -> p j e", p=128)), writes=[b_A])
    P.dve(lambda e: e.tensor_copy(out=Ae[:], in_=A[:].rearrange("p j e -> p e j")), reads=[b_A], writes=[b_Ae])
    us, b_us = C.sb([128, 128])
    rm, b_rm = C.sb([128, NCOL])
    onesf, b_of = C.sb([128, 128])
    P.dma(lambda e: e.dma_start(out=us[:], in_=ustrict), writes=[b_us])
    P.dma(lambda e: e.dma_start(out=rm[:], in_=rmask), writes=[b_rm])
    P.dve(lambda e: e.memset(onesf[:], 1.0), writes=[b_of])
    lo, b_lo = C.sb([128, 16])
    hi, b_hi = C.sb([128, 16])
    mid, b_mid = C.sb([128, 16])
    cnt, b_cnt = C.sb([128, 16])
    ge, b_ge = C.sb([128, 16])
    d1, b_d1 = C.sb([128, 16])
    cmps = [C.sb([128, NJ]) for _ in range(2)]
    ptot, b_ptot = C.ps([128, 16])
    P.dve(lambda e: e.memset(lo[:], 0.0), writes=[b_lo])
    P.dve(lambda e: e.memset(hi[:], 2.0), writes=[b_hi])
    for it in range(34):
        P.dve(lambda e: e.tensor_tensor(out=mid[:], in0=lo[:], in1=hi[:], op=ALU.add), reads=[b_lo, b_hi], writes=[b_mid])
        P.dve(lambda e: e.tensor_scalar(out=mid[:], in0=mid[:], scalar1=0.5, scalar2=None, op0=ALU.mult), reads=[b_mid], writes=[b_mid])
        for ex in range(16):
            cm, b_cm = cmps[ex % 2]
            P.dve(lambda e, ex=ex, cm=cm: e.tensor_scalar(out=cm[:], in0=Ae[:, ex, :], scalar1=mid[:, ex:ex + 1], scalar2=None, op0=ALU.is_ge, op1=ALU.add, accum_out=cnt[:, ex:ex + 1]),
                  reads=[b_Ae, b_mid], writes=[b_cm, b_cnt])
        P.pe(lambda e: e.matmul(ptot[:], onesf[:], cnt[:], start=True, stop=True), reads=[b_of, b_cnt], writes=[b_ptot])
        P.dve(lambda e: e.tensor_scalar(out=ge[:], in0=ptot[:], scalar1=float(CAP) - 0.5, scalar2=None, op0=ALU.is_ge), reads=[b_ptot], writes=[b_ge])
        P.dve(lambda e: e.tensor_tensor(out=d1[:], in0=mid[:], in1=lo[:], op=ALU.subtract), reads=[b_mid, b_lo], writes=[b_d1])
        P.dve(lambda e: e.tensor_tensor(out=d1[:], in0=d1[:], in1=ge[:], op=ALU.mult), reads=[b_d1, b_ge], writes=[b_d1])
        P.dve(lambda e: e.tensor_tensor(out=lo[:], in0=lo[:], in1=d1[:], op=ALU.add), reads=[b_lo, b_d1], writes=[b_lo])
        P.dve(lambda e: e.tensor_tensor(out=d1[:], in0=hi[:], in1=mid[:], op=ALU.subtract), reads=[b_hi, b_mid], writes=[b_d1])
        P.dve(lambda e: e.tensor_tensor(out=d1[:], in0=d1[:], in1=ge[:], op=ALU.mult), reads=[b_d1, b_ge], writes=[b_d1])
        P.dve(lambda e: e.tensor_tensor(out=hi[:], in0=mid[:], in1=d1[:], op=ALU.add), reads=[b_mid, b_d1], writes=[b_hi])
    Me, b_Me = C.sb([128, 16, NJ])
    gm, b_gm = C.sb([128, 16, NJ])
    for ex in range(16):
        P.dve(lambda e, ex=ex: e.tensor_scalar(out=Me[:, ex, :], in0=Ae[:, ex, :], scalar1=lo[:, ex:ex + 1], scalar2=None, op0=ALU.is_ge), reads=[b_Ae, b_lo], writes=[b_Me])
    P.dve(lambda e: e.tensor_tensor(out=gm[:], in0=Ae[:], in1=Me[:], op=ALU.mult), reads=[b_Ae, b_Me], writes=[b_gm])
    Mf = Me[:].rearrange("p e j -> p (e j)")
    pre, b_pre = C.sb([128, NCOL])
    cn, b_cn = C.sb([128, NCOL])
    off, b_off = C.sb([128, NCOL])
    pp, b_pp = C.ps([128, min(512, NCOL)])
    pc, b_pc = C.ps([128, min(512, NCOL)])
    CW = min(512, NCOL)
    for c0 in range(0, NCOL, CW):
        P.pe(lambda e, c0=c0: e.matmul(pp[:], us[:], Mf[:, c0:c0 + CW], start=True, stop=True), reads=[b_us, b_Me], writes=[b_pp])
        P.act(lambda e, c0=c0: e.activation(out=pre[:, c0:c0 + CW], in_=pp[:], func=AF.Copy), reads=[b_pp], writes=[b_pre])
        P.pe(lambda e, c0=c0: e.matmul(pc[:], onesf[:], Mf[:, c0:c0 + CW], start=True, stop=True), reads=[b_of, b_Me], writes=[b_pc])
        P.act(lambda e, c0=c0: e.activation(out=cn[:, c0:c0 + CW], in_=pc[:], func=AF.Copy), reads=[b_pc], writes=[b_cn])
    P.dve(lambda e: e.tensor_tensor_scan(out=off[:], data0=rm[:], data1=cn[:], initial=0.0, op0=ALU.mult, op1=ALU.add), reads=[b_rm, b_cn], writes=[b_off])
    P.dve(lambda e: e.tensor_tensor(out=off[:], in0=off[:], in1=cn[:], op=ALU.subtract), reads=[b_off, b_cn], writes=[b_off])
    P.dve(lambda e: e.tensor_tensor(out=pre[:], in0=pre[:], in1=off[:], op=ALU.add), reads=[b_pre, b_off], writes=[b_pre])
    P.dve(lambda e: e.tensor_scalar(out=pre[:], in0=pre[:], scalar1=-BIG, scalar2=None, op0=ALU.add), reads=[b_pre], writes=[b_pre])
    P.dve(lambda e: e.tensor_tensor(out=pre[:], in0=pre[:], in1=Mf, op=ALU.mult), reads=[b_pre, b_Me], writes=[b_pre])
    P.dve(lambda e: e.tensor_scalar(out=pre[:], in0=pre[:], scalar1=BIG, scalar2=None, op0=ALU.add), reads=[b_pre], writes=[b_pre])
    pi, b_pi = C.sb([128, NCOL], I32)
    P.dve(lambda e: e.tensor_copy(out=pi[:], in_=pre[:]), reads=[b_pre], writes=[b_pi])
    P.dma(lambda e: e.dma_start(out=posd, in_=pi[:]), reads=[b_pi], writes=[Buf()])
    P.dma(lambda e: e.dma_start(out=gmd, in_=gm[:].rearrange("p e j -> p (e j)")), reads=[b_gm], writes=[Buf()])
    C.end()


def stage_dispatch(C, htok, posd, xe, L):
    P = C.P
    C.begin()
    NJ = L // 128
    CAP = L // 8
    pi, b_pi = C.sb([128, 16, NJ], I32)
    P.dma(lambda e: e.dma_start(out=pi[:].rearrange("p e j -> p (e j)"), in_=posd), writes=[b_pi])
    rc = {}
    hts = [C.sb([128, 1024]) for _ in range(3)]
    ixs = [C.sb([128, 1], I32) for _ in range(12)]
    ni = 0
    for j in range(NJ):
        ht, b_ht = hts[j % 3]
        P.dma(lambda e, ht=ht, j=j: e.dma_start(out=ht[:], in_=htok[j * 128:(j + 1) * 128, :]), writes=[b_ht])
        for ex in range(16):
            ix, b_ix = ixs[ni % 12]
            ni += 1
            P.dve(lambda e, ix=ix, ex=ex, j=j: e.tensor_copy(out=ix[:], in_=pi[:, ex, j:j + 1]), reads=[b_pi], writes=[b_ix])
            P.dma(lambda e, ht=ht, ix=ix, ex=ex: e.indirect_dma_start(out=xe[ex][:, :], out_offset=bass.IndirectOffsetOnAxis(ap=ix[:, :], axis=0),
                                                                      in_=ht[:, :], in_offset=None, bounds_check=_breg(e, rc, CAP - 1), oob_is_err=False),
                  reads=[b_ht, b_ix], writes=[Buf()], q="pool")
    C.end()


def stage_ffn(C, xe, w1, w3, w2, ye, ident, L, FF):
    P = C.P
    C.begin()
    CAP = L // 8
    SB = min(512, CAP)
    NSB = CAP // SB
    NST = SB // 128
    NF = FF // 128
    idt, b_id = C.sb([128, 128])
    P.dma(lambda e: e.dma_start(out=idt[:], in_=ident), writes=[b_id])
    w1b, _ = C.sb([128, 8, FF], BF16)
    w3b, _ = C.sb([128, 8, FF], BF16)
    w2b, _ = C.sb([128, NF, 1024], BF16)
    b_w1 = [Buf() for _ in range(8)]
    b_w3 = [Buf() for _ in range(8)]
    b_w2 = [Buf() for _ in range(NF)]
    xrs = [C.sb([128, 1024]) for _ in range(2)]
    xeT, b_xeT = C.sb([128, 8, SB], BF16)
    hid, b_hid = C.sb([128, NF, SB], BF16)
    sas = [C.sb([128, SB]) for _ in range(2)]
    yrows = [C.sb([128, 1024]) for _ in range(2)]
    ptrs = [C.ps([128, 128]) for _ in range(2)]
    pas = [C.ps([128, SB]) for _ in range(2)]
    pbs = [C.ps([128, SB]) for _ in range(2)]
    pys = [C.ps([128, 512]) for _ in range(2)]
    nx = 0
    ny = 0
    NSTG = 3
    stg = [C.sb([128, max(FF, 1024)]) for _ in range(NSTG)]
    ns = 0
    for ex in range(16):
        jobs = []
        for kc in range(8):
            jobs.append((w1[ex, kc * 128:(kc + 1) * 128, :], FF, w1b, kc, b_w1[kc]))
            jobs.append((w3[ex, kc * 128:(kc + 1) * 128, :], FF, w3b, kc, b_w3[kc]))
        for fc in range(NF):
            jobs.append((w2[ex, fc * 128:(fc + 1) * 128, :], 1024, w2b, fc, b_w2[fc]))
        for (src, width, dstt, di, b_dst) in jobs:
            sg_, b_sg = stg[ns % NSTG]
            ns += 1
            P.dma(lambda e, sg_=sg_, src=src, width=width: e.dma_start(out=sg_[:, 0:width], in_=src), writes=[b_sg])
            P.act(lambda e, sg_=sg_, dstt=dstt, di=di, width=width: e.activation(out=dstt[:, di, :], in_=sg_[:, 0:width], func=AF.Copy), reads=[b_sg], writes=[b_dst])
        for sb_ in range(NSB):
            for st in range(NST):
                r0 = sb_ * SB + st * 128
                xr, b_xr = xrs[nx % 2]
                nx += 1
                P.dma(lambda e, xr=xr, ex=ex, r0=r0: e.dma_start(out=xr[:], in_=xe[ex][r0:r0 + 128, :]), writes=[b_xr], q="pool")
                for kc in range(8):
                    ptr, b_ptr = ptrs[kc % 2]
                    P.pe(lambda e, ptr=ptr, xr=xr, kc=kc: e.transpose(ptr[:], xr[:, kc * 128:(kc + 1) * 128], idt[:]), reads=[b_xr, b_id], writes=[b_ptr])
                    if kc % 2 == 0:
                        P.act(lambda e, ptr=ptr, kc=kc, st=st: e.activation(out=xeT[:, kc, st * 128:(st + 1) * 128], in_=ptr[:], func=AF.Copy), reads=[b_ptr], writes=[b_xeT])
                    else:
                        P.dve(lambda e, ptr=ptr, kc=kc, st=st: e.tensor_copy(out=xeT[:, kc, st * 128:(st + 1) * 128], in_=ptr[:]), reads=[b_ptr], writes=[b_xeT])
            for ft in range(NF):
                pa, b_pa = pas[ft % 2]
                pb, b_pb = pbs[ft % 2]
                sa, b_sa = sas[ft % 2]
                for kc in range(8):
                    P.pe(lambda e, pa=pa, kc=kc, ft=ft: e.matmul(pa[:], w1b[:, kc, ft * 128:(ft + 1) * 128], xeT[:, kc, :], start=(kc == 0), stop=(kc == 7)), reads=[b_w1[kc], b_xeT], writes=[b_pa])
                for kc in range(8):
                    P.pe(lambda e, pb=pb, kc=kc, ft=ft: e.matmul(pb[:], w3b[:, kc, ft * 128:(ft + 1) * 128], xeT[:, kc, :], start=(kc == 0), stop=(kc == 7)), reads=[b_w3[kc], b_xeT], writes=[b_pb])
                P.act(lambda e, sa=sa, pa=pa: e.activation(out=sa[:], in_=pa[:], func=AF.Exp, scale=-1.0), reads=[b_pa], writes=[b_sa])
                P.dve(lambda e, sa=sa: e.tensor_scalar(out=sa[:], in0=sa[:], scalar1=1.0, scalar2=None, op0=ALU.add), reads=[b_sa], writes=[b_sa])
                P.dve(lambda e, sa=sa: e.reciprocal(out=sa[:], in_=sa[:]), reads=[b_sa], writes=[b_sa])
                P.dve(lambda e, sa=sa, pa=pa: e.tensor_tensor(out=sa[:], in0=pa[:], in1=sa[:], op=ALU.mult), reads=[b_pa, b_sa], writes=[b_sa])
                P.dve(lambda e, sa=sa, pb=pb, ft=ft: e.tensor_tensor(out=hid[:, ft, :], in0=pb[:], in1=sa[:], op=ALU.mult), reads=[b_pb, b_sa], writes=[b_hid])
            for st in range(NST):
                r0 = sb_ * SB + st * 128
                yrow, b_yrow = yrows[ny % 2]
                ny += 1
                for dh in range(2):
                    py, b_py = pys[dh]
                    for fc in range(NF):
                        P.pe(lambda e, py=py, fc=fc, st=st, dh=dh: e.matmul(py[:], hid[:, fc, st * 128:(st + 1) * 128], w2b[:, fc, dh * 512:(dh + 1) * 512], start=(fc == 0), stop=(fc == NF - 1)),
                             reads=[b_hid, b_w2[fc]], writes=[b_py])
                    if dh == 0:
                        P.act(lambda e, py=py, yrow=yrow: e.activation(out=yrow[:, 0:512], in_=py[:], func=AF.Copy), reads=[b_py], writes=[b_yrow])
                    else:
                        P.dve(lambda e, py=py, yrow=yrow: e.tensor_copy(out=yrow[:, 512:1024], in_=py[:]), reads=[b_py], writes=[b_yrow])
                P.dma(lambda e, yrow=yrow, ex=ex, r0=r0: e.dma_start(out=ye[ex][r0:r0 + 128, :], in_=yrow[:]), reads=[b_yrow], writes=[Buf()], q="pool")
    C.end()


def stage_combine(C, ye, posd, gmd, x1T, ident, x2T, outT, gfin, L):
    P = C.P
    C.begin()
    NJ = L // 128
    CAP = L // 8
    idt, b_id = C.sb([128, 128])
    P.dma(lambda e: e.dma_start(out=idt[:], in_=ident), writes=[b_id])
    pi, b_pi = C.sb([128, 16, NJ], I32)
    gm, b_gm = C.sb([128, 16, NJ])
    P.dma(lambda e: e.dma_start(out=pi[:].rearrange("p e j -> p (e j)"), in_=posd), writes=[b_pi])
    P.dma(lambda e: e.dma_start(out=gm[:].rearrange("p e j -> p (e j)"), in_=gmd), writes=[b_gm])
    Gs = [C.sb([128, 1024]) for _ in range(6)]
    for G_, b_G in Gs:
        P.dve(lambda e, G_=G_: e.memset(G_[:], 0.0), writes=[b_G])
    accs = [C.sb([128, 1024]) for _ in range(2)]
    x1s = [C.sb([128, 8, 128]) for _ in range(2)]
    x2s = [C.sb([128, 8, 128]) for _ in range(2)]
    ptrs = [C.ps([128, 128]) for _ in range(2)]
    if outT is not None:
        gt, b_g = C.sb([128, 8])
        ones, b_ones = C.sb([128, 128], BF16)
        sq, b_sq = C.sb([128, 8, 128], BF16)
        rs, b_rs = C.sb([128, 128])
        ots = [C.sb([128, 8, 128]) for _ in range(2)]
        pss, b_pss = C.ps([128, 128])
        P.dma(lambda e: e.dma_start(out=gt[:], in_=gfin), writes=[b_g])
        P.dve(lambda e: e.memset(ones[:], 1.0), writes=[b_ones])
        epsc, b_epsc = C.sb([128, 1])
        P.dve(lambda e: e.memset(epsc[:], EPS), writes=[b_epsc])
        ov = outT.rearrange("(kc p) n -> p kc n", p=128)
    x1v = x1T.rearrange("(kc p) n -> p kc n", p=128)
    x2v = x2T.rearrange("(kc p) n -> p kc n", p=128)
    ng = 0
    rc = {}
    ixs = [C.sb([128, 1], I32) for _ in range(12)]
    for j in range(NJ):
        acc, b_acc = accs[j % 2]
        x1t, b_x1 = x1s[j % 2]
        x2t, b_x2 = x2s[j % 2]
        cs_ = slice(j * 128, (j + 1) * 128)
        P.dma(lambda e, x1t=x1t, cs_=cs_: e.dma_start(out=x1t[:], in_=x1v[:, :, cs_]), writes=[b_x1])
        for ex in range(16):
            G_, b_G = Gs[ng % 6]
            ix, b_ix = ixs[ng % 12]
            ng += 1
            P.act(lambda e, ix=ix, ex=ex, j=j: e.activation(out=ix[:], in_=pi[:, ex, j:j + 1], func=AF.Copy), reads=[b_pi], writes=[b_ix])
            P.dma(lambda e, G_=G_, ex=ex, ix=ix: e.indirect_dma_start(out=G_[:, :], out_offset=None, in_=ye[ex][:, :],
                                                                      in_offset=bass.IndirectOffsetOnAxis(ap=ix[:, :], axis=0), bounds_check=_breg(e, rc, CAP - 1), oob_is_err=False),
                  reads=[b_ix], writes=[b_G], q="pool")
            if ex == 0:
                P.dve(lambda e, acc=acc, G_=G_, ex=ex, j=j: e.tensor_scalar(out=acc[:], in0=G_[:], scalar1=gm[:, ex, j:j + 1], scalar2=None, op0=ALU.mult), reads=[b_G, b_gm], writes=[b_acc])
            else:
                P.dve(lambda e, acc=acc, G_=G_, ex=ex, j=j: e.scalar_tensor_tensor(out=acc[:], in0=G_[:], scalar=gm[:, ex, j:j + 1], in1=acc[:], op0=ALU.mult, op1=ALU.add),
                      reads=[b_G, b_gm, b_acc], writes=[b_acc])
        for kc in range(8):
            ptr, b_ptr = ptrs[kc % 2]
            P.pe(lambda e, ptr=ptr, acc=acc, kc=kc: e.transpose(ptr[:], acc[:, kc * 128:(kc + 1) * 128], idt[:]), reads=[b_acc, b_id], writes=[b_ptr])
            P.dve(lambda e, ptr=ptr, x2t=x2t, x1t=x1t, kc=kc: e.tensor_tensor(out=x2t[:, kc, :], in0=ptr[:], in1=x1t[:, kc, :], op=ALU.add), reads=[b_ptr, b_x1], writes=[b_x2])
        P.dma(lambda e, x2t=x2t, cs_=cs_: e.dma_start(out=x2v[:, :, cs_], in_=x2t[:]), reads=[b_x2], writes=[Buf()])
        if outT is not None:
            ot, b_ot = ots[j % 2]
            P.pool(lambda e, x2t=x2t: e.tensor_tensor(out=sq[:], in0=x2t[:], in1=x2t[:], op=ALU.mult), reads=[b_x2], writes=[b_sq])
            for kc in range(8):
                P.pe(lambda e, kc=kc: e.matmul(pss[:], ones[:], sq[:, kc, :], start=(kc == 0), stop=(kc == 7)), reads=[b_sq, b_ones], writes=[b_pss])
            P.act(lambda e: e.activation(out=rs[:], in_=pss[:], func=AF.Ln, scale=1.0 / D, bias=epsc[:, 0:1]), reads=[b_pss, b_epsc], writes=[b_rs])
            P.act(lambda e: e.activation(out=rs[:], in_=rs[:], func=AF.Exp, scale=-0.5), reads=[b_rs], writes=[b_rs])
            for kc in range(8):
                P.dve(lambda e, ot=ot, x2t=x2t, kc=kc: e.scalar_tensor_tensor(out=ot[:, kc, :], in0=x2t[:, kc, :], scalar=gt[:, kc:kc + 1], in1=rs[:], op0=ALU.mult, op1=ALU.mult),
                      reads=[b_x2, b_g, b_rs], writes=[b_ot])
            P.dma(lambda e, ot=ot, cs_=cs_: e.dma_start(out=ov[:, :, cs_], in_=ot[:]), reads=[b_ot], writes=[Buf()])
    C.end()


DILS = (1, 4, 16)
DSPAN = (1, 2, 8)


def dil_masks():
    kk = np.arange(128)[:, None]
    qq = np.arange(128)[None, :]
    ms = []
    for g, d in enumerate(DILS):
        for dl in range(-DSPAN[g], DSPAN[g] + 1):
            rel = 128 * dl + kk - qq
            ms.append(((rel % d == 0) & (np.abs(rel) <= 64 * d)).astype(np.float32))
    return np.stack(ms)


def stage_vprep(C, pT, ident, vtok, L, v_row):
    P = C.P
    C.begin()
    TB = min(512, L)
    NT = TB // 128
    idt, b_id = C.sb([128, 128])
    P.dma(lambda e: e.dma_start(out=idt[:], in_=ident), writes=[b_id])
    vts = [C.sb([64, TB]) for _ in range(2)]
    vos = [C.sb([128, NT, 66], BF16) for _ in range(2)]
    for vo, b_vo in vos:
        P.dve(lambda e, vo=vo: e.memset(vo[:], 1.0), writes=[b_vo])
    ptrs = [C.ps([128, 64]) for _ in range(2)]
    it = 0
    for hd in range(12):
        for tb in range(L // TB):
            vt, b_vt = vts[it % 2]
            vo, b_vo = vos[it % 2]
            it += 1
            P.dma(lambda e, vt=vt, hd=hd, tb=tb: e.dma_start(out=vt[:], in_=pT[v_row + hd * 64:v_row + (hd + 1) * 64, tb * TB:(tb + 1) * TB]), writes=[b_vt])
            for t in range(NT):
                ptr, b_ptr = ptrs[t % 2]
                P.pe(lambda e, ptr=ptr, vt=vt, t=t: e.transpose(ptr[:], vt[:, t * 128:(t + 1) * 128], idt[0:64, 0:64]), reads=[b_vt, b_id], writes=[b_ptr])
                if t % 2 == 0:
                    P.act(lambda e, ptr=ptr, vo=vo, t=t: e.activation(out=vo[:, t, 0:64], in_=ptr[:], func=AF.Copy), reads=[b_ptr], writes=[b_vo])
                else:
                    P.dve(lambda e, ptr=ptr, vo=vo, t=t: e.tensor_copy(out=vo[:, t, 0:64], in_=ptr[:]), reads=[b_ptr], writes=[b_vo])
            P.dma(lambda e, vo=vo, hd=hd, tb=tb: e.dma_start(out=vtok[hd][tb * TB:(tb + 1) * TB, :].rearrange("(t p) c -> p t c", p=128), in_=vo[:]), reads=[b_vo], writes=[Buf()])
    C.end()


def stage_qk16(C, pT, qk16, L, nrows):
    P = C.P
    C.begin()
    TB = min(512, L)
    xs = [C.sb([128, TB]) for _ in range(3)]
    ys = [C.sb([128, TB], BF16) for _ in range(3)]
    it = 0
    for rt in range(nrows // 128):
        for tb in range(L // TB):
            x_, b_x = xs[it % 3]
            y_, b_y = ys[it % 3]
            it += 1
            P.dma(lambda e, x_=x_, rt=rt, tb=tb: e.dma_start(out=x_[:], in_=pT[rt * 128:(rt + 1) * 128, tb * TB:(tb + 1) * TB]), writes=[b_x])
            if it % 2 == 0:
                P.act(lambda e, x_=x_, y_=y_: e.activation(out=y_[:], in_=x_[:], func=AF.Copy), reads=[b_x], writes=[b_y])
            else:
                P.dve(lambda e, x_=x_, y_=y_: e.tensor_copy(out=y_[:], in_=x_[:]), reads=[b_x], writes=[b_y])
            P.dma(lambda e, y_=y_, rt=rt, tb=tb: e.dma_start(out=qk16[rt * 128:(rt + 1) * 128, tb * TB:(tb + 1) * TB], in_=y_[:]), reads=[b_y], writes=[Buf()])
    C.end()


def stage_dil(C, pT, ident, masks, vtok, mixT, L, q_row, k_row, out_row):
    P = C.P
    NCH = L // 128
    for i in range(4):
        C.begin()
        idt, b_id = C.sb([128, 128])
        mk, b_mk = C.sb([128, 25, 128], BF16)
        P.dma(lambda e: e.dma_start(out=idt[:], in_=ident), writes=[b_id])
        for m0_ in range(0, 25, 5):
            P.dma(lambda e, m0_=m0_: e.dma_start(out=mk[:, m0_:m0_ + 5, :], in_=masks[m0_:m0_ + 5].rearrange("m p n -> p m n")), writes=[b_mk], q="pool")
        qs = [[C.sb([64, 128], BF16) for _ in range(2)] for _ in range(3)]
        ks = [[C.sb([64, (2 * DSPAN[g] + 1) * 128], BF16) for _ in range(2)] for g in range(3)]
        vs = [[C.sb([128, 2 * DSPAN[g] + 1, 66], BF16) for _ in range(2)] for g in range(3)]
        pss = [C.ps([128, 512]) for _ in range(2)]
        pacc = [C.ps([128, 65]) for _ in range(2)]
        ptr, b_ptr = C.ps([64, 128])
        es = [C.sb([128, 512], BF16) for _ in range(3)]
        pms = [C.sb([128, 512], BF16) for _ in range(3)]
        rd, b_rd = C.sb([128, 1])
        ots = [C.sb([128, 64]) for _ in range(2)]
        oTs = [C.sb([64, 128]) for _ in range(2)]
        moff = (0, 3, 8)
        ngrp = 0
        for n in range(NCH):
            pa, b_pa = pacc[n % 2]
            first = True
            plan = []
            for g in range(3):
                hd = 4 * g + i
                D_ = DSPAN[g]
                m0 = max(0, n - D_)
                m1 = min(NCH - 1, n + D_)
                q_, b_q = qs[g][n % 2]
                k_, b_k = ks[g][n % 2]
                v_, b_v = vs[g][n % 2]
                nm = m1 - m0 + 1
                P.dma(lambda e, q_=q_, hd=hd, n=n: e.dma_start(out=q_[:], in_=pT[q_row + hd * 64:q_row + (hd + 1) * 64, n * 128:(n + 1) * 128]), writes=[b_q])
                P.dma(lambda e, k_=k_, hd=hd, m0=m0, nm=nm: e.dma_start(out=k_[:, 0:nm * 128], in_=pT[k_row + hd * 64:k_row + (hd + 1) * 64, m0 * 128:(m0 + nm) * 128]), writes=[b_k])
                P.dma(lambda e, v_=v_, hd=hd, m0=m0, nm=nm: e.dma_start(out=v_[:, 0:nm, :], in_=vtok[hd][m0 * 128:(m0 + nm) * 128, :].rearrange("(t p) c -> p t c", p=128)), writes=[b_v])
                tiles = [(g, m, m - m0, moff[g] + (m - n) + D_) for m in range(m0, m1 + 1)]
                for c0 in range(0, len(tiles), 4):
                    plan.append((g, tiles[c0:c0 + 4], (q_, b_q), (k_, b_k), (v_, b_v)))
            total = sum(len(p[1]) for p in plan)
            done = 0
            for (g, tl, (q_, b_q), (k_, b_k), (v_, b_v)) in plan:
                ps_, b_ps = pss[ngrp % 2]
                e_, b_e = es[ngrp % 3]
                pm, b_pm = pms[ngrp % 3]
                nt = len(tl)
                for a, (_, m, ml, mi) in enumerate(tl):
                    P.pe(lambda e, ps_=ps_, k_=k_, q_=q_, a=a, ml=ml: e.matmul(ps_[:, a * 128:(a + 1) * 128], k_[:, ml * 128:(ml + 1) * 128], q_[:], start=True, stop=True),
                         reads=[b_k, b_q], writes=[b_ps])
                P.act(lambda e, e_=e_, ps_=ps_, nt=nt: e.activation(out=e_[:, 0:nt * 128], in_=ps_[:, 0:nt * 128], func=AF.Exp, scale=0.125), reads=[b_ps], writes=[b_e])
                mi0 = tl[0][3]
                eng = P.dve if ngrp % 2 == 0 else P.pool
                eng(lambda e, pm=pm, e_=e_, nt=nt, mi0=mi0: e.tensor_tensor(out=pm[:, 0:nt * 128], in0=e_[:, 0:nt * 128], in1=mk[:, mi0:mi0 + nt, :].rearrange("p m n -> p (m n)"), op=ALU.mult),
                    reads=[b_e, b_mk], writes=[b_pm])
                for a, (_, m, ml, mi) in enumerate(tl):
                    done += 1
                    P.pe(lambda e, pa=pa, pm=pm, v_=v_, a=a, ml=ml, st=(done == 1), sp=(done == total): e.matmul(pa[:], pm[:, a * 128:(a + 1) * 128], v_[:, ml, 0:65], start=st, stop=sp),
                         reads=[b_pm, b_v], writes=[b_pa])
                ngrp += 1
            ot, b_ot = ots[n % 2]
            oT, b_oT = oTs[n % 2]
            P.dve(lambda e, pa=pa: e.reciprocal(out=rd[:], in_=pa[:, 64:65]), reads=[b_pa], writes=[b_rd])
            P.dve(lambda e, ot=ot, pa=pa: e.tensor_scalar(out=ot[:], in0=pa[:, 0:64], scalar1=rd[:, 0:1], scalar2=None, op0=ALU.mult), reads=[b_pa, b_rd], writes=[b_ot])
            P.pe(lambda e, ot=ot: e.transpose(ptr[:], ot[:], idt[:]), reads=[b_ot, b_id], writes=[b_ptr])
            P.act(lambda e, oT=oT: e.activation(out=oT[:], in_=ptr[:], func=AF.Copy), reads=[b_ptr], writes=[b_oT])
            P.dma(lambda e, oT=oT, n=n: e.dma_start(out=mixT[out_row + i * 64:out_row + (i + 1) * 64, n * 128:(n + 1) * 128], in_=oT[:]), reads=[b_oT], writes=[Buf()])
        C.end()


def rope_tables(L, half, period):
    inv = (10000.0 ** (-np.arange(half, dtype=np.float32) / half)).astype(np.float32)
    ang = (np.arange(L, dtype=np.float32)[None, :] * inv[:, None]).astype(np.float32)
    cos = np.cos(ang).astype(np.float32)
    sin = np.sin(ang).astype(np.float32)
    rows = np.arange(128) % period
    c = cos[rows % half]
    s = np.where((rows < half)[:, None], -sin[rows % half], sin[rows % half])
    return np.ascontiguousarray(c, np.float32), np.ascontiguousarray(s, np.float32)


def swap_cols(w, ncols, period):
    half = period // 2
    idx = np.arange(ncols)
    src = (idx // period) * period + (idx % period + half) % period
    return np.ascontiguousarray(w[:, src])


def ret_tables(L):
    NCH = L // 128
    s = 128.0 ** -0.5
    tab = np.zeros((128, 24, NCH), np.float64)
    t = np.arange(128, dtype=np.float64)
    for h in range(4):
        lg = np.log1p(-(2.0 ** (-5.0 - h)))
        for dr in range(2):
            e = (t + 1) if dr == 0 else (128 - t)
            j0 = (h * 2 + dr) * 3
            tab[:, j0, :] = np.exp(lg * e)[:, None]
            tab[:, j0 + 1, :] = (np.exp(-lg * e) * s)[:, None]
            tab[:, j0 + 2, :] = np.exp(lg * 128)
    return tab.astype(np.float32)


def la_masks():
    j = np.arange(128)[:, None]
    i = np.arange(128)[None, :]
    return np.stack([(j <= i), (j > i), (j >= i)]).astype(np.float32)


def g8(v):
    return np.ascontiguousarray(v.reshape(8, 128).T)


def s5_host(d, T):
    a_re, a_im = d['s5_a_re'][0], d['s5_a_im'][0]
    are2 = np.concatenate([a_re.reshape(64, 64).T] * 2, 0)
    aim2 = np.concatenate([a_im.reshape(64, 64).T] * 2, 0)
    lstep = np.tile(d['s5_log_step'][0].reshape(1, 64), (128, 1))
    dsk = d['s5_d'][0].reshape(4, 128).T
    prm = {"are2": are2, "aim2": aim2, "lstep": lstep, "dsk": dsk, "b_re": d['s5_b_re'][0], "b_im": d['s5_b_im'][0],
           "c_re": d['s5_c_re'][0], "c_im": d['s5_c_im'][0]}
    psw = np.zeros((128, 128), np.float32)
    for k in range(128):
        psw[k, (k + 64) % 128] = 1
    ksel = np.zeros((128, 3), np.float32)
    ksel[:64, 0] = 1; ksel[64:, 1] = 1; ksel[:64, 2] = 1; ksel[64:, 2] = -1
    tau = np.tile(np.arange(1, T + 1, dtype=np.float32)[None, :], (128, 1))
    consts = {"psw": psw, "ksel": ksel, "tau": tau}
    return {k: np.ascontiguousarray(v, np.float32) for k, v in prm.items()}, consts


def route_consts(L):
    NJ = L // 128
    q = np.arange(128)[:, None]; p = np.arange(128)[None, :]
    us = (q < p).astype(np.float32)
    rm = np.ones((16, NJ), np.float32); rm[:, 0] = 0
    rm = np.tile(rm.reshape(1, -1), (128, 1))
    return us, np.ascontiguousarray(rm)


class RowSplit:
    def __init__(self, C, name, rows, L, chunk=1024):
        self.chunk = chunk
        self.parts = [C.dscr("%s_%d" % (name, k), [min(chunk, rows - k * chunk), L]) for k in range(-(-rows // chunk))]

    def __getitem__(self, key):
        rs, cs = key
        k = rs.start // self.chunk
        assert (rs.stop - 1) // self.chunk == k
        return self.parts[k][rs.start - k * self.chunk:rs.stop - k * self.chunk, cs]


def build_program(L, FF):
    NCH = L // 128
    NJ = L // 128
    CAP = L // 8
    T = min(512, L)
    C = Ctx()
    i = {}
    def din(name, shape, dt=F32):
        i[name] = C.din(name, shape, dt)
        return i[name]
    xT = din("xT", [D, L])
    w_in = [din("w_in0", [D, 2560]), din("w_in1", [D, 4480])]
    wsw = [din("wsw0", [D, 1024]), din("wsw1", [D, 1536])]
    w_out = [din("w_out0", [1024, 1024]), din("w_out1", [768, 1024])]
    gmix = [din("gmix0", [128, 8]), din("gmix1", [128, 8])]
    gffn = [din("gffn0", [128, 8]), din("gffn1", [128, 8])]
    gfin = din("gfin", [128, 8])
    cos = [din("cos0", [128, L]), din("cos1", [128, L])]
    sin = [din("sin0", [128, L]), din("sin1", [128, L])]
    ident = din("ident", [128, 128])
    lam = din("lamask", [3, 128, 128])
    rtab = din("rtab", [128, 24, NCH])
    prm_shapes = {"are2": [128, 64], "aim2": [128, 64], "lstep": [128, 64], "dsk": [128, 4], "b_re": [2, 32, 64, 16], "b_im": [2, 32, 64, 16],
                  "c_re": [2, 32, 16, 64], "c_im": [2, 32, 16, 64]}
    prm = {}
    for k, shp in prm_shapes.items():
        a = din("s5_" + k, shp)
        prm[k] = a[:, :] if len(shp) == 2 else a
    consts = {"ident": ident[:, :], "psw": din("c_psw", [128, 128])[:, :], "ksel": din("c_ksel", [128, 3])[:, :], "tau": din("c_tau", [128, T])[:, :]}
    gluw = din("gluw", [512, 512])
    glub = din("glub", [128, 4])
    router = [din("router0", [1024, 16]), din("router1", [1024, 16])]
    ustrict = din("ustrict", [128, 128])
    rmask = din("rmask", [128, 16 * NJ])
    mlb = din("mlb", [128, 16])
    dmasks = din("dmasks", [25, 128, 128])
    w1 = [din("w1_0", [16, 1024, FF]), din("w1_1", [16, 1024, FF])]
    w3 = [din("w3_0", [16, 1024, FF]), din("w3_1", [16, 1024, FF])]
    w2 = [din("w2_0", [16, FF, 1024]), din("w2_1", [16, FF, 1024])]
    outT = C.dout("outT", [D, L])
    pT = RowSplit(C, "pT", 4480, L)
    mixT = C.dscr("mixT", [1024, L])
    o1 = C.dscr("o1", [4, L, 128])
    yf = C.dscr("yf", [512, L])
    x1T = C.dscr("x1T", [1024, L])
    x2T = C.dscr("x2T", [1024, L])
    x3T = C.dscr("x3T", [1024, L])
    htok = C.dscr("htok", [L, 1024])
    aff = C.dscr("aff", [L, 16])
    posd = C.dscr("posd", [128, 16 * NJ], I32)
    gmd = C.dscr("gmd", [128, 16 * NJ])
    xe = [C.dscr("xe%d" % e, [CAP, 1024]) for e in range(16)]
    ye = [C.dscr("ye%d" % e, [CAP, 1024]) for e in range(16)]
    tabd = C.dscr("tabd", [128, 24, NCH])
    vtok = [C.dscr("vtok%d" % h, [L, 66], BF16) for h in range(12)]
    qk16 = C.dscr("qk16", [1536, L], BF16)

    def moe(layer, xin1T, xoutT, final):
        stage_route(C, aff, ustrict[:, :], rmask[:, :], posd[:, :], gmd[:, :], L)
        stage_dispatch(C, htok, posd[:, :], xe, L)
        stage_ffn(C, xe, w1[layer], w3[layer], w2[layer], ye, ident[:, :], L, FF)
        stage_combine(C, ye, posd[:, :], gmd[:, :], xin1T, ident[:, :], xoutT, outT if final else None, gfin[:, :], L)

    stage_proj(C, xT, w_in[0], wsw[0], gmix[0][:, :], cos[0], sin[0], pT, L, 20, 8)
    stage_linattn(C, pT, rtab[:, :, :], ident[:, :], lam, o1, mixT, L, 0, 512, 1024, 1536, 0, False)
    stage_s5(C, pT, prm, consts, yf, mixT, L, 2048, 512)
    stage_out(C, xT, mixT, w_out[0], gluw, glub[:, :], gffn[0][:, :], router[0], ident[:, :], x1T, htok, aff, L, True)
    moe(0, x1T, x2T, False)
    stage_proj(C, x2T, w_in[1], wsw[1], gmix[1][:, :], cos[1], sin[1], pT, L, 35, 12)
    stage_gates(C, pT, mlb[:, :], ident[:, :], tabd, L, 4352)
    stage_linattn(C, pT, tabd[:, :, :], ident[:, :], lam, o1, mixT, L, 2304, 2816, 3328, 3840, 256, True)
    stage_vprep(C, pT, ident[:, :], vtok, L, 1536)
    stage_qk16(C, pT, qk16, L, 1536)
    stage_dil(C, qk16, ident[:, :], dmasks, vtok, mixT, L, 0, 768, 0)
    stage_out(C, x2T, mixT, w_out[1], None, None, gffn[1][:, :], router[1], ident[:, :], x1T, htok, aff, L, False)
    moe(1, x1T, x3T, True)
    ninst = C.P.ninst
    return C.finish(), ninst


def host_inputs(inp, b, L, FF):
    f = lambda a: np.ascontiguousarray(a, np.float32)
    T = min(512, L)
    im = {"xT": f(inp["x"][b].T)}
    W0 = inp["ev_w_in"][0]
    W1 = np.zeros((D, 4480), np.float32)
    W1[:, :4368] = inp["od_w_in"][0]
    im["w_in0"] = f(W0); im["w_in1"] = W1
    im["wsw0"] = swap_cols(W0[:, :1024], 1024, 128); im["wsw1"] = swap_cols(W1[:, :1536], 1536, 64)
    im["w_out0"] = f(inp["ev_w_out"][0]); im["w_out1"] = f(inp["od_w_out"][0])
    for l in range(2):
        im["gmix%d" % l] = g8(inp["norm_mix_g"][l]); im["gffn%d" % l] = g8(inp["norm_ffn_g"][l])
        im["router%d" % l] = f(inp["moe_router"][l])
        im["w1_%d" % l] = f(inp["moe_w1"][l]); im["w3_%d" % l] = f(inp["moe_w3"][l]); im["w2_%d" % l] = f(inp["moe_w2"][l])
    im["gfin"] = g8(inp["final_g"])
    im["cos0"], im["sin0"] = rope_tables(L, 64, 128)
    im["cos1"], im["sin1"] = rope_tables(L, 32, 64)
    im["ident"] = np.eye(128, dtype=np.float32)
    im["lamask"] = la_masks()
    im["rtab"] = ret_tables(L)
    prm, consts = s5_host({k: inp[k] for k in ("s5_a_re", "s5_a_im", "s5_b_re", "s5_b_im", "s5_c_re", "s5_c_im", "s5_log_step", "s5_d")}, T)
    for k, v in prm.items():
        im["s5_" + k] = v
    for k, v in consts.items():
        im["c_" + k] = f(v)
    im["gluw"] = f(inp["s5_glu_w"][0]); im["glub"] = f(inp["s5_glu_b"][0].reshape(4, 128).T)
    im["ustrict"], im["rmask"] = route_consts(L)
    im["mlb"] = f(np.tile(np.concatenate([inp["ml_i_bias"][0].reshape(-1), inp["ml_f_bias"][0].reshape(-1)])[None, :], (128, 1)))
    im["dmasks"] = dil_masks()
    return im


def run_model(inp, batches):
    L = inp["x"].shape[1]
    FF = inp["moe_w1"].shape[-1]
    nc, ninst = build_program(L, FF)
    ims = [host_inputs(inp, b, L, FF) for b in batches]
    res = run_bass_kernel_spmd(nc, ims, core_ids=list(range(len(batches))))
    return [np.ascontiguousarray(r["outT"].T) for r in res.results]


def kernel(**inputs):
    inp = {k: np.asarray(v) for k, v in inputs.items()}
    B = inp["x"].shape[0]
    outs = run_model(inp, list(range(B)))
    return np.stack(outs, 0).astype(np.float32)
```
